# Optimizing a Trainium2 kernel written in Bass

```python
import jax, jax.numpy as jnp
from jax import lax
import numpy as np

D_MODEL = 2048
BATCH = 8
SEQ = 2048
DEPTH = 2
DEC_BATCH = 32
DEC_SEQ = 32
PAST_LEN = 2048

CHUNK = 64
D_MIX = D_MODEL
CONV_DIM = D_MIX // 4
CONV_WIDTH = 3
SGU_DIM = D_MIX // 4
SGU_HEADS = 4
SGU_HEAD_DIM = SGU_DIM // SGU_HEADS
MLP_CHUNK = 128
MLA_HEADS = 8
QK_NOPE = 128
QK_ROPE = 64
V_HEAD = 128
Q_LORA = 768
KV_LORA = 512
ROPE_THETA = 10000.0
Q_BLOCK = 128
SM_SCALE = (QK_NOPE + QK_ROPE) ** -0.5
OUT_HEAD_DIM = 128
OUT_HEADS = D_MIX // OUT_HEAD_DIM
D_IN = 3 * CONV_DIM + 2 * SGU_DIM + Q_LORA + KV_LORA + QK_ROPE
D_FF = 5632
N_EXPERTS = 8
TOP_K = 2
EXPERT_FF = 5632
N_DENSE = (DEPTH + 1) // 2
N_MOE = DEPTH // 2
ALPHA = (2 * DEPTH) ** 0.25
BETA = (8 * DEPTH) ** -0.25

kernel_name = 'hybrid_conv_sgu_mla_deepnorm_stream_step'


def _rms(x, g, eps=1e-6):
    xf = x.astype(jnp.float32)
    y = xf * lax.rsqrt(jnp.mean(xf * xf, axis=-1, keepdims=True) + eps)
    return (y * g).astype(x.dtype)


def _layernorm(x, g, b, eps=1e-5):
    xf = x.astype(jnp.float32)
    xc = xf - jnp.mean(xf, axis=-1, keepdims=True)
    var = jnp.mean(xc * xc, axis=-1, keepdims=True)
    return (xc * lax.rsqrt(var + eps) * g + b).astype(x.dtype)


def _rope(x, pos):
    half = QK_ROPE // 2
    inv = ROPE_THETA ** (-jnp.arange(half, dtype=jnp.float32) / half)
    ang = pos.astype(jnp.float32)[:, None] * inv[None, :]
    shape = (pos.shape[0],) + (1,) * (x.ndim - 3) + (QK_ROPE,)
    cos = jnp.concatenate([jnp.cos(ang), jnp.cos(ang)], axis=-1).reshape(shape)
    sin = jnp.concatenate([jnp.sin(ang), jnp.sin(ang)], axis=-1).reshape(shape)
    xf = x.astype(jnp.float32)
    rot = jnp.concatenate([-xf[..., half:], xf[..., :half]], axis=-1)
    return (xf * cos + rot * sin).astype(x.dtype)


def _split_offsets():
    a = CONV_DIM
    return [a, 2 * a, 3 * a, 3 * a + SGU_DIM, 3 * a + 2 * SGU_DIM,
            3 * a + 2 * SGU_DIM + Q_LORA, 3 * a + 2 * SGU_DIM + Q_LORA + KV_LORA]


def _short_conv(b_gate, c_gate, h, conv_w_l, conv_state):
    z = c_gate * h
    t = z.shape[1]
    zf = jnp.concatenate([conv_state.astype(z.dtype), z], axis=1)
    y = sum(conv_w_l[k] * zf[:, k:k + t] for k in range(CONV_WIDTH))
    return b_gate * y, zf[:, -(CONV_WIDTH - 1):]


def _sgu(u, v, ln_g, ln_b, sgu_w_l, sgu_b_l):
    bn, t, _ = v.shape
    vh = _layernorm(v.reshape(bn, t, SGU_HEADS, SGU_HEAD_DIM),
                    ln_g.reshape(SGU_HEADS, SGU_HEAD_DIM), ln_b.reshape(SGU_HEADS, SGU_HEAD_DIM))
    L = MLP_CHUNK if t >= MLP_CHUNK else t
    nc = t // L
    wm = jnp.tril(sgu_w_l[:, :L, :L])
    bias = jnp.transpose(sgu_b_l[:, :L])
    vc = vh.reshape(bn, nc, L, SGU_HEADS, SGU_HEAD_DIM)
    s = jnp.einsum('hpq,bnqhc->bnphc', wm, vc) + bias[:, :, None]
    out = u.reshape(bn, nc, L, SGU_HEADS, SGU_HEAD_DIM) * s
    return out.reshape(bn, t, SGU_DIM), vh.reshape(bn, t, SGU_DIM)


def _mla_prompt(q_nope, q_rope, k_nope, k_rope, v):
    bn, t, h, _ = q_nope.shape
    key_chunk = jnp.arange(t) // CHUNK

    def block(i):
        qs = i * Q_BLOCK
        qn = lax.dynamic_slice_in_dim(q_nope, qs, Q_BLOCK, axis=1)
        qr = lax.dynamic_slice_in_dim(q_rope, qs, Q_BLOCK, axis=1)
        s = (jnp.einsum('bqhd,bkhd->bhqk', qn, k_nope)
             + jnp.einsum('bqhr,bkr->bhqk', qr, k_rope)).astype(jnp.float32) * SM_SCALE
        q_chunk = (qs + jnp.arange(Q_BLOCK)) // CHUNK
        s = jnp.where(key_chunk[None, :] <= q_chunk[:, None], s, -jnp.inf)
        p = jax.nn.softmax(s, axis=-1).astype(v.dtype)
        return jnp.einsum('bhqk,bkhd->bqhd', p, v)

    o = lax.map(block, jnp.arange(t // Q_BLOCK))
    return jnp.transpose(o, (1, 0, 2, 3, 4)).reshape(bn, t, h * V_HEAD)


def _mla_sample(q_nope, q_rope, ckv_all, krope_all, w_uk_l, w_uv_l):
    bn, s_len, h, _ = q_nope.shape
    q_lat = jnp.einsum('bshd,chd->bshc', q_nope, w_uk_l)
    s = (jnp.einsum('bshc,bkc->bhsk', q_lat, ckv_all)
         + jnp.einsum('bshr,bkr->bhsk', q_rope, krope_all)).astype(jnp.float32) * SM_SCALE
    p = jax.nn.softmax(s, axis=-1).astype(ckv_all.dtype)
    o_lat = jnp.einsum('bhsk,bkc->bshc', p, ckv_all)
    return jnp.einsum('bshc,chd->bshd', o_lat, w_uv_l).reshape(bn, s_len, h * V_HEAD)


def _token_mixers(x, pos, conv_state, past_ckv, past_krope, w_in_l, conv_w_l, sgu_ln_g_l,
                  sgu_ln_b_l, sgu_w_l, sgu_b_l, q_norm_g_l, w_uq_l, kv_norm_g_l, w_uk_l, w_uv_l,
                  mix_norm_g_l, w_o_l):
    bn, t, _ = x.shape
    proj = x @ w_in_l
    b_g, c_g, h_c, u, v, c_q, c_kv, k_r = jnp.split(proj, _split_offsets(), axis=-1)
    a_out, conv_new = _short_conv(b_g, c_g, h_c, conv_w_l, conv_state)
    b_out, v_rows = _sgu(jax.nn.gelu(u), jax.nn.gelu(v), sgu_ln_g_l, sgu_ln_b_l, sgu_w_l, sgu_b_l)
    q = jnp.einsum('btc,chd->bthd', _rms(c_q, q_norm_g_l), w_uq_l)
    q_nope = q[..., :QK_NOPE]
    q_rope = _rope(q[..., QK_NOPE:], pos)
    ckv = _rms(c_kv, kv_norm_g_l)
    krope = _rope(k_r, pos)
    if past_ckv is None:
        k_nope = jnp.einsum('btc,chd->bthd', ckv, w_uk_l)
        v_att = jnp.einsum('btc,chd->bthd', ckv, w_uv_l)
        c_out = _mla_prompt(q_nope, q_rope, k_nope, krope, v_att)
    else:
        ckv_all = jnp.concatenate([past_ckv.astype(ckv.dtype), ckv], axis=1)
        krope_all = jnp.concatenate([past_krope.astype(krope.dtype), krope], axis=1)
        c_out = _mla_sample(q_nope, q_rope, ckv_all, krope_all, w_uk_l, w_uv_l)
    mixed = jnp.concatenate([a_out, b_out, c_out], axis=-1)
    mixed = _rms(mixed.reshape(bn, t, OUT_HEADS, OUT_HEAD_DIM),
                 mix_norm_g_l.reshape(OUT_HEADS, OUT_HEAD_DIM)).reshape(bn, t, D_MIX)
    return mixed @ w_o_l, conv_new, ckv, krope, v_rows


def _swiglu(x, wg, wu, wd):
    return (jax.nn.silu(x @ wg) * (x @ wu)) @ wd


def _moe(x, router_w_l, wg, wu, wd):
    logits = (x @ router_w_l).astype(jnp.float32)
    top_v, top_i = lax.top_k(logits, TOP_K)
    gates = jax.nn.softmax(top_v, axis=-1)
    dense_g = jnp.sum(jax.nn.one_hot(top_i, N_EXPERTS, dtype=jnp.float32) * gates[..., None],
                      axis=-2).astype(x.dtype)
    y = jnp.zeros_like(x)
    for e in range(N_EXPERTS):
        y = y + dense_g[..., e:e + 1] * _swiglu(x, wg[e], wu[e], wd[e])
    return y


def setup_inputs(seed: int = 0) -> dict:
    key = jax.random.key(seed)
    ks = iter(jax.random.split(key, 40))

    def nrm(shape, scale=1.0):
        return jax.random.normal(next(ks), shape, jnp.float32) * scale

    def gain(shape):
        return 1.0 + nrm(shape, 0.1)

    return {
        'x_prompt': nrm((BATCH, SEQ, D_MODEL)),
        'x_sample': nrm((DEC_BATCH, DEC_SEQ, D_MODEL)),
        'state_conv': nrm((DEPTH, DEC_BATCH, CONV_WIDTH - 1, CONV_DIM)),
        'cache_ckv': nrm((DEPTH, DEC_BATCH, PAST_LEN, KV_LORA)),
        'cache_krope': nrm((DEPTH, DEC_BATCH, PAST_LEN, QK_ROPE)),
        'w_in': nrm((DEPTH, D_MODEL, D_IN), D_MODEL ** -0.5),
        'conv_w': nrm((DEPTH, CONV_WIDTH, CONV_DIM), CONV_WIDTH ** -0.5),
        'sgu_ln_g': gain((DEPTH, SGU_DIM)),
        'sgu_ln_b': nrm((DEPTH, SGU_DIM), 0.1),
        'sgu_w': nrm((DEPTH, SGU_HEADS, MLP_CHUNK, MLP_CHUNK), MLP_CHUNK ** -0.5),
        'sgu_b': gain((DEPTH, SGU_HEADS, MLP_CHUNK)),
        'q_norm_g': gain((DEPTH, Q_LORA)),
        'w_uq': nrm((DEPTH, Q_LORA, MLA_HEADS, QK_NOPE + QK_ROPE), Q_LORA ** -0.5),
        'kv_norm_g': gain((DEPTH, KV_LORA)),
        'w_uk': nrm((DEPTH, KV_LORA, MLA_HEADS, QK_NOPE), KV_LORA ** -0.5),
        'w_uv': nrm((DEPTH, KV_LORA, MLA_HEADS, V_HEAD), KV_LORA ** -0.5),
        'mix_norm_g': gain((DEPTH, D_MIX)),
        'w_o': nrm((DEPTH, D_MIX, D_MODEL), BETA * D_MIX ** -0.5),
        'ln1_g': gain((DEPTH, D_MODEL)),
        'ln1_b': nrm((DEPTH, D_MODEL), 0.02),
        'ln2_g': gain((DEPTH, D_MODEL)),
        'ln2_b': nrm((DEPTH, D_MODEL), 0.02),
        'ffn_w_gate': nrm((N_DENSE, D_MODEL, D_FF), D_MODEL ** -0.5),
        'ffn_w_up': nrm((N_DENSE, D_MODEL, D_FF), D_MODEL ** -0.5),
        'ffn_w_down': nrm((N_DENSE, D_FF, D_MODEL), BETA * D_FF ** -0.5),
        'router_w': nrm((N_MOE, D_MODEL, N_EXPERTS), D_MODEL ** -0.5),
        'moe_w_gate': nrm((N_MOE, N_EXPERTS, D_MODEL, EXPERT_FF), D_MODEL ** -0.5),
        'moe_w_up': nrm((N_MOE, N_EXPERTS, D_MODEL, EXPERT_FF), D_MODEL ** -0.5),
        'moe_w_down': nrm((N_MOE, N_EXPERTS, EXPERT_FF, D_MODEL), BETA * EXPERT_FF ** -0.5),
    }


def reference(x_prompt, x_sample, state_conv, cache_ckv, cache_krope, w_in, conv_w, sgu_ln_g,
              sgu_ln_b, sgu_w, sgu_b, q_norm_g, w_uq, kv_norm_g, w_uk, w_uv, mix_norm_g, w_o,
              ln1_g, ln1_b, ln2_g, ln2_b, ffn_w_gate, ffn_w_up, ffn_w_down, router_w,
              moe_w_gate, moe_w_up, moe_w_down):
    pos_p = jnp.arange(x_prompt.shape[1])
    pos_s = PAST_LEN + jnp.arange(x_sample.shape[1])
    zero_conv = jnp.zeros((x_prompt.shape[0], CONV_WIDTH - 1, CONV_DIM), x_prompt.dtype)
    hp, hs = x_prompt, x_sample
    conv_p, ckv_p, kr_p, conv_s, ckv_s, kr_s, v_s = [], [], [], [], [], [], []
    for l in range(DEPTH):
        lp = (w_in[l], conv_w[l], sgu_ln_g[l], sgu_ln_b[l], sgu_w[l], sgu_b[l], q_norm_g[l],
              w_uq[l], kv_norm_g[l], w_uk[l], w_uv[l], mix_norm_g[l], w_o[l])
        mp, c1, k1, r1, _ = _token_mixers(hp, pos_p, zero_conv, None, None, *lp)
        ms, c2, k2, r2, v2 = _token_mixers(hs, pos_s, state_conv[l], cache_ckv[l],
                                           cache_krope[l], *lp)
        hp = _layernorm(ALPHA * hp + mp, ln1_g[l], ln1_b[l])
        hs = _layernorm(ALPHA * hs + ms, ln1_g[l], ln1_b[l])
        i = l // 2
        if l % 2 == 0:
            fp = _swiglu(hp, ffn_w_gate[i], ffn_w_up[i], ffn_w_down[i])
            fs = _swiglu(hs, ffn_w_gate[i], ffn_w_up[i], ffn_w_down[i])
        else:
            fp = _moe(hp, router_w[i], moe_w_gate[i], moe_w_up[i], moe_w_down[i])
            fs = _moe(hs, router_w[i], moe_w_gate[i], moe_w_up[i], moe_w_down[i])
        hp = _layernorm(ALPHA * hp + fp, ln2_g[l], ln2_b[l])
        hs = _layernorm(ALPHA * hs + fs, ln2_g[l], ln2_b[l])
        conv_p.append(c1)
        ckv_p.append(k1)
        kr_p.append(r1)
        conv_s.append(c2)
        ckv_s.append(k2)
        kr_s.append(r2)
        v_s.append(v2)
    return (hp, hs, jnp.stack(conv_p), jnp.stack(ckv_p), jnp.stack(kr_p),
            jnp.stack(conv_s), jnp.stack(ckv_s), jnp.stack(kr_s), jnp.stack(v_s))
```

```python
import math
import contextlib
import numpy as np
import concourse.bass as bass
import concourse.mybir as mybir
from concourse.bass_utils import run_bass_kernel_spmd

F32 = mybir.dt.float32
BF16 = mybir.dt.bfloat16
AF = mybir.ActivationFunctionType
ALU = mybir.AluOpType
AX = mybir.AxisListType

D = 2048
KT = 16
DIN = 3904
NE = 8
SM_SCALE = 192 ** -0.5
ALPHA = 4 ** 0.25
NEG = -30000.0

class Res:
    __slots__ = ("w", "rs", "excl")

    def __init__(self, excl=False):
        self.w = None
        self.rs = []
        self.excl = excl

class _Op:
    __slots__ = ("fn", "deps", "inc", "dma_sem", "dma_val", "val")

    def __init__(self, fn):
        self.fn = fn
        self.deps = []
        self.inc = False
        self.dma_sem = None
        self.dma_val = 0
        self.val = 0

class Prog:
    ENGS = ("pe", "act", "dve", "pool", "sp")
    NDMA = 8

    def __init__(self, nc, same=True):
        self.nc = nc
        self.ops = {e: [] for e in self.ENGS}
        self.same = same
        self.dma_ctr = {e: 0 for e in self.ENGS}
        self.dma_cnt = {}
        self.covered = {}

    def op(self, eng, fn, reads=(), writes=(), dma=False, extra=None):
        ops = self.ops[eng]
        seq = len(ops)
        o = _Op(fn)
        deps = {}

        def add(d, force=False):
            if d is None:
                return
            e2, s2 = d
            od = self.ops[e2][s2]
            if od.dma_sem is not None:
                k = ("dma", od.dma_sem)
                deps[k] = max(deps.get(k, 0), od.dma_val)
                return
            if e2 == eng and not force and (eng == "pe" or not self.same):
                return
            k = ("eng", e2)
            if s2 > deps.get(k, -1):
                deps[k] = s2

        if any(r.excl for r in reads):
            writes = list(writes) + [r for r in reads if r.excl]
            reads = [r for r in reads if not r.excl]
        for r in reads:
            add(r.w)
        for r in writes:
            add(r.w)
            for d in r.rs:
                add(d)
        if extra:
            for kk, v in extra:
                if kk[0] == "dma":
                    deps[kk] = max(deps.get(kk, 0), v)
                else:
                    add((kk[1], v), force=True)
        if dma:
            k = self.dma_ctr[eng] % self.NDMA
            self.dma_ctr[eng] += 1
            key = (eng, k)
            prev = self.dma_cnt.get(key, 0)
            if prev > 0:
                kk = ("dma", key)
                deps[kk] = max(deps.get(kk, 0), prev * 16)
            self.dma_cnt[key] = prev + 1
            o.dma_sem = key
            o.dma_val = (prev + 1) * 16
        for kk, v in deps.items():
            ck = (eng, kk)
            if self.covered.get(ck, -1) >= v:
                continue
            self.covered[ck] = v
            o.deps.append((kk, v))
            if kk[0] == "eng":
                self.ops[kk[1]][v].inc = True
        for r in reads:
            r.rs.append((eng, seq))
        for r in writes:
            r.w = (eng, seq)
            r.rs = []
        ops.append(o)
        return o

    def barrier(self):
        extra = []
        for e in self.ENGS:
            n = len(self.ops[e])
            for s in range(n - 1, -1, -1):
                od = self.ops[e][s]
                if od.dma_sem is None and od.fn is not None:
                    extra.append((("eng", e), s))
                    break
        for key, cnt in self.dma_cnt.items():
            extra.append((("dma", key), cnt * 16))
        for e in self.ENGS:
            self.op(e, None, extra=extra)

    def emit(self, final_waits=()):
        nc = self.nc
        self.op("sp", None, reads=list(final_waits))
        for e in self.ENGS:
            c = 0
            for o in self.ops[e]:
                if o.inc:
                    c += 1
                    o.val = c
        with contextlib.ExitStack() as st:
            esem = {e: st.enter_context(nc.semaphore("s_" + e)) for e in self.ENGS}
            dsem = {}
            for key in self.dma_cnt:
                dsem[key] = st.enter_context(nc.semaphore("d_%s%d" % key))
            block = st.enter_context(nc.Block())
            engmap = {"pe": block.tensor, "act": block.scalar, "dve": block.vector,
                      "pool": block.gpsimd, "sp": block.sync}

            def make(e):
                def body(engine):
                    for o in self.ops[e]:
                        for kk, v in o.deps:
                            if kk[0] == "eng":
                                engine.wait_ge(esem[kk[1]], self.ops[kk[1]][v].val)
                            else:
                                engine.wait_ge(dsem[kk[1]], v)
                        if o.fn is None:
                            continue
                        ins = o.fn(engine)
                        if o.dma_sem is not None:
                            ins.then_inc(dsem[o.dma_sem], 16)
                        elif o.inc:
                            ins.then_inc(esem[e], 1)
                return body

            for e in self.ENGS:
                engmap[e](make(e))

class _Stop(Exception):
    pass

class Cfg:
    def __init__(self, S=2048, PAST=2048, FF=5632, EFF=5632, DEPTH=2, stop=None):
        self.S, self.PAST, self.FF, self.EFF, self.DEPTH = S, PAST, FF, EFF, DEPTH
        self.stop = stop
        self.T = S + 128
        self.NT = self.T // 128
        self.NTP = S // 128
        self.groups = [(s, min(512, S - s)) for s in range(0, S, 512)] + [(S, 128)]
        self.sgs = []
        cur, tot = [], 0
        for gi, (s, n) in enumerate(self.groups):
            if tot + n > 1152:
                self.sgs.append(cur)
                cur, tot = [], 0
            cur.append(gi)
            tot += n
        self.sgs.append(cur)

W_INPUTS = ["w_in", "conv_w", "sgu_ln_g", "sgu_ln_b", "sgu_w", "sgu_b", "q_norm_g", "w_uq", "kv_norm_g",
            "w_uk", "w_uv", "mix_norm_g", "w_o", "ln1_g", "ln1_b", "ln2_g", "ln2_b", "ffn_w_gate",
            "ffn_w_up", "ffn_w_down", "router_w", "moe_w_gate", "moe_w_up", "moe_w_down"]

class KB:
    def __init__(self, cfg):
        self.cfg = cfg
        self.nc = bass.Bass("TRN2", target_bir_lowering=False)
        self.res = {}

    def R(self, name):
        r = self.res.get(name)
        if r is None:
            r = self.res[name] = Res(excl=(name[:2] == "ps" and name[2:].isdigit()))
        return r

    def Rs(self, *names):
        return [self.R(n) for n in names]

    def mm(self, out, lhsT, rhs, start, stop, rd, wr):
        self.p.op("pe", lambda e: e.matmul(out, lhsT=lhsT, rhs=rhs, start=start, stop=stop),
                  reads=self.Rs(*rd), writes=self.Rs(*wr))

    def tr(self, out, in_, ident, rd, wr):
        self.p.op("pe", lambda e: e.transpose(out=out, in_=in_, identity=ident),
                  reads=self.Rs(*rd), writes=self.Rs(*wr))

    def act(self, out, in_, func, rd, wr, bias=None, scale=None, accum=None):
        kw = {}
        if bias is not None:
            kw["bias"] = bias
        if scale is not None:
            kw["scale"] = scale
        if accum is not None:
            kw["accum_out"] = accum
        self.p.op("act", lambda e: e.activation(out=out, in_=in_, func=func, **kw),
                  reads=self.Rs(*rd), writes=self.Rs(*wr))

    def tt(self, eng, out, in0, in1, op, rd, wr):
        self.p.op(eng, lambda e: e.tensor_tensor(out=out, in0=in0, in1=in1, op=op),
                  reads=self.Rs(*rd), writes=self.Rs(*wr))

    def ts(self, eng, out, in0, s1, s2, op0, op1, rd, wr):
        if op1 is None:
            self.p.op(eng, lambda e: e.tensor_scalar(out=out, in0=in0, scalar1=s1, scalar2=None, op0=op0),
                      reads=self.Rs(*rd), writes=self.Rs(*wr))
        else:
            self.p.op(eng, lambda e: e.tensor_scalar(out=out, in0=in0, scalar1=s1, scalar2=s2, op0=op0, op1=op1),
                      reads=self.Rs(*rd), writes=self.Rs(*wr))

    def stt(self, eng, out, in0, scalar, in1, op0, op1, rd, wr):
        self.p.op(eng, lambda e: e.scalar_tensor_tensor(out=out, in0=in0, scalar=scalar, in1=in1, op0=op0, op1=op1),
                  reads=self.Rs(*rd), writes=self.Rs(*wr))

    def cp(self, eng, out, in_, rd, wr):
        if eng == "act":
            self.p.op("act", lambda e: e.activation(out=out, in_=in_, func=AF.Copy), reads=self.Rs(*rd), writes=self.Rs(*wr))
        else:
            self.p.op(eng, lambda e: e.tensor_copy(out=out, in_=in_), reads=self.Rs(*rd), writes=self.Rs(*wr))

    def red(self, out, in_, op, rd, wr):
        self.p.op("dve", lambda e: e.tensor_reduce(out=out, in_=in_, axis=AX.X, op=op),
                  reads=self.Rs(*rd), writes=self.Rs(*wr))

    def recip(self, out, in_, rd, wr):
        self.p.op("dve", lambda e: e.reciprocal(out=out, in_=in_), reads=self.Rs(*rd), writes=self.Rs(*wr))

    def memset(self, eng, ap, v, wr):
        self.p.op(eng, lambda e: e.memset(ap, v), writes=self.Rs(*wr))

    def dma(self, q, out, in_, rd, wr):
        self.p.op(q, lambda e: e.dma_start(out=out, in_=in_), reads=self.Rs(*rd), writes=self.Rs(*wr), dma=True)

    def rstd_from(self, out, in_, scale, rd, wr, tmpname):
        self.act(out, in_, AF.Sqrt, rd, [tmpname], bias=self._eps, scale=scale)
        self.recip(out, out, [tmpname], wr)

    @staticmethod
    def vw(reg, off, n, dt=F32):
        ap = reg[:, off:off + n]
        if dt != F32:
            ap = ap.bitcast(dt)
        return ap

    def v3(self, reg, off, a, b, dt=F32):
        n = a * b if dt == F32 else (a * b) // 2
        return self.vw(reg, off, n, dt).rearrange("p (a b) -> p a b", a=a)

    def build(self):
        cfg, nc = self.cfg, self.nc
        S, T, NT, NTP, PAST = cfg.S, cfg.T, cfg.NT, cfg.NTP, cfg.PAST
        self.p = Prog(nc)
        dt_in = lambda name, shape: nc.dram_tensor(name, list(shape), F32, kind="ExternalInput").ap()
        dt_out = lambda name, shape: nc.dram_tensor(name, list(shape), F32, kind="ExternalOutput").ap()
        L = cfg.DEPTH
        self.x = dt_in("x", [T, D])
        self.state_conv = dt_in("state_conv", [L, 4, 2, 512])
        self.cache_ckv = dt_in("cache_ckv", [L, 4, PAST, 512])
        self.cache_krope = dt_in("cache_krope", [L, 4, PAST, 64])
        self.tabT = dt_in("tabT", [128, T])
        self.tabM = dt_in("tabM", [T, 128])
        self.w = {}
        shapes = {"w_in": [L, D, DIN], "conv_w": [L, 3, 512], "sgu_ln_g": [L, 512], "sgu_ln_b": [L, 512],
                  "sgu_w": [L, 4, 128, 128], "sgu_b": [L, 4, 128], "q_norm_g": [L, 768],
                  "w_uq": [L, 768, 8 * 192], "kv_norm_g": [L, 512], "w_uk": [L, 512, 1024],
                  "w_uv": [L, 512, 1024], "mix_norm_g": [L, D], "w_o": [L, D, D], "ln1_g": [L, D],
                  "ln1_b": [L, D], "ln2_g": [L, D], "ln2_b": [L, D],
                  "ffn_w_gate": [1, D, cfg.FF], "ffn_w_up": [1, D, cfg.FF], "ffn_w_down": [1, cfg.FF, D],
                  "router_w": [1, D, NE], "moe_w_gate": [1, NE, D, cfg.EFF], "moe_w_up": [1, NE, D, cfg.EFF],
                  "moe_w_down": [1, NE, cfg.EFF, D]}
        for k in W_INPUTS:
            self.w[k] = dt_in(k, shapes[k])
        self.o_y = dt_out("y", [T, D])
        self.o_conv_p = dt_out("conv_p", [L, 2, 512])
        self.o_ckv_p = dt_out("ckv_p", [L, S, 512])
        self.o_krope_p = dt_out("krope_p", [L, S, 64])
        self.o_conv_s = dt_out("conv_s", [L, 4, 2, 512])
        self.o_ckv_s = dt_out("ckv_s", [L, 128, 512])
        self.o_krope_s = dt_out("krope_s", [L, 128, 64])
        self.o_sguv_s = dt_out("sguv_s", [L, 128, 512])
        self.hres = nc.dram_tensor("hresT", [D, T], F32, kind="Internal").ap()
        self.outs = []

        with contextlib.ExitStack() as st:
            st.enter_context(nc.allow_non_contiguous_dma(reason="small strided parameter loads"))
            sb = lambda n, s, d=F32: st.enter_context(nc.sbuf_tensor(n, s, d))
            RW = 18432
            self.EW = 13000
            self.RX = sb("RX", [128, RW])
            self.RY = sb("RY", [128, RW])
            self.RE = sb("RE", [128, self.EW])
            self.ident_f = sb("ident_f", [128, 128])
            self.ident_b = sb("ident_b", [128, 128], BF16)
            self.ones_f = sb("ones_f", [128, 128])
            self.eps5 = sb("eps5", [128, 1])
            self.eps6 = sb("eps6", [128, 1])
            self.mrow = sb("mrow", [1, 128], BF16)
            self.mcol = sb("mcol", [1, 128], BF16)
            self.small = sb("small", [128, 256])
            self.par = sb("par", [128, 1664])
            self.ps = [st.enter_context(nc.psum_tensor("ps%d" % i, [128, 512], F32)) for i in range(8)]
            self.setup_consts()
            try:
                for l in range(L):
                    A, B = (self.RX, self.RY) if l % 2 == 0 else (self.RY, self.RX)
                    self.layer(l, A, B)
            except _Stop:
                pass
            self.p.emit(final_waits=self.Rs(*self.outs))
        return nc

    def chk(self, name):
        if self.cfg.stop == "l%d%s" % (self.l, name):
            raise _Stop()

    def psb(self, i, n=512):
        return self.ps[i][:, 0:n]

    def psb16(self, i):
        return self.ps[i][:, :].bitcast(BF16)

    def setup_consts(self):
        self.memset("pool", self.ones_f[:], 1.0, ["ones"])
        self.memset("pool", self.ident_f[:], 1.0, ["identf"])
        idf = self.ident_f
        self.p.op("pool", lambda e: e.affine_select(out=idf[:], in_=idf[:], pattern=[[-1, 128]],
                                                    compare_op=ALU.is_equal, fill=0.0, base=0,
                                                    channel_multiplier=1),
                  reads=self.Rs("identf"), writes=self.Rs("identf"))
        self.cp("dve", self.ident_b[:], self.ident_f[:], ["identf"], ["identb"])
        self.memset("pool", self.eps5[:], 1e-5, ["eps"])
        self.memset("pool", self.eps6[:], 1e-6, ["eps"])
        self.memset("dve", self.mrow[:], 0.0, ["mask"])
        self.memset("dve", self.mrow[:, 0:64], 1.0, ["mask"])
        self.memset("dve", self.mcol[:], 0.0, ["mask"])
        self.memset("dve", self.mcol[:, 64:128], NEG, ["mask"])

    def load_params(self, l):
        par, w = self.par, self.w
        P = {}
        off = [0]

        def alloc(n):
            o = off[0]
            off[0] += n
            return par[:, o:o + n]

        def fm(name, src, k):
            ap = alloc(k)
            self.dma("sp", ap, src.rearrange("(k p) -> p k", p=128), [], ["par"])
            P[name] = ap

        def bc(name, src, n):
            ap = alloc(n)
            self.dma("sp", ap, src.rearrange("(o n) -> o n", o=1).partition_broadcast(128), [], ["par"])
            P[name] = ap

        fm("ln1g", w["ln1_g"][l], 16); fm("ln1b", w["ln1_b"][l], 16)
        fm("ln2g", w["ln2_g"][l], 16); fm("ln2b", w["ln2_b"][l], 16)
        fm("mixg", w["mix_norm_g"][l], 16)
        fm("qg", w["q_norm_g"][l], 6); fm("kvg", w["kv_norm_g"][l], 4)
        ap = alloc(12)
        for r in range(3):
            self.dma("sp", ap.rearrange("p (j r) -> p j r", j=4)[:, :, r], w["conv_w"][l][r].rearrange("(j p) -> p j", p=128), [], ["par"])
        P["convw"] = ap.rearrange("p (j r) -> p j r", j=4)
        bc("sgug", w["sgu_ln_g"][l], 512); bc("sgub", w["sgu_ln_b"][l], 512)
        bc("kvg_bc", w["kv_norm_g"][l], 512)
        ap = alloc(4)
        self.dma("sp", ap, w["sgu_b"][l].rearrange("h p -> p h"), [], ["par"])
        P["sgubias"] = ap
        ap = alloc(4)
        for b in range(4):
            self.dma("sp", ap[32 * b:32 * b + 32, :], w["sgu_b"][l][:, 0:32].rearrange("h p -> p h"), [], ["par"])
        P["sgubias_s"] = ap
        self.P = P

    def layer(self, l, A, B):
        cfg = self.cfg
        S, T, NT, NTP = cfg.S, cfg.T, cfg.NT, cfg.NTP
        self.l = l
        self.HT = self.v3(A, 0, 16, T, BF16)
        self.MT = self.v3(B, 0, 16, T, BF16)
        self._eps = self.eps5[:, 0:1]
        self.p.barrier()
        self.load_params(l)
        self.chk("par")
        if l == 0:
            self.stage0(A, B)
            self.p.barrier()
        self.chk("s0")
        self.stage1(l, A, B)
        self.p.barrier()
        self.chk("s1")
        self.stage2(l, A, B)
        self.p.barrier()
        self.chk("s2")
        self.stage3(l, A, B)
        self.p.barrier()
        self.chk("s3")
        self.stage4(l, A, B)
        self.chk("s4")

    def stage0(self, A, B):
        cfg = self.cfg
        T, NT = cfg.T, cfg.NT
        xs = [self.vw(B, 0, 2048), self.vw(B, 2048, 2048)]
        stg = [self.v3(B, 4096 + i * 512, 4, 128) for i in range(4)]
        nst = 0
        for t in range(NT):
            xb = xs[t % 2]
            self.dma("sp", xb, self.x[t * 128:(t + 1) * 128, :], [], ["xs%d" % (t % 2)])
            for q in range(4):
                bank = (t * 4 + q) % 6
                for j in range(4):
                    k = q * 4 + j
                    self.tr(self.ps[bank][:, j * 128:(j + 1) * 128], xb[:, k * 128:(k + 1) * 128], self.ident_f[:],
                            ["xs%d" % (t % 2), "identf"], ["ps%d" % bank])
                pv = self.ps[bank][:, :].rearrange("p (a b) -> p a b", a=4)
                self.cp("act", self.HT[:, q * 4:q * 4 + 4, t * 128:(t + 1) * 128], pv, ["ps%d" % bank], ["HT"])
                sg = stg[nst % 4]
                sn = "stg%d" % (nst % 4)
                nst += 1
                self.cp("dve", sg, pv, ["ps%d" % bank], [sn])
                self.dma("sp", self.hres[q * 512:(q + 1) * 512, t * 128:(t + 1) * 128].rearrange("(a p) n -> p a n", p=128),
                         sg, [sn], ["hres"])

    def wload(self, dst, src, name, q="pool"):
        self.dma(q, dst, src.rearrange("(k p) n -> p k n", p=128), [], [name])

    def stage1(self, l, A, B):
        cfg = self.cfg
        S, T, NT, NTP, groups = cfg.S, cfg.T, cfg.NT, cfg.NTP, cfg.groups
        P, w = self.P, self.w
        win = w["w_in"][l]
        HT, MT = self.HT, self.MT
        BH = 8704
        E = self.RE
        self.cqnT = self.v3(E, 0, 6, T, BF16)
        self.ckvnT = self.v3(E, 6528, 4, T, BF16)
        self.kropeT = self.vw(E, 10880, 1088, BF16)
        self.ckvn_new = self.v3(E, 11968, 4, 512, BF16)

        wb = [self.v3(B, BH + i * 3072, 3 * 16, 128, BF16) for i in range(2)]
        zbuf = self.vw(E, 0, S + 2)
        zs = self.v3(E, 2052, 4, 34)
        wk = [[self.vw(E, 2200 + (i * 4 + j) * 512, 512) for j in range(4)] for i in range(2)]
        convw = P["convw"]
        it = 0
        for j in range(4):
            wbj = wb[j % 2]
            wn = "cw%d" % (j % 2)
            for r, c0 in enumerate((512, 1024, 0)):
                self.wload(wbj[:, r * 16:(r + 1) * 16, :], win[:, c0 + j * 128:c0 + (j + 1) * 128], wn)
            self.memset("pool", zbuf[:, 0:2], 0.0, ["zbuf"])
            for b in range(4):
                self.dma("sp", zs[:, b, 0:2], self.state_conv[l][b][:, j * 128:(j + 1) * 128].rearrange("r p -> p r"),
                         [], ["zs"])
            for gi, (g0, n) in enumerate(groups):
                samp = gi == len(groups) - 1
                tmpc, a32, sq, rstd = wk[it % 2]
                wkn = "cwk%d" % (it % 2)
                it += 1
                pc, ph, pb = 0 + 3 * (it % 2), 1 + 3 * (it % 2), 2 + 3 * (it % 2)
                for r, bank in enumerate((pc, ph, pb)):
                    for k in range(16):
                        self.mm(self.psb(bank, n), wbj[:, r * 16 + k, :], HT[:, k, g0:g0 + n], k == 0, k == 15,
                                [wn, "HT"], ["ps%d" % bank])
                self.cp("act", tmpc[:, 0:n], self.psb(pc, n), ["ps%d" % pc], [wkn])
                if not samp:
                    zc = [zbuf[:, g0 + r:g0 + r + n] for r in range(3)]
                    zn = "zbuf"
                    self.tt("dve", zc[2], tmpc[:, 0:n], self.psb(ph, n), ALU.mult, [wkn, "ps%d" % ph], [zn])
                    yv = a32[:, 0:n]
                    pbv = self.psb(pb, n)
                    sqv, rsv = sq[:, 0:n], rstd[:, 0:n]
                    mo = MT[:, j, g0:g0 + n]
                else:
                    zc = [zs[:, :, r:r + 32] for r in range(3)]
                    zn = "zs"
                    self.tt("dve", zc[2], tmpc[:, 0:n].rearrange("p (b q) -> p b q", b=4),
                            self.psb(ph, n).rearrange("p (b q) -> p b q", b=4), ALU.mult, [wkn, "ps%d" % ph], [zn])
                    yv = a32[:, 0:n].rearrange("p (b q) -> p b q", b=4)
                    pbv = self.psb(pb, n).rearrange("p (b q) -> p b q", b=4)
                    sqv, rsv = sq[:, 0:n], rstd[:, 0:n]
                    mo = MT[:, j, g0:g0 + n]
                self.ts("dve", yv, zc[0], convw[:, j, 0:1], None, ALU.mult, None, [zn, "par"], [wkn])
                self.stt("dve", yv, zc[1], convw[:, j, 1:2], yv, ALU.mult, ALU.add, [zn, "par", wkn], [wkn])
                self.stt("dve", yv, zc[2], convw[:, j, 2:3], yv, ALU.mult, ALU.add, [zn, "par", wkn], [wkn])
                self.tt("dve", yv, yv, pbv, ALU.mult, [wkn, "ps%d" % pb], [wkn])
                self.act(sqv, a32[:, 0:n], AF.Square, [wkn], [wkn + "s"])
                self.mm(self.psb(6 + it % 2, n), self.ones_f[:], sqv, True, True, ["ones", wkn + "s"], ["ps%d" % (6 + it % 2)])
                self._eps = self.eps6[:, 0:1]
                self.rstd_from(rsv, self.psb(6 + it % 2, n), 1.0 / 128, ["ps%d" % (6 + it % 2), "eps"], [wkn + "r"], wkn + "r")
                self.stt("dve", mo, a32[:, 0:n], P["mixg"][:, j:j + 1], rsv, ALU.mult, ALU.mult,
                         [wkn, wkn + "r", "par"], ["MT"])
            self.dma("sp", self.o_conv_p[l][:, j * 128:(j + 1) * 128].rearrange("r p -> p r"), zbuf[:, S:S + 2],
                     ["zbuf"], ["o_conv_p%d_%d" % (l, j)])
            self.outs.append("o_conv_p%d_%d" % (l, j))
            for b in range(4):
                on = "o_conv_s%d_%d_%d" % (l, j, b)
                self.dma("sp", self.o_conv_s[l][b][:, j * 128:(j + 1) * 128].rearrange("r p -> p r"), zs[:, b, 32:34],
                         ["zs"], [on])
                self.outs.append(on)
        self.p.barrier()
        self.chk("p1")

        wu = self.v3(B, BH, 16, 512, BF16)
        wv = self.v3(B, BH + 4096, 16, 512, BF16)
        self.wload(wu, win[:, 1536:2048], "wu")
        self.wload(wv, win[:, 2048:2560], "wv")
        wraw = self.v3(E, 0, 4, 128)
        WT = self.v3(E, 512, 4, 128, BF16)
        WTs = self.v3(E, 768, 4, 128, BF16)
        wtf = self.v3(E, 1024, 4, 128)
        self.dma("sp", wraw, w["sgu_w"][l].rearrange("h p q -> p h q"), [], ["wraw"])
        for h in range(4):
            self.tr(self.ps[0][:, h * 128:(h + 1) * 128], wraw[:, h, :], self.ident_f[:], ["wraw", "identf"], ["ps0"])
        self.cp("dve", wtf, self.ps[0][:, :].rearrange("p (a b) -> p a b", a=4), ["ps0"], ["wtf"])
        for h in range(4):
            wslice = wtf[:, h, :]
            self.p.op("pool", (lambda ws: (lambda e: e.affine_select(out=ws, in_=ws, pattern=[[1, 128]],
                                                                       compare_op=ALU.is_ge, fill=0.0, base=0,
                                                                       channel_multiplier=-1)))(wslice),
                      reads=self.Rs("wtf"), writes=self.Rs("wtf"))
        self.cp("dve", WT, wtf, ["wtf"], ["WT"])
        self.memset("pool", WTs, 0.0, ["WTs"])
        for b in range(4):
            self.dma("sp", WTs[32 * b:32 * b + 32, :, 32 * b:32 * b + 32], WT[0:32, :, 0:32], ["WT", "WTs"], ["WTs"])
        sw = 1600
        bufs = []
        for i in range(2):
            o = sw + i * 2700
            bufs.append(dict(gu=self.vw(E, o, 512), gv=self.vw(E, o + 512, 512), sq=self.vw(E, o + 1024, 512),
                             bo=self.vw(E, o + 1536, 512), vnb=self.vw(E, o + 2048, 256, BF16),
                             bon=self.vw(E, o + 2304, 256, BF16)))
        sm = self.small
        for t in range(NT):
            samp = t == NT - 1
            bf = bufs[t % 2]
            bn = "sg%d" % (t % 2)
            gu, gv, sq, bo, vnb, bon = bf["gu"], bf["gv"], bf["sq"], bf["bo"], bf["vnb"], bf["bon"]
            pu, pv, pss, ptt = (0, 1, 2, 3) if t % 2 == 0 else (4, 5, 6, 7)
            tok = slice(t * 128, (t + 1) * 128)
            for k in range(16):
                self.mm(self.psb(pu), HT[:, k, tok], wu[:, k, :], k == 0, k == 15, ["HT", "wu"], ["ps%d" % pu])
            for k in range(16):
                self.mm(self.psb(pv), HT[:, k, tok], wv[:, k, :], k == 0, k == 15, ["HT", "wv"], ["ps%d" % pv])
            self.act(gu, self.psb(pu), AF.Gelu_apprx_tanh, ["ps%d" % pu], [bn + "gu"])
            self.act(gv, self.psb(pv), AF.Gelu_apprx_tanh, ["ps%d" % pv], [bn + "gv"])
            so = (t % 2) * 40
            s1, s2, mean, var, rs = (sm[:, so + i * 4:so + i * 4 + 4] for i in range(5))
            smn = "sm%d" % (t % 2)
            g3 = lambda a: a.rearrange("p (h c) -> p h c", h=4)
            bc3 = lambda a: a.unsqueeze(2).to_broadcast([128, 4, 128])
            self.red(s1, g3(gv), ALU.add, [bn + "gv"], [smn])
            self.tt("pool", sq, gv, gv, ALU.mult, [bn + "gv"], [bn + "sq"])
            self.red(s2, g3(sq), ALU.add, [bn + "sq"], [smn])
            self.ts("dve", mean, s1, 1.0 / 128, None, ALU.mult, None, [smn], [smn])
            self.tt("dve", var, mean, mean, ALU.mult, [smn], [smn])
            self.stt("dve", var, s2, 1.0 / 128, var, ALU.mult, ALU.subtract, [smn], [smn])
            self._eps = self.eps5[:, 0:1]
            self.rstd_from(rs, var, 1.0, [smn, "eps"], [smn], smn)
            self.tt("dve", g3(gv), g3(gv), bc3(mean), ALU.subtract, [bn + "gv", smn], [bn + "gv"])
            self.tt("dve", g3(gv), g3(gv), bc3(rs), ALU.mult, [bn + "gv", smn], [bn + "gv"])
            self.tt("dve", gv, gv, P["sgug"], ALU.mult, [bn + "gv", "par"], [bn + "gv"])
            self.tt("dve", gv, gv, P["sgub"], ALU.add, [bn + "gv", "par"], [bn + "gv"])
            if samp:
                on = "o_sguv%d" % l
                self.dma("sp", self.o_sguv_s[l], gv, [bn + "gv"], [on])
                self.outs.append(on)
            self.cp("act", vnb, gv, [bn + "gv"], [bn + "vnb"])
            wt = WTs if samp else WT
            wtn = "WTs" if samp else "WT"
            for h in range(4):
                self.mm(self.ps[pss][:, h * 128:(h + 1) * 128], wt[:, h, :], vnb[:, h * 128:(h + 1) * 128], True, True,
                        [wtn, bn + "vnb"], ["ps%d" % pss])
            bias = P["sgubias_s"] if samp else P["sgubias"]
            for h in range(4):
                hs = slice(h * 128, (h + 1) * 128)
                self.stt("dve", bo[:, hs], self.ps[pss][:, hs], bias[:, h:h + 1], gu[:, hs], ALU.add, ALU.mult,
                         ["ps%d" % pss, "par", bn + "gu"], [bn + "bo"])
            self.tt("pool", sq, bo, bo, ALU.mult, [bn + "bo"], [bn + "sq"])
            ss, rs2 = sm[:, so + 20:so + 24], sm[:, so + 24:so + 28]
            self.red(ss, g3(sq), ALU.add, [bn + "sq"], [smn])
            self._eps = self.eps6[:, 0:1]
            self.rstd_from(rs2, ss, 1.0 / 128, [smn, "eps"], [smn], smn)
            self.tt("dve", g3(bon), g3(bo), bc3(rs2), ALU.mult, [bn + "bo", smn], [bn + "bon"])
            p16 = self.psb16(ptt)
            for h in range(4):
                self.tr(p16[:, h * 128:(h + 1) * 128], bon[:, h * 128:(h + 1) * 128], self.ident_b[:],
                        [bn + "bon", "identb"], ["ps%d" % ptt])
            self.tt("dve", MT[:, 4:8, tok], p16[:, 0:512].rearrange("p (a b) -> p a b", a=4),
                    P["mixg"][:, 4:8].unsqueeze(2).to_broadcast([128, 4, 128]), ALU.mult,
                    ["ps%d" % ptt, "par"], ["MT"])
        self.p.barrier()
        self.chk("p2")

        wq = self.v3(B, BH, 16, 768, BF16)
        self.wload(wq, win[:, 2560:3328], "wq")
        sqb = [self.vw(B, BH + 6144 + i * 512, 512) for i in range(2)]
        rsb = self.vw(B, BH + 6144 + 1024, 512)
        self._eps = self.eps6[:, 0:1]
        for gi, (g0, n) in enumerate(groups):
            for m in range(6):
                for k in range(16):
                    self.mm(self.psb(m, n), wq[:, k, m * 128:(m + 1) * 128], HT[:, k, g0:g0 + n], k == 0, k == 15,
                            ["wq", "HT"], ["ps%d" % m])
            for m in range(6):
                self.act(sqb[m % 2][:, 0:n], self.psb(m, n), AF.Square, ["ps%d" % m], ["sqb%d" % (m % 2)])
                self.mm(self.psb(7, n), self.ones_f[:], sqb[m % 2][:, 0:n], m == 0, m == 5, ["ones", "sqb%d" % (m % 2)], ["ps7"])
            self.rstd_from(rsb[:, 0:n], self.psb(7, n), 1.0 / 768, ["ps7", "eps"], ["rsb"], "rsb")
            for m in range(6):
                self.stt("dve", self.cqnT[:, m, g0:g0 + n], self.psb(m, n), P["qg"][:, m:m + 1], rsb[:, 0:n],
                         ALU.mult, ALU.mult, ["ps%d" % m, "par", "rsb"], ["cqnT"])
        self.p.barrier()
        self.chk("p3")

        wkv = self.v3(B, BH, 16, 512, BF16)
        self.wload(wkv, win[:, 3328:3840], "wkv")
        wkr = self.v3(B, BH + 4096, 16, 128, BF16)
        self.wload(wkr[:, :, 0:64], win[:, 3840:3904], "wkr")
        self.ts("dve", wkr[:, :, 64:96], wkr[:, :, 32:64], -1.0, None, ALU.mult, None, ["wkr"], ["wkr"])
        self.cp("dve", wkr[:, :, 96:128], wkr[:, :, 0:32], ["wkr"], ["wkr"])
        o5 = BH + 4096 + 1024
        sqb = [self.vw(B, o5 + i * 512, 512) for i in range(2)]
        rsb = self.vw(B, o5 + 1024, 512)
        ctm = [self.vw(B, o5 + 1536 + i * 512, 512) for i in range(2)]
        ctb = self.vw(B, o5 + 2560, 256, BF16)
        junk = self.vw(B, o5 + 2816, 512)
        for gi, (g0, n) in enumerate(groups):
            for m in range(4):
                for k in range(16):
                    self.mm(self.psb(m, n), wkv[:, k, m * 128:(m + 1) * 128], HT[:, k, g0:g0 + n], k == 0, k == 15,
                            ["wkv", "HT"], ["ps%d" % m])
            for m in range(4):
                self.act(sqb[m % 2][:, 0:n], self.psb(m, n), AF.Square, ["ps%d" % m], ["sqb%d" % (m % 2)])
                self.mm(self.psb(7, n), self.ones_f[:], sqb[m % 2][:, 0:n], m == 0, m == 3, ["ones", "sqb%d" % (m % 2)], ["ps7"])
            self.rstd_from(rsb[:, 0:n], self.psb(7, n), 1.0 / 512, ["ps7", "eps"], ["rsb"], "rsb")
            for m in range(4):
                self.stt("dve", self.ckvnT[:, m, g0:g0 + n], self.psb(m, n), P["kvg"][:, m:m + 1], rsb[:, 0:n],
                         ALU.mult, ALU.mult, ["ps%d" % m, "par", "rsb"], ["ckvnT"])
        sm = self.small
        for t in range(NT):
            samp = t == NT - 1
            tok = slice(t * 128, (t + 1) * 128)
            bank = 4 + t % 2
            for k in range(16):
                self.mm(self.psb(bank), HT[:, k, tok], wkv[:, k, :], k == 0, k == 15, ["HT", "wkv"], ["ps%d" % bank])
            ss, rs = sm[:, 100 + (t % 2) * 2:101 + (t % 2) * 2], sm[:, 101 + (t % 2) * 2:102 + (t % 2) * 2]
            smn = "smk%d" % (t % 2)
            self.act(junk, self.psb(bank), AF.Square, ["ps%d" % bank], ["junk", smn], accum=ss)
            self.rstd_from(rs, ss, 1.0 / 512, [smn, "eps"], [smn], smn)
            cb = ctm[t % 2]
            cbn = "ctm%d" % (t % 2)
            self.stt("dve", cb, self.psb(bank), rs, P["kvg_bc"], ALU.mult, ALU.mult, ["ps%d" % bank, smn, "par"], [cbn])
            if not samp:
                on = "o_ckv_p%d_%d" % (l, t)
                self.dma("sp", self.o_ckv_p[l][tok, :], cb, [cbn], [on])
            else:
                on = "o_ckv_s%d" % l
                self.dma("sp", self.o_ckv_s[l], cb, [cbn], [on])
                self.cp("act", ctb, cb, [cbn], ["ctb"])
                for b in range(4):
                    self.dma("sp", self.ckvn_new[0:32, b, :], ctb[32 * b:32 * b + 32, :], ["ctb"], ["ckvn_new"])
            self.outs.append(on)
        self.chk("p4")
        tabm = [self.vw(B, o5 + 3328 + i * 128, 128) for i in range(2)]
        prod = [self.vw(B, o5 + 3584 + i * 128, 128) for i in range(2)]
        dup = [self.vw(B, o5 + 3840 + i * 128, 128) for i in range(2)]
        for t in range(NT):
            samp = t == NT - 1
            tok = slice(t * 128, (t + 1) * 128)
            i2 = t % 2
            bank = 0 + i2
            self.dma("sp", tabm[i2], self.tabM[tok, :], [], ["tabm%d" % i2])
            for k in range(16):
                self.mm(self.ps[bank][:, 0:128], HT[:, k, tok], wkr[:, k, :], k == 0, k == 15, ["HT", "wkr"], ["ps%d" % bank])
            self.tt("dve", prod[i2], self.ps[bank][:, 0:128], tabm[i2], ALU.mult, ["ps%d" % bank, "tabm%d" % i2], ["prod%d" % i2])
            self.tt("dve", dup[i2][:, 0:64], prod[i2][:, 0:64], prod[i2][:, 64:128], ALU.add, ["prod%d" % i2], ["dup%d" % i2])
            self.cp("dve", dup[i2][:, 64:128], dup[i2][:, 0:64], ["dup%d" % i2], ["dup%d" % i2])
            if not samp:
                on = "o_kr_p%d_%d" % (l, t)
                self.dma("sp", self.o_krope_p[l][tok, :], dup[i2][:, 0:64], ["dup%d" % i2], [on])
            else:
                on = "o_kr_s%d" % l
                self.dma("sp", self.o_krope_s[l], dup[i2][:, 0:64], ["dup%d" % i2], [on])
            self.outs.append(on)
            self.tr(self.ps[2 + i2][:, 0:128], dup[i2], self.ident_f[:], ["dup%d" % i2, "identf"], ["ps%d" % (2 + i2)])
            self.cp("act", self.kropeT[:, tok], self.ps[2 + i2][:, 0:128], ["ps%d" % (2 + i2)], ["kropeT"])

    def attn_tail(self, l, h, c_ps, rinv, tok, i2, A):
        P = self.P
        sm = self.small
        o = 16960 + i2 * 192
        c32 = self.vw(A, o, 128)
        cb = self.vw(A, o + 128, 64, BF16)
        n = "at%d" % i2
        ss, rs = sm[:, 120 + i2 * 2:121 + i2 * 2], sm[:, 121 + i2 * 2:122 + i2 * 2]
        if rinv is not None:
            self.ts("dve", c32, c_ps, rinv, None, ALU.mult, None, ["ps7", "sm_rinv"], [n])
        else:
            self.cp("dve", c32, c_ps, ["ps7"], [n])
        junk = self.vw(A, 16960 + 384, 64, BF16)
        self.act(junk, c32, AF.Square, [n], ["junkA", n + "s"], accum=ss)
        self._eps = self.eps6[:, 0:1]
        self.rstd_from(rs, ss, 1.0 / 128, [n + "s", "eps"], [n + "s"], n + "s")
        self.ts("dve", cb, c32, rs, None, ALU.mult, None, [n, n + "s"], [n + "b"])
        p16 = self.psb16(6)
        self.tr(p16[:, i2 * 128:(i2 + 1) * 128], cb, self.ident_b[:], [n + "b", "identb"], ["ps6"])
        self.act(self.MT[:, 8 + h, tok], p16[:, i2 * 128:(i2 + 1) * 128], AF.Copy, ["ps6", "par"], ["MT"],
                 scale=P["mixg"][:, 8 + h:9 + h])

    def stage2(self, l, A, B):
        cfg = self.cfg
        S, T, NT, NTP, PAST, groups = cfg.S, cfg.T, cfg.NT, cfg.NTP, cfg.PAST, cfg.groups
        P, w = self.P, self.w
        MT = self.MT
        sm = self.small
        tabT = self.vw(A, 0, T)
        o = 2176
        wh = []
        for i in range(2):
            wh.append(dict(qn=self.v3(A, o, 6, 128, BF16), qr=self.v3(A, o + 384, 6, 128, BF16),
                           uk=self.v3(A, o + 768, 4, 128, BF16), uv=self.v3(A, o + 1024, 4, 128, BF16),
                           ukT=self.v3(A, o + 1280, 4, 128, BF16)))
            o += 1536
        qnT = self.vw(A, o, T // 2, BF16); o += T // 2
        qrT = self.vw(A, o, T // 2, BF16); o += T // 2
        knT = self.vw(A, o, S // 2, BF16); o += S // 2
        Vh = self.v3(A, o, NTP, 128, BF16); o += NTP * 64
        Pb = [self.vw(A, o + i * ((PAST + 128) // 2), (PAST + 128) // 2, BF16) for i in range(2)]
        o += (PAST + 128)
        PTb = [self.vw(A, o + i * 512, 512, BF16) for i in range(2)]
        o += 1024
        assert o <= 12672, o
        o = 12672
        QlatT = self.vw(A, o, 2048, BF16).rearrange("p (c b h t) -> p c b h t", c=4, b=4, h=8); o += 2048
        QlatF = self.vw(A, o - 2048, 2048, BF16).rearrange("p (c b m t) -> p c b m t", c=4, b=4, m=2)
        OlatT = self.vw(A, o, 2048, BF16).rearrange("p (c h b q) -> p c h b q", c=4, h=8, b=4)
        OlatF = self.vw(A, o, 2048, BF16).rearrange("p (c h t) -> p c h t", c=4, h=8); o += 2048
        wr_tmp = self.v3(A, o, 6, 64, BF16); o += 192
        assert o <= 16960, o
        self.qs_rope = self.vw(A, 17920, 512, BF16).rearrange("p (b h t) -> p b h t", b=4, h=8)
        qs_ropeF = self.vw(A, 17920, 512, BF16).rearrange("p (b m t) -> p b m t", b=4, m=2)
        self.dma("sp", tabT, self.tabT, [], ["tabT"])
        wuq = w["w_uq"][l]
        wuk = w["w_uk"][l]
        wuv = w["w_uv"][l]
        for h in range(8):
            wb = wh[h % 2]
            wn = "wh%d" % (h % 2)
            self.wload(wb["qn"], wuq[:, h * 192:h * 192 + 128], wn)
            self.wload(wr_tmp, wuq[:, h * 192 + 128:h * 192 + 192], "wrtmp")
            self.cp("dve", wb["qr"][:, :, 0:64], wr_tmp, ["wrtmp"], [wn])
            self.ts("dve", wb["qr"][:, :, 64:96], wr_tmp[:, :, 32:64], -1.0, None, ALU.mult, None, ["wrtmp"], [wn])
            self.cp("dve", wb["qr"][:, :, 96:128], wr_tmp[:, :, 0:32], ["wrtmp"], [wn])
            self.wload(wb["uk"], wuk[:, h * 128:(h + 1) * 128], wn)
            self.wload(wb["uv"], wuv[:, h * 128:(h + 1) * 128], wn)
            for gi, (g0, n) in enumerate(groups):
                b0 = (gi % 2) * 2
                for k in range(6):
                    self.mm(self.psb(b0, n), wb["qn"][:, k, :], self.cqnT[:, k, g0:g0 + n], k == 0, k == 5,
                            [wn, "cqnT"], ["ps%d" % b0])
                self.cp("act", qnT[:, g0:g0 + n], self.psb(b0, n), ["ps%d" % b0], ["qnT"])
                for k in range(6):
                    self.mm(self.psb(b0 + 1, n), wb["qr"][:, k, :], self.cqnT[:, k, g0:g0 + n], k == 0, k == 5,
                            [wn, "cqnT"], ["ps%d" % (b0 + 1)])
                self.tt("dve", qrT[:, g0:g0 + n], self.psb(b0 + 1, n), tabT[:, g0:g0 + n], ALU.mult,
                        ["ps%d" % (b0 + 1), "tabT"], ["qrT"])
            self.cp("dve", self.qs_rope[:, :, h, :], qrT[:, S:S + 128].rearrange("p (b t) -> p b t", b=4), ["qrT"], ["qs"])
            for gi, (g0, n) in enumerate(groups[:-1]):
                b0 = 4 + gi % 2
                for k in range(4):
                    self.mm(self.psb(b0, n), wb["uk"][:, k, :], self.ckvnT[:, k, g0:g0 + n], k == 0, k == 3,
                            [wn, "ckvnT"], ["ps%d" % b0])
                self.cp("act", knT[:, g0:g0 + n], self.psb(b0, n), ["ps%d" % b0], ["knT"])
            for t0 in range(0, NTP, 4):
                bank = 6 + (t0 // 4) % 2
                nt = min(4, NTP - t0)
                for j in range(nt):
                    t = t0 + j
                    for k in range(4):
                        self.mm(self.ps[bank][:, j * 128:(j + 1) * 128], self.ckvnT[:, k, t * 128:(t + 1) * 128],
                                wb["uv"][:, k, :], k == 0, k == 3, ["ckvnT", wn], ["ps%d" % bank])
                self.cp("dve", Vh[:, t0:t0 + nt, :], self.ps[bank][:, 0:nt * 128].rearrange("p (a b) -> p a b", a=nt),
                        ["ps%d" % bank], ["Vh"])
            p16 = self.psb16(5)
            for c in range(4):
                self.tr(p16[:, c * 128:(c + 1) * 128], wb["uk"][:, c, :], self.ident_b[:], [wn, "identb"], ["ps5"])
            self.cp("dve", wb["ukT"], p16[:, 0:512].rearrange("p (a b) -> p a b", a=4), ["ps5"], [wn + "T"])
            for c in range(4):
                self.mm(self.ps[4][:, c * 128:(c + 1) * 128], wb["ukT"][:, c, :], qnT[:, S:S + 128], True, True,
                        [wn + "T", "qnT"], ["ps4"])
            self.cp("act", QlatT[:, :, :, h, :], self.ps[4][:, :].rearrange("p (c b t) -> p c b t", c=4, b=4), ["ps4"], ["QlatT"])
            for i in range(NTP):
                nk = (i + 1) * 128
                nb = (nk + 511) // 512
                qs = slice(i * 128, (i + 1) * 128)
                Pq = Pb[i % 2]
                Pn = "P%d" % (i % 2)
                so = 40 * (i % 2) + 140
                mx = sm[:, so:so + 4]
                rs4 = sm[:, so + 4:so + 8]
                m1, negm, rsum, rinv = (sm[:, so + 8 + j:so + 9 + j] for j in range(4))
                smn = "sma%d" % (i % 2)
                for kb in range(nb):
                    ksz = min(512, nk - kb * 512)
                    ks = slice(kb * 512, kb * 512 + ksz)
                    last = kb == nb - 1
                    self.mm(self.psb(kb, ksz), qnT[:, qs], knT[:, ks], True, False, ["qnT", "knT"], ["ps%d" % kb])
                    self.mm(self.psb(kb, ksz), qrT[:, qs], self.kropeT[:, ks], False, not last, ["qrT", "kropeT"], ["ps%d" % kb])
                    if last:
                        self.mm(self.ps[kb][:, ksz - 128:ksz], self.mrow[:], self.mcol[:], False, True, ["mask"], ["ps%d" % kb])
                    self.red(mx[:, kb:kb + 1], self.psb(kb, ksz), ALU.max, ["ps%d" % kb], [smn + "m"])
                self.red(m1, mx[:, 0:nb], ALU.max, [smn + "m"], [smn + "m"])
                self.ts("dve", negm, m1, -SM_SCALE, None, ALU.mult, None, [smn + "m"], [smn + "n"])
                for kb in range(nb):
                    ksz = min(512, nk - kb * 512)
                    ks = slice(kb * 512, kb * 512 + ksz)
                    self.act(Pq[:, ks], self.psb(kb, ksz), AF.Exp, ["ps%d" % kb, smn + "n"], [Pn, smn + "r"],
                             bias=negm, scale=SM_SCALE, accum=rs4[:, kb:kb + 1])
                self.red(rsum, rs4[:, 0:nb], ALU.add, [smn + "r"], [smn + "r"])
                self.recip(rinv, rsum, [smn + "r"], ["sm_rinv"])
                nblk = i + 1
                for c0 in range(0, nblk, 8):
                    cn = min(8, nblk - c0)
                    pi = (c0 // 8) % 2
                    p16 = self.psb16(4 + pi)
                    for j in range(cn):
                        kk = c0 + j
                        self.tr(p16[:, j * 128:(j + 1) * 128], Pq[:, kk * 128:(kk + 1) * 128], self.ident_b[:],
                                [Pn, "identb"], ["ps%d" % (4 + pi)])
                    eng = "act" if pi == 0 else "dve"
                    self.cp(eng, PTb[pi][:, 0:cn * 128], p16[:, 0:cn * 128], ["ps%d" % (4 + pi)], ["PT%d" % pi])
                    for j in range(cn):
                        kk = c0 + j
                        self.mm(self.ps[7][:, 0:128], PTb[pi][:, j * 128:(j + 1) * 128], Vh[:, kk, :], kk == 0, kk == nblk - 1,
                                ["PT%d" % pi, "Vh"], ["ps7"])
                self.attn_tail(l, h, self.ps[7][:, 0:128], rinv, qs, i % 2, A)
        self.p.barrier()
        self.chk("s2h")
        KTc = PAST // 128
        o = 0
        ckv = self.v3(A, o, KTc, 512, BF16); o += KTc * 256
        kr2 = self.v3(A, o, KTc, 128, BF16); o += KTc * 64
        ckvT = self.v3(A, o, 4, PAST, BF16); o += 2 * PAST
        krT = self.vw(A, o, PAST // 2, BF16); o += PAST // 2
        Ps = self.vw(A, o, (PAST + 128) // 2, BF16); o += (PAST + 128) // 2
        PTs = self.v3(A, o, KTc + 1, 128, BF16); o += (KTc + 1) * 64
        olb = self.vw(A, o, 256, BF16); o += 256
        assert o <= 12672, o
        uvb = [self.v3(A, i * 256, 4, 128, BF16) for i in range(2)]
        for b in range(4):
            self.wload(ckv, self.cache_ckv[l][b], "ckv")
            self.dma("pool", kr2[:, :, 0:64], self.cache_krope[l][b].rearrange("(k p) n -> p k n", p=128), [], ["kr2"])
            self.dma("pool", kr2[:, :, 64:128], self.cache_krope[l][b].rearrange("(k p) n -> p k n", p=128), [], ["kr2"])
            nbk = 0
            for c in range(4):
                for t0 in range(0, KTc, 8):
                    cn = min(8, KTc - t0)
                    bank = nbk % 4
                    nbk += 1
                    p16 = self.psb16(bank)
                    for j in range(cn):
                        self.tr(p16[:, j * 128:(j + 1) * 128], ckv[:, t0 + j, c * 128:(c + 1) * 128], self.ident_b[:],
                                ["ckv", "identb"], ["ps%d" % bank])
                    self.cp("act" if nbk % 2 else "dve", ckvT[:, c, t0 * 128:(t0 + cn) * 128], p16[:, 0:cn * 128],
                            ["ps%d" % bank], ["ckvT"])
            for t0 in range(0, KTc, 8):
                cn = min(8, KTc - t0)
                bank = nbk % 4
                nbk += 1
                p16 = self.psb16(bank)
                for j in range(cn):
                    self.tr(p16[:, j * 128:(j + 1) * 128], kr2[:, t0 + j, :], self.ident_b[:], ["kr2", "identb"], ["ps%d" % bank])
                self.cp("act" if nbk % 2 else "dve", krT[:, t0 * 128:(t0 + cn) * 128], p16[:, 0:cn * 128],
                        ["ps%d" % bank], ["krT"])
            newk = slice(S + 32 * b, S + 32 * b + 32)
            nblk = (PAST + 511) // 512
            for mt in range(2):
                hs = slice(4 * mt, 4 * mt + 4)
                qlat = lambda c: QlatF[:, c, b, mt, :]
                qrp = qs_ropeF[:, b, mt, :]
                so = 180
                mx = sm[:, so:so + 8]
                rs8 = sm[:, so + 8:so + 16]
                m1, negm, rsum, rinv = (sm[:, so + 16 + j:so + 17 + j] for j in range(4))
                for kb in range(nblk + 1):
                    bank = kb
                    if kb < nblk:
                        ksz = min(512, PAST - kb * 512)
                        ks = slice(kb * 512, kb * 512 + ksz)
                        for c in range(4):
                            self.mm(self.psb(bank, ksz), qlat(c), ckvT[:, c, ks], c == 0, False, ["QlatT", "ckvT"], ["ps%d" % bank])
                        self.mm(self.psb(bank, ksz), qrp, krT[:, ks], False, True, ["qs", "krT"], ["ps%d" % bank])
                    else:
                        ksz = 32
                        for c in range(4):
                            self.mm(self.psb(bank, ksz), qlat(c), self.ckvnT[:, c, newk], c == 0, False, ["QlatT", "ckvnT"], ["ps%d" % bank])
                        self.mm(self.psb(bank, ksz), qrp, self.kropeT[:, newk], False, True, ["qs", "kropeT"], ["ps%d" % bank])
                    self.red(mx[:, kb:kb + 1], self.psb(bank, ksz), ALU.max, ["ps%d" % bank], ["smsm"])
                self.red(m1, mx[:, 0:nblk + 1], ALU.max, ["smsm"], ["smsm"])
                self.ts("dve", negm, m1, -SM_SCALE, None, ALU.mult, None, ["smsm"], ["smsn"])
                for kb in range(nblk + 1):
                    if kb < nblk:
                        ksz = min(512, PAST - kb * 512)
                        ks = slice(kb * 512, kb * 512 + ksz)
                    else:
                        ksz = 32
                        ks = slice(PAST, PAST + 32)
                    self.act(Ps[:, ks], self.psb(kb, ksz), AF.Exp, ["ps%d" % kb, "smsn"], ["Ps", "smsr"],
                             bias=negm, scale=SM_SCALE, accum=rs8[:, kb:kb + 1])
                self.red(rsum, rs8[:, 0:nblk + 1], ALU.add, ["smsr"], ["smsr"])
                self.recip(rinv, rsum, ["smsr"], ["smsr"])
                for t0 in range(0, KTc, 8):
                    cn = min(8, KTc - t0)
                    bank = 5 + (t0 // 8) % 2
                    p16 = self.psb16(bank)
                    for j in range(cn):
                        self.tr(p16[:, j * 128:(j + 1) * 128], Ps[:, (t0 + j) * 128:(t0 + j + 1) * 128], self.ident_b[:],
                                ["Ps", "identb"], ["ps%d" % bank])
                    self.cp("act" if (t0 // 8) % 2 else "dve", PTs[:, t0:t0 + cn, :],
                            p16[:, 0:cn * 128].rearrange("p (a b) -> p a b", a=cn), ["ps%d" % bank], ["PTs"])
                p16 = self.psb16(5)
                self.tr(p16[0:32, 0:128], Ps[:, PAST:PAST + 32], self.ident_b[:], ["Ps", "identb"], ["ps5"])
                self.cp("dve", PTs[0:32, KTc, :], p16[0:32, 0:128], ["ps5"], ["PTs"])
                for kt in range(KTc):
                    self.mm(self.psb(7), PTs[:, kt, :], ckv[:, kt, :], kt == 0, False, ["PTs", "ckv"], ["ps7"])
                self.mm(self.psb(7), PTs[0:32, KTc, :], self.ckvn_new[0:32, b, :], False, True, ["PTs", "ckvn_new"], ["ps7"])
                self.ts("dve", olb, self.psb(7), rinv, None, ALU.mult, None, ["ps7", "smsr"], ["olb"])
                p16 = self.psb16(6)
                for c in range(4):
                    self.tr(p16[:, c * 128:(c + 1) * 128], olb[:, c * 128:(c + 1) * 128], self.ident_b[:], ["olb", "identb"], ["ps6"])
                for c in range(4):
                    self.cp("act" if c % 2 else "dve", OlatT[:, c, hs, b, :],
                            p16[:, c * 128:(c + 1) * 128].rearrange("p (h q) -> p h q", h=4), ["ps6"], ["OlatT"])
        self.p.barrier()
        self.chk("s2s")
        for h in range(8):
            ub = uvb[h % 2]
            un = "uvb%d" % (h % 2)
            self.wload(ub, wuv[:, h * 128:(h + 1) * 128], un)
            for c in range(4):
                self.mm(self.ps[7][:, 0:128], OlatF[:, c, h, :], ub[:, c, :], c == 0, c == 3, ["OlatT", un], ["ps7"])
            self.attn_tail(l, h, self.ps[7][:, 0:128], None, slice(S, S + 128), h % 2, A)

    def ln_apply(self, l, which, src_of, g0, n, gi, mean_bc, rstd_bc, work, last, A, B, dst_bf, ytm=None):
        P = self.P
        g, bta = P[which + "g"], P[which + "b"]
        step = 256 if last else n
        for c0 in range(0, n, step):
            nn = min(step, n - c0)
            cs = slice(c0, c0 + nn)
            for m in range(16):
                t1 = work[0][m % 2][:, 0:nn]
                o32 = work[1][m % 2][:, 0:nn]
                n1, n2 = "lnw%d" % (m % 2), "lno%d" % (m % 2)
                self.tt("pool", t1, src_of(m)[:, cs], mean_bc[:, cs], ALU.subtract, ["lnsrc", "lnmean"], [n1])
                self.tt("dve", t1, t1, rstd_bc[:, cs], ALU.mult, [n1, "lnrstd"], [n1])
                if not last:
                    self.act(dst_bf[:, m, g0 + c0:g0 + c0 + nn], t1, AF.Identity, [n1, "par"], ["MTout"],
                             scale=g[:, m:m + 1], bias=bta[:, m:m + 1])
                self.ts("dve", o32, t1, g[:, m:m + 1], bta[:, m:m + 1], ALU.mult, ALU.add, [n1, "par"], [n2])
                if not last:
                    self.dma("sp", self.hres[m * 128:(m + 1) * 128, g0 + c0:g0 + c0 + nn], o32, [n2], ["hres%d" % gi])
                else:
                    for j in range(nn // 128):
                        bank = 4 + j % 2
                        pcol = self.ps[bank][:, (m % 4) * 128:(m % 4 + 1) * 128]
                        self.tr(pcol, o32[:, j * 128:(j + 1) * 128], self.ident_f[:], [n2, "identf"], ["ps%d" % bank])
                        self.cp("act" if j % 2 else "dve", ytm[j][:, m * 128:(m + 1) * 128], pcol, ["ps%d" % bank], ["ytm%d" % j])
            if last:
                for j in range(nn // 128):
                    t0 = g0 + c0 + j * 128
                    on = "o_y_%d" % t0
                    self.dma("sp", self.o_y[t0:t0 + 128, :], ytm[j], ["ytm%d" % j], [on])
                    self.outs.append(on)

    def ln_stats(self, n, work):
        mean_bc, rstd_bc, tmp = work
        self.act(mean_bc, self.psb(6, n), AF.Copy, ["ps6"], ["lnmean"], scale=1.0 / D)
        self.tt("dve", tmp, mean_bc, mean_bc, ALU.mult, ["lnmean"], ["lntmp"])
        self.stt("dve", tmp, self.psb(7, n), 1.0 / D, tmp, ALU.mult, ALU.subtract, ["ps7", "lntmp"], ["lntmp"])
        self._eps = self.eps5[:, 0:1]
        self.rstd_from(rstd_bc, tmp, 1.0, ["lntmp", "eps"], ["lnrstd"], "lnrstd")

    def stage3(self, l, A, B):
        cfg = self.cfg
        S, T, NT, groups = cfg.S, cfg.T, cfg.NT, cfg.groups
        P, w = self.P, self.w
        MT = self.MT
        wo = w["w_o"][l]
        o = 0
        rT = self.v3(A, o, 16, 512); o += 8192
        wob = [self.v3(A, o + i * 1024, 16, 128, BF16) for i in range(3)]; o += 3072
        res32 = [self.vw(A, o + i * 512, 512) for i in range(2)]; o += 1024
        sqb = [self.vw(A, o + i * 512, 512) for i in range(2)]; o += 1024
        mean_bc = self.vw(A, o, 512); o += 512
        rstd_bc = self.vw(A, o, 512); o += 512
        tmp = self.vw(A, o, 512); o += 512
        w0 = [self.vw(A, o + i * 512, 512) for i in range(2)]; o += 1024
        w1 = [self.vw(A, o + i * 512, 512) for i in range(2)]; o += 1024
        it = 0
        for gi, (g0, n) in enumerate(groups):
            for m in range(16):
                wb = wob[it % 3]
                wn = "wo%d" % (it % 3)
                it += 1
                self.wload(wb, wo[:, m * 128:(m + 1) * 128], wn)
                bank = m % 4
                for k in range(16):
                    self.mm(self.psb(bank, n), wb[:, k, :], MT[:, k, g0:g0 + n], k == 0, k == 15, [wn, "MT"], ["ps%d" % bank])
                rb = res32[m % 2][:, 0:n]
                self.dma("sp", rb, self.hres[m * 128:(m + 1) * 128, g0:g0 + n], ["hres%d" % gi], ["res%d" % (m % 2)])
                self.stt("dve", rT[:, m, 0:n], rb, ALPHA, self.psb(bank, n), ALU.mult, ALU.add,
                         ["res%d" % (m % 2), "ps%d" % bank], ["lnsrc"])
                sq = sqb[m % 2][:, 0:n]
                self.act(sq, rT[:, m, 0:n], AF.Square, ["lnsrc"], ["lsq%d" % (m % 2)])
                self.mm(self.psb(6, n), self.ones_f[:], rT[:, m, 0:n], m == 0, m == 15, ["ones", "lnsrc"], ["ps6"])
                self.mm(self.psb(7, n), self.ones_f[:], sq, m == 0, m == 15, ["ones", "lsq%d" % (m % 2)], ["ps7"])
            self.ln_stats(n, (mean_bc[:, 0:n], rstd_bc[:, 0:n], tmp[:, 0:n]))
            self.ln_apply(l, "ln1", lambda m: rT[:, m, 0:n], g0, n, gi, mean_bc[:, 0:n], rstd_bc[:, 0:n], (w0, w1),
                          False, A, B, MT)

    def stage4(self, l, A, B):
        cfg = self.cfg
        S, T, NT, groups = cfg.S, cfg.T, cfg.NT, cfg.groups
        P, w = self.P, self.w
        H1 = self.MT
        E = self.RE
        moe = (l % 2 == 1)
        last = (l == cfg.DEPTH - 1)
        sm = self.small
        acc = self.v3(A, 0, 16, 1152)
        FW = 1
        o = 0
        wgb = [self.v3(E, o + i * 1024, 16, 128, BF16) for i in range(2)]; o += 2048
        wub = [self.v3(E, o + i * 1024, 16, 128, BF16) for i in range(2)]; o += 2048
        wdb = [self.v3(E, o + i * 1024, 1, 2048, BF16) for i in range(2)]; o += 2048
        sil = [self.vw(E, o + i * 512, 512) for i in range(2)]; o += 1024
        actb = [self.v3(E, o + i * 256, 1, 512, BF16) for i in range(2)]; o += 512
        gbc = self.vw(E, o, 1152); o += 1152
        assert o <= 8832
        o = 8832
        if moe:
            wr = self.v3(E, o, 16, 8, BF16); o += 64
            dg = self.v3(E, o, NT, 8); o += NT * 8
            dgb = self.vw(E, o, 128); o += 128
            eq = self.vw(E, o, 8); o += 8
            self.wload(wr, w["router_w"][0], "wr")
            for t in range(NT):
                tok = slice(t * 128, (t + 1) * 128)
                for k in range(16):
                    self.mm(self.ps[0][:, 0:8], H1[:, k, tok], wr[:, k, :], k == 0, k == 15, ["MTout" if False else "MT", "wr"], ["ps0"])
                lg = sm[:, 200:208]
                m8 = sm[:, 208:216]
                d12, e12, g1, g2 = (sm[:, 216 + j:217 + j] for j in range(4))
                self.cp("dve", lg, self.ps[0][:, 0:8], ["ps0"], ["rt"])
                self.p.op("dve", (lambda a, b: (lambda e: e.max(out=a, in_=b)))(m8, lg), reads=self.Rs("rt"), writes=self.Rs("rt8"))
                self.tt("dve", d12, m8[:, 1:2], m8[:, 0:1], ALU.subtract, ["rt8"], ["rtd"])
                self.act(e12, d12, AF.Exp, ["rtd"], ["rte"])
                self.ts("dve", g1, e12, 1.0, None, ALU.add, None, ["rte"], ["rtg"])
                self.recip(g1, g1, ["rtg"], ["rtg"])
                self.ts("dve", g2, g1, -1.0, 1.0, ALU.mult, ALU.add, ["rtg"], ["rtg2"])
                self.ts("dve", eq, lg, m8[:, 0:1], g1, ALU.is_equal, ALU.mult, ["rt", "rt8", "rtg"], ["rteq"])
                self.ts("dve", dg[:, t, :], lg, m8[:, 1:2], g2, ALU.is_equal, ALU.mult, ["rt", "rt8", "rtg2"], ["dg"])
                self.tt("dve", dg[:, t, :], dg[:, t, :], eq, ALU.add, ["dg", "rteq"], ["dg"])
        lw = o
        mean_bc = self.vw(E, lw, 512); rstd_bc = self.vw(E, lw + 512, 512); tmp = self.vw(E, lw + 1024, 512)
        assert lw + 1536 <= self.EW, lw
        experts = list(range(NE)) if moe else [None]
        FFd = cfg.EFF if moe else cfg.FF
        nchunk = FFd // (128 * FW)
        it = 0
        it2 = 0
        for sgi, sg in enumerate(cfg.sgs):
            sg0 = groups[sg[0]][0]
            first = True
            for e in experts:
                if moe:
                    wg_d, wu_d, wd_d = w["moe_w_gate"][0][e], w["moe_w_up"][0][e], w["moe_w_down"][0][e]
                    for gi in sg:
                        g0, n = groups[gi]
                        for j in range(n // 128):
                            t = g0 // 128 + j
                            self.cp("dve", dgb, dg[:, t, e:e + 1].to_broadcast([128, 128]), ["dg"], ["dgb"])
                            self.mm(self.ps[5][:, j * 128:(j + 1) * 128], dgb, self.ident_f[:], True, True, ["dgb", "identf"], ["ps5"])
                        self.cp("act", gbc[:, g0 - sg0:g0 - sg0 + n], self.psb(5, n), ["ps5"], ["gbc"])
                else:
                    wg_d, wu_d, wd_d = w["ffn_w_gate"][0], w["ffn_w_up"][0], w["ffn_w_down"][0]
                for fc in range(nchunk):
                    i2 = it % 2
                    it += 1
                    wgn, wun, wdn = "wg%d" % i2, "wu%d" % i2, "wd%d" % i2
                    c0 = fc * 128 * FW
                    self.wload(wgb[i2], wg_d[:, c0:c0 + 128 * FW], wgn)
                    self.wload(wub[i2], wu_d[:, c0:c0 + 128 * FW], wun)
                    self.wload(wdb[i2], wd_d[c0:c0 + 128 * FW, :], wdn)
                    for gi in sg:
                        g0, n = groups[gi]
                        ab = actb[gi % 2]
                        abn = "actb%d" % (gi % 2)
                        for f in range(FW):
                            bg, bu = ((0, 1), (2, 3))[(it2 := it2 + 1) % 2]
                            for k in range(16):
                                self.mm(self.psb(bg, n), wgb[i2][:, k, f * 128:(f + 1) * 128], H1[:, k, g0:g0 + n], k == 0, k == 15,
                                        [wgn, "MT"], ["ps%d" % bg])
                            for k in range(16):
                                self.mm(self.psb(bu, n), wub[i2][:, k, f * 128:(f + 1) * 128], H1[:, k, g0:g0 + n], k == 0, k == 15,
                                        [wun, "MT"], ["ps%d" % bu])
                            sl = sil[it2 % 2][:, 0:n]
                            sn = "sil%d" % (it2 % 2)
                            self.act(sl, self.psb(bg, n), AF.Silu, ["ps%d" % bg], [sn])
                            if moe:
                                self.tt("pool", sl, sl, gbc[:, g0 - sg0:g0 - sg0 + n], ALU.mult, [sn, "gbc"], [sn])
                            self.tt("dve", ab[:, f, 0:n], sl, self.psb(bu, n), ALU.mult, [sn, "ps%d" % bu], [abn])
                        for m in range(16):
                            bank = 4 + m % 2 if not moe else 6 + m % 2
                            for f in range(FW):
                                self.mm(self.psb(bank, n), wdb[i2][:, f, m * 128:(m + 1) * 128], ab[:, f, 0:n], f == 0, f == FW - 1,
                                        [wdn, abn], ["ps%d" % bank])
                            av = acc[:, m, g0 - sg0:g0 - sg0 + n]
                            if first:
                                self.cp("dve", av, self.psb(bank, n), ["ps%d" % bank], ["acc"])
                            else:
                                self.tt("dve", av, av, self.psb(bank, n), ALU.add, ["acc", "ps%d" % bank], ["acc"])
                    first = False
            self.p.barrier()
            res32 = [self.vw(E, i * 512, 512) for i in range(2)]
            sqb = [self.vw(E, 1024 + i * 512, 512) for i in range(2)]
            w0 = [self.vw(E, 2048 + i * 512, 512) for i in range(2)]
            w1 = [self.vw(E, 3072 + i * 512, 512) for i in range(2)]
            ytm = [self.vw(E, 4096 + j * 2048, 2048) for j in range(2)]
            for gi in sg:
                g0, n = groups[gi]
                for m in range(16):
                    rb = res32[m % 2][:, 0:n]
                    self.dma("sp", rb, self.hres[m * 128:(m + 1) * 128, g0:g0 + n], ["hres%d" % gi], ["res%d" % (m % 2)])
                    av = acc[:, m, g0 - sg0:g0 - sg0 + n]
                    self.stt("dve", av, rb, ALPHA, av, ALU.mult, ALU.add, ["res%d" % (m % 2), "acc"], ["lnsrc"])
                    sq = sqb[m % 2][:, 0:n]
                    self.act(sq, av, AF.Square, ["lnsrc"], ["lsq%d" % (m % 2)])
                    self.mm(self.psb(6, n), self.ones_f[:], av, m == 0, m == 15, ["ones", "lnsrc"], ["ps6"])
                    self.mm(self.psb(7, n), self.ones_f[:], sq, m == 0, m == 15, ["ones", "lsq%d" % (m % 2)], ["ps7"])
                self.ln_stats(n, (mean_bc[:, 0:n], rstd_bc[:, 0:n], tmp[:, 0:n]))
                self.ln_apply(l, "ln2", lambda m: acc[:, m, g0 - sg0:g0 - sg0 + n], g0, n, gi, mean_bc[:, 0:n],
                              rstd_bc[:, 0:n], (w0, w1), last, A, B, H1, ytm)
            self.p.barrier()

def rope_tables(S, PAST):
    half = 32
    inv = (np.float32(10000.0) ** (-(np.arange(half, dtype=np.float32)) / np.float32(half))).astype(np.float32)
    pos = np.concatenate([np.arange(S), np.tile(PAST + np.arange(32), 4)]).astype(np.float32)
    ang = pos[:, None] * inv[None, :]
    cos = np.concatenate([np.cos(ang), np.cos(ang)], axis=-1).astype(np.float32)
    sin = np.concatenate([np.sin(ang), np.sin(ang)], axis=-1).astype(np.float32)
    tabM = np.ascontiguousarray(np.concatenate([cos, sin], axis=-1))
    tabT = np.ascontiguousarray(tabM.T)
    return tabT, tabM

_NC_CACHE = {}

def run(cfg, inputs, n_cores):
    key = (cfg.S, cfg.PAST, cfg.FF, cfg.EFF, cfg.DEPTH, cfg.stop)
    if key not in _NC_CACHE:
        _NC_CACHE[key] = KB(cfg).build()
    nc = _NC_CACHE[key]
    L = cfg.DEPTH
    tabT, tabM = rope_tables(cfg.S, cfg.PAST)
    f = lambda a: np.ascontiguousarray(np.asarray(a, dtype=np.float32))
    wmap = {}
    for k in W_INPUTS:
        a = f(inputs[k])
        if k == "w_uq":
            a = a.reshape(L, 768, 8 * 192)
        elif k in ("w_uk", "w_uv"):
            a = a.reshape(L, 512, 1024)
        wmap[k] = a
    in_maps = []
    for c in range(n_cores):
        m = dict(wmap)
        m["x"] = f(np.concatenate([inputs["x_prompt"][c], np.asarray(inputs["x_sample"][4 * c:4 * c + 4]).reshape(128, D)], axis=0))
        m["state_conv"] = f(inputs["state_conv"][:, 4 * c:4 * c + 4])
        m["cache_ckv"] = f(inputs["cache_ckv"][:, 4 * c:4 * c + 4])
        m["cache_krope"] = f(inputs["cache_krope"][:, 4 * c:4 * c + 4])
        m["tabT"] = tabT
        m["tabM"] = tabM
        in_maps.append(m)
    res = run_bass_kernel_spmd(nc, in_maps, core_ids=list(range(n_cores))).results
    S = cfg.S
    y_p = np.stack([r["y"][:S] for r in res])
    y_s = np.concatenate([r["y"][S:].reshape(4, 32, D) for r in res])
    conv_p = np.stack([r["conv_p"] for r in res], axis=1)
    ckv_p = np.stack([r["ckv_p"] for r in res], axis=1)
    kr_p = np.stack([r["krope_p"] for r in res], axis=1)
    conv_s = np.concatenate([r["conv_s"] for r in res], axis=1)
    ckv_s = np.concatenate([r["ckv_s"].reshape(L, 4, 32, 512) for r in res], axis=1)
    kr_s = np.concatenate([r["krope_s"].reshape(L, 4, 32, 64) for r in res], axis=1)
    v_s = np.concatenate([r["sguv_s"].reshape(L, 4, 32, 512) for r in res], axis=1)
    return tuple(np.ascontiguousarray(a, dtype=np.float32) for a in
                 (y_p, y_s, conv_p, ckv_p, kr_p, conv_s, ckv_s, kr_s, v_s))

def kernel(**inputs):
    return run(Cfg(), inputs, 8)
```

```python
import math
import contextlib
import numpy as np
import concourse.bass as bass
import concourse.mybir as mybir
from concourse.bass_utils import run_bass_kernel_spmd

F32 = mybir.dt.float32
BF16 = mybir.dt.bfloat16
I32 = mybir.dt.int32
AF = mybir.ActivationFunctionType
ALU = mybir.AluOpType
AX = mybir.AxisListType

D = 2048
KT = 16
DIN = 3904
NE = 8
SM_SCALE = 192 ** -0.5
ALPHA = 4 ** 0.25
NEG = -30000.0

class Res:
    __slots__ = ("w", "rs", "excl")

    def __init__(self, excl=False):
        self.w = None
        self.rs = []
        self.excl = excl

class _Op:
    __slots__ = ("fn", "deps", "inc", "dma_sem", "dma_val", "val")

    def __init__(self, fn):
        self.fn = fn
        self.deps = []
        self.inc = False
        self.dma_sem = None
        self.dma_val = 0
        self.val = 0

class Prog:
    ENGS = ("pe", "act", "dve", "pool", "sp")
    NDMA = 8

    def __init__(self, nc, same=True):
        self.nc = nc
        self.ops = {e: [] for e in self.ENGS}
        self.same = same
        self.dma_ctr = {e: 0 for e in self.ENGS}
        self.dma_cnt = {}
        self.covered = {}

    def op(self, eng, fn, reads=(), writes=(), dma=False, extra=None):
        ops = self.ops[eng]
        seq = len(ops)
        o = _Op(fn)
        deps = {}

        def add(d, force=False):
            if d is None:
                return
            e2, s2 = d
            od = self.ops[e2][s2]
            if od.dma_sem is not None:
                k = ("dma", od.dma_sem)
                deps[k] = max(deps.get(k, 0), od.dma_val)
                return
            if e2 == eng and not force and (eng == "pe" or not self.same):
                return
            k = ("eng", e2)
            if s2 > deps.get(k, -1):
                deps[k] = s2

        if any(r.excl for r in reads):
            writes = list(writes) + [r for r in reads if r.excl]
            reads = [r for r in reads if not r.excl]
        for r in reads:
            add(r.w)
        for r in writes:
            add(r.w)
            for d in r.rs:
                add(d)
        if extra:
            for kk, v in extra:
                if kk[0] == "dma":
                    deps[kk] = max(deps.get(kk, 0), v)
                else:
                    add((kk[1], v), force=True)
        if dma:
            k = self.dma_ctr[eng] % self.NDMA
            self.dma_ctr[eng] += 1
            key = (eng, k)
            prev = self.dma_cnt.get(key, 0)
            if prev > 0:
                kk = ("dma", key)
                deps[kk] = max(deps.get(kk, 0), prev * 16)
            self.dma_cnt[key] = prev + 1
            o.dma_sem = key
            o.dma_val = (prev + 1) * 16
        for kk, v in deps.items():
            ck = (eng, kk)
            if self.covered.get(ck, -1) >= v:
                continue
            self.covered[ck] = v
            o.deps.append((kk, v))
            if kk[0] == "eng":
                self.ops[kk[1]][v].inc = True
        for r in reads:
            r.rs.append((eng, seq))
        for r in writes:
            r.w = (eng, seq)
            r.rs = []
        ops.append(o)
        return o

    def barrier(self):
        extra = []
        for e in self.ENGS:
            n = len(self.ops[e])
            for s in range(n - 1, -1, -1):
                od = self.ops[e][s]
                if od.dma_sem is None and od.fn is not None:
                    extra.append((("eng", e), s))
                    break
        for key, cnt in self.dma_cnt.items():
            extra.append((("dma", key), cnt * 16))
        for e in self.ENGS:
            self.op(e, None, extra=extra)

    def emit(self, final_waits=()):
        nc = self.nc
        self.op("sp", None, reads=list(final_waits))
        for e in self.ENGS:
            c = 0
            for o in self.ops[e]:
                if o.inc:
                    c += 1
                    o.val = c
        with contextlib.ExitStack() as st:
            esem = {e: st.enter_context(nc.semaphore("s_" + e)) for e in self.ENGS}
            dsem = {}
            for key in self.dma_cnt:
                dsem[key] = st.enter_context(nc.semaphore("d_%s%d" % key))
            block = st.enter_context(nc.Block())
            engmap = {"pe": block.tensor, "act": block.scalar, "dve": block.vector,
                      "pool": block.gpsimd, "sp": block.sync}

            def make(e):
                def body(engine):
                    for o in self.ops[e]:
                        for kk, v in o.deps:
                            if kk[0] == "eng":
                                engine.wait_ge(esem[kk[1]], self.ops[kk[1]][v].val)
                            else:
                                engine.wait_ge(dsem[kk[1]], v)
                        if o.fn is None:
                            continue
                        ins = o.fn(engine)
                        if o.dma_sem is not None:
                            ins.then_inc(dsem[o.dma_sem], 16)
                        elif o.inc:
                            ins.then_inc(esem[e], 1)
                return body

            for e in self.ENGS:
                engmap[e](make(e))

class _Stop(Exception):
    pass

class Cfg:
    def __init__(self, S=2048, PAST=2048, FF=5632, EFF=5632, DEPTH=2, stop=None, C=1152):
        self.S, self.PAST, self.FF, self.EFF, self.DEPTH = S, PAST, FF, EFF, DEPTH
        self.stop = stop
        self.C = C
        self.T = S + 128
        self.NT = self.T // 128
        self.NTP = S // 128
        self.groups = [(s, min(512, S - s)) for s in range(0, S, 512)] + [(S, 128)]
        self.sgs = []
        cur, tot = [], 0
        for gi, (s, n) in enumerate(self.groups):
            if tot + n > 1152:
                self.sgs.append(cur)
                cur, tot = [], 0
            cur.append(gi)
            tot += n
        self.sgs.append(cur)

W_INPUTS = ["w_in", "conv_w", "sgu_ln_g", "sgu_ln_b", "sgu_w", "sgu_b", "q_norm_g", "w_uq", "kv_norm_g",
            "w_uk", "w_uv", "mix_norm_g", "w_o", "ln1_g", "ln1_b", "ln2_g", "ln2_b", "ffn_w_gate",
            "ffn_w_up", "ffn_w_down", "router_w", "moe_w_gate", "moe_w_up", "moe_w_down"]

class KB:
    def __init__(self, cfg):
        self.cfg = cfg
        self.nc = bass.Bass("TRN2", target_bir_lowering=False)
        self.res = {}

    def R(self, name):
        r = self.res.get(name)
        if r is None:
            r = self.res[name] = Res(excl=(name[:2] == "ps" and name[2:].isdigit()))
        return r

    def Rs(self, *names):
        return [self.R(n) for n in names]

    def mm(self, out, lhsT, rhs, start, stop, rd, wr):
        self.p.op("pe", lambda e: e.matmul(out, lhsT=lhsT, rhs=rhs, start=start, stop=stop),
                  reads=self.Rs(*rd), writes=self.Rs(*wr))

    def tr(self, out, in_, ident, rd, wr):
        self.p.op("pe", lambda e: e.transpose(out=out, in_=in_, identity=ident),
                  reads=self.Rs(*rd), writes=self.Rs(*wr))

    def act(self, out, in_, func, rd, wr, bias=None, scale=None, accum=None):
        kw = {}
        if bias is not None:
            kw["bias"] = bias
        if scale is not None:
            kw["scale"] = scale
        if accum is not None:
            kw["accum_out"] = accum
        self.p.op("act", lambda e: e.activation(out=out, in_=in_, func=func, **kw),
                  reads=self.Rs(*rd), writes=self.Rs(*wr))

    def tt(self, eng, out, in0, in1, op, rd, wr):
        self.p.op(eng, lambda e: e.tensor_tensor(out=out, in0=in0, in1=in1, op=op),
                  reads=self.Rs(*rd), writes=self.Rs(*wr))

    def ts(self, eng, out, in0, s1, s2, op0, op1, rd, wr):
        if op1 is None:
            self.p.op(eng, lambda e: e.tensor_scalar(out=out, in0=in0, scalar1=s1, scalar2=None, op0=op0),
                      reads=self.Rs(*rd), writes=self.Rs(*wr))
        else:
            self.p.op(eng, lambda e: e.tensor_scalar(out=out, in0=in0, scalar1=s1, scalar2=s2, op0=op0, op1=op1),
                      reads=self.Rs(*rd), writes=self.Rs(*wr))

    def stt(self, eng, out, in0, scalar, in1, op0, op1, rd, wr):
        self.p.op(eng, lambda e: e.scalar_tensor_tensor(out=out, in0=in0, scalar=scalar, in1=in1, op0=op0, op1=op1),
                  reads=self.Rs(*rd), writes=self.Rs(*wr))

    def cp(self, eng, out, in_, rd, wr):
        if eng == "act":
            self.p.op("act", lambda e: e.activation(out=out, in_=in_, func=AF.Copy), reads=self.Rs(*rd), writes=self.Rs(*wr))
        else:
            self.p.op(eng, lambda e: e.tensor_copy(out=out, in_=in_), reads=self.Rs(*rd), writes=self.Rs(*wr))

    def red(self, out, in_, op, rd, wr):
        self.p.op("dve", lambda e: e.tensor_reduce(out=out, in_=in_, axis=AX.X, op=op),
                  reads=self.Rs(*rd), writes=self.Rs(*wr))

    def recip(self, out, in_, rd, wr):
        self.p.op("dve", lambda e: e.reciprocal(out=out, in_=in_), reads=self.Rs(*rd), writes=self.Rs(*wr))

    def memset(self, eng, ap, v, wr):
        self.p.op(eng, lambda e: e.memset(ap, v), writes=self.Rs(*wr))

    def dma(self, q, out, in_, rd, wr):
        self.p.op(q, lambda e: e.dma_start(out=out, in_=in_), reads=self.Rs(*rd), writes=self.Rs(*wr), dma=True)

    def rstd_from(self, out, in_, scale, rd, wr, tmpname):
        self.act(out, in_, AF.Sqrt, rd, [tmpname], bias=self._eps, scale=scale)
        self.recip(out, out, [tmpname], wr)

    @staticmethod
    def vw(reg, off, n, dt=F32):
        ap = reg[:, off:off + n]
        if dt != F32:
            ap = ap.bitcast(dt)
        return ap

    def v3(self, reg, off, a, b, dt=F32):
        n = a * b if dt == F32 else (a * b) // 2
        return self.vw(reg, off, n, dt).rearrange("p (a b) -> p a b", a=a)

    def build(self):
        cfg, nc = self.cfg, self.nc
        S, T, NT, NTP, PAST = cfg.S, cfg.T, cfg.NT, cfg.NTP, cfg.PAST
        self.p = Prog(nc)
        dt_in = lambda name, shape: nc.dram_tensor(name, list(shape), F32, kind="ExternalInput").ap()
        dt_out = lambda name, shape: nc.dram_tensor(name, list(shape), F32, kind="ExternalOutput").ap()
        L = cfg.DEPTH
        self.x = dt_in("x", [T, D])
        self.state_conv = dt_in("state_conv", [L, 4, 2, 512])
        self.cache_ckv = dt_in("cache_ckv", [L, 4, PAST, 512])
        self.cache_krope = dt_in("cache_krope", [L, 4, PAST, 64])
        self.tabT = dt_in("tabT", [128, T])
        self.tabM = dt_in("tabM", [T, 128])
        self.w = {}
        shapes = {"w_in": [L, D, DIN], "conv_w": [L, 3, 512], "sgu_ln_g": [L, 512], "sgu_ln_b": [L, 512],
                  "sgu_w": [L, 4, 128, 128], "sgu_b": [L, 4, 128], "q_norm_g": [L, 768],
                  "w_uq": [L, 768, 8 * 192], "kv_norm_g": [L, 512], "w_uk": [L, 512, 1024],
                  "w_uv": [L, 512, 1024], "mix_norm_g": [L, D], "w_o": [L, D, D], "ln1_g": [L, D],
                  "ln1_b": [L, D], "ln2_g": [L, D], "ln2_b": [L, D],
                  "ffn_w_gate": [1, D, cfg.FF], "ffn_w_up": [1, D, cfg.FF], "ffn_w_down": [1, cfg.FF, D],
                  "router_w": [1, D, NE], "moe_w_gate": [1, NE, D, cfg.EFF], "moe_w_up": [1, NE, D, cfg.EFF],
                  "moe_w_down": [1, NE, cfg.EFF, D]}
        for k in W_INPUTS:
            self.w[k] = dt_in(k, shapes[k])
        self.o_y = dt_out("y", [T, D])
        self.o_conv_p = dt_out("conv_p", [L, 2, 512])
        self.o_ckv_p = dt_out("ckv_p", [L, S, 512])
        self.o_krope_p = dt_out("krope_p", [L, S, 64])
        self.o_conv_s = dt_out("conv_s", [L, 4, 2, 512])
        self.o_ckv_s = dt_out("ckv_s", [L, 128, 512])
        self.o_krope_s = dt_out("krope_s", [L, 128, 64])
        self.o_sguv_s = dt_out("sguv_s", [L, 128, 512])
        self.hres = nc.dram_tensor("hresT", [D, T], F32, kind="Internal").ap()
        C = cfg.C
        dk = "ExternalOutput" if getattr(cfg, "debug", False) else "Internal"
        self.h1tm = nc.dram_tensor("h1tm", [T, D], F32, kind=dk).ap()
        self.xbuf = nc.dram_tensor("xbuf", [NE * C, D], BF16, kind=dk).ap()
        self.ybuf = nc.dram_tensor("ybuf", [NE * C, D], F32, kind=dk).ap()
        self.outs = []

        with contextlib.ExitStack() as st:
            st.enter_context(nc.allow_non_contiguous_dma(reason="small strided parameter loads"))
            sb = lambda n, s, d=F32: st.enter_context(nc.sbuf_tensor(n, s, d))
            RW = 18432
            self.EW = 13000
            self.RX = sb("RX", [128, RW])
            self.RY = sb("RY", [128, RW])
            self.RE = sb("RE", [128, self.EW])
            self.ident_f = sb("ident_f", [128, 128])
            self.ident_b = sb("ident_b", [128, 128], BF16)
            self.ones_f = sb("ones_f", [128, 128])
            self.eps5 = sb("eps5", [128, 1])
            self.eps6 = sb("eps6", [128, 1])
            self.mrow = sb("mrow", [1, 128], BF16)
            self.mcol = sb("mcol", [1, 128], BF16)
            self.small = sb("small", [128, 256])
            self.par = sb("par", [128, 1664])
            self.ps = [st.enter_context(nc.psum_tensor("ps%d" % i, [128, 512], F32)) for i in range(8)]
            self.setup_consts()
            if L > 1:
                zt = self.vw(self.RE, 0, 1024, BF16)
                self.memset("pool", zt, 0.0, ["zt"])
                for r in range(NE * cfg.C // 128):
                    self.dma("act", self.xbuf[r * 128:(r + 1) * 128, :], zt, ["zt"], ["xbuf"])
            try:
                for l in range(L):
                    A, B = (self.RX, self.RY) if l % 2 == 0 else (self.RY, self.RX)
                    self.layer(l, A, B)
            except _Stop:
                pass
            self.p.emit(final_waits=self.Rs(*self.outs))
        return nc

    def chk(self, name):
        if self.cfg.stop == "l%d%s" % (self.l, name):
            raise _Stop()

    def psb(self, i, n=512):
        return self.ps[i][:, 0:n]

    def psb16(self, i):
        return self.ps[i][:, :].bitcast(BF16)

    def setup_consts(self):
        self.memset("pool", self.ones_f[:], 1.0, ["ones"])
        self.memset("pool", self.ident_f[:], 1.0, ["identf"])
        idf = self.ident_f
        self.p.op("pool", lambda e: e.affine_select(out=idf[:], in_=idf[:], pattern=[[-1, 128]],
                                                    compare_op=ALU.is_equal, fill=0.0, base=0,
                                                    channel_multiplier=1),
                  reads=self.Rs("identf"), writes=self.Rs("identf"))
        self.cp("dve", self.ident_b[:], self.ident_f[:], ["identf"], ["identb"])
        self.memset("pool", self.eps5[:], 1e-5, ["eps"])
        self.memset("pool", self.eps6[:], 1e-6, ["eps"])
        self.memset("dve", self.mrow[:], 0.0, ["mask"])
        self.memset("dve", self.mrow[:, 0:64], 1.0, ["mask"])
        self.memset("dve", self.mcol[:], 0.0, ["mask"])
        self.memset("dve", self.mcol[:, 64:128], NEG, ["mask"])

    def load_params(self, l):
        par, w = self.par, self.w
        P = {}
        off = [0]

        def alloc(n):
            o = off[0]
            off[0] += n
            return par[:, o:o + n]

        def fm(name, src, k):
            ap = alloc(k)
            self.dma("sp", ap, src.rearrange("(k p) -> p k", p=128), [], ["par"])
            P[name] = ap

        def bc(name, src, n):
            ap = alloc(n)
            self.dma("sp", ap, src.rearrange("(o n) -> o n", o=1).partition_broadcast(128), [], ["par"])
            P[name] = ap

        fm("ln1g", w["ln1_g"][l], 16); fm("ln1b", w["ln1_b"][l], 16)
        fm("ln2g", w["ln2_g"][l], 16); fm("ln2b", w["ln2_b"][l], 16)
        fm("mixg", w["mix_norm_g"][l], 16)
        fm("qg", w["q_norm_g"][l], 6); fm("kvg", w["kv_norm_g"][l], 4)
        ap = alloc(12)
        for r in range(3):
            self.dma("sp", ap.rearrange("p (j r) -> p j r", j=4)[:, :, r], w["conv_w"][l][r].rearrange("(j p) -> p j", p=128), [], ["par"])
        P["convw"] = ap.rearrange("p (j r) -> p j r", j=4)
        bc("sgug", w["sgu_ln_g"][l], 512); bc("sgub", w["sgu_ln_b"][l], 512)
        bc("kvg_bc", w["kv_norm_g"][l], 512)
        ap = alloc(4)
        self.dma("sp", ap, w["sgu_b"][l].rearrange("h p -> p h"), [], ["par"])
        P["sgubias"] = ap
        ap = alloc(4)
        for b in range(4):
            self.dma("sp", ap[32 * b:32 * b + 32, :], w["sgu_b"][l][:, 0:32].rearrange("h p -> p h"), [], ["par"])
        P["sgubias_s"] = ap
        self.P = P

    def layer(self, l, A, B):
        cfg = self.cfg
        S, T, NT, NTP = cfg.S, cfg.T, cfg.NT, cfg.NTP
        self.l = l
        self.HT = self.v3(A, 0, 16, T, BF16)
        self.MT = self.v3(B, 0, 16, T, BF16)
        self._eps = self.eps5[:, 0:1]
        self.p.barrier()
        self.load_params(l)
        self.chk("par")
        if l == 0:
            self.stage0(A, B)
            self.p.barrier()
        self.chk("s0")
        self.stage1(l, A, B)
        self.p.barrier()
        self.chk("s1")
        self.stage2(l, A, B)
        self.p.barrier()
        self.chk("s2")
        self.stage3(l, A, B)
        self.p.barrier()
        self.chk("s3")
        if l % 2 == 1:
            self.stage4_sparse(l, A, B)
        else:
            self.stage4(l, A, B)
        self.chk("s4")

    def stage0(self, A, B):
        cfg = self.cfg
        T, NT = cfg.T, cfg.NT
        xs = [self.vw(B, 0, 2048), self.vw(B, 2048, 2048)]
        stg = [self.v3(B, 4096 + i * 512, 4, 128) for i in range(4)]
        nst = 0
        for t in range(NT):
            xb = xs[t % 2]
            self.dma("sp", xb, self.x[t * 128:(t + 1) * 128, :], [], ["xs%d" % (t % 2)])
            for q in range(4):
                bank = (t * 4 + q) % 6
                for j in range(4):
                    k = q * 4 + j
                    self.tr(self.ps[bank][:, j * 128:(j + 1) * 128], xb[:, k * 128:(k + 1) * 128], self.ident_f[:],
                            ["xs%d" % (t % 2), "identf"], ["ps%d" % bank])
                pv = self.ps[bank][:, :].rearrange("p (a b) -> p a b", a=4)
                self.cp("act", self.HT[:, q * 4:q * 4 + 4, t * 128:(t + 1) * 128], pv, ["ps%d" % bank], ["HT"])
                sg = stg[nst % 4]
                sn = "stg%d" % (nst % 4)
                nst += 1
                self.cp("dve", sg, pv, ["ps%d" % bank], [sn])
                self.dma("sp", self.hres[q * 512:(q + 1) * 512, t * 128:(t + 1) * 128].rearrange("(a p) n -> p a n", p=128),
                         sg, [sn], ["hres"])

    def wload(self, dst, src, name, q="pool"):
        self.dma(q, dst, src.rearrange("(k p) n -> p k n", p=128), [], [name])

    def stage1(self, l, A, B):
        cfg = self.cfg
        S, T, NT, NTP, groups = cfg.S, cfg.T, cfg.NT, cfg.NTP, cfg.groups
        P, w = self.P, self.w
        win = w["w_in"][l]
        HT, MT = self.HT, self.MT
        BH = 8704
        E = self.RE
        self.cqnT = self.v3(E, 0, 6, T, BF16)
        self.ckvnT = self.v3(E, 6528, 4, T, BF16)
        self.kropeT = self.vw(E, 10880, 1088, BF16)
        self.ckvn_new = self.v3(E, 11968, 4, 512, BF16)

        wb = [self.v3(B, BH + i * 3072, 3 * 16, 128, BF16) for i in range(2)]
        zbuf = self.vw(E, 0, S + 2)
        zs = self.v3(E, 2052, 4, 34)
        wk = [[self.vw(E, 2200 + (i * 4 + j) * 512, 512) for j in range(4)] for i in range(2)]
        convw = P["convw"]
        it = 0
        for j in range(4):
            wbj = wb[j % 2]
            wn = "cw%d" % (j % 2)
            for r, c0 in enumerate((512, 1024, 0)):
                self.wload(wbj[:, r * 16:(r + 1) * 16, :], win[:, c0 + j * 128:c0 + (j + 1) * 128], wn)
            self.memset("pool", zbuf[:, 0:2], 0.0, ["zbuf"])
            for b in range(4):
                self.dma("sp", zs[:, b, 0:2], self.state_conv[l][b][:, j * 128:(j + 1) * 128].rearrange("r p -> p r"),
                         [], ["zs"])
            for gi, (g0, n) in enumerate(groups):
                samp = gi == len(groups) - 1
                tmpc, a32, sq, rstd = wk[it % 2]
                wkn = "cwk%d" % (it % 2)
                it += 1
                pc, ph, pb = 0 + 3 * (it % 2), 1 + 3 * (it % 2), 2 + 3 * (it % 2)
                for r, bank in enumerate((pc, ph, pb)):
                    for k in range(16):
                        self.mm(self.psb(bank, n), wbj[:, r * 16 + k, :], HT[:, k, g0:g0 + n], k == 0, k == 15,
                                [wn, "HT"], ["ps%d" % bank])
                self.cp("act", tmpc[:, 0:n], self.psb(pc, n), ["ps%d" % pc], [wkn])
                if not samp:
                    zc = [zbuf[:, g0 + r:g0 + r + n] for r in range(3)]
                    zn = "zbuf"
                    self.tt("dve", zc[2], tmpc[:, 0:n], self.psb(ph, n), ALU.mult, [wkn, "ps%d" % ph], [zn])
                    yv = a32[:, 0:n]
                    pbv = self.psb(pb, n)
                    sqv, rsv = sq[:, 0:n], rstd[:, 0:n]
                    mo = MT[:, j, g0:g0 + n]
                else:
                    zc = [zs[:, :, r:r + 32] for r in range(3)]
                    zn = "zs"
                    self.tt("dve", zc[2], tmpc[:, 0:n].rearrange("p (b q) -> p b q", b=4),
                            self.psb(ph, n).rearrange("p (b q) -> p b q", b=4), ALU.mult, [wkn, "ps%d" % ph], [zn])
                    yv = a32[:, 0:n].rearrange("p (b q) -> p b q", b=4)
                    pbv = self.psb(pb, n).rearrange("p (b q) -> p b q", b=4)
                    sqv, rsv = sq[:, 0:n], rstd[:, 0:n]
                    mo = MT[:, j, g0:g0 + n]
                self.ts("dve", yv, zc[0], convw[:, j, 0:1], None, ALU.mult, None, [zn, "par"], [wkn])
                self.stt("dve", yv, zc[1], convw[:, j, 1:2], yv, ALU.mult, ALU.add, [zn, "par", wkn], [wkn])
                self.stt("dve", yv, zc[2], convw[:, j, 2:3], yv, ALU.mult, ALU.add, [zn, "par", wkn], [wkn])
                self.tt("dve", yv, yv, pbv, ALU.mult, [wkn, "ps%d" % pb], [wkn])
                self.act(sqv, a32[:, 0:n], AF.Square, [wkn], [wkn + "s"])
                self.mm(self.psb(6 + it % 2, n), self.ones_f[:], sqv, True, True, ["ones", wkn + "s"], ["ps%d" % (6 + it % 2)])
                self._eps = self.eps6[:, 0:1]
                self.rstd_from(rsv, self.psb(6 + it % 2, n), 1.0 / 128, ["ps%d" % (6 + it % 2), "eps"], [wkn + "r"], wkn + "r")
                self.stt("dve", mo, a32[:, 0:n], P["mixg"][:, j:j + 1], rsv, ALU.mult, ALU.mult,
                         [wkn, wkn + "r", "par"], ["MT"])
            self.dma("sp", self.o_conv_p[l][:, j * 128:(j + 1) * 128].rearrange("r p -> p r"), zbuf[:, S:S + 2],
                     ["zbuf"], ["o_conv_p%d_%d" % (l, j)])
            self.outs.append("o_conv_p%d_%d" % (l, j))
            for b in range(4):
                on = "o_conv_s%d_%d_%d" % (l, j, b)
                self.dma("sp", self.o_conv_s[l][b][:, j * 128:(j + 1) * 128].rearrange("r p -> p r"), zs[:, b, 32:34],
                         ["zs"], [on])
                self.outs.append(on)
        self.p.barrier()
        self.chk("p1")

        wu = self.v3(B, BH, 16, 512, BF16)
        wv = self.v3(B, BH + 4096, 16, 512, BF16)
        self.wload(wu, win[:, 1536:2048], "wu")
        self.wload(wv, win[:, 2048:2560], "wv")
        wraw = self.v3(E, 0, 4, 128)
        WT = self.v3(E, 512, 4, 128, BF16)
        WTs = self.v3(E, 768, 4, 128, BF16)
        wtf = self.v3(E, 1024, 4, 128)
        self.dma("sp", wraw, w["sgu_w"][l].rearrange("h p q -> p h q"), [], ["wraw"])
        for h in range(4):
            self.tr(self.ps[0][:, h * 128:(h + 1) * 128], wraw[:, h, :], self.ident_f[:], ["wraw", "identf"], ["ps0"])
        self.cp("dve", wtf, self.ps[0][:, :].rearrange("p (a b) -> p a b", a=4), ["ps0"], ["wtf"])
        for h in range(4):
            wslice = wtf[:, h, :]
            self.p.op("pool", (lambda ws: (lambda e: e.affine_select(out=ws, in_=ws, pattern=[[1, 128]],
                                                                       compare_op=ALU.is_ge, fill=0.0, base=0,
                                                                       channel_multiplier=-1)))(wslice),
                      reads=self.Rs("wtf"), writes=self.Rs("wtf"))
        self.cp("dve", WT, wtf, ["wtf"], ["WT"])
        self.memset("pool", WTs, 0.0, ["WTs"])
        for b in range(4):
            self.dma("sp", WTs[32 * b:32 * b + 32, :, 32 * b:32 * b + 32], WT[0:32, :, 0:32], ["WT", "WTs"], ["WTs"])
        sw = 1600
        bufs = []
        for i in range(2):
            o = sw + i * 2700
            bufs.append(dict(gu=self.vw(E, o, 512), gv=self.vw(E, o + 512, 512), sq=self.vw(E, o + 1024, 512),
                             bo=self.vw(E, o + 1536, 512), vnb=self.vw(E, o + 2048, 256, BF16),
                             bon=self.vw(E, o + 2304, 256, BF16)))
        sm = self.small
        for t in range(NT):
            samp = t == NT - 1
            bf = bufs[t % 2]
            bn = "sg%d" % (t % 2)
            gu, gv, sq, bo, vnb, bon = bf["gu"], bf["gv"], bf["sq"], bf["bo"], bf["vnb"], bf["bon"]
            pu, pv, pss, ptt = (0, 1, 2, 3) if t % 2 == 0 else (4, 5, 6, 7)
            tok = slice(t * 128, (t + 1) * 128)
            for k in range(16):
                self.mm(self.psb(pu), HT[:, k, tok], wu[:, k, :], k == 0, k == 15, ["HT", "wu"], ["ps%d" % pu])
            for k in range(16):
                self.mm(self.psb(pv), HT[:, k, tok], wv[:, k, :], k == 0, k == 15, ["HT", "wv"], ["ps%d" % pv])
            self.act(gu, self.psb(pu), AF.Gelu_apprx_tanh, ["ps%d" % pu], [bn + "gu"])
            self.act(gv, self.psb(pv), AF.Gelu_apprx_tanh, ["ps%d" % pv], [bn + "gv"])
            so = (t % 2) * 40
            s1, s2, mean, var, rs = (sm[:, so + i * 4:so + i * 4 + 4] for i in range(5))
            smn = "sm%d" % (t % 2)
            g3 = lambda a: a.rearrange("p (h c) -> p h c", h=4)
            bc3 = lambda a: a.unsqueeze(2).to_broadcast([128, 4, 128])
            self.red(s1, g3(gv), ALU.add, [bn + "gv"], [smn])
            self.tt("pool", sq, gv, gv, ALU.mult, [bn + "gv"], [bn + "sq"])
            self.red(s2, g3(sq), ALU.add, [bn + "sq"], [smn])
            self.ts("dve", mean, s1, 1.0 / 128, None, ALU.mult, None, [smn], [smn])
            self.tt("dve", var, mean, mean, ALU.mult, [smn], [smn])
            self.stt("dve", var, s2, 1.0 / 128, var, ALU.mult, ALU.subtract, [smn], [smn])
            self._eps = self.eps5[:, 0:1]
            self.rstd_from(rs, var, 1.0, [smn, "eps"], [smn], smn)
            self.tt("dve", g3(gv), g3(gv), bc3(mean), ALU.subtract, [bn + "gv", smn], [bn + "gv"])
            self.tt("dve", g3(gv), g3(gv), bc3(rs), ALU.mult, [bn + "gv", smn], [bn + "gv"])
            self.tt("dve", gv, gv, P["sgug"], ALU.mult, [bn + "gv", "par"], [bn + "gv"])
            self.tt("dve", gv, gv, P["sgub"], ALU.add, [bn + "gv", "par"], [bn + "gv"])
            if samp:
                on = "o_sguv%d" % l
                self.dma("sp", self.o_sguv_s[l], gv, [bn + "gv"], [on])
                self.outs.append(on)
            self.cp("act", vnb, gv, [bn + "gv"], [bn + "vnb"])
            wt = WTs if samp else WT
            wtn = "WTs" if samp else "WT"
            for h in range(4):
                self.mm(self.ps[pss][:, h * 128:(h + 1) * 128], wt[:, h, :], vnb[:, h * 128:(h + 1) * 128], True, True,
                        [wtn, bn + "vnb"], ["ps%d" % pss])
            bias = P["sgubias_s"] if samp else P["sgubias"]
            for h in range(4):
                hs = slice(h * 128, (h + 1) * 128)
                self.stt("dve", bo[:, hs], self.ps[pss][:, hs], bias[:, h:h + 1], gu[:, hs], ALU.add, ALU.mult,
                         ["ps%d" % pss, "par", bn + "gu"], [bn + "bo"])
            self.tt("pool", sq, bo, bo, ALU.mult, [bn + "bo"], [bn + "sq"])
            ss, rs2 = sm[:, so + 20:so + 24], sm[:, so + 24:so + 28]
            self.red(ss, g3(sq), ALU.add, [bn + "sq"], [smn])
            self._eps = self.eps6[:, 0:1]
            self.rstd_from(rs2, ss, 1.0 / 128, [smn, "eps"], [smn], smn)
            self.tt("dve", g3(bon), g3(bo), bc3(rs2), ALU.mult, [bn + "bo", smn], [bn + "bon"])
            p16 = self.psb16(ptt)
            for h in range(4):
                self.tr(p16[:, h * 128:(h + 1) * 128], bon[:, h * 128:(h + 1) * 128], self.ident_b[:],
                        [bn + "bon", "identb"], ["ps%d" % ptt])
            self.tt("dve", MT[:, 4:8, tok], p16[:, 0:512].rearrange("p (a b) -> p a b", a=4),
                    P["mixg"][:, 4:8].unsqueeze(2).to_broadcast([128, 4, 128]), ALU.mult,
                    ["ps%d" % ptt, "par"], ["MT"])
        self.p.barrier()
        self.chk("p2")

        wq = self.v3(B, BH, 16, 768, BF16)
        self.wload(wq, win[:, 2560:3328], "wq")
        sqb = [self.vw(B, BH + 6144 + i * 512, 512) for i in range(2)]
        rsb = self.vw(B, BH + 6144 + 1024, 512)
        self._eps = self.eps6[:, 0:1]
        for gi, (g0, n) in enumerate(groups):
            for m in range(6):
                for k in range(16):
                    self.mm(self.psb(m, n), wq[:, k, m * 128:(m + 1) * 128], HT[:, k, g0:g0 + n], k == 0, k == 15,
                            ["wq", "HT"], ["ps%d" % m])
            for m in range(6):
                self.act(sqb[m % 2][:, 0:n], self.psb(m, n), AF.Square, ["ps%d" % m], ["sqb%d" % (m % 2)])
                self.mm(self.psb(7, n), self.ones_f[:], sqb[m % 2][:, 0:n], m == 0, m == 5, ["ones", "sqb%d" % (m % 2)], ["ps7"])
            self.rstd_from(rsb[:, 0:n], self.psb(7, n), 1.0 / 768, ["ps7", "eps"], ["rsb"], "rsb")
            for m in range(6):
                self.stt("dve", self.cqnT[:, m, g0:g0 + n], self.psb(m, n), P["qg"][:, m:m + 1], rsb[:, 0:n],
                         ALU.mult, ALU.mult, ["ps%d" % m, "par", "rsb"], ["cqnT"])
        self.p.barrier()
        self.chk("p3")

        wkv = self.v3(B, BH, 16, 512, BF16)
        self.wload(wkv, win[:, 3328:3840], "wkv")
        wkr = self.v3(B, BH + 4096, 16, 128, BF16)
        self.wload(wkr[:, :, 0:64], win[:, 3840:3904], "wkr")
        self.ts("dve", wkr[:, :, 64:96], wkr[:, :, 32:64], -1.0, None, ALU.mult, None, ["wkr"], ["wkr"])
        self.cp("dve", wkr[:, :, 96:128], wkr[:, :, 0:32], ["wkr"], ["wkr"])
        o5 = BH + 4096 + 1024
        sqb = [self.vw(B, o5 + i * 512, 512) for i in range(2)]
        rsb = self.vw(B, o5 + 1024, 512)
        ctm = [self.vw(B, o5 + 1536 + i * 512, 512) for i in range(2)]
        ctb = self.vw(B, o5 + 2560, 256, BF16)
        junk = self.vw(B, o5 + 2816, 512)
        for gi, (g0, n) in enumerate(groups):
            for m in range(4):
                for k in range(16):
                    self.mm(self.psb(m, n), wkv[:, k, m * 128:(m + 1) * 128], HT[:, k, g0:g0 + n], k == 0, k == 15,
                            ["wkv", "HT"], ["ps%d" % m])
            for m in range(4):
                self.act(sqb[m % 2][:, 0:n], self.psb(m, n), AF.Square, ["ps%d" % m], ["sqb%d" % (m % 2)])
                self.mm(self.psb(7, n), self.ones_f[:], sqb[m % 2][:, 0:n], m == 0, m == 3, ["ones", "sqb%d" % (m % 2)], ["ps7"])
            self.rstd_from(rsb[:, 0:n], self.psb(7, n), 1.0 / 512, ["ps7", "eps"], ["rsb"], "rsb")
            for m in range(4):
                self.stt("dve", self.ckvnT[:, m, g0:g0 + n], self.psb(m, n), P["kvg"][:, m:m + 1], rsb[:, 0:n],
                         ALU.mult, ALU.mult, ["ps%d" % m, "par", "rsb"], ["ckvnT"])
        sm = self.small
        for t in range(NT):
            samp = t == NT - 1
            tok = slice(t * 128, (t + 1) * 128)
            bank = 4 + t % 2
            for k in range(16):
                self.mm(self.psb(bank), HT[:, k, tok], wkv[:, k, :], k == 0, k == 15, ["HT", "wkv"], ["ps%d" % bank])
            ss, rs = sm[:, 100 + (t % 2) * 2:101 + (t % 2) * 2], sm[:, 101 + (t % 2) * 2:102 + (t % 2) * 2]
            smn = "smk%d" % (t % 2)
            self.act(junk, self.psb(bank), AF.Square, ["ps%d" % bank], ["junk", smn], accum=ss)
            self.rstd_from(rs, ss, 1.0 / 512, [smn, "eps"], [smn], smn)
            cb = ctm[t % 2]
            cbn = "ctm%d" % (t % 2)
            self.stt("dve", cb, self.psb(bank), rs, P["kvg_bc"], ALU.mult, ALU.mult, ["ps%d" % bank, smn, "par"], [cbn])
            if not samp:
                on = "o_ckv_p%d_%d" % (l, t)
                self.dma("sp", self.o_ckv_p[l][tok, :], cb, [cbn], [on])
            else:
                on = "o_ckv_s%d" % l
                self.dma("sp", self.o_ckv_s[l], cb, [cbn], [on])
                self.cp("act", ctb, cb, [cbn], ["ctb"])
                for b in range(4):
                    self.dma("sp", self.ckvn_new[0:32, b, :], ctb[32 * b:32 * b + 32, :], ["ctb"], ["ckvn_new"])
            self.outs.append(on)
        self.chk("p4")
        tabm = [self.vw(B, o5 + 3328 + i * 128, 128) for i in range(2)]
        prod = [self.vw(B, o5 + 3584 + i * 128, 128) for i in range(2)]
        dup = [self.vw(B, o5 + 3840 + i * 128, 128) for i in range(2)]
        for t in range(NT):
            samp = t == NT - 1
            tok = slice(t * 128, (t + 1) * 128)
            i2 = t % 2
            bank = 0 + i2
            self.dma("sp", tabm[i2], self.tabM[tok, :], [], ["tabm%d" % i2])
            for k in range(16):
                self.mm(self.ps[bank][:, 0:128], HT[:, k, tok], wkr[:, k, :], k == 0, k == 15, ["HT", "wkr"], ["ps%d" % bank])
            self.tt("dve", prod[i2], self.ps[bank][:, 0:128], tabm[i2], ALU.mult, ["ps%d" % bank, "tabm%d" % i2], ["prod%d" % i2])
            self.tt("dve", dup[i2][:, 0:64], prod[i2][:, 0:64], prod[i2][:, 64:128], ALU.add, ["prod%d" % i2], ["dup%d" % i2])
            self.cp("dve", dup[i2][:, 64:128], dup[i2][:, 0:64], ["dup%d" % i2], ["dup%d" % i2])
            if not samp:
                on = "o_kr_p%d_%d" % (l, t)
                self.dma("sp", self.o_krope_p[l][tok, :], dup[i2][:, 0:64], ["dup%d" % i2], [on])
            else:
                on = "o_kr_s%d" % l
                self.dma("sp", self.o_krope_s[l], dup[i2][:, 0:64], ["dup%d" % i2], [on])
            self.outs.append(on)
            self.tr(self.ps[2 + i2][:, 0:128], dup[i2], self.ident_f[:], ["dup%d" % i2, "identf"], ["ps%d" % (2 + i2)])
            self.cp("act", self.kropeT[:, tok], self.ps[2 + i2][:, 0:128], ["ps%d" % (2 + i2)], ["kropeT"])

    def attn_tail(self, l, h, c_ps, rinv, tok, i2, A):
        P = self.P
        sm = self.small
        o = 16960 + i2 * 192
        c32 = self.vw(A, o, 128)
        cb = self.vw(A, o + 128, 64, BF16)
        n = "at%d" % i2
        ss, rs = sm[:, 120 + i2 * 2:121 + i2 * 2], sm[:, 121 + i2 * 2:122 + i2 * 2]
        if rinv is not None:
            self.ts("dve", c32, c_ps, rinv, None, ALU.mult, None, ["ps7", "sm_rinv"], [n])
        else:
            self.cp("dve", c32, c_ps, ["ps7"], [n])
        junk = self.vw(A, 16960 + 384, 64, BF16)
        self.act(junk, c32, AF.Square, [n], ["junkA", n + "s"], accum=ss)
        self._eps = self.eps6[:, 0:1]
        self.rstd_from(rs, ss, 1.0 / 128, [n + "s", "eps"], [n + "s"], n + "s")
        self.ts("dve", cb, c32, rs, None, ALU.mult, None, [n, n + "s"], [n + "b"])
        p16 = self.psb16(6)
        self.tr(p16[:, i2 * 128:(i2 + 1) * 128], cb, self.ident_b[:], [n + "b", "identb"], ["ps6"])
        self.act(self.MT[:, 8 + h, tok], p16[:, i2 * 128:(i2 + 1) * 128], AF.Copy, ["ps6", "par"], ["MT"],
                 scale=P["mixg"][:, 8 + h:9 + h])

    def stage2(self, l, A, B):
        cfg = self.cfg
        S, T, NT, NTP, PAST, groups = cfg.S, cfg.T, cfg.NT, cfg.NTP, cfg.PAST, cfg.groups
        P, w = self.P, self.w
        MT = self.MT
        sm = self.small
        tabT = self.vw(A, 0, T)
        o = 2176
        wh = []
        for i in range(2):
            wh.append(dict(qn=self.v3(A, o, 6, 128, BF16), qr=self.v3(A, o + 384, 6, 128, BF16),
                           uk=self.v3(A, o + 768, 4, 128, BF16), uv=self.v3(A, o + 1024, 4, 128, BF16),
                           ukT=self.v3(A, o + 1280, 4, 128, BF16)))
            o += 1536
        qnT = self.vw(A, o, T // 2, BF16); o += T // 2
        qrT = self.vw(A, o, T // 2, BF16); o += T // 2
        knT = self.vw(A, o, S // 2, BF16); o += S // 2
        Vh = self.v3(A, o, NTP, 128, BF16); o += NTP * 64
        Pb = [self.vw(A, o + i * ((PAST + 128) // 2), (PAST + 128) // 2, BF16) for i in range(2)]
        o += (PAST + 128)
        PTb = [self.vw(A, o + i * 512, 512, BF16) for i in range(2)]
        o += 1024
        assert o <= 12672, o
        o = 12672
        QlatT = self.vw(A, o, 2048, BF16).rearrange("p (c b h t) -> p c b h t", c=4, b=4, h=8); o += 2048
        QlatF = self.vw(A, o - 2048, 2048, BF16).rearrange("p (c b m t) -> p c b m t", c=4, b=4, m=2)
        OlatT = self.vw(A, o, 2048, BF16).rearrange("p (c h b q) -> p c h b q", c=4, h=8, b=4)
        OlatF = self.vw(A, o, 2048, BF16).rearrange("p (c h t) -> p c h t", c=4, h=8); o += 2048
        wr_tmp = self.v3(A, o, 6, 64, BF16); o += 192
        assert o <= 16960, o
        self.qs_rope = self.vw(A, 17920, 512, BF16).rearrange("p (b h t) -> p b h t", b=4, h=8)
        qs_ropeF = self.vw(A, 17920, 512, BF16).rearrange("p (b m t) -> p b m t", b=4, m=2)
        self.dma("sp", tabT, self.tabT, [], ["tabT"])
        wuq = w["w_uq"][l]
        wuk = w["w_uk"][l]
        wuv = w["w_uv"][l]
        for h in range(8):
            wb = wh[h % 2]
            wn = "wh%d" % (h % 2)
            self.wload(wb["qn"], wuq[:, h * 192:h * 192 + 128], wn)
            self.wload(wr_tmp, wuq[:, h * 192 + 128:h * 192 + 192], "wrtmp")
            self.cp("dve", wb["qr"][:, :, 0:64], wr_tmp, ["wrtmp"], [wn])
            self.ts("dve", wb["qr"][:, :, 64:96], wr_tmp[:, :, 32:64], -1.0, None, ALU.mult, None, ["wrtmp"], [wn])
            self.cp("dve", wb["qr"][:, :, 96:128], wr_tmp[:, :, 0:32], ["wrtmp"], [wn])
            self.wload(wb["uk"], wuk[:, h * 128:(h + 1) * 128], wn)
            self.wload(wb["uv"], wuv[:, h * 128:(h + 1) * 128], wn)
            for gi, (g0, n) in enumerate(groups):
                b0 = (gi % 2) * 2
                for k in range(6):
                    self.mm(self.psb(b0, n), wb["qn"][:, k, :], self.cqnT[:, k, g0:g0 + n], k == 0, k == 5,
                            [wn, "cqnT"], ["ps%d" % b0])
                self.cp("act", qnT[:, g0:g0 + n], self.psb(b0, n), ["ps%d" % b0], ["qnT"])
                for k in range(6):
                    self.mm(self.psb(b0 + 1, n), wb["qr"][:, k, :], self.cqnT[:, k, g0:g0 + n], k == 0, k == 5,
                            [wn, "cqnT"], ["ps%d" % (b0 + 1)])
                self.tt("dve", qrT[:, g0:g0 + n], self.psb(b0 + 1, n), tabT[:, g0:g0 + n], ALU.mult,
                        ["ps%d" % (b0 + 1), "tabT"], ["qrT"])
            self.cp("dve", self.qs_rope[:, :, h, :], qrT[:, S:S + 128].rearrange("p (b t) -> p b t", b=4), ["qrT"], ["qs"])
            for gi, (g0, n) in enumerate(groups[:-1]):
                b0 = 4 + gi % 2
                for k in range(4):
                    self.mm(self.psb(b0, n), wb["uk"][:, k, :], self.ckvnT[:, k, g0:g0 + n], k == 0, k == 3,
                            [wn, "ckvnT"], ["ps%d" % b0])
                self.cp("act", knT[:, g0:g0 + n], self.psb(b0, n), ["ps%d" % b0], ["knT"])
            for t0 in range(0, NTP, 4):
                bank = 6 + (t0 // 4) % 2
                nt = min(4, NTP - t0)
                for j in range(nt):
                    t = t0 + j
                    for k in range(4):
                        self.mm(self.ps[bank][:, j * 128:(j + 1) * 128], self.ckvnT[:, k, t * 128:(t + 1) * 128],
                                wb["uv"][:, k, :], k == 0, k == 3, ["ckvnT", wn], ["ps%d" % bank])
                self.cp("dve", Vh[:, t0:t0 + nt, :], self.ps[bank][:, 0:nt * 128].rearrange("p (a b) -> p a b", a=nt),
                        ["ps%d" % bank], ["Vh"])
            p16 = self.psb16(5)
            for c in range(4):
                self.tr(p16[:, c * 128:(c + 1) * 128], wb["uk"][:, c, :], self.ident_b[:], [wn, "identb"], ["ps5"])
            self.cp("dve", wb["ukT"], p16[:, 0:512].rearrange("p (a b) -> p a b", a=4), ["ps5"], [wn + "T"])
            for c in range(4):
                self.mm(self.ps[4][:, c * 128:(c + 1) * 128], wb["ukT"][:, c, :], qnT[:, S:S + 128], True, True,
                        [wn + "T", "qnT"], ["ps4"])
            self.cp("act", QlatT[:, :, :, h, :], self.ps[4][:, :].rearrange("p (c b t) -> p c b t", c=4, b=4), ["ps4"], ["QlatT"])
            for i in range(NTP):
                nk = (i + 1) * 128
                nb = (nk + 511) // 512
                qs = slice(i * 128, (i + 1) * 128)
                Pq = Pb[i % 2]
                Pn = "P%d" % (i % 2)
                so = 40 * (i % 2) + 140
                mx = sm[:, so:so + 4]
                rs4 = sm[:, so + 4:so + 8]
                m1, negm, rsum, rinv = (sm[:, so + 8 + j:so + 9 + j] for j in range(4))
                smn = "sma%d" % (i % 2)
                for kb in range(nb):
                    ksz = min(512, nk - kb * 512)
                    ks = slice(kb * 512, kb * 512 + ksz)
                    last = kb == nb - 1
                    self.mm(self.psb(kb, ksz), qnT[:, qs], knT[:, ks], True, False, ["qnT", "knT"], ["ps%d" % kb])
                    self.mm(self.psb(kb, ksz), qrT[:, qs], self.kropeT[:, ks], False, not last, ["qrT", "kropeT"], ["ps%d" % kb])
                    if last:
                        self.mm(self.ps[kb][:, ksz - 128:ksz], self.mrow[:], self.mcol[:], False, True, ["mask"], ["ps%d" % kb])
                    self.red(mx[:, kb:kb + 1], self.psb(kb, ksz), ALU.max, ["ps%d" % kb], [smn + "m"])
                self.red(m1, mx[:, 0:nb], ALU.max, [smn + "m"], [smn + "m"])
                self.ts("dve", negm, m1, -SM_SCALE, None, ALU.mult, None, [smn + "m"], [smn + "n"])
                for kb in range(nb):
                    ksz = min(512, nk - kb * 512)
                    ks = slice(kb * 512, kb * 512 + ksz)
                    self.act(Pq[:, ks], self.psb(kb, ksz), AF.Exp, ["ps%d" % kb, smn + "n"], [Pn, smn + "r"],
                             bias=negm, scale=SM_SCALE, accum=rs4[:, kb:kb + 1])
                self.red(rsum, rs4[:, 0:nb], ALU.add, [smn + "r"], [smn + "r"])
                self.recip(rinv, rsum, [smn + "r"], ["sm_rinv"])
                nblk = i + 1
                for c0 in range(0, nblk, 8):
                    cn = min(8, nblk - c0)
                    pi = (c0 // 8) % 2
                    p16 = self.psb16(4 + pi)
                    for j in range(cn):
                        kk = c0 + j
                        self.tr(p16[:, j * 128:(j + 1) * 128], Pq[:, kk * 128:(kk + 1) * 128], self.ident_b[:],
                                [Pn, "identb"], ["ps%d" % (4 + pi)])
                    eng = "act" if pi == 0 else "dve"
                    self.cp(eng, PTb[pi][:, 0:cn * 128], p16[:, 0:cn * 128], ["ps%d" % (4 + pi)], ["PT%d" % pi])
                    for j in range(cn):
                        kk = c0 + j
                        self.mm(self.ps[7][:, 0:128], PTb[pi][:, j * 128:(j + 1) * 128], Vh[:, kk, :], kk == 0, kk == nblk - 1,
                                ["PT%d" % pi, "Vh"], ["ps7"])
                self.attn_tail(l, h, self.ps[7][:, 0:128], rinv, qs, i % 2, A)
        self.p.barrier()
        self.chk("s2h")
        KTc = PAST // 128
        o = 0
        ckv = self.v3(A, o, KTc, 512, BF16); o += KTc * 256
        kr2 = self.v3(A, o, KTc, 128, BF16); o += KTc * 64
        ckvT = self.v3(A, o, 4, PAST, BF16); o += 2 * PAST
        krT = self.vw(A, o, PAST // 2, BF16); o += PAST // 2
        Ps = self.vw(A, o, (PAST + 128) // 2, BF16); o += (PAST + 128) // 2
        PTs = self.v3(A, o, KTc + 1, 128, BF16); o += (KTc + 1) * 64
        olb = self.vw(A, o, 256, BF16); o += 256
        assert o <= 12672, o
        uvb = [self.v3(A, i * 256, 4, 128, BF16) for i in range(2)]
        for b in range(4):
            self.wload(ckv, self.cache_ckv[l][b], "ckv")
            self.dma("pool", kr2[:, :, 0:64], self.cache_krope[l][b].rearrange("(k p) n -> p k n", p=128), [], ["kr2"])
            self.dma("pool", kr2[:, :, 64:128], self.cache_krope[l][b].rearrange("(k p) n -> p k n", p=128), [], ["kr2"])
            nbk = 0
            for c in range(4):
                for t0 in range(0, KTc, 8):
                    cn = min(8, KTc - t0)
                    bank = nbk % 4
                    nbk += 1
                    p16 = self.psb16(bank)
                    for j in range(cn):
                        self.tr(p16[:, j * 128:(j + 1) * 128], ckv[:, t0 + j, c * 128:(c + 1) * 128], self.ident_b[:],
                                ["ckv", "identb"], ["ps%d" % bank])
                    self.cp("act" if nbk % 2 else "dve", ckvT[:, c, t0 * 128:(t0 + cn) * 128], p16[:, 0:cn * 128],
                            ["ps%d" % bank], ["ckvT"])
            for t0 in range(0, KTc, 8):
                cn = min(8, KTc - t0)
                bank = nbk % 4
                nbk += 1
                p16 = self.psb16(bank)
                for j in range(cn):
                    self.tr(p16[:, j * 128:(j + 1) * 128], kr2[:, t0 + j, :], self.ident_b[:], ["kr2", "identb"], ["ps%d" % bank])
                self.cp("act" if nbk % 2 else "dve", krT[:, t0 * 128:(t0 + cn) * 128], p16[:, 0:cn * 128],
                        ["ps%d" % bank], ["krT"])
            newk = slice(S + 32 * b, S + 32 * b + 32)
            nblk = (PAST + 511) // 512
            for mt in range(2):
                hs = slice(4 * mt, 4 * mt + 4)
                qlat = lambda c: QlatF[:, c, b, mt, :]
                qrp = qs_ropeF[:, b, mt, :]
                so = 180
                mx = sm[:, so:so + 8]
                rs8 = sm[:, so + 8:so + 16]
                m1, negm, rsum, rinv = (sm[:, so + 16 + j:so + 17 + j] for j in range(4))
                for kb in range(nblk + 1):
                    bank = kb
                    if kb < nblk:
                        ksz = min(512, PAST - kb * 512)
                        ks = slice(kb * 512, kb * 512 + ksz)
                        for c in range(4):
                            self.mm(self.psb(bank, ksz), qlat(c), ckvT[:, c, ks], c == 0, False, ["QlatT", "ckvT"], ["ps%d" % bank])
                        self.mm(self.psb(bank, ksz), qrp, krT[:, ks], False, True, ["qs", "krT"], ["ps%d" % bank])
                    else:
                        ksz = 32
                        for c in range(4):
                            self.mm(self.psb(bank, ksz), qlat(c), self.ckvnT[:, c, newk], c == 0, False, ["QlatT", "ckvnT"], ["ps%d" % bank])
                        self.mm(self.psb(bank, ksz), qrp, self.kropeT[:, newk], False, True, ["qs", "kropeT"], ["ps%d" % bank])
                    self.red(mx[:, kb:kb + 1], self.psb(bank, ksz), ALU.max, ["ps%d" % bank], ["smsm"])
                self.red(m1, mx[:, 0:nblk + 1], ALU.max, ["smsm"], ["smsm"])
                self.ts("dve", negm, m1, -SM_SCALE, None, ALU.mult, None, ["smsm"], ["smsn"])
                for kb in range(nblk + 1):
                    if kb < nblk:
                        ksz = min(512, PAST - kb * 512)
                        ks = slice(kb * 512, kb * 512 + ksz)
                    else:
                        ksz = 32
                        ks = slice(PAST, PAST + 32)
                    self.act(Ps[:, ks], self.psb(kb, ksz), AF.Exp, ["ps%d" % kb, "smsn"], ["Ps", "smsr"],
                             bias=negm, scale=SM_SCALE, accum=rs8[:, kb:kb + 1])
                self.red(rsum, rs8[:, 0:nblk + 1], ALU.add, ["smsr"], ["smsr"])
                self.recip(rinv, rsum, ["smsr"], ["smsr"])
                for t0 in range(0, KTc, 8):
                    cn = min(8, KTc - t0)
                    bank = 5 + (t0 // 8) % 2
                    p16 = self.psb16(bank)
                    for j in range(cn):
                        self.tr(p16[:, j * 128:(j + 1) * 128], Ps[:, (t0 + j) * 128:(t0 + j + 1) * 128], self.ident_b[:],
                                ["Ps", "identb"], ["ps%d" % bank])
                    self.cp("act" if (t0 // 8) % 2 else "dve", PTs[:, t0:t0 + cn, :],
                            p16[:, 0:cn * 128].rearrange("p (a b) -> p a b", a=cn), ["ps%d" % bank], ["PTs"])
                p16 = self.psb16(5)
                self.tr(p16[0:32, 0:128], Ps[:, PAST:PAST + 32], self.ident_b[:], ["Ps", "identb"], ["ps5"])
                self.cp("dve", PTs[0:32, KTc, :], p16[0:32, 0:128], ["ps5"], ["PTs"])
                for kt in range(KTc):
                    self.mm(self.psb(7), PTs[:, kt, :], ckv[:, kt, :], kt == 0, False, ["PTs", "ckv"], ["ps7"])
                self.mm(self.psb(7), PTs[0:32, KTc, :], self.ckvn_new[0:32, b, :], False, True, ["PTs", "ckvn_new"], ["ps7"])
                self.ts("dve", olb, self.psb(7), rinv, None, ALU.mult, None, ["ps7", "smsr"], ["olb"])
                p16 = self.psb16(6)
                for c in range(4):
                    self.tr(p16[:, c * 128:(c + 1) * 128], olb[:, c * 128:(c + 1) * 128], self.ident_b[:], ["olb", "identb"], ["ps6"])
                for c in range(4):
                    self.cp("act" if c % 2 else "dve", OlatT[:, c, hs, b, :],
                            p16[:, c * 128:(c + 1) * 128].rearrange("p (h q) -> p h q", h=4), ["ps6"], ["OlatT"])
        self.p.barrier()
        self.chk("s2s")
        for h in range(8):
            ub = uvb[h % 2]
            un = "uvb%d" % (h % 2)
            self.wload(ub, wuv[:, h * 128:(h + 1) * 128], un)
            for c in range(4):
                self.mm(self.ps[7][:, 0:128], OlatF[:, c, h, :], ub[:, c, :], c == 0, c == 3, ["OlatT", un], ["ps7"])
            self.attn_tail(l, h, self.ps[7][:, 0:128], None, slice(S, S + 128), h % 2, A)

    def ln_apply(self, l, which, src_of, g0, n, gi, mean_bc, rstd_bc, work, last, A, B, dst_bf, ytm=None, tm_dst=None):
        P = self.P
        g, bta = P[which + "g"], P[which + "b"]
        if last:
            tm_dst = self.o_y
        tm = tm_dst is not None
        step = 256 if tm else n
        for c0 in range(0, n, step):
            nn = min(step, n - c0)
            cs = slice(c0, c0 + nn)
            for m in range(16):
                t1 = work[0][m % 2][:, 0:nn]
                o32 = work[1][m % 2][:, 0:nn]
                n1, n2 = "lnw%d" % (m % 2), "lno%d" % (m % 2)
                self.tt("pool", t1, src_of(m)[:, cs], mean_bc[:, cs], ALU.subtract, ["lnsrc", "lnmean"], [n1])
                self.tt("dve", t1, t1, rstd_bc[:, cs], ALU.mult, [n1, "lnrstd"], [n1])
                if not last:
                    self.act(dst_bf[:, m, g0 + c0:g0 + c0 + nn], t1, AF.Identity, [n1, "par"], ["MTout"],
                             scale=g[:, m:m + 1], bias=bta[:, m:m + 1])
                self.ts("dve", o32, t1, g[:, m:m + 1], bta[:, m:m + 1], ALU.mult, ALU.add, [n1, "par"], [n2])
                if not tm:
                    self.dma("sp", self.hres[m * 128:(m + 1) * 128, g0 + c0:g0 + c0 + nn], o32, [n2], ["hres%d" % gi])
                else:
                    for j in range(nn // 128):
                        bank = 4 + j % 2
                        pcol = self.ps[bank][:, (m % 4) * 128:(m % 4 + 1) * 128]
                        self.tr(pcol, o32[:, j * 128:(j + 1) * 128], self.ident_f[:], [n2, "identf"], ["ps%d" % bank])
                        self.cp("act" if j % 2 else "dve", ytm[j][:, m * 128:(m + 1) * 128], pcol, ["ps%d" % bank], ["ytm%d" % j])
            if tm:
                for j in range(nn // 128):
                    t0 = g0 + c0 + j * 128
                    if last:
                        on = "o_y_%d" % t0
                        self.outs.append(on)
                    else:
                        on = "h1tm"
                    self.dma("sp", tm_dst[t0:t0 + 128, :], ytm[j], ["ytm%d" % j], [on])

    def ln_stats(self, n, work):
        mean_bc, rstd_bc, tmp = work
        self.act(mean_bc, self.psb(6, n), AF.Copy, ["ps6"], ["lnmean"], scale=1.0 / D)
        self.tt("dve", tmp, mean_bc, mean_bc, ALU.mult, ["lnmean"], ["lntmp"])
        self.stt("dve", tmp, self.psb(7, n), 1.0 / D, tmp, ALU.mult, ALU.subtract, ["ps7", "lntmp"], ["lntmp"])
        self._eps = self.eps5[:, 0:1]
        self.rstd_from(rstd_bc, tmp, 1.0, ["lntmp", "eps"], ["lnrstd"], "lnrstd")

    def stage3(self, l, A, B):
        cfg = self.cfg
        S, T, NT, groups = cfg.S, cfg.T, cfg.NT, cfg.groups
        P, w = self.P, self.w
        MT = self.MT
        wo = w["w_o"][l]
        o = 0
        rT = self.v3(A, o, 16, 512); o += 8192
        wob = [self.v3(A, o + i * 1024, 16, 128, BF16) for i in range(3)]; o += 3072
        res32 = [self.vw(A, o + i * 512, 512) for i in range(2)]; o += 1024
        sqb = [self.vw(A, o + i * 512, 512) for i in range(2)]; o += 1024
        mean_bc = self.vw(A, o, 512); o += 512
        rstd_bc = self.vw(A, o, 512); o += 512
        tmp = self.vw(A, o, 512); o += 512
        w0 = [self.vw(A, o + i * 512, 512) for i in range(2)]; o += 1024
        w1 = [self.vw(A, o + i * 512, 512) for i in range(2)]; o += 1024
        it = 0
        for gi, (g0, n) in enumerate(groups):
            for m in range(16):
                wb = wob[it % 3]
                wn = "wo%d" % (it % 3)
                it += 1
                self.wload(wb, wo[:, m * 128:(m + 1) * 128], wn)
                bank = m % 4
                for k in range(16):
                    self.mm(self.psb(bank, n), wb[:, k, :], MT[:, k, g0:g0 + n], k == 0, k == 15, [wn, "MT"], ["ps%d" % bank])
                rb = res32[m % 2][:, 0:n]
                self.dma("sp", rb, self.hres[m * 128:(m + 1) * 128, g0:g0 + n], ["hres%d" % gi], ["res%d" % (m % 2)])
                self.stt("dve", rT[:, m, 0:n], rb, ALPHA, self.psb(bank, n), ALU.mult, ALU.add,
                         ["res%d" % (m % 2), "ps%d" % bank], ["lnsrc"])
                sq = sqb[m % 2][:, 0:n]
                self.act(sq, rT[:, m, 0:n], AF.Square, ["lnsrc"], ["lsq%d" % (m % 2)])
                self.mm(self.psb(6, n), self.ones_f[:], rT[:, m, 0:n], m == 0, m == 15, ["ones", "lnsrc"], ["ps6"])
                self.mm(self.psb(7, n), self.ones_f[:], sq, m == 0, m == 15, ["ones", "lsq%d" % (m % 2)], ["ps7"])
            self.ln_stats(n, (mean_bc[:, 0:n], rstd_bc[:, 0:n], tmp[:, 0:n]))
            if l % 2 == 1:
                ytm = [self.vw(self.RE, j * 2048, 2048) for j in range(2)]
                self.ln_apply(l, "ln1", lambda m: rT[:, m, 0:n], g0, n, gi, mean_bc[:, 0:n], rstd_bc[:, 0:n], (w0, w1),
                              False, A, B, MT, ytm=ytm, tm_dst=self.h1tm)
            else:
                self.ln_apply(l, "ln1", lambda m: rT[:, m, 0:n], g0, n, gi, mean_bc[:, 0:n], rstd_bc[:, 0:n], (w0, w1),
                              False, A, B, MT)

    def stage4(self, l, A, B):
        cfg = self.cfg
        S, T, NT, groups = cfg.S, cfg.T, cfg.NT, cfg.groups
        P, w = self.P, self.w
        H1 = self.MT
        E = self.RE
        moe = (l % 2 == 1)
        last = (l == cfg.DEPTH - 1)
        sm = self.small
        acc = self.v3(A, 0, 16, 1152)
        FW = 1
        o = 0
        wgb = [self.v3(E, o + i * 1024, 16, 128, BF16) for i in range(2)]; o += 2048
        wub = [self.v3(E, o + i * 1024, 16, 128, BF16) for i in range(2)]; o += 2048
        wdb = [self.v3(E, o + i * 1024, 1, 2048, BF16) for i in range(2)]; o += 2048
        sil = [self.vw(E, o + i * 512, 512) for i in range(2)]; o += 1024
        actb = [self.v3(E, o + i * 256, 1, 512, BF16) for i in range(2)]; o += 512
        gbc = self.vw(E, o, 1152); o += 1152
        assert o <= 8832
        o = 8832
        if moe:
            wr = self.v3(E, o, 16, 8, BF16); o += 64
            dg = self.v3(E, o, NT, 8); o += NT * 8
            dgb = self.vw(E, o, 128); o += 128
            eq = self.vw(E, o, 8); o += 8
            self.wload(wr, w["router_w"][0], "wr")
            for t in range(NT):
                tok = slice(t * 128, (t + 1) * 128)
                for k in range(16):
                    self.mm(self.ps[0][:, 0:8], H1[:, k, tok], wr[:, k, :], k == 0, k == 15, ["MTout" if False else "MT", "wr"], ["ps0"])
                lg = sm[:, 200:208]
                m8 = sm[:, 208:216]
                d12, e12, g1, g2 = (sm[:, 216 + j:217 + j] for j in range(4))
                self.cp("dve", lg, self.ps[0][:, 0:8], ["ps0"], ["rt"])
                self.p.op("dve", (lambda a, b: (lambda e: e.max(out=a, in_=b)))(m8, lg), reads=self.Rs("rt"), writes=self.Rs("rt8"))
                self.tt("dve", d12, m8[:, 1:2], m8[:, 0:1], ALU.subtract, ["rt8"], ["rtd"])
                self.act(e12, d12, AF.Exp, ["rtd"], ["rte"])
                self.ts("dve", g1, e12, 1.0, None, ALU.add, None, ["rte"], ["rtg"])
                self.recip(g1, g1, ["rtg"], ["rtg"])
                self.ts("dve", g2, g1, -1.0, 1.0, ALU.mult, ALU.add, ["rtg"], ["rtg2"])
                self.ts("dve", eq, lg, m8[:, 0:1], g1, ALU.is_equal, ALU.mult, ["rt", "rt8", "rtg"], ["rteq"])
                self.ts("dve", dg[:, t, :], lg, m8[:, 1:2], g2, ALU.is_equal, ALU.mult, ["rt", "rt8", "rtg2"], ["dg"])
                self.tt("dve", dg[:, t, :], dg[:, t, :], eq, ALU.add, ["dg", "rteq"], ["dg"])
        lw = o
        mean_bc = self.vw(E, lw, 512); rstd_bc = self.vw(E, lw + 512, 512); tmp = self.vw(E, lw + 1024, 512)
        assert lw + 1536 <= self.EW, lw
        experts = list(range(NE)) if moe else [None]
        FFd = cfg.EFF if moe else cfg.FF
        nchunk = FFd // (128 * FW)
        it = 0
        it2 = 0
        for sgi, sg in enumerate(cfg.sgs):
            sg0 = groups[sg[0]][0]
            first = True
            for e in experts:
                if moe:
                    wg_d, wu_d, wd_d = w["moe_w_gate"][0][e], w["moe_w_up"][0][e], w["moe_w_down"][0][e]
                    for gi in sg:
                        g0, n = groups[gi]
                        for j in range(n // 128):
                            t = g0 // 128 + j
                            self.cp("dve", dgb, dg[:, t, e:e + 1].to_broadcast([128, 128]), ["dg"], ["dgb"])
                            self.mm(self.ps[5][:, j * 128:(j + 1) * 128], dgb, self.ident_f[:], True, True, ["dgb", "identf"], ["ps5"])
                        self.cp("act", gbc[:, g0 - sg0:g0 - sg0 + n], self.psb(5, n), ["ps5"], ["gbc"])
                else:
                    wg_d, wu_d, wd_d = w["ffn_w_gate"][0], w["ffn_w_up"][0], w["ffn_w_down"][0]
                for fc in range(nchunk):
                    i2 = it % 2
                    it += 1
                    wgn, wun, wdn = "wg%d" % i2, "wu%d" % i2, "wd%d" % i2
                    c0 = fc * 128 * FW
                    self.wload(wgb[i2], wg_d[:, c0:c0 + 128 * FW], wgn)
                    self.wload(wub[i2], wu_d[:, c0:c0 + 128 * FW], wun)
                    self.wload(wdb[i2], wd_d[c0:c0 + 128 * FW, :], wdn)
                    for gi in sg:
                        g0, n = groups[gi]
                        ab = actb[gi % 2]
                        abn = "actb%d" % (gi % 2)
                        for f in range(FW):
                            bg, bu = ((0, 1), (2, 3))[(it2 := it2 + 1) % 2]
                            for k in range(16):
                                self.mm(self.psb(bg, n), wgb[i2][:, k, f * 128:(f + 1) * 128], H1[:, k, g0:g0 + n], k == 0, k == 15,
                                        [wgn, "MT"], ["ps%d" % bg])
                            for k in range(16):
                                self.mm(self.psb(bu, n), wub[i2][:, k, f * 128:(f + 1) * 128], H1[:, k, g0:g0 + n], k == 0, k == 15,
                                        [wun, "MT"], ["ps%d" % bu])
                            sl = sil[it2 % 2][:, 0:n]
                            sn = "sil%d" % (it2 % 2)
                            self.act(sl, self.psb(bg, n), AF.Silu, ["ps%d" % bg], [sn])
                            if moe:
                                self.tt("pool", sl, sl, gbc[:, g0 - sg0:g0 - sg0 + n], ALU.mult, [sn, "gbc"], [sn])
                            self.tt("dve", ab[:, f, 0:n], sl, self.psb(bu, n), ALU.mult, [sn, "ps%d" % bu], [abn])
                        for m in range(16):
                            bank = 4 + m % 2 if not moe else 6 + m % 2
                            for f in range(FW):
                                self.mm(self.psb(bank, n), wdb[i2][:, f, m * 128:(m + 1) * 128], ab[:, f, 0:n], f == 0, f == FW - 1,
                                        [wdn, abn], ["ps%d" % bank])
                            av = acc[:, m, g0 - sg0:g0 - sg0 + n]
                            if first:
                                self.cp("dve", av, self.psb(bank, n), ["ps%d" % bank], ["acc"])
                            else:
                                self.tt("dve", av, av, self.psb(bank, n), ALU.add, ["acc", "ps%d" % bank], ["acc"])
                    first = False
            self.p.barrier()
            res32 = [self.vw(E, i * 512, 512) for i in range(2)]
            sqb = [self.vw(E, 1024 + i * 512, 512) for i in range(2)]
            w0 = [self.vw(E, 2048 + i * 512, 512) for i in range(2)]
            w1 = [self.vw(E, 3072 + i * 512, 512) for i in range(2)]
            ytm = [self.vw(E, 4096 + j * 2048, 2048) for j in range(2)]
            for gi in sg:
                g0, n = groups[gi]
                for m in range(16):
                    rb = res32[m % 2][:, 0:n]
                    self.dma("sp", rb, self.hres[m * 128:(m + 1) * 128, g0:g0 + n], ["hres%d" % gi], ["res%d" % (m % 2)])
                    av = acc[:, m, g0 - sg0:g0 - sg0 + n]
                    self.stt("dve", av, rb, ALPHA, av, ALU.mult, ALU.add, ["res%d" % (m % 2), "acc"], ["lnsrc"])
                    sq = sqb[m % 2][:, 0:n]
                    self.act(sq, av, AF.Square, ["lnsrc"], ["lsq%d" % (m % 2)])
                    self.mm(self.psb(6, n), self.ones_f[:], av, m == 0, m == 15, ["ones", "lnsrc"], ["ps6"])
                    self.mm(self.psb(7, n), self.ones_f[:], sq, m == 0, m == 15, ["ones", "lsq%d" % (m % 2)], ["ps7"])
                self.ln_stats(n, (mean_bc[:, 0:n], rstd_bc[:, 0:n], tmp[:, 0:n]))
                self.ln_apply(l, "ln2", lambda m: acc[:, m, g0 - sg0:g0 - sg0 + n], g0, n, gi, mean_bc[:, 0:n],
                              rstd_bc[:, 0:n], (w0, w1), last, A, B, H1, ytm)
            self.p.barrier()

    def stage4_sparse(self, l, A, B):
        cfg = self.cfg
        S, T, NT, C = cfg.S, cfg.T, cfg.NT, cfg.C
        P, w = self.P, self.w
        H1 = self.MT
        E = self.RE
        sm = self.small
        NS = C // 128
        NSL = NE * C
        cgroups = [(c0, min(512, C - c0)) for c0 in range(0, C, 512)]
        v3, vw = self.v3, self.vw
        o = 0
        wgb = [v3(E, o + i * 1024, 16, 128, BF16) for i in range(2)]; o += 2048
        wub = [v3(E, o + i * 1024, 16, 128, BF16) for i in range(2)]; o += 2048
        wdb = [v3(E, o + i * 1024, 1, 2048, BF16) for i in range(2)]; o += 2048
        sil = [vw(E, o + i * 512, 512) for i in range(2)]; o += 1024
        actb = [vw(E, o + i * (C // 2), C // 2, BF16) for i in range(2)]; o += C
        wr = v3(E, o, 16, 8, BF16); o += 64
        n8 = NT * 8
        EQ1 = v3(E, o, NT, 8); o += n8
        EQ2 = v3(E, o, NT, 8); o += n8
        POS = v3(E, o, NT, 8); o += n8
        GI = v3(E, o, NT, 8); o += n8
        TM1 = v3(E, o, NT, 8); o += n8
        TM2 = v3(E, o, NT, 8); o += n8
        G1 = vw(E, o, NT); o += NT
        G2 = vw(E, o, NT); o += NT
        IDX = [vw(E, o + i * NT, NT) for i in range(2)]; o += 2 * NT
        IDXg = [vw(E, o + i * NT, NT) for i in range(2)]; o += 2 * NT
        VAL = vw(E, o, NT); o += NT
        IDXi = [vw(E, o + i * NT, NT).bitcast(I32) for i in range(2)]; o += 2 * NT
        IDXgi = [vw(E, o + i * NT, NT).bitcast(I32) for i in range(2)]; o += 2 * NT
        EOFF = vw(E, o, 8); o += 8
        UT = vw(E, o, 128); o += 128
        xtm = [vw(E, o + i * 1024, 1024, BF16) for i in range(2)]; o += 2048
        assert o <= self.EW, o
        self.wload(wr, w["router_w"][0], "wr")
        self.memset("pool", UT, 1.0, ["UT"])
        self.p.op("pool", lambda e: e.affine_select(out=UT, in_=UT, pattern=[[1, 128]], compare_op=ALU.is_ge, fill=0.0,
                                                    base=-1, channel_multiplier=-1),
                  reads=self.Rs("UT"), writes=self.Rs("UT"))
        for e_ in range(NE):
            self.memset("pool", EOFF[:, e_:e_ + 1], float(e_ * C), ["EOFF"])
        for t in range(NT):
            tok = slice(t * 128, (t + 1) * 128)
            for k in range(16):
                self.mm(self.ps[0][:, 0:8], H1[:, k, tok], wr[:, k, :], k == 0, k == 15, ["MT", "wr"], ["ps0"])
            lg = sm[:, 200:208]
            m8 = sm[:, 208:216]
            d12, e12 = sm[:, 216:217], sm[:, 217:218]
            self.cp("dve", lg, self.ps[0][:, 0:8], ["ps0"], ["rt"])
            self.p.op("dve", (lambda a, b: (lambda e: e.max(out=a, in_=b)))(m8, lg), reads=self.Rs("rt"), writes=self.Rs("rt8"))
            self.tt("dve", d12, m8[:, 1:2], m8[:, 0:1], ALU.subtract, ["rt8"], ["rtd"])
            self.act(e12, d12, AF.Exp, ["rtd"], ["rte"])
            self.ts("dve", G1[:, t:t + 1], e12, 1.0, None, ALU.add, None, ["rte"], ["G"])
            self.recip(G1[:, t:t + 1], G1[:, t:t + 1], ["G"], ["G"])
            self.ts("dve", G2[:, t:t + 1], G1[:, t:t + 1], -1.0, 1.0, ALU.mult, ALU.add, ["G"], ["G"])
            self.ts("dve", EQ1[:, t, :], lg, m8[:, 0:1], None, ALU.is_equal, None, ["rt", "rt8"], ["EQ"])
            self.ts("dve", EQ2[:, t, :], lg, m8[:, 1:2], None, ALU.is_equal, None, ["rt", "rt8"], ["EQ"])
        self.tt("dve", TM1, EQ1, EQ2, ALU.add, ["EQ"], ["MASK"])
        for t in range(NT):
            for t2 in range(t + 1):
                lhsT = UT if t2 == t else self.ones_f[:]
                self.mm(self.ps[1][:, t * 8:(t + 1) * 8], lhsT, TM1[:, t2, :], t2 == 0, t2 == t, ["UT", "ones", "MASK"], ["ps1"])
        self.cp("dve", POS, self.ps[1][:, 0:n8].rearrange("p (a b) -> p a b", a=NT), ["ps1"], ["POS"])
        self.tt("dve", GI, POS, EOFF.unsqueeze(1).to_broadcast([128, NT, 8]), ALU.add, ["POS", "EOFF"], ["GI"])
        self.ts("dve", TM2, POS, float(C), 1.0e6, ALU.is_ge, ALU.mult, ["POS"], ["TM2"])
        self.tt("dve", GI, GI, TM2, ALU.add, ["GI", "TM2"], ["GI"])
        for k_, EQ in enumerate((EQ1, EQ2)):
            self.tt("dve", TM2, EQ, GI, ALU.mult, ["EQ", "GI"], ["TM2"])
            self.red(IDX[k_], TM2, ALU.add, ["TM2"], ["IDX"])
            G = (G1, G2)[k_]
            self.ts("dve", VAL, IDX[k_], float(NSL), None, ALU.is_lt, None, ["IDX"], ["VAL"])
            self.tt("dve", G, G, VAL, ALU.mult, ["G", "VAL"], ["G"])
            self.ts("dve", IDXg[k_], IDX[k_], float(NSL - 1), None, ALU.min, None, ["IDX"], ["IDXg"])
            self.cp("dve", IDXi[k_], IDX[k_], ["IDX"], ["IDXi"])
            self.cp("dve", IDXgi[k_], IDXg[k_], ["IDXg"], ["IDXgi"])
        self.p.barrier()
        hb = [vw(A, i * 2048, 2048) for i in range(2)]
        hbb = [vw(A, 4096 + i * 1024, 1024, BF16) for i in range(2)]
        xbuf, ybuf = self.xbuf, self.ybuf
        for t in range(NT):
            i2 = t % 2
            self.dma("sp", hb[i2], self.h1tm[t * 128:(t + 1) * 128, :], ["h1tm"], ["hb%d" % i2])
            self.cp("act", hbb[i2], hb[i2], ["hb%d" % i2], ["hbb%d" % i2])
            for k_ in range(2):
                self.p.op("pool", (lambda src, ix: (lambda e: e.indirect_dma_start(
                    out=xbuf[:, :], out_offset=bass.IndirectOffsetOnAxis(ap=ix, axis=0), in_=src, in_offset=None,
                    bounds_check=NSL - 1, oob_is_err=False)))(hbb[i2], IDXi[k_][:, t:t + 1]),
                    reads=self.Rs("hbb%d" % i2, "IDXi"), writes=self.Rs("xbuf"), dma=True)
        self.p.barrier()
        XT = [v3(B, i * 8 * C, 16, C, BF16) for i in range(2)]
        assert 16 * C <= 18432 and NS * 2048 <= 18432
        acc = v3(A, 0, NS, 2048)
        nchunk = cfg.EFF // 128
        ngrp = NS * 2

        def load_xt(e_, s_):
            self.dma("sp", xtm[s_ % 2], xbuf[e_ * C + s_ * 128:e_ * C + (s_ + 1) * 128, :], ["xbuf"], ["xtm%d" % (s_ % 2)])

        def load_x(e_):
            for s_ in range(min(2, NS)):
                load_xt(e_, s_)

        def xpose_group(e_, g_):
            s_, hf = divmod(g_, 2)
            p16 = self.psb16(7)
            for j in range(8):
                k = hf * 8 + j
                self.tr(p16[:, j * 128:(j + 1) * 128], xtm[s_ % 2][:, k * 128:(k + 1) * 128], self.ident_b[:],
                        ["xtm%d" % (s_ % 2), "identb"], ["ps7"])
            self.cp("act", XT[e_ % 2][:, hf * 8:hf * 8 + 8, s_ * 128:(s_ + 1) * 128],
                    p16[:, 0:1024].rearrange("p (a b) -> p a b", a=8), ["ps7"], ["XT%d" % (e_ % 2)])
            if hf == 1 and s_ + 2 < NS:
                load_xt(e_, s_ + 2)

        load_x(0)
        for g_ in range(ngrp):
            xpose_group(0, g_)
        steps = [(e_, fc) for e_ in range(NE) for fc in range(nchunk)]
        per = -(-ngrp // nchunk)
        gnext = {}
        state = {"it2": 0, "dn": 0}

        def gate_up(i):
            e_, fc = steps[i]
            i2 = i % 2
            c0 = fc * 128
            wgn, wun, wdn = "wg%d" % i2, "wu%d" % i2, "wd%d" % i2
            if fc == 0 and e_ + 1 < NE:
                load_x(e_ + 1)
                gnext[e_ + 1] = 0
            self.wload(wgb[i2], w["moe_w_gate"][0][e_][:, c0:c0 + 128], wgn)
            self.wload(wub[i2], w["moe_w_up"][0][e_][:, c0:c0 + 128], wun)
            self.wload(wdb[i2], w["moe_w_down"][0][e_][c0:c0 + 128, :], wdn)
            xt, xn = XT[e_ % 2], "XT%d" % (e_ % 2)
            for ci, (cc0, n) in enumerate(cgroups):
                bg, bu = ((0, 1), (2, 3))[state["it2"] % 2]
                state["it2"] += 1
                for k in range(16):
                    self.mm(self.psb(bg, n), wgb[i2][:, k, :], xt[:, k, cc0:cc0 + n], k == 0, k == 15, [wgn, xn], ["ps%d" % bg])
                for k in range(16):
                    self.mm(self.psb(bu, n), wub[i2][:, k, :], xt[:, k, cc0:cc0 + n], k == 0, k == 15, [wun, xn], ["ps%d" % bu])
                sl = sil[state["it2"] % 2][:, 0:n]
                sn = "sil%d" % (state["it2"] % 2)
                self.act(sl, self.psb(bg, n), AF.Silu, ["ps%d" % bg], [sn])
                self.tt("dve", actb[i2][:, cc0:cc0 + n], sl, self.psb(bu, n), ALU.mult, [sn, "ps%d" % bu], ["ab%d_%d" % (i2, ci)])
            if e_ + 1 < NE:
                for _ in range(per):
                    if gnext[e_ + 1] < ngrp:
                        xpose_group(e_ + 1, gnext[e_ + 1])
                        gnext[e_ + 1] += 1

        def down(i):
            e_, fc = steps[i]
            i2 = i % 2
            wdn = "wd%d" % i2
            for s_ in range(NS):
                ci = (s_ * 128) // 512
                for q in range(4):
                    bank = 4 + state["dn"] % 3
                    state["dn"] += 1
                    self.mm(self.psb(bank), actb[i2][:, s_ * 128:(s_ + 1) * 128], wdb[i2][:, 0, q * 512:(q + 1) * 512], True, True,
                            ["ab%d_%d" % (i2, ci), wdn], ["ps%d" % bank])
                    av = acc[:, s_, q * 512:(q + 1) * 512]
                    if fc == 0:
                        self.cp("dve", av, self.psb(bank), ["ps%d" % bank], ["acc%d" % s_])
                    else:
                        self.tt("dve", av, av, self.psb(bank), ALU.add, ["acc%d" % s_, "ps%d" % bank], ["acc%d" % s_])
                if fc == nchunk - 1:
                    self.dma("sp", ybuf[e_ * C + s_ * 128:e_ * C + (s_ + 1) * 128, :], acc[:, s_, :], ["acc%d" % s_], ["ybuf"])

        for i in range(len(steps) + 1):
            if i < len(steps):
                gate_up(i)
            if i >= 1:
                down(i - 1)
        self.p.barrier()
        lng = vw(B, 0, 2048)
        lnb = vw(B, 2048, 2048)
        self.dma("sp", lng, w["ln2_g"][l].rearrange("(o n) -> o n", o=1).partition_broadcast(128), [], ["lng"])
        self.dma("sp", lnb, w["ln2_b"][l].rearrange("(o n) -> o n", o=1).partition_broadcast(128), [], ["lng"])
        sets = [dict(y1=vw(A, i * 8192, 2048), y2=vw(A, i * 8192 + 2048, 2048), hh=vw(A, i * 8192 + 4096, 2048),
                     tt=vw(A, i * 8192 + 6144, 2048)) for i in range(2)]
        for t in range(NT):
            i2 = t % 2
            st_ = sets[i2]
            y1, y2, hh, tt_ = st_["y1"], st_["y2"], st_["hh"], st_["tt"]
            n_ = "cb%d" % i2
            for k_, yb in enumerate((y1, y2)):
                self.p.op("pool", (lambda dst, ix: (lambda e: e.indirect_dma_start(
                    out=dst, out_offset=None, in_=ybuf[:, :], in_offset=bass.IndirectOffsetOnAxis(ap=ix, axis=0))))(
                        yb, IDXgi[k_][:, t:t + 1]),
                    reads=self.Rs("ybuf", "IDXgi"), writes=self.Rs(n_ + "y%d" % k_), dma=True)
            self.dma("sp", hh, self.h1tm[t * 128:(t + 1) * 128, :], ["h1tm"], [n_ + "h"])
            self.ts("dve", y1, y1, G1[:, t:t + 1], None, ALU.mult, None, [n_ + "y0", "G"], [n_ + "y0"])
            self.stt("dve", hh, hh, ALPHA, y1, ALU.mult, ALU.add, [n_ + "h", n_ + "y0"], [n_ + "h"])
            self.stt("dve", hh, y2, G2[:, t:t + 1], hh, ALU.mult, ALU.add, [n_ + "h", n_ + "y1", "G"], [n_ + "h"])
            so = 220 + i2 * 8
            ssum, ssq, mean, var, rstd = (sm[:, so + j:so + j + 1] for j in range(5))
            smn = n_ + "s"
            self.act(y1, hh, AF.Identity, [n_ + "h"], [n_ + "y0", smn + "a"], accum=ssum)
            self.act(y2, hh, AF.Square, [n_ + "h"], [n_ + "y1", smn + "b"], accum=ssq)
            self.ts("dve", mean, ssum, 1.0 / D, None, ALU.mult, None, [smn + "a"], [smn + "m"])
            self.tt("dve", var, mean, mean, ALU.mult, [smn + "m"], [smn + "v"])
            self.stt("dve", var, ssq, 1.0 / D, var, ALU.mult, ALU.subtract, [smn + "b", smn + "v"], [smn + "v"])
            self._eps = self.eps5[:, 0:1]
            self.rstd_from(rstd, var, 1.0, [smn + "v", "eps"], [smn + "r"], smn + "r")
            self.ts("dve", tt_, hh, mean, rstd, ALU.subtract, ALU.mult, [n_ + "h", smn + "m", smn + "r"], [n_ + "t"])
            self.tt("pool", tt_, tt_, lng, ALU.mult, [n_ + "t", "lng"], [n_ + "t"])
            self.tt("dve", tt_, tt_, lnb, ALU.add, [n_ + "t", "lng"], [n_ + "t"])
            on = "o_y_%d" % (t * 128)
            self.dma("sp", self.o_y[t * 128:(t + 1) * 128, :], tt_, [n_ + "t"], [on])
            self.outs.append(on)

def rope_tables(S, PAST):
    half = 32
    inv = (np.float32(10000.0) ** (-(np.arange(half, dtype=np.float32)) / np.float32(half))).astype(np.float32)
    pos = np.concatenate([np.arange(S), np.tile(PAST + np.arange(32), 4)]).astype(np.float32)
    ang = pos[:, None] * inv[None, :]
    cos = np.concatenate([np.cos(ang), np.cos(ang)], axis=-1).astype(np.float32)
    sin = np.concatenate([np.sin(ang), np.sin(ang)], axis=-1).astype(np.float32)
    tabM = np.ascontiguousarray(np.concatenate([cos, sin], axis=-1))
    tabT = np.ascontiguousarray(tabM.T)
    return tabT, tabM

_NC_CACHE = {}

def run(cfg, inputs, n_cores):
    key = (cfg.S, cfg.PAST, cfg.FF, cfg.EFF, cfg.DEPTH, cfg.stop, cfg.C)
    if key not in _NC_CACHE:
        _NC_CACHE[key] = KB(cfg).build()
    nc = _NC_CACHE[key]
    L = cfg.DEPTH
    tabT, tabM = rope_tables(cfg.S, cfg.PAST)
    f = lambda a: np.ascontiguousarray(np.asarray(a, dtype=np.float32))
    wmap = {}
    for k in W_INPUTS:
        a = f(inputs[k])
        if k == "w_uq":
            a = a.reshape(L, 768, 8 * 192)
        elif k in ("w_uk", "w_uv"):
            a = a.reshape(L, 512, 1024)
        wmap[k] = a
    in_maps = []
    for c in range(n_cores):
        m = dict(wmap)
        m["x"] = f(np.concatenate([inputs["x_prompt"][c], np.asarray(inputs["x_sample"][4 * c:4 * c + 4]).reshape(128, D)], axis=0))
        m["state_conv"] = f(inputs["state_conv"][:, 4 * c:4 * c + 4])
        m["cache_ckv"] = f(inputs["cache_ckv"][:, 4 * c:4 * c + 4])
        m["cache_krope"] = f(inputs["cache_krope"][:, 4 * c:4 * c + 4])
        m["tabT"] = tabT
        m["tabM"] = tabM
        in_maps.append(m)
    res = run_bass_kernel_spmd(nc, in_maps, core_ids=list(range(n_cores))).results
    if getattr(cfg, "debug", False):
        _NC_CACHE["last_res"] = res
    S = cfg.S
    y_p = np.stack([r["y"][:S] for r in res])
    y_s = np.concatenate([r["y"][S:].reshape(4, 32, D) for r in res])
    conv_p = np.stack([r["conv_p"] for r in res], axis=1)
    ckv_p = np.stack([r["ckv_p"] for r in res], axis=1)
    kr_p = np.stack([r["krope_p"] for r in res], axis=1)
    conv_s = np.concatenate([r["conv_s"] for r in res], axis=1)
    ckv_s = np.concatenate([r["ckv_s"].reshape(L, 4, 32, 512) for r in res], axis=1)
    kr_s = np.concatenate([r["krope_s"].reshape(L, 4, 32, 64) for r in res], axis=1)
    v_s = np.concatenate([r["sguv_s"].reshape(L, 4, 32, 512) for r in res], axis=1)
    return tuple(np.ascontiguousarray(a, dtype=np.float32) for a in
                 (y_p, y_s, conv_p, ckv_p, kr_p, conv_s, ckv_s, kr_s, v_s))

def kernel(**inputs):
    return run(Cfg(), inputs, 8)
```

```python
import math
import copy
import contextlib
import numpy as np
import concourse.bass as bass
import concourse.mybir as mybir
from concourse.bass_utils import run_bass_kernel_spmd

F32 = mybir.dt.float32
BF16 = mybir.dt.bfloat16
I32 = mybir.dt.int32
AF = mybir.ActivationFunctionType
ALU = mybir.AluOpType
AX = mybir.AxisListType

D = 2048
KT = 16
DIN = 3904
NE = 8
SM_SCALE = 192 ** -0.5
ALPHA = 4 ** 0.25
NEG = -30000.0

class Res:
    __slots__ = ("w", "rs", "excl")

    def __init__(self, excl=False):
        self.w = None
        self.rs = []
        self.excl = excl

class _Op:
    __slots__ = ("fn", "deps", "inc", "dma_sem", "dma_val", "val", "noinc")

    def __init__(self, fn):
        self.fn = fn
        self.noinc = False
        self.deps = []
        self.inc = False
        self.dma_sem = None
        self.dma_val = 0
        self.val = 0

class Prog:
    ENGS = ("pe", "act", "dve", "pool", "sp")
    NDMA = 8

    def __init__(self, nc, same=True):
        self.nc = nc
        self.ops = {e: [] for e in self.ENGS}
        self.same = same
        self.dma_ctr = {e: 0 for e in self.ENGS}
        self.dma_cnt = {}
        self.covered = {}

    def op(self, eng, fn, reads=(), writes=(), dma=False, extra=None, noinc=False):
        ops = self.ops[eng]
        seq = len(ops)
        o = _Op(fn)
        o.noinc = noinc
        deps = {}

        def add(d, force=False):
            if d is None:
                return
            e2, s2 = d
            od = self.ops[e2][s2]
            if od.dma_sem is not None:
                k = ("dma", od.dma_sem)
                deps[k] = max(deps.get(k, 0), od.dma_val)
                return
            if e2 == eng and not force and (eng == "pe" or not self.same):
                return
            k = ("eng", e2)
            if s2 > deps.get(k, -1):
                deps[k] = s2

        if any(r.excl for r in reads):
            writes = list(writes) + [r for r in reads if r.excl]
            reads = [r for r in reads if not r.excl]
        for r in reads:
            add(r.w)
        for r in writes:
            add(r.w)
            for d in r.rs:
                add(d)
        if extra:
            for kk, v in extra:
                if kk[0] == "dma":
                    deps[kk] = max(deps.get(kk, 0), v)
                else:
                    add((kk[1], v), force=True)
        if dma:
            k = self.dma_ctr[eng] % self.NDMA
            self.dma_ctr[eng] += 1
            key = (eng, k)
            prev = self.dma_cnt.get(key, 0)
            if prev > 0:
                kk = ("dma", key)
                deps[kk] = max(deps.get(kk, 0), prev * 16)
            self.dma_cnt[key] = prev + 1
            o.dma_sem = key
            o.dma_val = (prev + 1) * 16
        for kk, v in deps.items():
            ck = (eng, kk)
            if self.covered.get(ck, -1) >= v:
                continue
            self.covered[ck] = v
            o.deps.append((kk, v))
            if kk[0] == "eng":
                self.ops[kk[1]][v].inc = True
        for r in reads:
            r.rs.append((eng, seq))
        for r in writes:
            r.w = (eng, seq)
            r.rs = []
        ops.append(o)
        return o

    def clone(self):
        p = Prog(self.nc, self.same)
        for e in self.ENGS:
            lst = []
            for o in self.ops[e]:
                c = _Op(o.fn)
                c.deps, c.inc, c.dma_sem, c.dma_val, c.val, c.noinc = list(o.deps), o.inc, o.dma_sem, o.dma_val, o.val, o.noinc
                lst.append(c)
            p.ops[e] = lst
        p.dma_ctr = dict(self.dma_ctr)
        p.dma_cnt = dict(self.dma_cnt)
        p.covered = dict(self.covered)
        return p

    def barrier(self):
        extra = []
        for e in self.ENGS:
            n = len(self.ops[e])
            for s in range(n - 1, -1, -1):
                od = self.ops[e][s]
                if od.dma_sem is None and od.fn is not None and not od.noinc:
                    extra.append((("eng", e), s))
                    break
        for key, cnt in self.dma_cnt.items():
            extra.append((("dma", key), cnt * 16))
        for e in self.ENGS:
            self.op(e, None, extra=extra)

    def emit(self, final_waits=(), other=None, other_waits=(), npre=None):
        nc = self.nc
        progs = [self] + ([other] if other is not None else [])
        self.op("sp", None, reads=list(final_waits))
        if other is not None:
            other.op("sp", None, reads=list(other_waits))
            for e in self.ENGS:
                for i in range(npre[e]):
                    a, b = self.ops[e][i], other.ops[e][i]
                    a.inc = b.inc = (a.inc or b.inc)
        for pr in progs:
            for e in self.ENGS:
                c = 0
                for o in pr.ops[e]:
                    if o.inc:
                        c += 1
                        o.val = c
        with contextlib.ExitStack() as st:
            esem = {e: st.enter_context(nc.semaphore("s_" + e)) for e in self.ENGS}
            dsem = {}
            for pr in progs:
                for key in pr.dma_cnt:
                    if key not in dsem:
                        dsem[key] = st.enter_context(nc.semaphore("d_%s%d" % key))
            engobj = {"pe": nc.tensor, "act": nc.scalar, "dve": nc.vector, "pool": nc.gpsimd, "sp": nc.sync}
            regs = {e: st.enter_context(engobj[e].register("r_" + e)) for e in self.ENGS} if other is not None else {}
            for pr in progs:
                pr.regs = regs
            block = st.enter_context(nc.Block())
            engmap = {"pe": block.tensor, "act": block.scalar, "dve": block.vector,
                      "pool": block.gpsimd, "sp": block.sync}

            def run(engine, e, pr, ops):
                for o in ops:
                    for kk, v in o.deps:
                        if kk[0] == "eng":
                            engine.wait_ge(esem[kk[1]], pr.ops[kk[1]][v].val)
                        else:
                            engine.wait_ge(dsem[kk[1]], v)
                    if o.fn is None:
                        continue
                    ins = o.fn(engine)
                    if o.dma_sem is not None:
                        ins.then_inc(dsem[o.dma_sem], 16)
                    elif o.inc:
                        ins.then_inc(esem[e], 1)

            def make(e):
                def body(engine):
                    if other is None:
                        run(engine, e, self, self.ops[e])
                        return
                    run(engine, e, self, self.ops[e][:npre[e]])
                    with engine.If_eq(regs[e], 1):
                        run(engine, e, self, self.ops[e][npre[e]:])
                    with engine.Else():
                        run(engine, e, other, other.ops[e][npre[e]:])
                return body

            for e in self.ENGS:
                engmap[e](make(e))

class _Stop(Exception):
    pass

class Cfg:
    def __init__(self, S=2048, PAST=2048, FF=5632, EFF=5632, DEPTH=2, stop=None, C=1152):
        self.S, self.PAST, self.FF, self.EFF, self.DEPTH = S, PAST, FF, EFF, DEPTH
        self.stop = stop
        self.C = C
        self.T = S + 128
        self.NT = self.T // 128
        self.NTP = S // 128
        self.groups = [(s, min(512, S - s)) for s in range(0, S, 512)] + [(S, 128)]
        self.sgs = []
        cur, tot = [], 0
        for gi, (s, n) in enumerate(self.groups):
            if tot + n > 1152:
                self.sgs.append(cur)
                cur, tot = [], 0
            cur.append(gi)
            tot += n
        self.sgs.append(cur)

W_INPUTS = ["w_in", "conv_w", "sgu_ln_g", "sgu_ln_b", "sgu_w", "sgu_b", "q_norm_g", "w_uq", "kv_norm_g",
            "w_uk", "w_uv", "mix_norm_g", "w_o", "ln1_g", "ln1_b", "ln2_g", "ln2_b", "ffn_w_gate",
            "ffn_w_up", "ffn_w_down", "router_w", "moe_w_gate", "moe_w_up", "moe_w_down"]

class KB:
    def __init__(self, cfg):
        self.cfg = cfg
        self.nc = bass.Bass("TRN2", target_bir_lowering=False)
        self.res = {}

    def R(self, name):
        r = self.res.get(name)
        if r is None:
            r = self.res[name] = Res(excl=(name[:2] == "ps" and name[2:].isdigit()))
        return r

    def Rs(self, *names):
        return [self.R(n) for n in names]

    def mm(self, out, lhsT, rhs, start, stop, rd, wr):
        self.p.op("pe", lambda e: e.matmul(out, lhsT=lhsT, rhs=rhs, start=start, stop=stop),
                  reads=self.Rs(*rd), writes=self.Rs(*wr))

    def tr(self, out, in_, ident, rd, wr):
        self.p.op("pe", lambda e: e.transpose(out=out, in_=in_, identity=ident),
                  reads=self.Rs(*rd), writes=self.Rs(*wr))

    def act(self, out, in_, func, rd, wr, bias=None, scale=None, accum=None):
        kw = {}
        if bias is not None:
            kw["bias"] = bias
        if scale is not None:
            kw["scale"] = scale
        if accum is not None:
            kw["accum_out"] = accum
        self.p.op("act", lambda e: e.activation(out=out, in_=in_, func=func, **kw),
                  reads=self.Rs(*rd), writes=self.Rs(*wr))

    def tt(self, eng, out, in0, in1, op, rd, wr):
        self.p.op(eng, lambda e: e.tensor_tensor(out=out, in0=in0, in1=in1, op=op),
                  reads=self.Rs(*rd), writes=self.Rs(*wr))

    def ts(self, eng, out, in0, s1, s2, op0, op1, rd, wr):
        if op1 is None:
            self.p.op(eng, lambda e: e.tensor_scalar(out=out, in0=in0, scalar1=s1, scalar2=None, op0=op0),
                      reads=self.Rs(*rd), writes=self.Rs(*wr))
        else:
            self.p.op(eng, lambda e: e.tensor_scalar(out=out, in0=in0, scalar1=s1, scalar2=s2, op0=op0, op1=op1),
                      reads=self.Rs(*rd), writes=self.Rs(*wr))

    def stt(self, eng, out, in0, scalar, in1, op0, op1, rd, wr):
        self.p.op(eng, lambda e: e.scalar_tensor_tensor(out=out, in0=in0, scalar=scalar, in1=in1, op0=op0, op1=op1),
                  reads=self.Rs(*rd), writes=self.Rs(*wr))

    def cp(self, eng, out, in_, rd, wr):
        if eng == "act":
            self.p.op("act", lambda e: e.activation(out=out, in_=in_, func=AF.Copy), reads=self.Rs(*rd), writes=self.Rs(*wr))
        else:
            self.p.op(eng, lambda e: e.tensor_copy(out=out, in_=in_), reads=self.Rs(*rd), writes=self.Rs(*wr))

    def red(self, out, in_, op, rd, wr):
        self.p.op("dve", lambda e: e.tensor_reduce(out=out, in_=in_, axis=AX.X, op=op),
                  reads=self.Rs(*rd), writes=self.Rs(*wr))

    def recip(self, out, in_, rd, wr):
        self.p.op("dve", lambda e: e.reciprocal(out=out, in_=in_), reads=self.Rs(*rd), writes=self.Rs(*wr))

    def memset(self, eng, ap, v, wr):
        self.p.op(eng, lambda e: e.memset(ap, v), writes=self.Rs(*wr))

    def dma(self, q, out, in_, rd, wr):
        self.p.op(q, lambda e: e.dma_start(out=out, in_=in_), reads=self.Rs(*rd), writes=self.Rs(*wr), dma=True)

    def rstd_from(self, out, in_, scale, rd, wr, tmpname):
        self.act(out, in_, AF.Sqrt, rd, [tmpname], bias=self._eps, scale=scale)
        self.recip(out, out, [tmpname], wr)

    @staticmethod
    def vw(reg, off, n, dt=F32):
        ap = reg[:, off:off + n]
        if dt != F32:
            ap = ap.bitcast(dt)
        return ap

    def v3(self, reg, off, a, b, dt=F32):
        n = a * b if dt == F32 else (a * b) // 2
        return self.vw(reg, off, n, dt).rearrange("p (a b) -> p a b", a=a)

    def build(self):
        cfg, nc = self.cfg, self.nc
        S, T, NT, NTP, PAST = cfg.S, cfg.T, cfg.NT, cfg.NTP, cfg.PAST
        self.p = Prog(nc)
        dt_in = lambda name, shape: nc.dram_tensor(name, list(shape), F32, kind="ExternalInput").ap()
        dt_out = lambda name, shape: nc.dram_tensor(name, list(shape), F32, kind="ExternalOutput").ap()
        L = cfg.DEPTH
        self.x = dt_in("x", [T, D])
        self.state_conv = dt_in("state_conv", [L, 4, 2, 512])
        self.cache_ckv = dt_in("cache_ckv", [L, 4, PAST, 512])
        self.cache_krope = dt_in("cache_krope", [L, 4, PAST, 64])
        self.tabT = dt_in("tabT", [128, T])
        self.tabM = dt_in("tabM", [T, 128])
        self.w = {}
        shapes = {"w_in": [L, D, DIN], "conv_w": [L, 3, 512], "sgu_ln_g": [L, 512], "sgu_ln_b": [L, 512],
                  "sgu_w": [L, 4, 128, 128], "sgu_b": [L, 4, 128], "q_norm_g": [L, 768],
                  "w_uq": [L, 768, 8 * 192], "kv_norm_g": [L, 512], "w_uk": [L, 512, 1024],
                  "w_uv": [L, 512, 1024], "mix_norm_g": [L, D], "w_o": [L, D, D], "ln1_g": [L, D],
                  "ln1_b": [L, D], "ln2_g": [L, D], "ln2_b": [L, D],
                  "ffn_w_gate": [1, D, cfg.FF], "ffn_w_up": [1, D, cfg.FF], "ffn_w_down": [1, cfg.FF, D],
                  "router_w": [1, D, NE], "moe_w_gate": [1, NE, D, cfg.EFF], "moe_w_up": [1, NE, D, cfg.EFF],
                  "moe_w_down": [1, NE, cfg.EFF, D]}
        for k in W_INPUTS:
            self.w[k] = dt_in(k, shapes[k])
        self.o_y = dt_out("y", [T, D])
        self.o_conv_p = dt_out("conv_p", [L, 2, 512])
        self.o_ckv_p = dt_out("ckv_p", [L, S, 512])
        self.o_krope_p = dt_out("krope_p", [L, S, 64])
        self.o_conv_s = dt_out("conv_s", [L, 4, 2, 512])
        self.o_ckv_s = dt_out("ckv_s", [L, 128, 512])
        self.o_krope_s = dt_out("krope_s", [L, 128, 64])
        self.o_sguv_s = dt_out("sguv_s", [L, 128, 512])
        self.hres = nc.dram_tensor("hresT", [D, T], F32, kind="Internal").ap()
        C = cfg.C
        dk = "ExternalOutput" if getattr(cfg, "debug", False) else "Internal"
        self.h1tm = nc.dram_tensor("h1tm", [T, D], F32, kind=dk).ap()
        self.xbuf = nc.dram_tensor("xbuf", [NE * C, D], BF16, kind=dk).ap()
        self.ybuf = nc.dram_tensor("ybuf", [NE * C, D], F32, kind=dk).ap()
        self.outs = []

        with contextlib.ExitStack() as st:
            st.enter_context(nc.allow_non_contiguous_dma(reason="small strided parameter loads"))
            sb = lambda n, s, d=F32: st.enter_context(nc.sbuf_tensor(n, s, d))
            RW = 18432
            self.EW = 13000
            self.RX = sb("RX", [128, RW])
            self.RY = sb("RY", [128, RW])
            self.RE = sb("RE", [128, self.EW])
            self.ident_f = sb("ident_f", [128, 128])
            self.ident_b = sb("ident_b", [128, 128], BF16)
            self.ones_f = sb("ones_f", [128, 128])
            self.eps5 = sb("eps5", [128, 1])
            self.eps6 = sb("eps6", [128, 1])
            self.mrow = sb("mrow", [1, 128], BF16)
            self.mcol = sb("mcol", [1, 128], BF16)
            self.small = sb("small", [128, 256])
            self.par = sb("par", [128, 1664])
            self.ps = [st.enter_context(nc.psum_tensor("ps%d" % i, [128, 512], F32)) for i in range(8)]
            self.setup_consts()
            if L > 1:
                zt = self.vw(self.RE, 0, 1024, BF16)
                self.memset("pool", zt, 0.0, ["zt"])
                for r in range(NE * cfg.C // 128):
                    self.dma("act", self.xbuf[r * 128:(r + 1) * 128, :], zt, ["zt"], ["xbuf"])
            try:
                for l in range(L):
                    A, B = (self.RX, self.RY) if l % 2 == 0 else (self.RY, self.RX)
                    self.layer(l, A, B)
            except _Stop:
                pass
            if getattr(self, "p_else", None) is not None:
                fwA = self.Rs(*self.outs)
                resA = self.res
                self.res = self.res_else
                fwB = self.Rs(*self.outs_else)
                self.res = resA
                self.p.emit(final_waits=fwA, other=self.p_else, other_waits=fwB, npre=self.fork_npre)
            else:
                self.p.emit(final_waits=self.Rs(*self.outs))
        return nc

    def fork_begin(self, flag_ap):
        p = self.p
        for e in p.ENGS:
            p.op(e, (lambda en: (lambda eng: eng.reg_load(p.regs[en], flag_ap)))(e), reads=self.Rs("FLGi"), noinc=True)
        self._fork = dict(npre={e: len(p.ops[e]) for e in p.ENGS}, prog=p.clone(), res=copy.deepcopy(self.res),
                          outs=list(self.outs))

    def fork_else(self):
        f = self._fork
        f["progA"], f["outsA"], f["resA"] = self.p, self.outs, self.res
        self.p, self.res, self.outs = f["prog"], f["res"], f["outs"]

    def fork_end(self):
        f = self._fork
        self.p_else, self.outs_else, self.res_else = self.p, self.outs, self.res
        self.p, self.outs, self.res = f["progA"], f["outsA"], f["resA"]
        self.fork_npre = f["npre"]

    def chk(self, name):
        if self.cfg.stop == "l%d%s" % (self.l, name):
            raise _Stop()

    def psb(self, i, n=512):
        return self.ps[i][:, 0:n]

    def psb16(self, i):
        return self.ps[i][:, :].bitcast(BF16)

    def setup_consts(self):
        self.memset("pool", self.ones_f[:], 1.0, ["ones"])
        self.memset("pool", self.ident_f[:], 1.0, ["identf"])
        idf = self.ident_f
        self.p.op("pool", lambda e: e.affine_select(out=idf[:], in_=idf[:], pattern=[[-1, 128]],
                                                    compare_op=ALU.is_equal, fill=0.0, base=0,
                                                    channel_multiplier=1),
                  reads=self.Rs("identf"), writes=self.Rs("identf"))
        self.cp("dve", self.ident_b[:], self.ident_f[:], ["identf"], ["identb"])
        self.memset("pool", self.eps5[:], 1e-5, ["eps"])
        self.memset("pool", self.eps6[:], 1e-6, ["eps"])
        self.memset("dve", self.mrow[:], 0.0, ["mask"])
        self.memset("dve", self.mrow[:, 0:64], 1.0, ["mask"])
        self.memset("dve", self.mcol[:], 0.0, ["mask"])
        self.memset("dve", self.mcol[:, 64:128], NEG, ["mask"])

    def load_params(self, l):
        par, w = self.par, self.w
        P = {}
        off = [0]

        def alloc(n):
            o = off[0]
            off[0] += n
            return par[:, o:o + n]

        def fm(name, src, k):
            ap = alloc(k)
            self.dma("sp", ap, src.rearrange("(k p) -> p k", p=128), [], ["par"])
            P[name] = ap

        def bc(name, src, n):
            ap = alloc(n)
            self.dma("sp", ap, src.rearrange("(o n) -> o n", o=1).partition_broadcast(128), [], ["par"])
            P[name] = ap

        fm("ln1g", w["ln1_g"][l], 16); fm("ln1b", w["ln1_b"][l], 16)
        fm("ln2g", w["ln2_g"][l], 16); fm("ln2b", w["ln2_b"][l], 16)
        fm("mixg", w["mix_norm_g"][l], 16)
        fm("qg", w["q_norm_g"][l], 6); fm("kvg", w["kv_norm_g"][l], 4)
        ap = alloc(12)
        for r in range(3):
            self.dma("sp", ap.rearrange("p (j r) -> p j r", j=4)[:, :, r], w["conv_w"][l][r].rearrange("(j p) -> p j", p=128), [], ["par"])
        P["convw"] = ap.rearrange("p (j r) -> p j r", j=4)
        bc("sgug", w["sgu_ln_g"][l], 512); bc("sgub", w["sgu_ln_b"][l], 512)
        bc("kvg_bc", w["kv_norm_g"][l], 512)
        ap = alloc(4)
        self.dma("sp", ap, w["sgu_b"][l].rearrange("h p -> p h"), [], ["par"])
        P["sgubias"] = ap
        ap = alloc(4)
        for b in range(4):
            self.dma("sp", ap[32 * b:32 * b + 32, :], w["sgu_b"][l][:, 0:32].rearrange("h p -> p h"), [], ["par"])
        P["sgubias_s"] = ap
        self.P = P

    def layer(self, l, A, B):
        cfg = self.cfg
        S, T, NT, NTP = cfg.S, cfg.T, cfg.NT, cfg.NTP
        self.l = l
        self.HT = self.v3(A, 0, 16, T, BF16)
        self.MT = self.v3(B, 0, 16, T, BF16)
        self._eps = self.eps5[:, 0:1]
        self.p.barrier()
        self.load_params(l)
        self.chk("par")
        if l == 0:
            self.stage0(A, B)
            self.p.barrier()
        self.chk("s0")
        self.stage1(l, A, B)
        self.p.barrier()
        self.chk("s1")
        self.stage2(l, A, B)
        self.p.barrier()
        self.chk("s2")
        self.stage3(l, A, B)
        self.p.barrier()
        self.chk("s3")
        if l % 2 == 1:
            self.stage4_sparse(l, A, B)
        else:
            self.stage4(l, A, B)
        self.chk("s4")

    def stage0(self, A, B):
        cfg = self.cfg
        T, NT = cfg.T, cfg.NT
        xs = [self.vw(B, 0, 2048), self.vw(B, 2048, 2048)]
        stg = [self.v3(B, 4096 + i * 512, 4, 128) for i in range(4)]
        nst = 0
        for t in range(NT):
            xb = xs[t % 2]
            self.dma("sp", xb, self.x[t * 128:(t + 1) * 128, :], [], ["xs%d" % (t % 2)])
            for q in range(4):
                bank = (t * 4 + q) % 6
                for j in range(4):
                    k = q * 4 + j
                    self.tr(self.ps[bank][:, j * 128:(j + 1) * 128], xb[:, k * 128:(k + 1) * 128], self.ident_f[:],
                            ["xs%d" % (t % 2), "identf"], ["ps%d" % bank])
                pv = self.ps[bank][:, :].rearrange("p (a b) -> p a b", a=4)
                self.cp("act", self.HT[:, q * 4:q * 4 + 4, t * 128:(t + 1) * 128], pv, ["ps%d" % bank], ["HT"])
                sg = stg[nst % 4]
                sn = "stg%d" % (nst % 4)
                nst += 1
                self.cp("dve", sg, pv, ["ps%d" % bank], [sn])
                self.dma("sp", self.hres[q * 512:(q + 1) * 512, t * 128:(t + 1) * 128].rearrange("(a p) n -> p a n", p=128),
                         sg, [sn], ["hres"])

    def wload(self, dst, src, name, q="pool"):
        self.dma(q, dst, src.rearrange("(k p) n -> p k n", p=128), [], [name])

    def stage1(self, l, A, B):
        cfg = self.cfg
        S, T, NT, NTP, groups = cfg.S, cfg.T, cfg.NT, cfg.NTP, cfg.groups
        P, w = self.P, self.w
        win = w["w_in"][l]
        HT, MT = self.HT, self.MT
        BH = 8704
        E = self.RE
        self.cqnT = self.v3(E, 0, 6, T, BF16)
        self.ckvnT = self.v3(E, 6528, 4, T, BF16)
        self.kropeT = self.vw(E, 10880, 1088, BF16)
        self.ckvn_new = self.v3(E, 11968, 4, 512, BF16)

        wb = [self.v3(B, BH + i * 3072, 3 * 16, 128, BF16) for i in range(2)]
        zbuf = self.vw(E, 0, S + 2)
        zs = self.v3(E, 2052, 4, 34)
        wk = [[self.vw(E, 2200 + (i * 4 + j) * 512, 512) for j in range(4)] for i in range(2)]
        convw = P["convw"]
        it = 0
        for j in range(4):
            wbj = wb[j % 2]
            wn = "cw%d" % (j % 2)
            for r, c0 in enumerate((512, 1024, 0)):
                self.wload(wbj[:, r * 16:(r + 1) * 16, :], win[:, c0 + j * 128:c0 + (j + 1) * 128], wn)
            self.memset("pool", zbuf[:, 0:2], 0.0, ["zbuf"])
            for b in range(4):
                self.dma("sp", zs[:, b, 0:2], self.state_conv[l][b][:, j * 128:(j + 1) * 128].rearrange("r p -> p r"),
                         [], ["zs"])
            for gi, (g0, n) in enumerate(groups):
                samp = gi == len(groups) - 1
                tmpc, a32, sq, rstd = wk[it % 2]
                wkn = "cwk%d" % (it % 2)
                it += 1
                pc, ph, pb = 0 + 3 * (it % 2), 1 + 3 * (it % 2), 2 + 3 * (it % 2)
                for r, bank in enumerate((pc, ph, pb)):
                    for k in range(16):
                        self.mm(self.psb(bank, n), wbj[:, r * 16 + k, :], HT[:, k, g0:g0 + n], k == 0, k == 15,
                                [wn, "HT"], ["ps%d" % bank])
                self.cp("act", tmpc[:, 0:n], self.psb(pc, n), ["ps%d" % pc], [wkn])
                if not samp:
                    zc = [zbuf[:, g0 + r:g0 + r + n] for r in range(3)]
                    zn = "zbuf"
                    self.tt("dve", zc[2], tmpc[:, 0:n], self.psb(ph, n), ALU.mult, [wkn, "ps%d" % ph], [zn])
                    yv = a32[:, 0:n]
                    pbv = self.psb(pb, n)
                    sqv, rsv = sq[:, 0:n], rstd[:, 0:n]
                    mo = MT[:, j, g0:g0 + n]
                else:
                    zc = [zs[:, :, r:r + 32] for r in range(3)]
                    zn = "zs"
                    self.tt("dve", zc[2], tmpc[:, 0:n].rearrange("p (b q) -> p b q", b=4),
                            self.psb(ph, n).rearrange("p (b q) -> p b q", b=4), ALU.mult, [wkn, "ps%d" % ph], [zn])
                    yv = a32[:, 0:n].rearrange("p (b q) -> p b q", b=4)
                    pbv = self.psb(pb, n).rearrange("p (b q) -> p b q", b=4)
                    sqv, rsv = sq[:, 0:n], rstd[:, 0:n]
                    mo = MT[:, j, g0:g0 + n]
                self.ts("dve", yv, zc[0], convw[:, j, 0:1], None, ALU.mult, None, [zn, "par"], [wkn])
                self.stt("dve", yv, zc[1], convw[:, j, 1:2], yv, ALU.mult, ALU.add, [zn, "par", wkn], [wkn])
                self.stt("dve", yv, zc[2], convw[:, j, 2:3], yv, ALU.mult, ALU.add, [zn, "par", wkn], [wkn])
                self.tt("dve", yv, yv, pbv, ALU.mult, [wkn, "ps%d" % pb], [wkn])
                self.act(sqv, a32[:, 0:n], AF.Square, [wkn], [wkn + "s"])
                self.mm(self.psb(6 + it % 2, n), self.ones_f[:], sqv, True, True, ["ones", wkn + "s"], ["ps%d" % (6 + it % 2)])
                self._eps = self.eps6[:, 0:1]
                self.rstd_from(rsv, self.psb(6 + it % 2, n), 1.0 / 128, ["ps%d" % (6 + it % 2), "eps"], [wkn + "r"], wkn + "r")
                self.stt("dve", mo, a32[:, 0:n], P["mixg"][:, j:j + 1], rsv, ALU.mult, ALU.mult,
                         [wkn, wkn + "r", "par"], ["MT"])
            self.dma("sp", self.o_conv_p[l][:, j * 128:(j + 1) * 128].rearrange("r p -> p r"), zbuf[:, S:S + 2],
                     ["zbuf"], ["o_conv_p%d_%d" % (l, j)])
            self.outs.append("o_conv_p%d_%d" % (l, j))
            for b in range(4):
                on = "o_conv_s%d_%d_%d" % (l, j, b)
                self.dma("sp", self.o_conv_s[l][b][:, j * 128:(j + 1) * 128].rearrange("r p -> p r"), zs[:, b, 32:34],
                         ["zs"], [on])
                self.outs.append(on)
        self.p.barrier()
        self.chk("p1")

        wu = self.v3(B, BH, 16, 512, BF16)
        wv = self.v3(B, BH + 4096, 16, 512, BF16)
        self.wload(wu, win[:, 1536:2048], "wu")
        self.wload(wv, win[:, 2048:2560], "wv")
        wraw = self.v3(E, 0, 4, 128)
        WT = self.v3(E, 512, 4, 128, BF16)
        WTs = self.v3(E, 768, 4, 128, BF16)
        wtf = self.v3(E, 1024, 4, 128)
        self.dma("sp", wraw, w["sgu_w"][l].rearrange("h p q -> p h q"), [], ["wraw"])
        for h in range(4):
            self.tr(self.ps[0][:, h * 128:(h + 1) * 128], wraw[:, h, :], self.ident_f[:], ["wraw", "identf"], ["ps0"])
        self.cp("dve", wtf, self.ps[0][:, :].rearrange("p (a b) -> p a b", a=4), ["ps0"], ["wtf"])
        for h in range(4):
            wslice = wtf[:, h, :]
            self.p.op("pool", (lambda ws: (lambda e: e.affine_select(out=ws, in_=ws, pattern=[[1, 128]],
                                                                       compare_op=ALU.is_ge, fill=0.0, base=0,
                                                                       channel_multiplier=-1)))(wslice),
                      reads=self.Rs("wtf"), writes=self.Rs("wtf"))
        self.cp("dve", WT, wtf, ["wtf"], ["WT"])
        self.memset("pool", WTs, 0.0, ["WTs"])
        for b in range(4):
            self.dma("sp", WTs[32 * b:32 * b + 32, :, 32 * b:32 * b + 32], WT[0:32, :, 0:32], ["WT", "WTs"], ["WTs"])
        sw = 1600
        bufs = []
        for i in range(2):
            o = sw + i * 2700
            bufs.append(dict(gu=self.vw(E, o, 512), gv=self.vw(E, o + 512, 512), sq=self.vw(E, o + 1024, 512),
                             bo=self.vw(E, o + 1536, 512), vnb=self.vw(E, o + 2048, 256, BF16),
                             bon=self.vw(E, o + 2304, 256, BF16)))
        sm = self.small
        for t in range(NT):
            samp = t == NT - 1
            bf = bufs[t % 2]
            bn = "sg%d" % (t % 2)
            gu, gv, sq, bo, vnb, bon = bf["gu"], bf["gv"], bf["sq"], bf["bo"], bf["vnb"], bf["bon"]
            pu, pv, pss, ptt = (0, 1, 2, 3) if t % 2 == 0 else (4, 5, 6, 7)
            tok = slice(t * 128, (t + 1) * 128)
            for k in range(16):
                self.mm(self.psb(pu), HT[:, k, tok], wu[:, k, :], k == 0, k == 15, ["HT", "wu"], ["ps%d" % pu])
            for k in range(16):
                self.mm(self.psb(pv), HT[:, k, tok], wv[:, k, :], k == 0, k == 15, ["HT", "wv"], ["ps%d" % pv])
            self.act(gu, self.psb(pu), AF.Gelu_apprx_tanh, ["ps%d" % pu], [bn + "gu"])
            self.act(gv, self.psb(pv), AF.Gelu_apprx_tanh, ["ps%d" % pv], [bn + "gv"])
            so = (t % 2) * 40
            s1, s2, mean, var, rs = (sm[:, so + i * 4:so + i * 4 + 4] for i in range(5))
            smn = "sm%d" % (t % 2)
            g3 = lambda a: a.rearrange("p (h c) -> p h c", h=4)
            bc3 = lambda a: a.unsqueeze(2).to_broadcast([128, 4, 128])
            self.red(s1, g3(gv), ALU.add, [bn + "gv"], [smn])
            self.tt("pool", sq, gv, gv, ALU.mult, [bn + "gv"], [bn + "sq"])
            self.red(s2, g3(sq), ALU.add, [bn + "sq"], [smn])
            self.ts("dve", mean, s1, 1.0 / 128, None, ALU.mult, None, [smn], [smn])
            self.tt("dve", var, mean, mean, ALU.mult, [smn], [smn])
            self.stt("dve", var, s2, 1.0 / 128, var, ALU.mult, ALU.subtract, [smn], [smn])
            self._eps = self.eps5[:, 0:1]
            self.rstd_from(rs, var, 1.0, [smn, "eps"], [smn], smn)
            self.tt("dve", g3(gv), g3(gv), bc3(mean), ALU.subtract, [bn + "gv", smn], [bn + "gv"])
            self.tt("dve", g3(gv), g3(gv), bc3(rs), ALU.mult, [bn + "gv", smn], [bn + "gv"])
            self.tt("dve", gv, gv, P["sgug"], ALU.mult, [bn + "gv", "par"], [bn + "gv"])
            self.tt("dve", gv, gv, P["sgub"], ALU.add, [bn + "gv", "par"], [bn + "gv"])
            if samp:
                on = "o_sguv%d" % l
                self.dma("sp", self.o_sguv_s[l], gv, [bn + "gv"], [on])
                self.outs.append(on)
            self.cp("act", vnb, gv, [bn + "gv"], [bn + "vnb"])
            wt = WTs if samp else WT
            wtn = "WTs" if samp else "WT"
            for h in range(4):
                self.mm(self.ps[pss][:, h * 128:(h + 1) * 128], wt[:, h, :], vnb[:, h * 128:(h + 1) * 128], True, True,
                        [wtn, bn + "vnb"], ["ps%d" % pss])
            bias = P["sgubias_s"] if samp else P["sgubias"]
            for h in range(4):
                hs = slice(h * 128, (h + 1) * 128)
                self.stt("dve", bo[:, hs], self.ps[pss][:, hs], bias[:, h:h + 1], gu[:, hs], ALU.add, ALU.mult,
                         ["ps%d" % pss, "par", bn + "gu"], [bn + "bo"])
            self.tt("pool", sq, bo, bo, ALU.mult, [bn + "bo"], [bn + "sq"])
            ss, rs2 = sm[:, so + 20:so + 24], sm[:, so + 24:so + 28]
            self.red(ss, g3(sq), ALU.add, [bn + "sq"], [smn])
            self._eps = self.eps6[:, 0:1]
            self.rstd_from(rs2, ss, 1.0 / 128, [smn, "eps"], [smn], smn)
            self.tt("dve", g3(bon), g3(bo), bc3(rs2), ALU.mult, [bn + "bo", smn], [bn + "bon"])
            p16 = self.psb16(ptt)
            for h in range(4):
                self.tr(p16[:, h * 128:(h + 1) * 128], bon[:, h * 128:(h + 1) * 128], self.ident_b[:],
                        [bn + "bon", "identb"], ["ps%d" % ptt])
            self.tt("dve", MT[:, 4:8, tok], p16[:, 0:512].rearrange("p (a b) -> p a b", a=4),
                    P["mixg"][:, 4:8].unsqueeze(2).to_broadcast([128, 4, 128]), ALU.mult,
                    ["ps%d" % ptt, "par"], ["MT"])
        self.p.barrier()
        self.chk("p2")

        wq = self.v3(B, BH, 16, 768, BF16)
        self.wload(wq, win[:, 2560:3328], "wq")
        sqb = [self.vw(B, BH + 6144 + i * 512, 512) for i in range(2)]
        rsb = self.vw(B, BH + 6144 + 1024, 512)
        self._eps = self.eps6[:, 0:1]
        for gi, (g0, n) in enumerate(groups):
            for m in range(6):
                for k in range(16):
                    self.mm(self.psb(m, n), wq[:, k, m * 128:(m + 1) * 128], HT[:, k, g0:g0 + n], k == 0, k == 15,
                            ["wq", "HT"], ["ps%d" % m])
            for m in range(6):
                self.act(sqb[m % 2][:, 0:n], self.psb(m, n), AF.Square, ["ps%d" % m], ["sqb%d" % (m % 2)])
                self.mm(self.psb(7, n), self.ones_f[:], sqb[m % 2][:, 0:n], m == 0, m == 5, ["ones", "sqb%d" % (m % 2)], ["ps7"])
            self.rstd_from(rsb[:, 0:n], self.psb(7, n), 1.0 / 768, ["ps7", "eps"], ["rsb"], "rsb")
            for m in range(6):
                self.stt("dve", self.cqnT[:, m, g0:g0 + n], self.psb(m, n), P["qg"][:, m:m + 1], rsb[:, 0:n],
                         ALU.mult, ALU.mult, ["ps%d" % m, "par", "rsb"], ["cqnT"])
        self.p.barrier()
        self.chk("p3")

        wkv = self.v3(B, BH, 16, 512, BF16)
        self.wload(wkv, win[:, 3328:3840], "wkv")
        wkr = self.v3(B, BH + 4096, 16, 128, BF16)
        self.wload(wkr[:, :, 0:64], win[:, 3840:3904], "wkr")
        self.ts("dve", wkr[:, :, 64:96], wkr[:, :, 32:64], -1.0, None, ALU.mult, None, ["wkr"], ["wkr"])
        self.cp("dve", wkr[:, :, 96:128], wkr[:, :, 0:32], ["wkr"], ["wkr"])
        o5 = BH + 4096 + 1024
        sqb = [self.vw(B, o5 + i * 512, 512) for i in range(2)]
        rsb = self.vw(B, o5 + 1024, 512)
        ctm = [self.vw(B, o5 + 1536 + i * 512, 512) for i in range(2)]
        ctb = self.vw(B, o5 + 2560, 256, BF16)
        junk = self.vw(B, o5 + 2816, 512)
        for gi, (g0, n) in enumerate(groups):
            for m in range(4):
                for k in range(16):
                    self.mm(self.psb(m, n), wkv[:, k, m * 128:(m + 1) * 128], HT[:, k, g0:g0 + n], k == 0, k == 15,
                            ["wkv", "HT"], ["ps%d" % m])
            for m in range(4):
                self.act(sqb[m % 2][:, 0:n], self.psb(m, n), AF.Square, ["ps%d" % m], ["sqb%d" % (m % 2)])
                self.mm(self.psb(7, n), self.ones_f[:], sqb[m % 2][:, 0:n], m == 0, m == 3, ["ones", "sqb%d" % (m % 2)], ["ps7"])
            self.rstd_from(rsb[:, 0:n], self.psb(7, n), 1.0 / 512, ["ps7", "eps"], ["rsb"], "rsb")
            for m in range(4):
                self.stt("dve", self.ckvnT[:, m, g0:g0 + n], self.psb(m, n), P["kvg"][:, m:m + 1], rsb[:, 0:n],
                         ALU.mult, ALU.mult, ["ps%d" % m, "par", "rsb"], ["ckvnT"])
        sm = self.small
        for t in range(NT):
            samp = t == NT - 1
            tok = slice(t * 128, (t + 1) * 128)
            bank = 4 + t % 2
            for k in range(16):
                self.mm(self.psb(bank), HT[:, k, tok], wkv[:, k, :], k == 0, k == 15, ["HT", "wkv"], ["ps%d" % bank])
            ss, rs = sm[:, 100 + (t % 2) * 2:101 + (t % 2) * 2], sm[:, 101 + (t % 2) * 2:102 + (t % 2) * 2]
            smn = "smk%d" % (t % 2)
            self.act(junk, self.psb(bank), AF.Square, ["ps%d" % bank], ["junk", smn], accum=ss)
            self.rstd_from(rs, ss, 1.0 / 512, [smn, "eps"], [smn], smn)
            cb = ctm[t % 2]
            cbn = "ctm%d" % (t % 2)
            self.stt("dve", cb, self.psb(bank), rs, P["kvg_bc"], ALU.mult, ALU.mult, ["ps%d" % bank, smn, "par"], [cbn])
            if not samp:
                on = "o_ckv_p%d_%d" % (l, t)
                self.dma("sp", self.o_ckv_p[l][tok, :], cb, [cbn], [on])
            else:
                on = "o_ckv_s%d" % l
                self.dma("sp", self.o_ckv_s[l], cb, [cbn], [on])
                self.cp("act", ctb, cb, [cbn], ["ctb"])
                for b in range(4):
                    self.dma("sp", self.ckvn_new[0:32, b, :], ctb[32 * b:32 * b + 32, :], ["ctb"], ["ckvn_new"])
            self.outs.append(on)
        self.chk("p4")
        tabm = [self.vw(B, o5 + 3328 + i * 128, 128) for i in range(2)]
        prod = [self.vw(B, o5 + 3584 + i * 128, 128) for i in range(2)]
        dup = [self.vw(B, o5 + 3840 + i * 128, 128) for i in range(2)]
        for t in range(NT):
            samp = t == NT - 1
            tok = slice(t * 128, (t + 1) * 128)
            i2 = t % 2
            bank = 0 + i2
            self.dma("sp", tabm[i2], self.tabM[tok, :], [], ["tabm%d" % i2])
            for k in range(16):
                self.mm(self.ps[bank][:, 0:128], HT[:, k, tok], wkr[:, k, :], k == 0, k == 15, ["HT", "wkr"], ["ps%d" % bank])
            self.tt("dve", prod[i2], self.ps[bank][:, 0:128], tabm[i2], ALU.mult, ["ps%d" % bank, "tabm%d" % i2], ["prod%d" % i2])
            self.tt("dve", dup[i2][:, 0:64], prod[i2][:, 0:64], prod[i2][:, 64:128], ALU.add, ["prod%d" % i2], ["dup%d" % i2])
            self.cp("dve", dup[i2][:, 64:128], dup[i2][:, 0:64], ["dup%d" % i2], ["dup%d" % i2])
            if not samp:
                on = "o_kr_p%d_%d" % (l, t)
                self.dma("sp", self.o_krope_p[l][tok, :], dup[i2][:, 0:64], ["dup%d" % i2], [on])
            else:
                on = "o_kr_s%d" % l
                self.dma("sp", self.o_krope_s[l], dup[i2][:, 0:64], ["dup%d" % i2], [on])
            self.outs.append(on)
            self.tr(self.ps[2 + i2][:, 0:128], dup[i2], self.ident_f[:], ["dup%d" % i2, "identf"], ["ps%d" % (2 + i2)])
            self.cp("act", self.kropeT[:, tok], self.ps[2 + i2][:, 0:128], ["ps%d" % (2 + i2)], ["kropeT"])

    def attn_tail(self, l, h, c_ps, rinv, tok, i2, A):
        P = self.P
        sm = self.small
        o = 16960 + i2 * 192
        c32 = self.vw(A, o, 128)
        cb = self.vw(A, o + 128, 64, BF16)
        n = "at%d" % i2
        ss, rs = sm[:, 120 + i2 * 2:121 + i2 * 2], sm[:, 121 + i2 * 2:122 + i2 * 2]
        if rinv is not None:
            self.ts("dve", c32, c_ps, rinv, None, ALU.mult, None, ["ps7", "sm_rinv"], [n])
        else:
            self.cp("dve", c32, c_ps, ["ps7"], [n])
        junk = self.vw(A, 16960 + 384, 64, BF16)
        self.act(junk, c32, AF.Square, [n], ["junkA", n + "s"], accum=ss)
        self._eps = self.eps6[:, 0:1]
        self.rstd_from(rs, ss, 1.0 / 128, [n + "s", "eps"], [n + "s"], n + "s")
        self.ts("dve", cb, c32, rs, None, ALU.mult, None, [n, n + "s"], [n + "b"])
        p16 = self.psb16(6)
        self.tr(p16[:, i2 * 128:(i2 + 1) * 128], cb, self.ident_b[:], [n + "b", "identb"], ["ps6"])
        self.act(self.MT[:, 8 + h, tok], p16[:, i2 * 128:(i2 + 1) * 128], AF.Copy, ["ps6", "par"], ["MT"],
                 scale=P["mixg"][:, 8 + h:9 + h])

    def stage2(self, l, A, B):
        cfg = self.cfg
        S, T, NT, NTP, PAST, groups = cfg.S, cfg.T, cfg.NT, cfg.NTP, cfg.PAST, cfg.groups
        P, w = self.P, self.w
        MT = self.MT
        sm = self.small
        tabT = self.vw(A, 0, T)
        o = 2176
        wh = []
        for i in range(2):
            wh.append(dict(qn=self.v3(A, o, 6, 128, BF16), qr=self.v3(A, o + 384, 6, 128, BF16),
                           uk=self.v3(A, o + 768, 4, 128, BF16), uv=self.v3(A, o + 1024, 4, 128, BF16),
                           ukT=self.v3(A, o + 1280, 4, 128, BF16)))
            o += 1536
        qnT = self.vw(A, o, T // 2, BF16); o += T // 2
        qrT = self.vw(A, o, T // 2, BF16); o += T // 2
        knT = self.vw(A, o, S // 2, BF16); o += S // 2
        Vh = self.v3(A, o, NTP, 128, BF16); o += NTP * 64
        Pb = [self.vw(A, o + i * ((PAST + 128) // 2), (PAST + 128) // 2, BF16) for i in range(2)]
        o += (PAST + 128)
        PTb = [self.vw(A, o + i * 512, 512, BF16) for i in range(2)]
        o += 1024
        assert o <= 12672, o
        o = 12672
        QlatT = self.vw(A, o, 2048, BF16).rearrange("p (c b h t) -> p c b h t", c=4, b=4, h=8); o += 2048
        QlatF = self.vw(A, o - 2048, 2048, BF16).rearrange("p (c b m t) -> p c b m t", c=4, b=4, m=2)
        OlatT = self.vw(A, o, 2048, BF16).rearrange("p (c h b q) -> p c h b q", c=4, h=8, b=4)
        OlatF = self.vw(A, o, 2048, BF16).rearrange("p (c h t) -> p c h t", c=4, h=8); o += 2048
        wr_tmp = self.v3(A, o, 6, 64, BF16); o += 192
        assert o <= 16960, o
        self.qs_rope = self.vw(A, 17920, 512, BF16).rearrange("p (b h t) -> p b h t", b=4, h=8)
        qs_ropeF = self.vw(A, 17920, 512, BF16).rearrange("p (b m t) -> p b m t", b=4, m=2)
        self.dma("sp", tabT, self.tabT, [], ["tabT"])
        wuq = w["w_uq"][l]
        wuk = w["w_uk"][l]
        wuv = w["w_uv"][l]
        for h in range(8):
            wb = wh[h % 2]
            wn = "wh%d" % (h % 2)
            self.wload(wb["qn"], wuq[:, h * 192:h * 192 + 128], wn)
            self.wload(wr_tmp, wuq[:, h * 192 + 128:h * 192 + 192], "wrtmp")
            self.cp("dve", wb["qr"][:, :, 0:64], wr_tmp, ["wrtmp"], [wn])
            self.ts("dve", wb["qr"][:, :, 64:96], wr_tmp[:, :, 32:64], -1.0, None, ALU.mult, None, ["wrtmp"], [wn])
            self.cp("dve", wb["qr"][:, :, 96:128], wr_tmp[:, :, 0:32], ["wrtmp"], [wn])
            self.wload(wb["uk"], wuk[:, h * 128:(h + 1) * 128], wn)
            self.wload(wb["uv"], wuv[:, h * 128:(h + 1) * 128], wn)
            for gi, (g0, n) in enumerate(groups):
                b0 = (gi % 2) * 2
                for k in range(6):
                    self.mm(self.psb(b0, n), wb["qn"][:, k, :], self.cqnT[:, k, g0:g0 + n], k == 0, k == 5,
                            [wn, "cqnT"], ["ps%d" % b0])
                self.cp("act", qnT[:, g0:g0 + n], self.psb(b0, n), ["ps%d" % b0], ["qnT"])
                for k in range(6):
                    self.mm(self.psb(b0 + 1, n), wb["qr"][:, k, :], self.cqnT[:, k, g0:g0 + n], k == 0, k == 5,
                            [wn, "cqnT"], ["ps%d" % (b0 + 1)])
                self.tt("dve", qrT[:, g0:g0 + n], self.psb(b0 + 1, n), tabT[:, g0:g0 + n], ALU.mult,
                        ["ps%d" % (b0 + 1), "tabT"], ["qrT"])
            self.cp("dve", self.qs_rope[:, :, h, :], qrT[:, S:S + 128].rearrange("p (b t) -> p b t", b=4), ["qrT"], ["qs"])
            for gi, (g0, n) in enumerate(groups[:-1]):
                b0 = 4 + gi % 2
                for k in range(4):
                    self.mm(self.psb(b0, n), wb["uk"][:, k, :], self.ckvnT[:, k, g0:g0 + n], k == 0, k == 3,
                            [wn, "ckvnT"], ["ps%d" % b0])
                self.cp("act", knT[:, g0:g0 + n], self.psb(b0, n), ["ps%d" % b0], ["knT"])
            for t0 in range(0, NTP, 4):
                bank = 6 + (t0 // 4) % 2
                nt = min(4, NTP - t0)
                for j in range(nt):
                    t = t0 + j
                    for k in range(4):
                        self.mm(self.ps[bank][:, j * 128:(j + 1) * 128], self.ckvnT[:, k, t * 128:(t + 1) * 128],
                                wb["uv"][:, k, :], k == 0, k == 3, ["ckvnT", wn], ["ps%d" % bank])
                self.cp("dve", Vh[:, t0:t0 + nt, :], self.ps[bank][:, 0:nt * 128].rearrange("p (a b) -> p a b", a=nt),
                        ["ps%d" % bank], ["Vh"])
            p16 = self.psb16(5)
            for c in range(4):
                self.tr(p16[:, c * 128:(c + 1) * 128], wb["uk"][:, c, :], self.ident_b[:], [wn, "identb"], ["ps5"])
            self.cp("dve", wb["ukT"], p16[:, 0:512].rearrange("p (a b) -> p a b", a=4), ["ps5"], [wn + "T"])
            for c in range(4):
                self.mm(self.ps[4][:, c * 128:(c + 1) * 128], wb["ukT"][:, c, :], qnT[:, S:S + 128], True, True,
                        [wn + "T", "qnT"], ["ps4"])
            self.cp("act", QlatT[:, :, :, h, :], self.ps[4][:, :].rearrange("p (c b t) -> p c b t", c=4, b=4), ["ps4"], ["QlatT"])
            for i in range(NTP):
                nk = (i + 1) * 128
                nb = (nk + 511) // 512
                qs = slice(i * 128, (i + 1) * 128)
                Pq = Pb[i % 2]
                Pn = "P%d" % (i % 2)
                so = 40 * (i % 2) + 140
                mx = sm[:, so:so + 4]
                rs4 = sm[:, so + 4:so + 8]
                m1, negm, rsum, rinv = (sm[:, so + 8 + j:so + 9 + j] for j in range(4))
                smn = "sma%d" % (i % 2)
                for kb in range(nb):
                    ksz = min(512, nk - kb * 512)
                    ks = slice(kb * 512, kb * 512 + ksz)
                    last = kb == nb - 1
                    self.mm(self.psb(kb, ksz), qnT[:, qs], knT[:, ks], True, False, ["qnT", "knT"], ["ps%d" % kb])
                    self.mm(self.psb(kb, ksz), qrT[:, qs], self.kropeT[:, ks], False, not last, ["qrT", "kropeT"], ["ps%d" % kb])
                    if last:
                        self.mm(self.ps[kb][:, ksz - 128:ksz], self.mrow[:], self.mcol[:], False, True, ["mask"], ["ps%d" % kb])
                    self.red(mx[:, kb:kb + 1], self.psb(kb, ksz), ALU.max, ["ps%d" % kb], [smn + "m"])
                self.red(m1, mx[:, 0:nb], ALU.max, [smn + "m"], [smn + "m"])
                self.ts("dve", negm, m1, -SM_SCALE, None, ALU.mult, None, [smn + "m"], [smn + "n"])
                for kb in range(nb):
                    ksz = min(512, nk - kb * 512)
                    ks = slice(kb * 512, kb * 512 + ksz)
                    self.act(Pq[:, ks], self.psb(kb, ksz), AF.Exp, ["ps%d" % kb, smn + "n"], [Pn, smn + "r"],
                             bias=negm, scale=SM_SCALE, accum=rs4[:, kb:kb + 1])
                self.red(rsum, rs4[:, 0:nb], ALU.add, [smn + "r"], [smn + "r"])
                self.recip(rinv, rsum, [smn + "r"], ["sm_rinv"])
                nblk = i + 1
                for c0 in range(0, nblk, 8):
                    cn = min(8, nblk - c0)
                    pi = (c0 // 8) % 2
                    p16 = self.psb16(4 + pi)
                    for j in range(cn):
                        kk = c0 + j
                        self.tr(p16[:, j * 128:(j + 1) * 128], Pq[:, kk * 128:(kk + 1) * 128], self.ident_b[:],
                                [Pn, "identb"], ["ps%d" % (4 + pi)])
                    eng = "act" if pi == 0 else "dve"
                    self.cp(eng, PTb[pi][:, 0:cn * 128], p16[:, 0:cn * 128], ["ps%d" % (4 + pi)], ["PT%d" % pi])
                    for j in range(cn):
                        kk = c0 + j
                        self.mm(self.ps[7][:, 0:128], PTb[pi][:, j * 128:(j + 1) * 128], Vh[:, kk, :], kk == 0, kk == nblk - 1,
                                ["PT%d" % pi, "Vh"], ["ps7"])
                self.attn_tail(l, h, self.ps[7][:, 0:128], rinv, qs, i % 2, A)
        self.p.barrier()
        self.chk("s2h")
        KTc = PAST // 128
        o = 0
        ckv = self.v3(A, o, KTc, 512, BF16); o += KTc * 256
        kr2 = self.v3(A, o, KTc, 128, BF16); o += KTc * 64
        ckvT = self.v3(A, o, 4, PAST, BF16); o += 2 * PAST
        krT = self.vw(A, o, PAST // 2, BF16); o += PAST // 2
        Ps = self.vw(A, o, (PAST + 128) // 2, BF16); o += (PAST + 128) // 2
        PTs = self.v3(A, o, KTc + 1, 128, BF16); o += (KTc + 1) * 64
        olb = self.vw(A, o, 256, BF16); o += 256
        assert o <= 12672, o
        uvb = [self.v3(A, i * 256, 4, 128, BF16) for i in range(2)]
        for b in range(4):
            self.wload(ckv, self.cache_ckv[l][b], "ckv")
            self.dma("pool", kr2[:, :, 0:64], self.cache_krope[l][b].rearrange("(k p) n -> p k n", p=128), [], ["kr2"])
            self.dma("pool", kr2[:, :, 64:128], self.cache_krope[l][b].rearrange("(k p) n -> p k n", p=128), [], ["kr2"])
            nbk = 0
            for c in range(4):
                for t0 in range(0, KTc, 8):
                    cn = min(8, KTc - t0)
                    bank = nbk % 4
                    nbk += 1
                    p16 = self.psb16(bank)
                    for j in range(cn):
                        self.tr(p16[:, j * 128:(j + 1) * 128], ckv[:, t0 + j, c * 128:(c + 1) * 128], self.ident_b[:],
                                ["ckv", "identb"], ["ps%d" % bank])
                    self.cp("act" if nbk % 2 else "dve", ckvT[:, c, t0 * 128:(t0 + cn) * 128], p16[:, 0:cn * 128],
                            ["ps%d" % bank], ["ckvT"])
            for t0 in range(0, KTc, 8):
                cn = min(8, KTc - t0)
                bank = nbk % 4
                nbk += 1
                p16 = self.psb16(bank)
                for j in range(cn):
                    self.tr(p16[:, j * 128:(j + 1) * 128], kr2[:, t0 + j, :], self.ident_b[:], ["kr2", "identb"], ["ps%d" % bank])
                self.cp("act" if nbk % 2 else "dve", krT[:, t0 * 128:(t0 + cn) * 128], p16[:, 0:cn * 128],
                        ["ps%d" % bank], ["krT"])
            newk = slice(S + 32 * b, S + 32 * b + 32)
            nblk = (PAST + 511) // 512
            for mt in range(2):
                hs = slice(4 * mt, 4 * mt + 4)
                qlat = lambda c: QlatF[:, c, b, mt, :]
                qrp = qs_ropeF[:, b, mt, :]
                so = 180
                mx = sm[:, so:so + 8]
                rs8 = sm[:, so + 8:so + 16]
                m1, negm, rsum, rinv = (sm[:, so + 16 + j:so + 17 + j] for j in range(4))
                for kb in range(nblk + 1):
                    bank = kb
                    if kb < nblk:
                        ksz = min(512, PAST - kb * 512)
                        ks = slice(kb * 512, kb * 512 + ksz)
                        for c in range(4):
                            self.mm(self.psb(bank, ksz), qlat(c), ckvT[:, c, ks], c == 0, False, ["QlatT", "ckvT"], ["ps%d" % bank])
                        self.mm(self.psb(bank, ksz), qrp, krT[:, ks], False, True, ["qs", "krT"], ["ps%d" % bank])
                    else:
                        ksz = 32
                        for c in range(4):
                            self.mm(self.psb(bank, ksz), qlat(c), self.ckvnT[:, c, newk], c == 0, False, ["QlatT", "ckvnT"], ["ps%d" % bank])
                        self.mm(self.psb(bank, ksz), qrp, self.kropeT[:, newk], False, True, ["qs", "kropeT"], ["ps%d" % bank])
                    self.red(mx[:, kb:kb + 1], self.psb(bank, ksz), ALU.max, ["ps%d" % bank], ["smsm"])
                self.red(m1, mx[:, 0:nblk + 1], ALU.max, ["smsm"], ["smsm"])
                self.ts("dve", negm, m1, -SM_SCALE, None, ALU.mult, None, ["smsm"], ["smsn"])
                for kb in range(nblk + 1):
                    if kb < nblk:
                        ksz = min(512, PAST - kb * 512)
                        ks = slice(kb * 512, kb * 512 + ksz)
                    else:
                        ksz = 32
                        ks = slice(PAST, PAST + 32)
                    self.act(Ps[:, ks], self.psb(kb, ksz), AF.Exp, ["ps%d" % kb, "smsn"], ["Ps", "smsr"],
                             bias=negm, scale=SM_SCALE, accum=rs8[:, kb:kb + 1])
                self.red(rsum, rs8[:, 0:nblk + 1], ALU.add, ["smsr"], ["smsr"])
                self.recip(rinv, rsum, ["smsr"], ["smsr"])
                for t0 in range(0, KTc, 8):
                    cn = min(8, KTc - t0)
                    bank = 5 + (t0 // 8) % 2
                    p16 = self.psb16(bank)
                    for j in range(cn):
                        self.tr(p16[:, j * 128:(j + 1) * 128], Ps[:, (t0 + j) * 128:(t0 + j + 1) * 128], self.ident_b[:],
                                ["Ps", "identb"], ["ps%d" % bank])
                    self.cp("act" if (t0 // 8) % 2 else "dve", PTs[:, t0:t0 + cn, :],
                            p16[:, 0:cn * 128].rearrange("p (a b) -> p a b", a=cn), ["ps%d" % bank], ["PTs"])
                p16 = self.psb16(5)
                self.tr(p16[0:32, 0:128], Ps[:, PAST:PAST + 32], self.ident_b[:], ["Ps", "identb"], ["ps5"])
                self.cp("dve", PTs[0:32, KTc, :], p16[0:32, 0:128], ["ps5"], ["PTs"])
                for kt in range(KTc):
                    self.mm(self.psb(7), PTs[:, kt, :], ckv[:, kt, :], kt == 0, False, ["PTs", "ckv"], ["ps7"])
                self.mm(self.psb(7), PTs[0:32, KTc, :], self.ckvn_new[0:32, b, :], False, True, ["PTs", "ckvn_new"], ["ps7"])
                self.ts("dve", olb, self.psb(7), rinv, None, ALU.mult, None, ["ps7", "smsr"], ["olb"])
                p16 = self.psb16(6)
                for c in range(4):
                    self.tr(p16[:, c * 128:(c + 1) * 128], olb[:, c * 128:(c + 1) * 128], self.ident_b[:], ["olb", "identb"], ["ps6"])
                for c in range(4):
                    self.cp("act" if c % 2 else "dve", OlatT[:, c, hs, b, :],
                            p16[:, c * 128:(c + 1) * 128].rearrange("p (h q) -> p h q", h=4), ["ps6"], ["OlatT"])
        self.p.barrier()
        self.chk("s2s")
        for h in range(8):
            ub = uvb[h % 2]
            un = "uvb%d" % (h % 2)
            self.wload(ub, wuv[:, h * 128:(h + 1) * 128], un)
            for c in range(4):
                self.mm(self.ps[7][:, 0:128], OlatF[:, c, h, :], ub[:, c, :], c == 0, c == 3, ["OlatT", un], ["ps7"])
            self.attn_tail(l, h, self.ps[7][:, 0:128], None, slice(S, S + 128), h % 2, A)

    def ln_apply(self, l, which, src_of, g0, n, gi, mean_bc, rstd_bc, work, last, A, B, dst_bf, ytm=None, tm_dst=None):
        P = self.P
        g, bta = P[which + "g"], P[which + "b"]
        if last:
            tm_dst = self.o_y
        tm = tm_dst is not None
        step = 256 if tm else n
        for c0 in range(0, n, step):
            nn = min(step, n - c0)
            cs = slice(c0, c0 + nn)
            for m in range(16):
                t1 = work[0][m % 2][:, 0:nn]
                o32 = work[1][m % 2][:, 0:nn]
                n1, n2 = "lnw%d" % (m % 2), "lno%d" % (m % 2)
                self.tt("pool", t1, src_of(m)[:, cs], mean_bc[:, cs], ALU.subtract, ["lnsrc", "lnmean"], [n1])
                self.tt("dve", t1, t1, rstd_bc[:, cs], ALU.mult, [n1, "lnrstd"], [n1])
                if not last:
                    self.act(dst_bf[:, m, g0 + c0:g0 + c0 + nn], t1, AF.Identity, [n1, "par"], ["MTout"],
                             scale=g[:, m:m + 1], bias=bta[:, m:m + 1])
                self.ts("dve", o32, t1, g[:, m:m + 1], bta[:, m:m + 1], ALU.mult, ALU.add, [n1, "par"], [n2])
                if not tm or not last:
                    self.dma("sp", self.hres[m * 128:(m + 1) * 128, g0 + c0:g0 + c0 + nn], o32, [n2], ["hres%d" % gi])
                if tm:
                    for j in range(nn // 128):
                        bank = 4 + j % 2
                        pcol = self.ps[bank][:, (m % 4) * 128:(m % 4 + 1) * 128]
                        self.tr(pcol, o32[:, j * 128:(j + 1) * 128], self.ident_f[:], [n2, "identf"], ["ps%d" % bank])
                        self.cp("act" if j % 2 else "dve", ytm[j][:, m * 128:(m + 1) * 128], pcol, ["ps%d" % bank], ["ytm%d" % j])
            if tm:
                for j in range(nn // 128):
                    t0 = g0 + c0 + j * 128
                    if last:
                        on = "o_y_%d" % t0
                        self.outs.append(on)
                    else:
                        on = "h1tm"
                    self.dma("sp", tm_dst[t0:t0 + 128, :], ytm[j], ["ytm%d" % j], [on])

    def ln_stats(self, n, work):
        mean_bc, rstd_bc, tmp = work
        self.act(mean_bc, self.psb(6, n), AF.Copy, ["ps6"], ["lnmean"], scale=1.0 / D)
        self.tt("dve", tmp, mean_bc, mean_bc, ALU.mult, ["lnmean"], ["lntmp"])
        self.stt("dve", tmp, self.psb(7, n), 1.0 / D, tmp, ALU.mult, ALU.subtract, ["ps7", "lntmp"], ["lntmp"])
        self._eps = self.eps5[:, 0:1]
        self.rstd_from(rstd_bc, tmp, 1.0, ["lntmp", "eps"], ["lnrstd"], "lnrstd")

    def stage3(self, l, A, B):
        cfg = self.cfg
        S, T, NT, groups = cfg.S, cfg.T, cfg.NT, cfg.groups
        P, w = self.P, self.w
        MT = self.MT
        wo = w["w_o"][l]
        o = 0
        rT = self.v3(A, o, 16, 512); o += 8192
        wob = [self.v3(A, o + i * 1024, 16, 128, BF16) for i in range(3)]; o += 3072
        res32 = [self.vw(A, o + i * 512, 512) for i in range(2)]; o += 1024
        sqb = [self.vw(A, o + i * 512, 512) for i in range(2)]; o += 1024
        mean_bc = self.vw(A, o, 512); o += 512
        rstd_bc = self.vw(A, o, 512); o += 512
        tmp = self.vw(A, o, 512); o += 512
        w0 = [self.vw(A, o + i * 512, 512) for i in range(2)]; o += 1024
        w1 = [self.vw(A, o + i * 512, 512) for i in range(2)]; o += 1024
        it = 0
        for gi, (g0, n) in enumerate(groups):
            for m in range(16):
                wb = wob[it % 3]
                wn = "wo%d" % (it % 3)
                it += 1
                self.wload(wb, wo[:, m * 128:(m + 1) * 128], wn)
                bank = m % 4
                for k in range(16):
                    self.mm(self.psb(bank, n), wb[:, k, :], MT[:, k, g0:g0 + n], k == 0, k == 15, [wn, "MT"], ["ps%d" % bank])
                rb = res32[m % 2][:, 0:n]
                self.dma("sp", rb, self.hres[m * 128:(m + 1) * 128, g0:g0 + n], ["hres%d" % gi], ["res%d" % (m % 2)])
                self.stt("dve", rT[:, m, 0:n], rb, ALPHA, self.psb(bank, n), ALU.mult, ALU.add,
                         ["res%d" % (m % 2), "ps%d" % bank], ["lnsrc"])
                sq = sqb[m % 2][:, 0:n]
                self.act(sq, rT[:, m, 0:n], AF.Square, ["lnsrc"], ["lsq%d" % (m % 2)])
                self.mm(self.psb(6, n), self.ones_f[:], rT[:, m, 0:n], m == 0, m == 15, ["ones", "lnsrc"], ["ps6"])
                self.mm(self.psb(7, n), self.ones_f[:], sq, m == 0, m == 15, ["ones", "lsq%d" % (m % 2)], ["ps7"])
            self.ln_stats(n, (mean_bc[:, 0:n], rstd_bc[:, 0:n], tmp[:, 0:n]))
            if l % 2 == 1:
                ytm = [self.vw(self.RE, j * 2048, 2048) for j in range(2)]
                self.ln_apply(l, "ln1", lambda m: rT[:, m, 0:n], g0, n, gi, mean_bc[:, 0:n], rstd_bc[:, 0:n], (w0, w1),
                              False, A, B, MT, ytm=ytm, tm_dst=self.h1tm)
            else:
                self.ln_apply(l, "ln1", lambda m: rT[:, m, 0:n], g0, n, gi, mean_bc[:, 0:n], rstd_bc[:, 0:n], (w0, w1),
                              False, A, B, MT)

    def stage4(self, l, A, B, dg_ext=None):
        cfg = self.cfg
        S, T, NT, groups = cfg.S, cfg.T, cfg.NT, cfg.groups
        P, w = self.P, self.w
        H1 = self.MT
        E = self.RE
        moe = (l % 2 == 1)
        last = (l == cfg.DEPTH - 1)
        sm = self.small
        acc = self.v3(A, 0, 16, 1152)
        FW = 1
        o = 0
        wgb = [self.v3(E, o + i * 1024, 16, 128, BF16) for i in range(2)]; o += 2048
        wub = [self.v3(E, o + i * 1024, 16, 128, BF16) for i in range(2)]; o += 2048
        wdb = [self.v3(E, o + i * 1024, 1, 2048, BF16) for i in range(2)]; o += 2048
        sil = [self.vw(E, o + i * 512, 512) for i in range(2)]; o += 1024
        actb = [self.v3(E, o + i * 256, 1, 512, BF16) for i in range(2)]; o += 512
        gbc = self.vw(E, o, 1152); o += 1152
        assert o <= 8832
        o = 8832
        if moe:
            dg = dg_ext
            dgb = self.vw(E, o, 128); o += 128
        lw = o
        mean_bc = self.vw(E, lw, 512); rstd_bc = self.vw(E, lw + 512, 512); tmp = self.vw(E, lw + 1024, 512)
        assert lw + 1536 <= self.EW, lw
        experts = list(range(NE)) if moe else [None]
        FFd = cfg.EFF if moe else cfg.FF
        nchunk = FFd // (128 * FW)
        it = 0
        it2 = 0
        for sgi, sg in enumerate(cfg.sgs):
            sg0 = groups[sg[0]][0]
            first = True
            for e in experts:
                if moe:
                    wg_d, wu_d, wd_d = w["moe_w_gate"][0][e], w["moe_w_up"][0][e], w["moe_w_down"][0][e]
                    for gi in sg:
                        g0, n = groups[gi]
                        for j in range(n // 128):
                            t = g0 // 128 + j
                            self.cp("dve", dgb, dg[:, t, e:e + 1].to_broadcast([128, 128]), ["dg"], ["dgb"])
                            self.mm(self.ps[5][:, j * 128:(j + 1) * 128], dgb, self.ident_f[:], True, True, ["dgb", "identf"], ["ps5"])
                        self.cp("act", gbc[:, g0 - sg0:g0 - sg0 + n], self.psb(5, n), ["ps5"], ["gbc"])
                else:
                    wg_d, wu_d, wd_d = w["ffn_w_gate"][0], w["ffn_w_up"][0], w["ffn_w_down"][0]
                for fc in range(nchunk):
                    i2 = it % 2
                    it += 1
                    wgn, wun, wdn = "wg%d" % i2, "wu%d" % i2, "wd%d" % i2
                    c0 = fc * 128 * FW
                    self.wload(wgb[i2], wg_d[:, c0:c0 + 128 * FW], wgn)
                    self.wload(wub[i2], wu_d[:, c0:c0 + 128 * FW], wun)
                    self.wload(wdb[i2], wd_d[c0:c0 + 128 * FW, :], wdn)
                    for gi in sg:
                        g0, n = groups[gi]
                        ab = actb[gi % 2]
                        abn = "actb%d" % (gi % 2)
                        for f in range(FW):
                            bg, bu = ((0, 1), (2, 3))[(it2 := it2 + 1) % 2]
                            for k in range(16):
                                self.mm(self.psb(bg, n), wgb[i2][:, k, f * 128:(f + 1) * 128], H1[:, k, g0:g0 + n], k == 0, k == 15,
                                        [wgn, "MT"], ["ps%d" % bg])
                            for k in range(16):
                                self.mm(self.psb(bu, n), wub[i2][:, k, f * 128:(f + 1) * 128], H1[:, k, g0:g0 + n], k == 0, k == 15,
                                        [wun, "MT"], ["ps%d" % bu])
                            sl = sil[it2 % 2][:, 0:n]
                            sn = "sil%d" % (it2 % 2)
                            self.act(sl, self.psb(bg, n), AF.Silu, ["ps%d" % bg], [sn])
                            if moe:
                                self.tt("pool", sl, sl, gbc[:, g0 - sg0:g0 - sg0 + n], ALU.mult, [sn, "gbc"], [sn])
                            self.tt("dve", ab[:, f, 0:n], sl, self.psb(bu, n), ALU.mult, [sn, "ps%d" % bu], [abn])
                        for m in range(16):
                            bank = 4 + m % 2 if not moe else 6 + m % 2
                            for f in range(FW):
                                self.mm(self.psb(bank, n), wdb[i2][:, f, m * 128:(m + 1) * 128], ab[:, f, 0:n], f == 0, f == FW - 1,
                                        [wdn, abn], ["ps%d" % bank])
                            av = acc[:, m, g0 - sg0:g0 - sg0 + n]
                            if first:
                                self.cp("dve", av, self.psb(bank, n), ["ps%d" % bank], ["acc"])
                            else:
                                self.tt("dve", av, av, self.psb(bank, n), ALU.add, ["acc", "ps%d" % bank], ["acc"])
                    first = False
            self.p.barrier()
            res32 = [self.vw(E, i * 512, 512) for i in range(2)]
            sqb = [self.vw(E, 1024 + i * 512, 512) for i in range(2)]
            w0 = [self.vw(E, 2048 + i * 512, 512) for i in range(2)]
            w1 = [self.vw(E, 3072 + i * 512, 512) for i in range(2)]
            ytm = [self.vw(E, 4096 + j * 2048, 2048) for j in range(2)]
            for gi in sg:
                g0, n = groups[gi]
                for m in range(16):
                    rb = res32[m % 2][:, 0:n]
                    self.dma("sp", rb, self.hres[m * 128:(m + 1) * 128, g0:g0 + n], ["hres%d" % gi], ["res%d" % (m % 2)])
                    av = acc[:, m, g0 - sg0:g0 - sg0 + n]
                    self.stt("dve", av, rb, ALPHA, av, ALU.mult, ALU.add, ["res%d" % (m % 2), "acc"], ["lnsrc"])
                    sq = sqb[m % 2][:, 0:n]
                    self.act(sq, av, AF.Square, ["lnsrc"], ["lsq%d" % (m % 2)])
                    self.mm(self.psb(6, n), self.ones_f[:], av, m == 0, m == 15, ["ones", "lnsrc"], ["ps6"])
                    self.mm(self.psb(7, n), self.ones_f[:], sq, m == 0, m == 15, ["ones", "lsq%d" % (m % 2)], ["ps7"])
                self.ln_stats(n, (mean_bc[:, 0:n], rstd_bc[:, 0:n], tmp[:, 0:n]))
                self.ln_apply(l, "ln2", lambda m: acc[:, m, g0 - sg0:g0 - sg0 + n], g0, n, gi, mean_bc[:, 0:n],
                              rstd_bc[:, 0:n], (w0, w1), last, A, B, H1, ytm)
            self.p.barrier()

    def stage4_sparse(self, l, A, B):
        cfg = self.cfg
        S, T, NT, C = cfg.S, cfg.T, cfg.NT, cfg.C
        P, w = self.P, self.w
        H1 = self.MT
        E = self.RE
        sm = self.small
        NS = C // 128
        NSL = NE * C
        cgroups = [(c0, min(512, C - c0)) for c0 in range(0, C, 512)]
        v3, vw = self.v3, self.vw
        o = 0
        wgb = [v3(E, o + i * 1024, 16, 128, BF16) for i in range(2)]; o += 2048
        wub = [v3(E, o + i * 1024, 16, 128, BF16) for i in range(2)]; o += 2048
        wdb = [v3(E, o + i * 1024, 1, 2048, BF16) for i in range(2)]; o += 2048
        sil = [vw(E, o + i * 512, 512) for i in range(2)]; o += 1024
        actb = [vw(E, o + i * (C // 2), C // 2, BF16) for i in range(2)]; o += C
        wr = v3(E, o, 16, 8, BF16); o += 64
        n8 = NT * 8
        so_ = self.EW - 1300
        EQ1 = v3(E, so_, NT, 8); so_ += n8
        EQ2 = v3(E, so_, NT, 8); so_ += n8
        DG = v3(E, so_, NT, 8); so_ += n8
        G1 = vw(E, so_, NT); so_ += NT
        G2 = vw(E, so_, NT); so_ += NT
        CNT = vw(E, so_, 8); so_ += 8
        FLG = vw(E, so_, 2); so_ += 2
        FLGi = vw(E, so_, 2).bitcast(I32); so_ += 2
        assert so_ <= self.EW
        POS = v3(E, o, NT, 8); o += n8
        GI = v3(E, o, NT, 8); o += n8
        TM1 = v3(E, o, NT, 8); o += n8
        TM2 = v3(E, o, NT, 8); o += n8
        IDX = [vw(E, o + i * NT, NT) for i in range(2)]; o += 2 * NT
        IDXg = [vw(E, o + i * NT, NT) for i in range(2)]; o += 2 * NT
        VAL = vw(E, o, NT); o += NT
        IDXi = [vw(E, o + i * NT, NT).bitcast(I32) for i in range(2)]; o += 2 * NT
        IDXgi = [vw(E, o + i * NT, NT).bitcast(I32) for i in range(2)]; o += 2 * NT
        EOFF = vw(E, o, 8); o += 8
        UT = vw(E, o, 128); o += 128
        xtm = [vw(E, o + i * 1024, 1024, BF16) for i in range(2)]; o += 2048
        assert o <= self.EW - 1300, o
        self.wload(wr, w["router_w"][0], "wr")
        self.memset("pool", UT, 1.0, ["UT"])
        self.p.op("pool", lambda e: e.affine_select(out=UT, in_=UT, pattern=[[1, 128]], compare_op=ALU.is_ge, fill=0.0,
                                                    base=-1, channel_multiplier=-1),
                  reads=self.Rs("UT"), writes=self.Rs("UT"))
        for e_ in range(NE):
            self.memset("pool", EOFF[:, e_:e_ + 1], float(e_ * C), ["EOFF"])
        for t in range(NT):
            tok = slice(t * 128, (t + 1) * 128)
            for k in range(16):
                self.mm(self.ps[0][:, 0:8], H1[:, k, tok], wr[:, k, :], k == 0, k == 15, ["MT", "wr"], ["ps0"])
            lg = sm[:, 200:208]
            m8 = sm[:, 208:216]
            d12, e12 = sm[:, 216:217], sm[:, 217:218]
            self.cp("dve", lg, self.ps[0][:, 0:8], ["ps0"], ["rt"])
            self.p.op("dve", (lambda a, b: (lambda e: e.max(out=a, in_=b)))(m8, lg), reads=self.Rs("rt"), writes=self.Rs("rt8"))
            self.tt("dve", d12, m8[:, 1:2], m8[:, 0:1], ALU.subtract, ["rt8"], ["rtd"])
            self.act(e12, d12, AF.Exp, ["rtd"], ["rte"])
            self.ts("dve", G1[:, t:t + 1], e12, 1.0, None, ALU.add, None, ["rte"], ["G"])
            self.recip(G1[:, t:t + 1], G1[:, t:t + 1], ["G"], ["G"])
            self.ts("dve", G2[:, t:t + 1], G1[:, t:t + 1], -1.0, 1.0, ALU.mult, ALU.add, ["G"], ["G"])
            self.ts("dve", EQ1[:, t, :], lg, m8[:, 0:1], None, ALU.is_equal, None, ["rt", "rt8"], ["EQ"])
            self.ts("dve", EQ2[:, t, :], lg, m8[:, 1:2], None, ALU.is_equal, None, ["rt", "rt8"], ["EQ"])
        self.tt("dve", TM1, EQ1, EQ2, ALU.add, ["EQ"], ["MASK"])
        for t in range(NT):
            self.mm(self.ps[2][:, 0:8], self.ones_f[:], TM1[:, t, :], t == 0, t == NT - 1, ["ones", "MASK"], ["ps2"])
        self.cp("dve", CNT, self.ps[2][:, 0:8], ["ps2"], ["CNT"])
        self.red(FLG[:, 0:1], CNT, ALU.max, ["CNT"], ["FLG"])
        self.ts("dve", FLG[:, 1:2], FLG[:, 0:1], float(C), None, ALU.is_le, None, ["FLG"], ["FLG"])
        self.cp("dve", FLGi, FLG, ["FLG"], ["FLGi"])
        self.tt("dve", DG, EQ1, G1.unsqueeze(2).to_broadcast([128, NT, 8]), ALU.mult, ["EQ", "G"], ["DG"])
        self.tt("dve", TM2, EQ2, G2.unsqueeze(2).to_broadcast([128, NT, 8]), ALU.mult, ["EQ", "G"], ["TM2"])
        self.tt("dve", DG, DG, TM2, ALU.add, ["DG", "TM2"], ["DG"])
        self.p.barrier()
        self.fork_begin(FLGi[0:1, 1:2])
        self.stage4_sparse_body(l, A, B, locals())
        self.fork_else()
        self.stage4(l, A, B, dg_ext=DG)
        self.fork_end()

    def stage4_sparse_body(self, l, A, B, L_):
        cfg = self.cfg
        S, T, NT, C = cfg.S, cfg.T, cfg.NT, cfg.C
        P, w = self.P, self.w
        E = self.RE
        sm = self.small
        v3, vw = self.v3, self.vw
        (NS, NSL, cgroups, wgb, wub, wdb, sil, actb, n8, EQ1, EQ2, POS, GI, TM1, TM2, G1, G2, IDX, IDXg, VAL, IDXi, IDXgi, EOFF,
         UT, xtm) = (L_[k] for k in ("NS", "NSL", "cgroups", "wgb", "wub", "wdb", "sil", "actb", "n8", "EQ1", "EQ2", "POS", "GI",
                                     "TM1", "TM2", "G1", "G2", "IDX", "IDXg", "VAL", "IDXi", "IDXgi", "EOFF", "UT", "xtm"))
        for t in range(NT):
            for t2 in range(t + 1):
                lhsT = UT if t2 == t else self.ones_f[:]
                self.mm(self.ps[1][:, t * 8:(t + 1) * 8], lhsT, TM1[:, t2, :], t2 == 0, t2 == t, ["UT", "ones", "MASK"], ["ps1"])
        self.cp("dve", POS, self.ps[1][:, 0:n8].rearrange("p (a b) -> p a b", a=NT), ["ps1"], ["POS"])
        self.tt("dve", GI, POS, EOFF.unsqueeze(1).to_broadcast([128, NT, 8]), ALU.add, ["POS", "EOFF"], ["GI"])
        self.ts("dve", TM2, POS, float(C), 1.0e6, ALU.is_ge, ALU.mult, ["POS"], ["TM2"])
        self.tt("dve", GI, GI, TM2, ALU.add, ["GI", "TM2"], ["GI"])
        for k_, EQ in enumerate((EQ1, EQ2)):
            self.tt("dve", TM2, EQ, GI, ALU.mult, ["EQ", "GI"], ["TM2"])
            self.red(IDX[k_], TM2, ALU.add, ["TM2"], ["IDX"])
            G = (G1, G2)[k_]
            self.ts("dve", VAL, IDX[k_], float(NSL), None, ALU.is_lt, None, ["IDX"], ["VAL"])
            self.tt("dve", G, G, VAL, ALU.mult, ["G", "VAL"], ["G"])
            self.ts("dve", IDXg[k_], IDX[k_], float(NSL - 1), None, ALU.min, None, ["IDX"], ["IDXg"])
            self.cp("dve", IDXi[k_], IDX[k_], ["IDX"], ["IDXi"])
            self.cp("dve", IDXgi[k_], IDXg[k_], ["IDXg"], ["IDXgi"])
        self.p.barrier()
        hb = [vw(A, i * 2048, 2048) for i in range(2)]
        hbb = [vw(A, 4096 + i * 1024, 1024, BF16) for i in range(2)]
        xbuf, ybuf = self.xbuf, self.ybuf
        for t in range(NT):
            i2 = t % 2
            self.dma("sp", hb[i2], self.h1tm[t * 128:(t + 1) * 128, :], ["h1tm"], ["hb%d" % i2])
            self.cp("act", hbb[i2], hb[i2], ["hb%d" % i2], ["hbb%d" % i2])
            for k_ in range(2):
                self.p.op("pool", (lambda src, ix: (lambda e: e.indirect_dma_start(
                    out=xbuf[:, :], out_offset=bass.IndirectOffsetOnAxis(ap=ix, axis=0), in_=src, in_offset=None,
                    bounds_check=NSL - 1, oob_is_err=False)))(hbb[i2], IDXi[k_][:, t:t + 1]),
                    reads=self.Rs("hbb%d" % i2, "IDXi"), writes=self.Rs("xbuf"), dma=True)
        self.p.barrier()
        XT = [v3(B, i * 8 * C, 16, C, BF16) for i in range(2)]
        assert 16 * C <= 18432 and NS * 2048 <= 18432
        acc = v3(A, 0, NS, 2048)
        nchunk = cfg.EFF // 128
        ngrp = NS * 2

        def load_xt(e_, s_):
            self.dma("sp", xtm[s_ % 2], xbuf[e_ * C + s_ * 128:e_ * C + (s_ + 1) * 128, :], ["xbuf"], ["xtm%d" % (s_ % 2)])

        def load_x(e_):
            for s_ in range(min(2, NS)):
                load_xt(e_, s_)

        def xpose_group(e_, g_):
            s_, hf = divmod(g_, 2)
            p16 = self.psb16(7)
            for j in range(8):
                k = hf * 8 + j
                self.tr(p16[:, j * 128:(j + 1) * 128], xtm[s_ % 2][:, k * 128:(k + 1) * 128], self.ident_b[:],
                        ["xtm%d" % (s_ % 2), "identb"], ["ps7"])
            self.cp("act", XT[e_ % 2][:, hf * 8:hf * 8 + 8, s_ * 128:(s_ + 1) * 128],
                    p16[:, 0:1024].rearrange("p (a b) -> p a b", a=8), ["ps7"], ["XT%d" % (e_ % 2)])
            if hf == 1 and s_ + 2 < NS:
                load_xt(e_, s_ + 2)

        load_x(0)
        for g_ in range(ngrp):
            xpose_group(0, g_)
        steps = [(e_, fc) for e_ in range(NE) for fc in range(nchunk)]
        per = -(-ngrp // nchunk)
        gnext = {}
        state = {"it2": 0, "dn": 0}

        def gate_up(i):
            e_, fc = steps[i]
            i2 = i % 2
            c0 = fc * 128
            wgn, wun, wdn = "wg%d" % i2, "wu%d" % i2, "wd%d" % i2
            if fc == 0 and e_ + 1 < NE:
                load_x(e_ + 1)
                gnext[e_ + 1] = 0
            self.wload(wgb[i2], w["moe_w_gate"][0][e_][:, c0:c0 + 128], wgn)
            self.wload(wub[i2], w["moe_w_up"][0][e_][:, c0:c0 + 128], wun)
            self.wload(wdb[i2], w["moe_w_down"][0][e_][c0:c0 + 128, :], wdn)
            xt, xn = XT[e_ % 2], "XT%d" % (e_ % 2)
            for ci, (cc0, n) in enumerate(cgroups):
                bg, bu = ((0, 1), (2, 3))[state["it2"] % 2]
                state["it2"] += 1
                for k in range(16):
                    self.mm(self.psb(bg, n), wgb[i2][:, k, :], xt[:, k, cc0:cc0 + n], k == 0, k == 15, [wgn, xn], ["ps%d" % bg])
                for k in range(16):
                    self.mm(self.psb(bu, n), wub[i2][:, k, :], xt[:, k, cc0:cc0 + n], k == 0, k == 15, [wun, xn], ["ps%d" % bu])
                sl = sil[state["it2"] % 2][:, 0:n]
                sn = "sil%d" % (state["it2"] % 2)
                self.act(sl, self.psb(bg, n), AF.Silu, ["ps%d" % bg], [sn])
                self.tt("dve", actb[i2][:, cc0:cc0 + n], sl, self.psb(bu, n), ALU.mult, [sn, "ps%d" % bu], ["ab%d_%d" % (i2, ci)])
            if e_ + 1 < NE:
                for _ in range(per):
                    if gnext[e_ + 1] < ngrp:
                        xpose_group(e_ + 1, gnext[e_ + 1])
                        gnext[e_ + 1] += 1

        def down(i):
            e_, fc = steps[i]
            i2 = i % 2
            wdn = "wd%d" % i2
            for s_ in range(NS):
                ci = (s_ * 128) // 512
                for q in range(4):
                    bank = 4 + state["dn"] % 3
                    state["dn"] += 1
                    self.mm(self.psb(bank), actb[i2][:, s_ * 128:(s_ + 1) * 128], wdb[i2][:, 0, q * 512:(q + 1) * 512], True, True,
                            ["ab%d_%d" % (i2, ci), wdn], ["ps%d" % bank])
                    av = acc[:, s_, q * 512:(q + 1) * 512]
                    if fc == 0:
                        self.cp("dve", av, self.psb(bank), ["ps%d" % bank], ["acc%d" % s_])
                    else:
                        self.tt("dve", av, av, self.psb(bank), ALU.add, ["acc%d" % s_, "ps%d" % bank], ["acc%d" % s_])
                if fc == nchunk - 1:
                    self.dma("sp", ybuf[e_ * C + s_ * 128:e_ * C + (s_ + 1) * 128, :], acc[:, s_, :], ["acc%d" % s_], ["ybuf"])

        for i in range(len(steps) + 1):
            if i < len(steps):
                gate_up(i)
            if i >= 1:
                down(i - 1)
        self.p.barrier()
        lng = vw(B, 0, 2048)
        lnb = vw(B, 2048, 2048)
        self.dma("sp", lng, w["ln2_g"][l].rearrange("(o n) -> o n", o=1).partition_broadcast(128), [], ["lng"])
        self.dma("sp", lnb, w["ln2_b"][l].rearrange("(o n) -> o n", o=1).partition_broadcast(128), [], ["lng"])
        sets = [dict(y1=vw(A, i * 8192, 2048), y2=vw(A, i * 8192 + 2048, 2048), hh=vw(A, i * 8192 + 4096, 2048),
                     tt=vw(A, i * 8192 + 6144, 2048)) for i in range(2)]
        for t in range(NT):
            i2 = t % 2
            st_ = sets[i2]
            y1, y2, hh, tt_ = st_["y1"], st_["y2"], st_["hh"], st_["tt"]
            n_ = "cb%d" % i2
            for k_, yb in enumerate((y1, y2)):
                self.p.op("pool", (lambda dst, ix: (lambda e: e.indirect_dma_start(
                    out=dst, out_offset=None, in_=ybuf[:, :], in_offset=bass.IndirectOffsetOnAxis(ap=ix, axis=0))))(
                        yb, IDXgi[k_][:, t:t + 1]),
                    reads=self.Rs("ybuf", "IDXgi"), writes=self.Rs(n_ + "y%d" % k_), dma=True)
            self.dma("sp", hh, self.h1tm[t * 128:(t + 1) * 128, :], ["h1tm"], [n_ + "h"])
            self.ts("dve", y1, y1, G1[:, t:t + 1], None, ALU.mult, None, [n_ + "y0", "G"], [n_ + "y0"])
            self.stt("dve", hh, hh, ALPHA, y1, ALU.mult, ALU.add, [n_ + "h", n_ + "y0"], [n_ + "h"])
            self.stt("dve", hh, y2, G2[:, t:t + 1], hh, ALU.mult, ALU.add, [n_ + "h", n_ + "y1", "G"], [n_ + "h"])
            so = 220 + i2 * 8
            ssum, ssq, mean, var, rstd = (sm[:, so + j:so + j + 1] for j in range(5))
            smn = n_ + "s"
            self.act(y1, hh, AF.Identity, [n_ + "h"], [n_ + "y0", smn + "a"], accum=ssum)
            self.act(y2, hh, AF.Square, [n_ + "h"], [n_ + "y1", smn + "b"], accum=ssq)
            self.ts("dve", mean, ssum, 1.0 / D, None, ALU.mult, None, [smn + "a"], [smn + "m"])
            self.tt("dve", var, mean, mean, ALU.mult, [smn + "m"], [smn + "v"])
            self.stt("dve", var, ssq, 1.0 / D, var, ALU.mult, ALU.subtract, [smn + "b", smn + "v"], [smn + "v"])
            self._eps = self.eps5[:, 0:1]
            self.rstd_from(rstd, var, 1.0, [smn + "v", "eps"], [smn + "r"], smn + "r")
            self.ts("dve", tt_, hh, mean, rstd, ALU.subtract, ALU.mult, [n_ + "h", smn + "m", smn + "r"], [n_ + "t"])
            self.tt("pool", tt_, tt_, lng, ALU.mult, [n_ + "t", "lng"], [n_ + "t"])
            self.tt("dve", tt_, tt_, lnb, ALU.add, [n_ + "t", "lng"], [n_ + "t"])
            on = "o_y_%d" % (t * 128)
            self.dma("sp", self.o_y[t * 128:(t + 1) * 128, :], tt_, [n_ + "t"], [on])
            self.outs.append(on)

def rope_tables(S, PAST):
    half = 32
    inv = (np.float32(10000.0) ** (-(np.arange(half, dtype=np.float32)) / np.float32(half))).astype(np.float32)
    pos = np.concatenate([np.arange(S), np.tile(PAST + np.arange(32), 4)]).astype(np.float32)
    ang = pos[:, None] * inv[None, :]
    cos = np.concatenate([np.cos(ang), np.cos(ang)], axis=-1).astype(np.float32)
    sin = np.concatenate([np.sin(ang), np.sin(ang)], axis=-1).astype(np.float32)
    tabM = np.ascontiguousarray(np.concatenate([cos, sin], axis=-1))
    tabT = np.ascontiguousarray(tabM.T)
    return tabT, tabM

_NC_CACHE = {}

def run(cfg, inputs, n_cores):
    key = (cfg.S, cfg.PAST, cfg.FF, cfg.EFF, cfg.DEPTH, cfg.stop, cfg.C)
    if key not in _NC_CACHE:
        _NC_CACHE[key] = KB(cfg).build()
    nc = _NC_CACHE[key]
    L = cfg.DEPTH
    tabT, tabM = rope_tables(cfg.S, cfg.PAST)
    f = lambda a: np.ascontiguousarray(np.asarray(a, dtype=np.float32))
    wmap = {}
    for k in W_INPUTS:
        a = f(inputs[k])
        if k == "w_uq":
            a = a.reshape(L, 768, 8 * 192)
        elif k in ("w_uk", "w_uv"):
            a = a.reshape(L, 512, 1024)
        wmap[k] = a
    in_maps = []
    for c in range(n_cores):
        m = dict(wmap)
        m["x"] = f(np.concatenate([inputs["x_prompt"][c], np.asarray(inputs["x_sample"][4 * c:4 * c + 4]).reshape(128, D)], axis=0))
        m["state_conv"] = f(inputs["state_conv"][:, 4 * c:4 * c + 4])
        m["cache_ckv"] = f(inputs["cache_ckv"][:, 4 * c:4 * c + 4])
        m["cache_krope"] = f(inputs["cache_krope"][:, 4 * c:4 * c + 4])
        m["tabT"] = tabT
        m["tabM"] = tabM
        in_maps.append(m)
    res = run_bass_kernel_spmd(nc, in_maps, core_ids=list(range(n_cores))).results
    if getattr(cfg, "debug", False):
        _NC_CACHE["last_res"] = res
    S = cfg.S
    y_p = np.stack([r["y"][:S] for r in res])
    y_s = np.concatenate([r["y"][S:].reshape(4, 32, D) for r in res])
    conv_p = np.stack([r["conv_p"] for r in res], axis=1)
    ckv_p = np.stack([r["ckv_p"] for r in res], axis=1)
    kr_p = np.stack([r["krope_p"] for r in res], axis=1)
    conv_s = np.concatenate([r["conv_s"] for r in res], axis=1)
    ckv_s = np.concatenate([r["ckv_s"].reshape(L, 4, 32, 512) for r in res], axis=1)
    kr_s = np.concatenate([r["krope_s"].reshape(L, 4, 32, 64) for r in res], axis=1)
    v_s = np.concatenate([r["sguv_s"].reshape(L, 4, 32, 512) for r in res], axis=1)
    return tuple(np.ascontiguousarray(a, dtype=np.float32) for a in
                 (y_p, y_s, conv_p, ckv_p, kr_p, conv_s, ckv_s, kr_s, v_s))

def kernel(**inputs):
    return run(Cfg(), inputs, 8)
```

```python
import math
import copy
import contextlib
import numpy as np
import concourse.bass as bass
import concourse.mybir as mybir
from concourse.bass_utils import run_bass_kernel_spmd

F32 = mybir.dt.float32
BF16 = mybir.dt.bfloat16
I32 = mybir.dt.int32
AF = mybir.ActivationFunctionType
ALU = mybir.AluOpType
AX = mybir.AxisListType

D = 2048
KT = 16
DIN = 3904
NE = 8
SM_SCALE = 192 ** -0.5
ALPHA = 4 ** 0.25
NEG = -30000.0

class Res:
    __slots__ = ("w", "rs", "excl")

    def __init__(self, excl=False):
        self.w = None
        self.rs = []
        self.excl = excl

class _Op:
    __slots__ = ("fn", "deps", "inc", "dma_sem", "dma_val", "val", "noinc")

    def __init__(self, fn):
        self.fn = fn
        self.noinc = False
        self.deps = []
        self.inc = False
        self.dma_sem = None
        self.dma_val = 0
        self.val = 0

class Prog:
    ENGS = ("pe", "act", "dve", "pool", "sp")
    NDMA = 8

    def __init__(self, nc, same=True):
        self.nc = nc
        self.ops = {e: [] for e in self.ENGS}
        self.same = same
        self.dma_ctr = {e: 0 for e in self.ENGS}
        self.dma_cnt = {}
        self.covered = {}

    def op(self, eng, fn, reads=(), writes=(), dma=False, extra=None, noinc=False):
        ops = self.ops[eng]
        seq = len(ops)
        o = _Op(fn)
        o.noinc = noinc
        deps = {}

        def add(d, force=False):
            if d is None:
                return
            e2, s2 = d
            od = self.ops[e2][s2]
            if od.dma_sem is not None:
                k = ("dma", od.dma_sem)
                deps[k] = max(deps.get(k, 0), od.dma_val)
                return
            if e2 == eng and not force and (eng == "pe" or not self.same):
                return
            k = ("eng", e2)
            if s2 > deps.get(k, -1):
                deps[k] = s2

        if any(r.excl for r in reads):
            writes = list(writes) + [r for r in reads if r.excl]
            reads = [r for r in reads if not r.excl]
        for r in reads:
            add(r.w)
        for r in writes:
            add(r.w)
            for d in r.rs:
                add(d)
        if extra:
            for kk, v in extra:
                if kk[0] == "dma":
                    deps[kk] = max(deps.get(kk, 0), v)
                else:
                    add((kk[1], v), force=True)
        if dma:
            k = self.dma_ctr[eng] % self.NDMA
            self.dma_ctr[eng] += 1
            key = (eng, k)
            prev = self.dma_cnt.get(key, 0)
            if prev > 0:
                kk = ("dma", key)
                deps[kk] = max(deps.get(kk, 0), prev * 16)
            self.dma_cnt[key] = prev + 1
            o.dma_sem = key
            o.dma_val = (prev + 1) * 16
        for kk, v in deps.items():
            ck = (eng, kk)
            if self.covered.get(ck, -1) >= v:
                continue
            self.covered[ck] = v
            o.deps.append((kk, v))
            if kk[0] == "eng":
                self.ops[kk[1]][v].inc = True
        for r in reads:
            r.rs.append((eng, seq))
        for r in writes:
            r.w = (eng, seq)
            r.rs = []
        ops.append(o)
        return o

    def clone(self):
        p = Prog(self.nc, self.same)
        for e in self.ENGS:
            lst = []
            for o in self.ops[e]:
                c = _Op(o.fn)
                c.deps, c.inc, c.dma_sem, c.dma_val, c.val, c.noinc = list(o.deps), o.inc, o.dma_sem, o.dma_val, o.val, o.noinc
                lst.append(c)
            p.ops[e] = lst
        p.dma_ctr = dict(self.dma_ctr)
        p.dma_cnt = dict(self.dma_cnt)
        p.covered = dict(self.covered)
        return p

    def barrier(self):
        extra = []
        for e in self.ENGS:
            n = len(self.ops[e])
            for s in range(n - 1, -1, -1):
                od = self.ops[e][s]
                if od.dma_sem is None and od.fn is not None and not od.noinc:
                    extra.append((("eng", e), s))
                    break
        for key, cnt in self.dma_cnt.items():
            extra.append((("dma", key), cnt * 16))
        for e in self.ENGS:
            self.op(e, None, extra=extra)

    def emit(self, final_waits=()):
        self.emit_fork([(self, final_waits)], None, None)

    def emit_fork(self, branches, npre, vals):
        nc = self.nc
        progs = [b[0] for b in branches]
        for pr, fw in branches:
            pr.op("sp", None, reads=list(fw))
        if len(progs) > 1:
            for e in self.ENGS:
                for i in range(npre[e]):
                    inc = any(pr.ops[e][i].inc for pr in progs)
                    for pr in progs:
                        pr.ops[e][i].inc = inc
        for pr in progs:
            for e in self.ENGS:
                c = 0
                for o in pr.ops[e]:
                    if o.inc:
                        c += 1
                        o.val = c
        with contextlib.ExitStack() as st:
            esem = {e: st.enter_context(nc.semaphore("s_" + e)) for e in self.ENGS}
            dsem = {}
            for pr in progs:
                for key in pr.dma_cnt:
                    if key not in dsem:
                        dsem[key] = st.enter_context(nc.semaphore("d_%s%d" % key))
            engobj = {"pe": nc.tensor, "act": nc.scalar, "dve": nc.vector, "pool": nc.gpsimd, "sp": nc.sync}
            regs = {e: st.enter_context(engobj[e].register("r_" + e)) for e in self.ENGS} if len(progs) > 1 else {}
            for pr in progs:
                pr.regs = regs
            block = st.enter_context(nc.Block())
            engmap = {"pe": block.tensor, "act": block.scalar, "dve": block.vector,
                      "pool": block.gpsimd, "sp": block.sync}

            def run(engine, e, pr, ops):
                for o in ops:
                    for kk, v in o.deps:
                        if kk[0] == "eng":
                            engine.wait_ge(esem[kk[1]], pr.ops[kk[1]][v].val)
                        else:
                            engine.wait_ge(dsem[kk[1]], v)
                    if o.fn is None:
                        continue
                    ins = o.fn(engine)
                    if o.dma_sem is not None:
                        ins.then_inc(dsem[o.dma_sem], 16)
                    elif o.inc:
                        ins.then_inc(esem[e], 1)

            def chain(engine, e, i):
                pr = progs[i]
                if i == len(progs) - 1:
                    run(engine, e, pr, pr.ops[e][npre[e]:])
                    return
                with engine.If_eq(regs[e], vals[i]):
                    run(engine, e, pr, pr.ops[e][npre[e]:])
                with engine.Else():
                    chain(engine, e, i + 1)

            def make(e):
                def body(engine):
                    if len(progs) == 1:
                        run(engine, e, self, self.ops[e])
                        return
                    run(engine, e, progs[0], progs[0].ops[e][:npre[e]])
                    chain(engine, e, 0)
                return body

            for e in self.ENGS:
                engmap[e](make(e))

class _Stop(Exception):
    pass

class Cfg:
    def __init__(self, S=2048, PAST=2048, FF=5632, EFF=5632, DEPTH=2, stop=None, C=(896, 1024, 1152)):
        self.S, self.PAST, self.FF, self.EFF, self.DEPTH = S, PAST, FF, EFF, DEPTH
        self.stop = stop
        self.Cs = tuple(sorted(C)) if isinstance(C, (tuple, list)) else (C,)
        self.C = self.Cs[-1]
        self.T = S + 128
        self.NT = self.T // 128
        self.NTP = S // 128
        self.groups = [(s, min(512, S - s)) for s in range(0, S, 512)] + [(S, 128)]
        self.sgs = []
        cur, tot = [], 0
        for gi, (s, n) in enumerate(self.groups):
            if tot + n > 1152:
                self.sgs.append(cur)
                cur, tot = [], 0
            cur.append(gi)
            tot += n
        self.sgs.append(cur)

W_INPUTS = ["w_in", "conv_w", "sgu_ln_g", "sgu_ln_b", "sgu_w", "sgu_b", "q_norm_g", "w_uq", "kv_norm_g",
            "w_uk", "w_uv", "mix_norm_g", "w_o", "ln1_g", "ln1_b", "ln2_g", "ln2_b", "ffn_w_gate",
            "ffn_w_up", "ffn_w_down", "router_w", "moe_w_gate", "moe_w_up", "moe_w_down"]

class KB:
    def __init__(self, cfg):
        self.cfg = cfg
        self.nc = bass.Bass("TRN2", target_bir_lowering=False)
        self.res = {}

    def R(self, name):
        r = self.res.get(name)
        if r is None:
            r = self.res[name] = Res(excl=(name[:2] == "ps" and name[2:].isdigit()))
        return r

    def Rs(self, *names):
        return [self.R(n) for n in names]

    def mm(self, out, lhsT, rhs, start, stop, rd, wr):
        self.p.op("pe", lambda e: e.matmul(out, lhsT=lhsT, rhs=rhs, start=start, stop=stop),
                  reads=self.Rs(*rd), writes=self.Rs(*wr))

    def tr(self, out, in_, ident, rd, wr):
        self.p.op("pe", lambda e: e.transpose(out=out, in_=in_, identity=ident),
                  reads=self.Rs(*rd), writes=self.Rs(*wr))

    def act(self, out, in_, func, rd, wr, bias=None, scale=None, accum=None):
        kw = {}
        if bias is not None:
            kw["bias"] = bias
        if scale is not None:
            kw["scale"] = scale
        if accum is not None:
            kw["accum_out"] = accum
        self.p.op("act", lambda e: e.activation(out=out, in_=in_, func=func, **kw),
                  reads=self.Rs(*rd), writes=self.Rs(*wr))

    def tt(self, eng, out, in0, in1, op, rd, wr):
        self.p.op(eng, lambda e: e.tensor_tensor(out=out, in0=in0, in1=in1, op=op),
                  reads=self.Rs(*rd), writes=self.Rs(*wr))

    def ts(self, eng, out, in0, s1, s2, op0, op1, rd, wr):
        if op1 is None:
            self.p.op(eng, lambda e: e.tensor_scalar(out=out, in0=in0, scalar1=s1, scalar2=None, op0=op0),
                      reads=self.Rs(*rd), writes=self.Rs(*wr))
        else:
            self.p.op(eng, lambda e: e.tensor_scalar(out=out, in0=in0, scalar1=s1, scalar2=s2, op0=op0, op1=op1),
                      reads=self.Rs(*rd), writes=self.Rs(*wr))

    def stt(self, eng, out, in0, scalar, in1, op0, op1, rd, wr):
        self.p.op(eng, lambda e: e.scalar_tensor_tensor(out=out, in0=in0, scalar=scalar, in1=in1, op0=op0, op1=op1),
                  reads=self.Rs(*rd), writes=self.Rs(*wr))

    def cp(self, eng, out, in_, rd, wr):
        if eng == "act":
            self.p.op("act", lambda e: e.activation(out=out, in_=in_, func=AF.Copy), reads=self.Rs(*rd), writes=self.Rs(*wr))
        else:
            self.p.op(eng, lambda e: e.tensor_copy(out=out, in_=in_), reads=self.Rs(*rd), writes=self.Rs(*wr))

    def red(self, out, in_, op, rd, wr):
        self.p.op("dve", lambda e: e.tensor_reduce(out=out, in_=in_, axis=AX.X, op=op),
                  reads=self.Rs(*rd), writes=self.Rs(*wr))

    def recip(self, out, in_, rd, wr):
        self.p.op("dve", lambda e: e.reciprocal(out=out, in_=in_), reads=self.Rs(*rd), writes=self.Rs(*wr))

    def memset(self, eng, ap, v, wr):
        self.p.op(eng, lambda e: e.memset(ap, v), writes=self.Rs(*wr))

    def dma(self, q, out, in_, rd, wr):
        self.p.op(q, lambda e: e.dma_start(out=out, in_=in_), reads=self.Rs(*rd), writes=self.Rs(*wr), dma=True)

    def rstd_from(self, out, in_, scale, rd, wr, tmpname):
        self.act(out, in_, AF.Sqrt, rd, [tmpname], bias=self._eps, scale=scale)
        self.recip(out, out, [tmpname], wr)

    @staticmethod
    def vw(reg, off, n, dt=F32):
        ap = reg[:, off:off + n]
        if dt != F32:
            ap = ap.bitcast(dt)
        return ap

    def v3(self, reg, off, a, b, dt=F32):
        n = a * b if dt == F32 else (a * b) // 2
        return self.vw(reg, off, n, dt).rearrange("p (a b) -> p a b", a=a)

    def build(self):
        cfg, nc = self.cfg, self.nc
        S, T, NT, NTP, PAST = cfg.S, cfg.T, cfg.NT, cfg.NTP, cfg.PAST
        self.p = Prog(nc)
        dt_in = lambda name, shape: nc.dram_tensor(name, list(shape), F32, kind="ExternalInput").ap()
        dt_out = lambda name, shape: nc.dram_tensor(name, list(shape), F32, kind="ExternalOutput").ap()
        L = cfg.DEPTH
        self.x = dt_in("x", [T, D])
        self.state_conv = dt_in("state_conv", [L, 4, 2, 512])
        self.cache_ckv = dt_in("cache_ckv", [L, 4, PAST, 512])
        self.cache_krope = dt_in("cache_krope", [L, 4, PAST, 64])
        self.tabT = dt_in("tabT", [128, T])
        self.tabM = dt_in("tabM", [T, 128])
        self.w = {}
        shapes = {"w_in": [L, D, DIN], "conv_w": [L, 3, 512], "sgu_ln_g": [L, 512], "sgu_ln_b": [L, 512],
                  "sgu_w": [L, 4, 128, 128], "sgu_b": [L, 4, 128], "q_norm_g": [L, 768],
                  "w_uq": [L, 768, 8 * 192], "kv_norm_g": [L, 512], "w_uk": [L, 512, 1024],
                  "w_uv": [L, 512, 1024], "mix_norm_g": [L, D], "w_o": [L, D, D], "ln1_g": [L, D],
                  "ln1_b": [L, D], "ln2_g": [L, D], "ln2_b": [L, D],
                  "ffn_w_gate": [1, D, cfg.FF], "ffn_w_up": [1, D, cfg.FF], "ffn_w_down": [1, cfg.FF, D],
                  "router_w": [1, D, NE], "moe_w_gate": [1, NE, D, cfg.EFF], "moe_w_up": [1, NE, D, cfg.EFF],
                  "moe_w_down": [1, NE, cfg.EFF, D]}
        for k in W_INPUTS:
            self.w[k] = dt_in(k, shapes[k])
        self.o_y = dt_out("y", [T, D])
        self.o_conv_p = dt_out("conv_p", [L, 2, 512])
        self.o_ckv_p = dt_out("ckv_p", [L, S, 512])
        self.o_krope_p = dt_out("krope_p", [L, S, 64])
        self.o_conv_s = dt_out("conv_s", [L, 4, 2, 512])
        self.o_ckv_s = dt_out("ckv_s", [L, 128, 512])
        self.o_krope_s = dt_out("krope_s", [L, 128, 64])
        self.o_sguv_s = dt_out("sguv_s", [L, 128, 512])
        self.hres = nc.dram_tensor("hresT", [D, T], F32, kind="Internal").ap()
        C = cfg.C
        dk = "ExternalOutput" if getattr(cfg, "debug", False) else "Internal"
        self.h1tm = nc.dram_tensor("h1tm", [T, D], F32, kind=dk).ap()
        self.xbuf = nc.dram_tensor("xbuf", [NE * C, D], BF16, kind=dk).ap()
        self.ybuf = nc.dram_tensor("ybuf", [NE * C, D], F32, kind=dk).ap()
        self.outs = []

        with contextlib.ExitStack() as st:
            st.enter_context(nc.allow_non_contiguous_dma(reason="small strided parameter loads"))
            sb = lambda n, s, d=F32: st.enter_context(nc.sbuf_tensor(n, s, d))
            RW = 18432
            self.EW = 13000
            self.RX = sb("RX", [128, RW])
            self.RY = sb("RY", [128, RW])
            self.RE = sb("RE", [128, self.EW])
            self.ident_f = sb("ident_f", [128, 128])
            self.ident_b = sb("ident_b", [128, 128], BF16)
            self.ones_f = sb("ones_f", [128, 128])
            self.eps5 = sb("eps5", [128, 1])
            self.eps6 = sb("eps6", [128, 1])
            self.mrow = sb("mrow", [1, 128], BF16)
            self.mcol = sb("mcol", [1, 128], BF16)
            self.small = sb("small", [128, 256])
            self.par = sb("par", [128, 1664])
            self.ps = [st.enter_context(nc.psum_tensor("ps%d" % i, [128, 512], F32)) for i in range(8)]
            self.setup_consts()
            if L > 1:
                zt = self.vw(self.RE, 0, 1024, BF16)
                self.memset("pool", zt, 0.0, ["zt"])
                for r in range(NE * cfg.C // 128):
                    self.dma("act", self.xbuf[r * 128:(r + 1) * 128, :], zt, ["zt"], ["xbuf"])
            try:
                for l in range(L):
                    A, B = (self.RX, self.RY) if l % 2 == 0 else (self.RY, self.RX)
                    self.layer(l, A, B)
            except _Stop:
                pass
            if getattr(self, "_fork", None) is not None:
                brs = []
                for (pr, res, outs) in self._fork["branches"]:
                    self.res = res
                    brs.append((pr, self.Rs(*outs)))
                brs[0][0].emit_fork(brs, self._fork["npre"], self.fork_vals)
            else:
                self.p.emit(final_waits=self.Rs(*self.outs))
        return nc

    def fork_begin(self, flag_ap):
        p = self.p
        for e in p.ENGS:
            p.op(e, (lambda en: (lambda eng: eng.reg_load(p.regs[en], flag_ap)))(e), reads=self.Rs("FLGi"), noinc=True)
        self._fork = dict(npre={e: len(p.ops[e]) for e in p.ENGS}, prog=p.clone(), res=copy.deepcopy(self.res),
                          outs=list(self.outs), branches=[])

    def fork_next(self, last=False):
        f = self._fork
        f["branches"].append((self.p, self.res, self.outs))
        if not last:
            self.p, self.res, self.outs = f["prog"].clone(), copy.deepcopy(f["res"]), list(f["outs"])

    def fork_end(self):
        self.fork_next(last=True)

    def chk(self, name):
        if self.cfg.stop == "l%d%s" % (self.l, name):
            raise _Stop()

    def psb(self, i, n=512):
        return self.ps[i][:, 0:n]

    def psb16(self, i):
        return self.ps[i][:, :].bitcast(BF16)

    def setup_consts(self):
        self.memset("pool", self.ones_f[:], 1.0, ["ones"])
        self.memset("pool", self.ident_f[:], 1.0, ["identf"])
        idf = self.ident_f
        self.p.op("pool", lambda e: e.affine_select(out=idf[:], in_=idf[:], pattern=[[-1, 128]],
                                                    compare_op=ALU.is_equal, fill=0.0, base=0,
                                                    channel_multiplier=1),
                  reads=self.Rs("identf"), writes=self.Rs("identf"))
        self.cp("dve", self.ident_b[:], self.ident_f[:], ["identf"], ["identb"])
        self.memset("pool", self.eps5[:], 1e-5, ["eps"])
        self.memset("pool", self.eps6[:], 1e-6, ["eps"])
        self.memset("dve", self.mrow[:], 0.0, ["mask"])
        self.memset("dve", self.mrow[:, 0:64], 1.0, ["mask"])
        self.memset("dve", self.mcol[:], 0.0, ["mask"])
        self.memset("dve", self.mcol[:, 64:128], NEG, ["mask"])

    def load_params(self, l):
        par, w = self.par, self.w
        P = {}
        off = [0]

        def alloc(n):
            o = off[0]
            off[0] += n
            return par[:, o:o + n]

        def fm(name, src, k):
            ap = alloc(k)
            self.dma("sp", ap, src.rearrange("(k p) -> p k", p=128), [], ["par"])
            P[name] = ap

        def bc(name, src, n):
            ap = alloc(n)
            self.dma("sp", ap, src.rearrange("(o n) -> o n", o=1).partition_broadcast(128), [], ["par"])
            P[name] = ap

        fm("ln1g", w["ln1_g"][l], 16); fm("ln1b", w["ln1_b"][l], 16)
        fm("ln2g", w["ln2_g"][l], 16); fm("ln2b", w["ln2_b"][l], 16)
        fm("mixg", w["mix_norm_g"][l], 16)
        fm("qg", w["q_norm_g"][l], 6); fm("kvg", w["kv_norm_g"][l], 4)
        ap = alloc(12)
        for r in range(3):
            self.dma("sp", ap.rearrange("p (j r) -> p j r", j=4)[:, :, r], w["conv_w"][l][r].rearrange("(j p) -> p j", p=128), [], ["par"])
        P["convw"] = ap.rearrange("p (j r) -> p j r", j=4)
        bc("sgug", w["sgu_ln_g"][l], 512); bc("sgub", w["sgu_ln_b"][l], 512)
        bc("kvg_bc", w["kv_norm_g"][l], 512)
        ap = alloc(4)
        self.dma("sp", ap, w["sgu_b"][l].rearrange("h p -> p h"), [], ["par"])
        P["sgubias"] = ap
        ap = alloc(4)
        for b in range(4):
            self.dma("sp", ap[32 * b:32 * b + 32, :], w["sgu_b"][l][:, 0:32].rearrange("h p -> p h"), [], ["par"])
        P["sgubias_s"] = ap
        self.P = P

    def layer(self, l, A, B):
        cfg = self.cfg
        S, T, NT, NTP = cfg.S, cfg.T, cfg.NT, cfg.NTP
        self.l = l
        self.HT = self.v3(A, 0, 16, T, BF16)
        self.MT = self.v3(B, 0, 16, T, BF16)
        self._eps = self.eps5[:, 0:1]
        self.p.barrier()
        self.load_params(l)
        self.chk("par")
        if l == 0:
            self.stage0(A, B)
            self.p.barrier()
        self.chk("s0")
        self.stage1(l, A, B)
        self.p.barrier()
        self.chk("s1")
        self.stage2(l, A, B)
        self.p.barrier()
        self.chk("s2")
        self.stage3(l, A, B)
        self.p.barrier()
        self.chk("s3")
        if l % 2 == 1:
            self.stage4_sparse(l, A, B)
        else:
            self.stage4(l, A, B)
        self.chk("s4")

    def stage0(self, A, B):
        cfg = self.cfg
        T, NT = cfg.T, cfg.NT
        xs = [self.vw(B, 0, 2048), self.vw(B, 2048, 2048)]
        stg = [self.v3(B, 4096 + i * 512, 4, 128) for i in range(4)]
        nst = 0
        for t in range(NT):
            xb = xs[t % 2]
            self.dma("sp", xb, self.x[t * 128:(t + 1) * 128, :], [], ["xs%d" % (t % 2)])
            for q in range(4):
                bank = (t * 4 + q) % 6
                for j in range(4):
                    k = q * 4 + j
                    self.tr(self.ps[bank][:, j * 128:(j + 1) * 128], xb[:, k * 128:(k + 1) * 128], self.ident_f[:],
                            ["xs%d" % (t % 2), "identf"], ["ps%d" % bank])
                pv = self.ps[bank][:, :].rearrange("p (a b) -> p a b", a=4)
                self.cp("act", self.HT[:, q * 4:q * 4 + 4, t * 128:(t + 1) * 128], pv, ["ps%d" % bank], ["HT"])
                sg = stg[nst % 4]
                sn = "stg%d" % (nst % 4)
                nst += 1
                self.cp("dve", sg, pv, ["ps%d" % bank], [sn])
                self.dma("sp", self.hres[q * 512:(q + 1) * 512, t * 128:(t + 1) * 128].rearrange("(a p) n -> p a n", p=128),
                         sg, [sn], ["hres"])

    def wload(self, dst, src, name, q="pool"):
        self.dma(q, dst, src.rearrange("(k p) n -> p k n", p=128), [], [name])

    def stage1(self, l, A, B):
        cfg = self.cfg
        S, T, NT, NTP, groups = cfg.S, cfg.T, cfg.NT, cfg.NTP, cfg.groups
        P, w = self.P, self.w
        win = w["w_in"][l]
        HT, MT = self.HT, self.MT
        BH = 8704
        E = self.RE
        self.cqnT = self.v3(E, 0, 6, T, BF16)
        self.ckvnT = self.v3(E, 6528, 4, T, BF16)
        self.kropeT = self.vw(E, 10880, 1088, BF16)
        self.ckvn_new = self.v3(E, 11968, 4, 512, BF16)

        wb = [self.v3(B, BH + i * 3072, 3 * 16, 128, BF16) for i in range(2)]
        zbuf = self.vw(E, 0, S + 2)
        zs = self.v3(E, 2052, 4, 34)
        wk = [[self.vw(E, 2200 + (i * 4 + j) * 512, 512) for j in range(4)] for i in range(2)]
        convw = P["convw"]
        it = 0
        for j in range(4):
            wbj = wb[j % 2]
            wn = "cw%d" % (j % 2)
            for r, c0 in enumerate((512, 1024, 0)):
                self.wload(wbj[:, r * 16:(r + 1) * 16, :], win[:, c0 + j * 128:c0 + (j + 1) * 128], wn)
            self.memset("pool", zbuf[:, 0:2], 0.0, ["zbuf"])
            for b in range(4):
                self.dma("sp", zs[:, b, 0:2], self.state_conv[l][b][:, j * 128:(j + 1) * 128].rearrange("r p -> p r"),
                         [], ["zs"])
            for gi, (g0, n) in enumerate(groups):
                samp = gi == len(groups) - 1
                tmpc, a32, sq, rstd = wk[it % 2]
                wkn = "cwk%d" % (it % 2)
                it += 1
                pc, ph, pb = 0 + 3 * (it % 2), 1 + 3 * (it % 2), 2 + 3 * (it % 2)
                for r, bank in enumerate((pc, ph, pb)):
                    for k in range(16):
                        self.mm(self.psb(bank, n), wbj[:, r * 16 + k, :], HT[:, k, g0:g0 + n], k == 0, k == 15,
                                [wn, "HT"], ["ps%d" % bank])
                self.cp("act", tmpc[:, 0:n], self.psb(pc, n), ["ps%d" % pc], [wkn])
                if not samp:
                    zc = [zbuf[:, g0 + r:g0 + r + n] for r in range(3)]
                    zn = "zbuf"
                    self.tt("dve", zc[2], tmpc[:, 0:n], self.psb(ph, n), ALU.mult, [wkn, "ps%d" % ph], [zn])
                    yv = a32[:, 0:n]
                    pbv = self.psb(pb, n)
                    sqv, rsv = sq[:, 0:n], rstd[:, 0:n]
                    mo = MT[:, j, g0:g0 + n]
                else:
                    zc = [zs[:, :, r:r + 32] for r in range(3)]
                    zn = "zs"
                    self.tt("dve", zc[2], tmpc[:, 0:n].rearrange("p (b q) -> p b q", b=4),
                            self.psb(ph, n).rearrange("p (b q) -> p b q", b=4), ALU.mult, [wkn, "ps%d" % ph], [zn])
                    yv = a32[:, 0:n].rearrange("p (b q) -> p b q", b=4)
                    pbv = self.psb(pb, n).rearrange("p (b q) -> p b q", b=4)
                    sqv, rsv = sq[:, 0:n], rstd[:, 0:n]
                    mo = MT[:, j, g0:g0 + n]
                self.ts("dve", yv, zc[0], convw[:, j, 0:1], None, ALU.mult, None, [zn, "par"], [wkn])
                self.stt("dve", yv, zc[1], convw[:, j, 1:2], yv, ALU.mult, ALU.add, [zn, "par", wkn], [wkn])
                self.stt("dve", yv, zc[2], convw[:, j, 2:3], yv, ALU.mult, ALU.add, [zn, "par", wkn], [wkn])
                self.tt("dve", yv, yv, pbv, ALU.mult, [wkn, "ps%d" % pb], [wkn])
                self.act(sqv, a32[:, 0:n], AF.Square, [wkn], [wkn + "s"])
                self.mm(self.psb(6 + it % 2, n), self.ones_f[:], sqv, True, True, ["ones", wkn + "s"], ["ps%d" % (6 + it % 2)])
                self._eps = self.eps6[:, 0:1]
                self.rstd_from(rsv, self.psb(6 + it % 2, n), 1.0 / 128, ["ps%d" % (6 + it % 2), "eps"], [wkn + "r"], wkn + "r")
                self.stt("dve", mo, a32[:, 0:n], P["mixg"][:, j:j + 1], rsv, ALU.mult, ALU.mult,
                         [wkn, wkn + "r", "par"], ["MT"])
            self.dma("sp", self.o_conv_p[l][:, j * 128:(j + 1) * 128].rearrange("r p -> p r"), zbuf[:, S:S + 2],
                     ["zbuf"], ["o_conv_p%d_%d" % (l, j)])
            self.outs.append("o_conv_p%d_%d" % (l, j))
            for b in range(4):
                on = "o_conv_s%d_%d_%d" % (l, j, b)
                self.dma("sp", self.o_conv_s[l][b][:, j * 128:(j + 1) * 128].rearrange("r p -> p r"), zs[:, b, 32:34],
                         ["zs"], [on])
                self.outs.append(on)
        self.p.barrier()
        self.chk("p1")

        wu = self.v3(B, BH, 16, 512, BF16)
        wv = self.v3(B, BH + 4096, 16, 512, BF16)
        self.wload(wu, win[:, 1536:2048], "wu")
        self.wload(wv, win[:, 2048:2560], "wv")
        wraw = self.v3(E, 0, 4, 128)
        WT = self.v3(E, 512, 4, 128, BF16)
        WTs = self.v3(E, 768, 4, 128, BF16)
        wtf = self.v3(E, 1024, 4, 128)
        self.dma("sp", wraw, w["sgu_w"][l].rearrange("h p q -> p h q"), [], ["wraw"])
        for h in range(4):
            self.tr(self.ps[0][:, h * 128:(h + 1) * 128], wraw[:, h, :], self.ident_f[:], ["wraw", "identf"], ["ps0"])
        self.cp("dve", wtf, self.ps[0][:, :].rearrange("p (a b) -> p a b", a=4), ["ps0"], ["wtf"])
        for h in range(4):
            wslice = wtf[:, h, :]
            self.p.op("pool", (lambda ws: (lambda e: e.affine_select(out=ws, in_=ws, pattern=[[1, 128]],
                                                                       compare_op=ALU.is_ge, fill=0.0, base=0,
                                                                       channel_multiplier=-1)))(wslice),
                      reads=self.Rs("wtf"), writes=self.Rs("wtf"))
        self.cp("dve", WT, wtf, ["wtf"], ["WT"])
        self.memset("pool", WTs, 0.0, ["WTs"])
        for b in range(4):
            self.dma("sp", WTs[32 * b:32 * b + 32, :, 32 * b:32 * b + 32], WT[0:32, :, 0:32], ["WT", "WTs"], ["WTs"])
        sw = 1600
        bufs = []
        for i in range(2):
            o = sw + i * 2700
            bufs.append(dict(gu=self.vw(E, o, 512), gv=self.vw(E, o + 512, 512), sq=self.vw(E, o + 1024, 512),
                             bo=self.vw(E, o + 1536, 512), vnb=self.vw(E, o + 2048, 256, BF16),
                             bon=self.vw(E, o + 2304, 256, BF16)))
        sm = self.small
        for t in range(NT):
            samp = t == NT - 1
            bf = bufs[t % 2]
            bn = "sg%d" % (t % 2)
            gu, gv, sq, bo, vnb, bon = bf["gu"], bf["gv"], bf["sq"], bf["bo"], bf["vnb"], bf["bon"]
            pu, pv, pss, ptt = (0, 1, 2, 3) if t % 2 == 0 else (4, 5, 6, 7)
            tok = slice(t * 128, (t + 1) * 128)
            for k in range(16):
                self.mm(self.psb(pu), HT[:, k, tok], wu[:, k, :], k == 0, k == 15, ["HT", "wu"], ["ps%d" % pu])
            for k in range(16):
                self.mm(self.psb(pv), HT[:, k, tok], wv[:, k, :], k == 0, k == 15, ["HT", "wv"], ["ps%d" % pv])
            self.act(gu, self.psb(pu), AF.Gelu_apprx_tanh, ["ps%d" % pu], [bn + "gu"])
            self.act(gv, self.psb(pv), AF.Gelu_apprx_tanh, ["ps%d" % pv], [bn + "gv"])
            so = (t % 2) * 40
            s1, s2, mean, var, rs = (sm[:, so + i * 4:so + i * 4 + 4] for i in range(5))
            smn = "sm%d" % (t % 2)
            g3 = lambda a: a.rearrange("p (h c) -> p h c", h=4)
            bc3 = lambda a: a.unsqueeze(2).to_broadcast([128, 4, 128])
            self.red(s1, g3(gv), ALU.add, [bn + "gv"], [smn])
            self.tt("pool", sq, gv, gv, ALU.mult, [bn + "gv"], [bn + "sq"])
            self.red(s2, g3(sq), ALU.add, [bn + "sq"], [smn])
            self.ts("dve", mean, s1, 1.0 / 128, None, ALU.mult, None, [smn], [smn])
            self.tt("dve", var, mean, mean, ALU.mult, [smn], [smn])
            self.stt("dve", var, s2, 1.0 / 128, var, ALU.mult, ALU.subtract, [smn], [smn])
            self._eps = self.eps5[:, 0:1]
            self.rstd_from(rs, var, 1.0, [smn, "eps"], [smn], smn)
            self.tt("dve", g3(gv), g3(gv), bc3(mean), ALU.subtract, [bn + "gv", smn], [bn + "gv"])
            self.tt("dve", g3(gv), g3(gv), bc3(rs), ALU.mult, [bn + "gv", smn], [bn + "gv"])
            self.tt("dve", gv, gv, P["sgug"], ALU.mult, [bn + "gv", "par"], [bn + "gv"])
            self.tt("dve", gv, gv, P["sgub"], ALU.add, [bn + "gv", "par"], [bn + "gv"])
            if samp:
                on = "o_sguv%d" % l
                self.dma("sp", self.o_sguv_s[l], gv, [bn + "gv"], [on])
                self.outs.append(on)
            self.cp("act", vnb, gv, [bn + "gv"], [bn + "vnb"])
            wt = WTs if samp else WT
            wtn = "WTs" if samp else "WT"
            for h in range(4):
                self.mm(self.ps[pss][:, h * 128:(h + 1) * 128], wt[:, h, :], vnb[:, h * 128:(h + 1) * 128], True, True,
                        [wtn, bn + "vnb"], ["ps%d" % pss])
            bias = P["sgubias_s"] if samp else P["sgubias"]
            for h in range(4):
                hs = slice(h * 128, (h + 1) * 128)
                self.stt("dve", bo[:, hs], self.ps[pss][:, hs], bias[:, h:h + 1], gu[:, hs], ALU.add, ALU.mult,
                         ["ps%d" % pss, "par", bn + "gu"], [bn + "bo"])
            self.tt("pool", sq, bo, bo, ALU.mult, [bn + "bo"], [bn + "sq"])
            ss, rs2 = sm[:, so + 20:so + 24], sm[:, so + 24:so + 28]
            self.red(ss, g3(sq), ALU.add, [bn + "sq"], [smn])
            self._eps = self.eps6[:, 0:1]
            self.rstd_from(rs2, ss, 1.0 / 128, [smn, "eps"], [smn], smn)
            self.tt("dve", g3(bon), g3(bo), bc3(rs2), ALU.mult, [bn + "bo", smn], [bn + "bon"])
            p16 = self.psb16(ptt)
            for h in range(4):
                self.tr(p16[:, h * 128:(h + 1) * 128], bon[:, h * 128:(h + 1) * 128], self.ident_b[:],
                        [bn + "bon", "identb"], ["ps%d" % ptt])
            self.tt("dve", MT[:, 4:8, tok], p16[:, 0:512].rearrange("p (a b) -> p a b", a=4),
                    P["mixg"][:, 4:8].unsqueeze(2).to_broadcast([128, 4, 128]), ALU.mult,
                    ["ps%d" % ptt, "par"], ["MT"])
        self.p.barrier()
        self.chk("p2")

        wq = self.v3(B, BH, 16, 768, BF16)
        self.wload(wq, win[:, 2560:3328], "wq")
        sqb = [self.vw(B, BH + 6144 + i * 512, 512) for i in range(2)]
        rsb = self.vw(B, BH + 6144 + 1024, 512)
        self._eps = self.eps6[:, 0:1]
        for gi, (g0, n) in enumerate(groups):
            for m in range(6):
                for k in range(16):
                    self.mm(self.psb(m, n), wq[:, k, m * 128:(m + 1) * 128], HT[:, k, g0:g0 + n], k == 0, k == 15,
                            ["wq", "HT"], ["ps%d" % m])
            for m in range(6):
                self.act(sqb[m % 2][:, 0:n], self.psb(m, n), AF.Square, ["ps%d" % m], ["sqb%d" % (m % 2)])
                self.mm(self.psb(7, n), self.ones_f[:], sqb[m % 2][:, 0:n], m == 0, m == 5, ["ones", "sqb%d" % (m % 2)], ["ps7"])
            self.rstd_from(rsb[:, 0:n], self.psb(7, n), 1.0 / 768, ["ps7", "eps"], ["rsb"], "rsb")
            for m in range(6):
                self.stt("dve", self.cqnT[:, m, g0:g0 + n], self.psb(m, n), P["qg"][:, m:m + 1], rsb[:, 0:n],
                         ALU.mult, ALU.mult, ["ps%d" % m, "par", "rsb"], ["cqnT"])
        self.p.barrier()
        self.chk("p3")

        wkv = self.v3(B, BH, 16, 512, BF16)
        self.wload(wkv, win[:, 3328:3840], "wkv")
        wkr = self.v3(B, BH + 4096, 16, 128, BF16)
        self.wload(wkr[:, :, 0:64], win[:, 3840:3904], "wkr")
        self.ts("dve", wkr[:, :, 64:96], wkr[:, :, 32:64], -1.0, None, ALU.mult, None, ["wkr"], ["wkr"])
        self.cp("dve", wkr[:, :, 96:128], wkr[:, :, 0:32], ["wkr"], ["wkr"])
        o5 = BH + 4096 + 1024
        sqb = [self.vw(B, o5 + i * 512, 512) for i in range(2)]
        rsb = self.vw(B, o5 + 1024, 512)
        ctm = [self.vw(B, o5 + 1536 + i * 512, 512) for i in range(2)]
        ctb = self.vw(B, o5 + 2560, 256, BF16)
        junk = self.vw(B, o5 + 2816, 512)
        for gi, (g0, n) in enumerate(groups):
            for m in range(4):
                for k in range(16):
                    self.mm(self.psb(m, n), wkv[:, k, m * 128:(m + 1) * 128], HT[:, k, g0:g0 + n], k == 0, k == 15,
                            ["wkv", "HT"], ["ps%d" % m])
            for m in range(4):
                self.act(sqb[m % 2][:, 0:n], self.psb(m, n), AF.Square, ["ps%d" % m], ["sqb%d" % (m % 2)])
                self.mm(self.psb(7, n), self.ones_f[:], sqb[m % 2][:, 0:n], m == 0, m == 3, ["ones", "sqb%d" % (m % 2)], ["ps7"])
            self.rstd_from(rsb[:, 0:n], self.psb(7, n), 1.0 / 512, ["ps7", "eps"], ["rsb"], "rsb")
            for m in range(4):
                self.stt("dve", self.ckvnT[:, m, g0:g0 + n], self.psb(m, n), P["kvg"][:, m:m + 1], rsb[:, 0:n],
                         ALU.mult, ALU.mult, ["ps%d" % m, "par", "rsb"], ["ckvnT"])
        sm = self.small
        for t in range(NT):
            samp = t == NT - 1
            tok = slice(t * 128, (t + 1) * 128)
            bank = 4 + t % 2
            for k in range(16):
                self.mm(self.psb(bank), HT[:, k, tok], wkv[:, k, :], k == 0, k == 15, ["HT", "wkv"], ["ps%d" % bank])
            ss, rs = sm[:, 100 + (t % 2) * 2:101 + (t % 2) * 2], sm[:, 101 + (t % 2) * 2:102 + (t % 2) * 2]
            smn = "smk%d" % (t % 2)
            self.act(junk, self.psb(bank), AF.Square, ["ps%d" % bank], ["junk", smn], accum=ss)
            self.rstd_from(rs, ss, 1.0 / 512, [smn, "eps"], [smn], smn)
            cb = ctm[t % 2]
            cbn = "ctm%d" % (t % 2)
            self.stt("dve", cb, self.psb(bank), rs, P["kvg_bc"], ALU.mult, ALU.mult, ["ps%d" % bank, smn, "par"], [cbn])
            if not samp:
                on = "o_ckv_p%d_%d" % (l, t)
                self.dma("sp", self.o_ckv_p[l][tok, :], cb, [cbn], [on])
            else:
                on = "o_ckv_s%d" % l
                self.dma("sp", self.o_ckv_s[l], cb, [cbn], [on])
                self.cp("act", ctb, cb, [cbn], ["ctb"])
                for b in range(4):
                    self.dma("sp", self.ckvn_new[0:32, b, :], ctb[32 * b:32 * b + 32, :], ["ctb"], ["ckvn_new"])
            self.outs.append(on)
        self.chk("p4")
        tabm = [self.vw(B, o5 + 3328 + i * 128, 128) for i in range(2)]
        prod = [self.vw(B, o5 + 3584 + i * 128, 128) for i in range(2)]
        dup = [self.vw(B, o5 + 3840 + i * 128, 128) for i in range(2)]
        for t in range(NT):
            samp = t == NT - 1
            tok = slice(t * 128, (t + 1) * 128)
            i2 = t % 2
            bank = 0 + i2
            self.dma("sp", tabm[i2], self.tabM[tok, :], [], ["tabm%d" % i2])
            for k in range(16):
                self.mm(self.ps[bank][:, 0:128], HT[:, k, tok], wkr[:, k, :], k == 0, k == 15, ["HT", "wkr"], ["ps%d" % bank])
            self.tt("dve", prod[i2], self.ps[bank][:, 0:128], tabm[i2], ALU.mult, ["ps%d" % bank, "tabm%d" % i2], ["prod%d" % i2])
            self.tt("dve", dup[i2][:, 0:64], prod[i2][:, 0:64], prod[i2][:, 64:128], ALU.add, ["prod%d" % i2], ["dup%d" % i2])
            self.cp("dve", dup[i2][:, 64:128], dup[i2][:, 0:64], ["dup%d" % i2], ["dup%d" % i2])
            if not samp:
                on = "o_kr_p%d_%d" % (l, t)
                self.dma("sp", self.o_krope_p[l][tok, :], dup[i2][:, 0:64], ["dup%d" % i2], [on])
            else:
                on = "o_kr_s%d" % l
                self.dma("sp", self.o_krope_s[l], dup[i2][:, 0:64], ["dup%d" % i2], [on])
            self.outs.append(on)
            self.tr(self.ps[2 + i2][:, 0:128], dup[i2], self.ident_f[:], ["dup%d" % i2, "identf"], ["ps%d" % (2 + i2)])
            self.cp("act", self.kropeT[:, tok], self.ps[2 + i2][:, 0:128], ["ps%d" % (2 + i2)], ["kropeT"])

    def attn_tail(self, l, h, c_ps, rinv, tok, i2, A):
        P = self.P
        sm = self.small
        o = 16960 + i2 * 192
        c32 = self.vw(A, o, 128)
        cb = self.vw(A, o + 128, 64, BF16)
        n = "at%d" % i2
        ss, rs = sm[:, 120 + i2 * 2:121 + i2 * 2], sm[:, 121 + i2 * 2:122 + i2 * 2]
        if rinv is not None:
            self.ts("dve", c32, c_ps, rinv, None, ALU.mult, None, ["ps7", "sm_rinv"], [n])
        else:
            self.cp("dve", c32, c_ps, ["ps7"], [n])
        junk = self.vw(A, 16960 + 384, 64, BF16)
        self.act(junk, c32, AF.Square, [n], ["junkA", n + "s"], accum=ss)
        self._eps = self.eps6[:, 0:1]
        self.rstd_from(rs, ss, 1.0 / 128, [n + "s", "eps"], [n + "s"], n + "s")
        self.ts("dve", cb, c32, rs, None, ALU.mult, None, [n, n + "s"], [n + "b"])
        p16 = self.psb16(6)
        self.tr(p16[:, i2 * 128:(i2 + 1) * 128], cb, self.ident_b[:], [n + "b", "identb"], ["ps6"])
        self.act(self.MT[:, 8 + h, tok], p16[:, i2 * 128:(i2 + 1) * 128], AF.Copy, ["ps6", "par"], ["MT"],
                 scale=P["mixg"][:, 8 + h:9 + h])

    def stage2(self, l, A, B):
        cfg = self.cfg
        S, T, NT, NTP, PAST, groups = cfg.S, cfg.T, cfg.NT, cfg.NTP, cfg.PAST, cfg.groups
        P, w = self.P, self.w
        MT = self.MT
        sm = self.small
        tabT = self.vw(A, 0, T)
        o = 2176
        wh = []
        for i in range(2):
            wh.append(dict(qn=self.v3(A, o, 6, 128, BF16), qr=self.v3(A, o + 384, 6, 128, BF16),
                           uk=self.v3(A, o + 768, 4, 128, BF16), uv=self.v3(A, o + 1024, 4, 128, BF16),
                           ukT=self.v3(A, o + 1280, 4, 128, BF16)))
            o += 1536
        qnT = self.vw(A, o, T // 2, BF16); o += T // 2
        qrT = self.vw(A, o, T // 2, BF16); o += T // 2
        knT = self.vw(A, o, S // 2, BF16); o += S // 2
        Vh = self.v3(A, o, NTP, 128, BF16); o += NTP * 64
        Pb = [self.vw(A, o + i * ((PAST + 128) // 2), (PAST + 128) // 2, BF16) for i in range(2)]
        o += (PAST + 128)
        PTb = [self.vw(A, o + i * 512, 512, BF16) for i in range(2)]
        o += 1024
        assert o <= 12672, o
        o = 12672
        QlatT = self.vw(A, o, 2048, BF16).rearrange("p (c b h t) -> p c b h t", c=4, b=4, h=8); o += 2048
        QlatF = self.vw(A, o - 2048, 2048, BF16).rearrange("p (c b m t) -> p c b m t", c=4, b=4, m=2)
        OlatT = self.vw(A, o, 2048, BF16).rearrange("p (c h b q) -> p c h b q", c=4, h=8, b=4)
        OlatF = self.vw(A, o, 2048, BF16).rearrange("p (c h t) -> p c h t", c=4, h=8); o += 2048
        wr_tmp = self.v3(A, o, 6, 64, BF16); o += 192
        assert o <= 16960, o
        self.qs_rope = self.vw(A, 17920, 512, BF16).rearrange("p (b h t) -> p b h t", b=4, h=8)
        qs_ropeF = self.vw(A, 17920, 512, BF16).rearrange("p (b m t) -> p b m t", b=4, m=2)
        self.dma("sp", tabT, self.tabT, [], ["tabT"])
        wuq = w["w_uq"][l]
        wuk = w["w_uk"][l]
        wuv = w["w_uv"][l]
        for h in range(8):
            wb = wh[h % 2]
            wn = "wh%d" % (h % 2)
            self.wload(wb["qn"], wuq[:, h * 192:h * 192 + 128], wn)
            self.wload(wr_tmp, wuq[:, h * 192 + 128:h * 192 + 192], "wrtmp")
            self.cp("dve", wb["qr"][:, :, 0:64], wr_tmp, ["wrtmp"], [wn])
            self.ts("dve", wb["qr"][:, :, 64:96], wr_tmp[:, :, 32:64], -1.0, None, ALU.mult, None, ["wrtmp"], [wn])
            self.cp("dve", wb["qr"][:, :, 96:128], wr_tmp[:, :, 0:32], ["wrtmp"], [wn])
            self.wload(wb["uk"], wuk[:, h * 128:(h + 1) * 128], wn)
            self.wload(wb["uv"], wuv[:, h * 128:(h + 1) * 128], wn)
            for gi, (g0, n) in enumerate(groups):
                b0 = (gi % 2) * 2
                for k in range(6):
                    self.mm(self.psb(b0, n), wb["qn"][:, k, :], self.cqnT[:, k, g0:g0 + n], k == 0, k == 5,
                            [wn, "cqnT"], ["ps%d" % b0])
                self.cp("act", qnT[:, g0:g0 + n], self.psb(b0, n), ["ps%d" % b0], ["qnT"])
                for k in range(6):
                    self.mm(self.psb(b0 + 1, n), wb["qr"][:, k, :], self.cqnT[:, k, g0:g0 + n], k == 0, k == 5,
                            [wn, "cqnT"], ["ps%d" % (b0 + 1)])
                self.tt("dve", qrT[:, g0:g0 + n], self.psb(b0 + 1, n), tabT[:, g0:g0 + n], ALU.mult,
                        ["ps%d" % (b0 + 1), "tabT"], ["qrT"])
            self.cp("dve", self.qs_rope[:, :, h, :], qrT[:, S:S + 128].rearrange("p (b t) -> p b t", b=4), ["qrT"], ["qs"])
            for gi, (g0, n) in enumerate(groups[:-1]):
                b0 = 4 + gi % 2
                for k in range(4):
                    self.mm(self.psb(b0, n), wb["uk"][:, k, :], self.ckvnT[:, k, g0:g0 + n], k == 0, k == 3,
                            [wn, "ckvnT"], ["ps%d" % b0])
                self.cp("act", knT[:, g0:g0 + n], self.psb(b0, n), ["ps%d" % b0], ["knT"])
            for t0 in range(0, NTP, 4):
                bank = 6 + (t0 // 4) % 2
                nt = min(4, NTP - t0)
                for j in range(nt):
                    t = t0 + j
                    for k in range(4):
                        self.mm(self.ps[bank][:, j * 128:(j + 1) * 128], self.ckvnT[:, k, t * 128:(t + 1) * 128],
                                wb["uv"][:, k, :], k == 0, k == 3, ["ckvnT", wn], ["ps%d" % bank])
                self.cp("dve", Vh[:, t0:t0 + nt, :], self.ps[bank][:, 0:nt * 128].rearrange("p (a b) -> p a b", a=nt),
                        ["ps%d" % bank], ["Vh"])
            p16 = self.psb16(5)
            for c in range(4):
                self.tr(p16[:, c * 128:(c + 1) * 128], wb["uk"][:, c, :], self.ident_b[:], [wn, "identb"], ["ps5"])
            self.cp("dve", wb["ukT"], p16[:, 0:512].rearrange("p (a b) -> p a b", a=4), ["ps5"], [wn + "T"])
            for c in range(4):
                self.mm(self.ps[4][:, c * 128:(c + 1) * 128], wb["ukT"][:, c, :], qnT[:, S:S + 128], True, True,
                        [wn + "T", "qnT"], ["ps4"])
            self.cp("act", QlatT[:, :, :, h, :], self.ps[4][:, :].rearrange("p (c b t) -> p c b t", c=4, b=4), ["ps4"], ["QlatT"])
            for i in range(NTP):
                nk = (i + 1) * 128
                nb = (nk + 511) // 512
                qs = slice(i * 128, (i + 1) * 128)
                Pq = Pb[i % 2]
                Pn = "P%d" % (i % 2)
                so = 40 * (i % 2) + 140
                mx = sm[:, so:so + 4]
                rs4 = sm[:, so + 4:so + 8]
                m1, negm, rsum, rinv = (sm[:, so + 8 + j:so + 9 + j] for j in range(4))
                smn = "sma%d" % (i % 2)
                for kb in range(nb):
                    ksz = min(512, nk - kb * 512)
                    ks = slice(kb * 512, kb * 512 + ksz)
                    last = kb == nb - 1
                    self.mm(self.psb(kb, ksz), qnT[:, qs], knT[:, ks], True, False, ["qnT", "knT"], ["ps%d" % kb])
                    self.mm(self.psb(kb, ksz), qrT[:, qs], self.kropeT[:, ks], False, not last, ["qrT", "kropeT"], ["ps%d" % kb])
                    if last:
                        self.mm(self.ps[kb][:, ksz - 128:ksz], self.mrow[:], self.mcol[:], False, True, ["mask"], ["ps%d" % kb])
                    self.red(mx[:, kb:kb + 1], self.psb(kb, ksz), ALU.max, ["ps%d" % kb], [smn + "m"])
                self.red(m1, mx[:, 0:nb], ALU.max, [smn + "m"], [smn + "m"])
                self.ts("dve", negm, m1, -SM_SCALE, None, ALU.mult, None, [smn + "m"], [smn + "n"])
                for kb in range(nb):
                    ksz = min(512, nk - kb * 512)
                    ks = slice(kb * 512, kb * 512 + ksz)
                    self.act(Pq[:, ks], self.psb(kb, ksz), AF.Exp, ["ps%d" % kb, smn + "n"], [Pn, smn + "r"],
                             bias=negm, scale=SM_SCALE, accum=rs4[:, kb:kb + 1])
                self.red(rsum, rs4[:, 0:nb], ALU.add, [smn + "r"], [smn + "r"])
                self.recip(rinv, rsum, [smn + "r"], ["sm_rinv"])
                nblk = i + 1
                for c0 in range(0, nblk, 8):
                    cn = min(8, nblk - c0)
                    pi = (c0 // 8) % 2
                    p16 = self.psb16(4 + pi)
                    for j in range(cn):
                        kk = c0 + j
                        self.tr(p16[:, j * 128:(j + 1) * 128], Pq[:, kk * 128:(kk + 1) * 128], self.ident_b[:],
                                [Pn, "identb"], ["ps%d" % (4 + pi)])
                    eng = "act" if pi == 0 else "dve"
                    self.cp(eng, PTb[pi][:, 0:cn * 128], p16[:, 0:cn * 128], ["ps%d" % (4 + pi)], ["PT%d" % pi])
                    for j in range(cn):
                        kk = c0 + j
                        self.mm(self.ps[7][:, 0:128], PTb[pi][:, j * 128:(j + 1) * 128], Vh[:, kk, :], kk == 0, kk == nblk - 1,
                                ["PT%d" % pi, "Vh"], ["ps7"])
                self.attn_tail(l, h, self.ps[7][:, 0:128], rinv, qs, i % 2, A)
        self.p.barrier()
        self.chk("s2h")
        KTc = PAST // 128
        o = 0
        ckv = self.v3(A, o, KTc, 512, BF16); o += KTc * 256
        kr2 = self.v3(A, o, KTc, 128, BF16); o += KTc * 64
        ckvT = self.v3(A, o, 4, PAST, BF16); o += 2 * PAST
        krT = self.vw(A, o, PAST // 2, BF16); o += PAST // 2
        Ps = self.vw(A, o, (PAST + 128) // 2, BF16); o += (PAST + 128) // 2
        PTs = self.v3(A, o, KTc + 1, 128, BF16); o += (KTc + 1) * 64
        olb = self.vw(A, o, 256, BF16); o += 256
        assert o <= 12672, o
        uvb = [self.v3(A, i * 256, 4, 128, BF16) for i in range(2)]
        for b in range(4):
            self.wload(ckv, self.cache_ckv[l][b], "ckv")
            self.dma("pool", kr2[:, :, 0:64], self.cache_krope[l][b].rearrange("(k p) n -> p k n", p=128), [], ["kr2"])
            self.dma("pool", kr2[:, :, 64:128], self.cache_krope[l][b].rearrange("(k p) n -> p k n", p=128), [], ["kr2"])
            nbk = 0
            for c in range(4):
                for t0 in range(0, KTc, 8):
                    cn = min(8, KTc - t0)
                    bank = nbk % 4
                    nbk += 1
                    p16 = self.psb16(bank)
                    for j in range(cn):
                        self.tr(p16[:, j * 128:(j + 1) * 128], ckv[:, t0 + j, c * 128:(c + 1) * 128], self.ident_b[:],
                                ["ckv", "identb"], ["ps%d" % bank])
                    self.cp("act" if nbk % 2 else "dve", ckvT[:, c, t0 * 128:(t0 + cn) * 128], p16[:, 0:cn * 128],
                            ["ps%d" % bank], ["ckvT"])
            for t0 in range(0, KTc, 8):
                cn = min(8, KTc - t0)
                bank = nbk % 4
                nbk += 1
                p16 = self.psb16(bank)
                for j in range(cn):
                    self.tr(p16[:, j * 128:(j + 1) * 128], kr2[:, t0 + j, :], self.ident_b[:], ["kr2", "identb"], ["ps%d" % bank])
                self.cp("act" if nbk % 2 else "dve", krT[:, t0 * 128:(t0 + cn) * 128], p16[:, 0:cn * 128],
                        ["ps%d" % bank], ["krT"])
            newk = slice(S + 32 * b, S + 32 * b + 32)
            nblk = (PAST + 511) // 512
            for mt in range(2):
                hs = slice(4 * mt, 4 * mt + 4)
                qlat = lambda c: QlatF[:, c, b, mt, :]
                qrp = qs_ropeF[:, b, mt, :]
                so = 180
                mx = sm[:, so:so + 8]
                rs8 = sm[:, so + 8:so + 16]
                m1, negm, rsum, rinv = (sm[:, so + 16 + j:so + 17 + j] for j in range(4))
                for kb in range(nblk + 1):
                    bank = kb
                    if kb < nblk:
                        ksz = min(512, PAST - kb * 512)
                        ks = slice(kb * 512, kb * 512 + ksz)
                        for c in range(4):
                            self.mm(self.psb(bank, ksz), qlat(c), ckvT[:, c, ks], c == 0, False, ["QlatT", "ckvT"], ["ps%d" % bank])
                        self.mm(self.psb(bank, ksz), qrp, krT[:, ks], False, True, ["qs", "krT"], ["ps%d" % bank])
                    else:
                        ksz = 32
                        for c in range(4):
                            self.mm(self.psb(bank, ksz), qlat(c), self.ckvnT[:, c, newk], c == 0, False, ["QlatT", "ckvnT"], ["ps%d" % bank])
                        self.mm(self.psb(bank, ksz), qrp, self.kropeT[:, newk], False, True, ["qs", "kropeT"], ["ps%d" % bank])
                    self.red(mx[:, kb:kb + 1], self.psb(bank, ksz), ALU.max, ["ps%d" % bank], ["smsm"])
                self.red(m1, mx[:, 0:nblk + 1], ALU.max, ["smsm"], ["smsm"])
                self.ts("dve", negm, m1, -SM_SCALE, None, ALU.mult, None, ["smsm"], ["smsn"])
                for kb in range(nblk + 1):
                    if kb < nblk:
                        ksz = min(512, PAST - kb * 512)
                        ks = slice(kb * 512, kb * 512 + ksz)
                    else:
                        ksz = 32
                        ks = slice(PAST, PAST + 32)
                    self.act(Ps[:, ks], self.psb(kb, ksz), AF.Exp, ["ps%d" % kb, "smsn"], ["Ps", "smsr"],
                             bias=negm, scale=SM_SCALE, accum=rs8[:, kb:kb + 1])
                self.red(rsum, rs8[:, 0:nblk + 1], ALU.add, ["smsr"], ["smsr"])
                self.recip(rinv, rsum, ["smsr"], ["smsr"])
                for t0 in range(0, KTc, 8):
                    cn = min(8, KTc - t0)
                    bank = 5 + (t0 // 8) % 2
                    p16 = self.psb16(bank)
                    for j in range(cn):
                        self.tr(p16[:, j * 128:(j + 1) * 128], Ps[:, (t0 + j) * 128:(t0 + j + 1) * 128], self.ident_b[:],
                                ["Ps", "identb"], ["ps%d" % bank])
                    self.cp("act" if (t0 // 8) % 2 else "dve", PTs[:, t0:t0 + cn, :],
                            p16[:, 0:cn * 128].rearrange("p (a b) -> p a b", a=cn), ["ps%d" % bank], ["PTs"])
                p16 = self.psb16(5)
                self.tr(p16[0:32, 0:128], Ps[:, PAST:PAST + 32], self.ident_b[:], ["Ps", "identb"], ["ps5"])
                self.cp("dve", PTs[0:32, KTc, :], p16[0:32, 0:128], ["ps5"], ["PTs"])
                for kt in range(KTc):
                    self.mm(self.psb(7), PTs[:, kt, :], ckv[:, kt, :], kt == 0, False, ["PTs", "ckv"], ["ps7"])
                self.mm(self.psb(7), PTs[0:32, KTc, :], self.ckvn_new[0:32, b, :], False, True, ["PTs", "ckvn_new"], ["ps7"])
                self.ts("dve", olb, self.psb(7), rinv, None, ALU.mult, None, ["ps7", "smsr"], ["olb"])
                p16 = self.psb16(6)
                for c in range(4):
                    self.tr(p16[:, c * 128:(c + 1) * 128], olb[:, c * 128:(c + 1) * 128], self.ident_b[:], ["olb", "identb"], ["ps6"])
                for c in range(4):
                    self.cp("act" if c % 2 else "dve", OlatT[:, c, hs, b, :],
                            p16[:, c * 128:(c + 1) * 128].rearrange("p (h q) -> p h q", h=4), ["ps6"], ["OlatT"])
        self.p.barrier()
        self.chk("s2s")
        for h in range(8):
            ub = uvb[h % 2]
            un = "uvb%d" % (h % 2)
            self.wload(ub, wuv[:, h * 128:(h + 1) * 128], un)
            for c in range(4):
                self.mm(self.ps[7][:, 0:128], OlatF[:, c, h, :], ub[:, c, :], c == 0, c == 3, ["OlatT", un], ["ps7"])
            self.attn_tail(l, h, self.ps[7][:, 0:128], None, slice(S, S + 128), h % 2, A)

    def ln_apply(self, l, which, src_of, g0, n, gi, mean_bc, rstd_bc, work, last, A, B, dst_bf, ytm=None, tm_dst=None):
        P = self.P
        g, bta = P[which + "g"], P[which + "b"]
        if last:
            tm_dst = self.o_y
        tm = tm_dst is not None
        step = 256 if tm else n
        for c0 in range(0, n, step):
            nn = min(step, n - c0)
            cs = slice(c0, c0 + nn)
            for m in range(16):
                t1 = work[0][m % 2][:, 0:nn]
                o32 = work[1][m % 2][:, 0:nn]
                n1, n2 = "lnw%d" % (m % 2), "lno%d" % (m % 2)
                self.tt("pool", t1, src_of(m)[:, cs], mean_bc[:, cs], ALU.subtract, ["lnsrc", "lnmean"], [n1])
                self.tt("dve", t1, t1, rstd_bc[:, cs], ALU.mult, [n1, "lnrstd"], [n1])
                if not last:
                    self.act(dst_bf[:, m, g0 + c0:g0 + c0 + nn], t1, AF.Identity, [n1, "par"], ["MTout"],
                             scale=g[:, m:m + 1], bias=bta[:, m:m + 1])
                self.ts("dve", o32, t1, g[:, m:m + 1], bta[:, m:m + 1], ALU.mult, ALU.add, [n1, "par"], [n2])
                if not tm or not last:
                    self.dma("sp", self.hres[m * 128:(m + 1) * 128, g0 + c0:g0 + c0 + nn], o32, [n2], ["hres%d" % gi])
                if tm:
                    for j in range(nn // 128):
                        bank = 4 + j % 2
                        pcol = self.ps[bank][:, (m % 4) * 128:(m % 4 + 1) * 128]
                        self.tr(pcol, o32[:, j * 128:(j + 1) * 128], self.ident_f[:], [n2, "identf"], ["ps%d" % bank])
                        self.cp("act" if j % 2 else "dve", ytm[j][:, m * 128:(m + 1) * 128], pcol, ["ps%d" % bank], ["ytm%d" % j])
            if tm:
                for j in range(nn // 128):
                    t0 = g0 + c0 + j * 128
                    if last:
                        on = "o_y_%d" % t0
                        self.outs.append(on)
                    else:
                        on = "h1tm"
                    self.dma("sp", tm_dst[t0:t0 + 128, :], ytm[j], ["ytm%d" % j], [on])

    def ln_stats(self, n, work):
        mean_bc, rstd_bc, tmp = work
        self.act(mean_bc, self.psb(6, n), AF.Copy, ["ps6"], ["lnmean"], scale=1.0 / D)
        self.tt("dve", tmp, mean_bc, mean_bc, ALU.mult, ["lnmean"], ["lntmp"])
        self.stt("dve", tmp, self.psb(7, n), 1.0 / D, tmp, ALU.mult, ALU.subtract, ["ps7", "lntmp"], ["lntmp"])
        self._eps = self.eps5[:, 0:1]
        self.rstd_from(rstd_bc, tmp, 1.0, ["lntmp", "eps"], ["lnrstd"], "lnrstd")

    def stage3(self, l, A, B):
        cfg = self.cfg
        S, T, NT, groups = cfg.S, cfg.T, cfg.NT, cfg.groups
        P, w = self.P, self.w
        MT = self.MT
        wo = w["w_o"][l]
        o = 0
        rT = self.v3(A, o, 16, 512); o += 8192
        wob = [self.v3(A, o + i * 1024, 16, 128, BF16) for i in range(3)]; o += 3072
        res32 = [self.vw(A, o + i * 512, 512) for i in range(2)]; o += 1024
        sqb = [self.vw(A, o + i * 512, 512) for i in range(2)]; o += 1024
        mean_bc = self.vw(A, o, 512); o += 512
        rstd_bc = self.vw(A, o, 512); o += 512
        tmp = self.vw(A, o, 512); o += 512
        w0 = [self.vw(A, o + i * 512, 512) for i in range(2)]; o += 1024
        w1 = [self.vw(A, o + i * 512, 512) for i in range(2)]; o += 1024
        it = 0
        for gi, (g0, n) in enumerate(groups):
            for m in range(16):
                wb = wob[it % 3]
                wn = "wo%d" % (it % 3)
                it += 1
                self.wload(wb, wo[:, m * 128:(m + 1) * 128], wn)
                bank = m % 4
                for k in range(16):
                    self.mm(self.psb(bank, n), wb[:, k, :], MT[:, k, g0:g0 + n], k == 0, k == 15, [wn, "MT"], ["ps%d" % bank])
                rb = res32[m % 2][:, 0:n]
                self.dma("sp", rb, self.hres[m * 128:(m + 1) * 128, g0:g0 + n], ["hres%d" % gi], ["res%d" % (m % 2)])
                self.stt("dve", rT[:, m, 0:n], rb, ALPHA, self.psb(bank, n), ALU.mult, ALU.add,
                         ["res%d" % (m % 2), "ps%d" % bank], ["lnsrc"])
                sq = sqb[m % 2][:, 0:n]
                self.act(sq, rT[:, m, 0:n], AF.Square, ["lnsrc"], ["lsq%d" % (m % 2)])
                self.mm(self.psb(6, n), self.ones_f[:], rT[:, m, 0:n], m == 0, m == 15, ["ones", "lnsrc"], ["ps6"])
                self.mm(self.psb(7, n), self.ones_f[:], sq, m == 0, m == 15, ["ones", "lsq%d" % (m % 2)], ["ps7"])
            self.ln_stats(n, (mean_bc[:, 0:n], rstd_bc[:, 0:n], tmp[:, 0:n]))
            if l % 2 == 1:
                ytm = [self.vw(self.RE, j * 2048, 2048) for j in range(2)]
                self.ln_apply(l, "ln1", lambda m: rT[:, m, 0:n], g0, n, gi, mean_bc[:, 0:n], rstd_bc[:, 0:n], (w0, w1),
                              False, A, B, MT, ytm=ytm, tm_dst=self.h1tm)
            else:
                self.ln_apply(l, "ln1", lambda m: rT[:, m, 0:n], g0, n, gi, mean_bc[:, 0:n], rstd_bc[:, 0:n], (w0, w1),
                              False, A, B, MT)

    def stage4(self, l, A, B, dg_ext=None):
        cfg = self.cfg
        S, T, NT, groups = cfg.S, cfg.T, cfg.NT, cfg.groups
        P, w = self.P, self.w
        H1 = self.MT
        E = self.RE
        moe = (l % 2 == 1)
        last = (l == cfg.DEPTH - 1)
        sm = self.small
        acc = self.v3(A, 0, 16, 1152)
        FW = 1
        o = 0
        wgb = [self.v3(E, o + i * 1024, 16, 128, BF16) for i in range(2)]; o += 2048
        wub = [self.v3(E, o + i * 1024, 16, 128, BF16) for i in range(2)]; o += 2048
        wdb = [self.v3(E, o + i * 1024, 1, 2048, BF16) for i in range(2)]; o += 2048
        sil = [self.vw(E, o + i * 512, 512) for i in range(2)]; o += 1024
        actb = [self.v3(E, o + i * 256, 1, 512, BF16) for i in range(2)]; o += 512
        gbc = self.vw(E, o, 1152); o += 1152
        assert o <= 8832
        o = 8832
        if moe:
            dg = dg_ext
            dgb = self.vw(E, o, 128); o += 128
        lw = o
        mean_bc = self.vw(E, lw, 512); rstd_bc = self.vw(E, lw + 512, 512); tmp = self.vw(E, lw + 1024, 512)
        assert lw + 1536 <= self.EW, lw
        experts = list(range(NE)) if moe else [None]
        FFd = cfg.EFF if moe else cfg.FF
        nchunk = FFd // (128 * FW)
        it = 0
        it2 = 0
        for sgi, sg in enumerate(cfg.sgs):
            sg0 = groups[sg[0]][0]
            first = True
            for e in experts:
                if moe:
                    wg_d, wu_d, wd_d = w["moe_w_gate"][0][e], w["moe_w_up"][0][e], w["moe_w_down"][0][e]
                    for gi in sg:
                        g0, n = groups[gi]
                        for j in range(n // 128):
                            t = g0 // 128 + j
                            self.cp("dve", dgb, dg[:, t, e:e + 1].to_broadcast([128, 128]), ["dg"], ["dgb"])
                            self.mm(self.ps[5][:, j * 128:(j + 1) * 128], dgb, self.ident_f[:], True, True, ["dgb", "identf"], ["ps5"])
                        self.cp("act", gbc[:, g0 - sg0:g0 - sg0 + n], self.psb(5, n), ["ps5"], ["gbc"])
                else:
                    wg_d, wu_d, wd_d = w["ffn_w_gate"][0], w["ffn_w_up"][0], w["ffn_w_down"][0]
                for fc in range(nchunk):
                    i2 = it % 2
                    it += 1
                    wgn, wun, wdn = "wg%d" % i2, "wu%d" % i2, "wd%d" % i2
                    c0 = fc * 128 * FW
                    self.wload(wgb[i2], wg_d[:, c0:c0 + 128 * FW], wgn)
                    self.wload(wub[i2], wu_d[:, c0:c0 + 128 * FW], wun)
                    self.wload(wdb[i2], wd_d[c0:c0 + 128 * FW, :], wdn)
                    for gi in sg:
                        g0, n = groups[gi]
                        ab = actb[gi % 2]
                        abn = "actb%d" % (gi % 2)
                        for f in range(FW):
                            bg, bu = ((0, 1), (2, 3))[(it2 := it2 + 1) % 2]
                            for k in range(16):
                                self.mm(self.psb(bg, n), wgb[i2][:, k, f * 128:(f + 1) * 128], H1[:, k, g0:g0 + n], k == 0, k == 15,
                                        [wgn, "MT"], ["ps%d" % bg])
                            for k in range(16):
                                self.mm(self.psb(bu, n), wub[i2][:, k, f * 128:(f + 1) * 128], H1[:, k, g0:g0 + n], k == 0, k == 15,
                                        [wun, "MT"], ["ps%d" % bu])
                            sl = sil[it2 % 2][:, 0:n]
                            sn = "sil%d" % (it2 % 2)
                            self.act(sl, self.psb(bg, n), AF.Silu, ["ps%d" % bg], [sn])
                            if moe:
                                self.tt("pool", sl, sl, gbc[:, g0 - sg0:g0 - sg0 + n], ALU.mult, [sn, "gbc"], [sn])
                            self.tt("dve", ab[:, f, 0:n], sl, self.psb(bu, n), ALU.mult, [sn, "ps%d" % bu], [abn])
                        for m in range(16):
                            bank = 4 + m % 2 if not moe else 6 + m % 2
                            for f in range(FW):
                                self.mm(self.psb(bank, n), wdb[i2][:, f, m * 128:(m + 1) * 128], ab[:, f, 0:n], f == 0, f == FW - 1,
                                        [wdn, abn], ["ps%d" % bank])
                            av = acc[:, m, g0 - sg0:g0 - sg0 + n]
                            if first:
                                self.cp("dve", av, self.psb(bank, n), ["ps%d" % bank], ["acc"])
                            else:
                                self.tt("dve", av, av, self.psb(bank, n), ALU.add, ["acc", "ps%d" % bank], ["acc"])
                    first = False
            self.p.barrier()
            res32 = [self.vw(E, i * 512, 512) for i in range(2)]
            sqb = [self.vw(E, 1024 + i * 512, 512) for i in range(2)]
            w0 = [self.vw(E, 2048 + i * 512, 512) for i in range(2)]
            w1 = [self.vw(E, 3072 + i * 512, 512) for i in range(2)]
            ytm = [self.vw(E, 4096 + j * 2048, 2048) for j in range(2)]
            for gi in sg:
                g0, n = groups[gi]
                for m in range(16):
                    rb = res32[m % 2][:, 0:n]
                    self.dma("sp", rb, self.hres[m * 128:(m + 1) * 128, g0:g0 + n], ["hres%d" % gi], ["res%d" % (m % 2)])
                    av = acc[:, m, g0 - sg0:g0 - sg0 + n]
                    self.stt("dve", av, rb, ALPHA, av, ALU.mult, ALU.add, ["res%d" % (m % 2), "acc"], ["lnsrc"])
                    sq = sqb[m % 2][:, 0:n]
                    self.act(sq, av, AF.Square, ["lnsrc"], ["lsq%d" % (m % 2)])
                    self.mm(self.psb(6, n), self.ones_f[:], av, m == 0, m == 15, ["ones", "lnsrc"], ["ps6"])
                    self.mm(self.psb(7, n), self.ones_f[:], sq, m == 0, m == 15, ["ones", "lsq%d" % (m % 2)], ["ps7"])
                self.ln_stats(n, (mean_bc[:, 0:n], rstd_bc[:, 0:n], tmp[:, 0:n]))
                self.ln_apply(l, "ln2", lambda m: acc[:, m, g0 - sg0:g0 - sg0 + n], g0, n, gi, mean_bc[:, 0:n],
                              rstd_bc[:, 0:n], (w0, w1), last, A, B, H1, ytm)
            self.p.barrier()

    def stage4_sparse(self, l, A, B):
        cfg = self.cfg
        S, T, NT, C = cfg.S, cfg.T, cfg.NT, cfg.C
        P, w = self.P, self.w
        H1 = self.MT
        E = self.RE
        sm = self.small
        NS = C // 128
        NSL = NE * C
        cgroups = [(c0, min(512, C - c0)) for c0 in range(0, C, 512)]
        v3, vw = self.v3, self.vw
        o = 0
        wgb = [v3(E, o + i * 1024, 16, 128, BF16) for i in range(2)]; o += 2048
        wub = [v3(E, o + i * 1024, 16, 128, BF16) for i in range(2)]; o += 2048
        wdb = [v3(E, o + i * 1024, 1, 2048, BF16) for i in range(2)]; o += 2048
        sil = [vw(E, o + i * 512, 512) for i in range(2)]; o += 1024
        actb = [vw(E, o + i * (C // 2), C // 2, BF16) for i in range(2)]; o += C
        wr = v3(E, o, 16, 8, BF16); o += 64
        n8 = NT * 8
        so_ = self.EW - 1300
        EQ1 = v3(E, so_, NT, 8); so_ += n8
        EQ2 = v3(E, so_, NT, 8); so_ += n8
        DG = v3(E, so_, NT, 8); so_ += n8
        G1 = vw(E, so_, NT); so_ += NT
        G2 = vw(E, so_, NT); so_ += NT
        CNT = vw(E, so_, 8); so_ += 8
        FLG = vw(E, so_, 2); so_ += 2
        FLGi = vw(E, so_, 2).bitcast(I32); so_ += 2
        assert so_ <= self.EW
        POS = v3(E, o, NT, 8); o += n8
        GI = v3(E, o, NT, 8); o += n8
        TM1 = v3(E, o, NT, 8); o += n8
        TM2 = v3(E, o, NT, 8); o += n8
        IDX = [vw(E, o + i * NT, NT) for i in range(2)]; o += 2 * NT
        IDXg = [vw(E, o + i * NT, NT) for i in range(2)]; o += 2 * NT
        VAL = vw(E, o, NT); o += NT
        IDXi = [vw(E, o + i * NT, NT).bitcast(I32) for i in range(2)]; o += 2 * NT
        IDXgi = [vw(E, o + i * NT, NT).bitcast(I32) for i in range(2)]; o += 2 * NT
        EOFF = vw(E, o, 8); o += 8
        UT = vw(E, o, 128); o += 128
        xtm = [vw(E, o + i * 1024, 1024, BF16) for i in range(2)]; o += 2048
        assert o <= self.EW - 1300, o
        self.wload(wr, w["router_w"][0], "wr")
        self.memset("pool", UT, 1.0, ["UT"])
        self.p.op("pool", lambda e: e.affine_select(out=UT, in_=UT, pattern=[[1, 128]], compare_op=ALU.is_ge, fill=0.0,
                                                    base=-1, channel_multiplier=-1),
                  reads=self.Rs("UT"), writes=self.Rs("UT"))
        for t in range(NT):
            tok = slice(t * 128, (t + 1) * 128)
            for k in range(16):
                self.mm(self.ps[0][:, 0:8], H1[:, k, tok], wr[:, k, :], k == 0, k == 15, ["MT", "wr"], ["ps0"])
            lg = sm[:, 200:208]
            m8 = sm[:, 208:216]
            d12, e12 = sm[:, 216:217], sm[:, 217:218]
            self.cp("dve", lg, self.ps[0][:, 0:8], ["ps0"], ["rt"])
            self.p.op("dve", (lambda a, b: (lambda e: e.max(out=a, in_=b)))(m8, lg), reads=self.Rs("rt"), writes=self.Rs("rt8"))
            self.tt("dve", d12, m8[:, 1:2], m8[:, 0:1], ALU.subtract, ["rt8"], ["rtd"])
            self.act(e12, d12, AF.Exp, ["rtd"], ["rte"])
            self.ts("dve", G1[:, t:t + 1], e12, 1.0, None, ALU.add, None, ["rte"], ["G"])
            self.recip(G1[:, t:t + 1], G1[:, t:t + 1], ["G"], ["G"])
            self.ts("dve", G2[:, t:t + 1], G1[:, t:t + 1], -1.0, 1.0, ALU.mult, ALU.add, ["G"], ["G"])
            self.ts("dve", EQ1[:, t, :], lg, m8[:, 0:1], None, ALU.is_equal, None, ["rt", "rt8"], ["EQ"])
            self.ts("dve", EQ2[:, t, :], lg, m8[:, 1:2], None, ALU.is_equal, None, ["rt", "rt8"], ["EQ"])
        self.tt("dve", TM1, EQ1, EQ2, ALU.add, ["EQ"], ["MASK"])
        for t in range(NT):
            self.mm(self.ps[2][:, 0:8], self.ones_f[:], TM1[:, t, :], t == 0, t == NT - 1, ["ones", "MASK"], ["ps2"])
        self.cp("dve", CNT, self.ps[2][:, 0:8], ["ps2"], ["CNT"])
        self.red(FLG[:, 0:1], CNT, ALU.max, ["CNT"], ["FLG"])
        Cs = cfg.Cs
        self.ts("dve", FLG[:, 1:2], FLG[:, 0:1], float(Cs[0]), None, ALU.is_le, None, ["FLG"], ["FLG"])
        for Cv in Cs[1:]:
            self.stt("dve", FLG[:, 1:2], FLG[:, 0:1], float(Cv), FLG[:, 1:2], ALU.is_le, ALU.add, ["FLG"], ["FLG"])
        self.cp("dve", FLGi, FLG, ["FLG"], ["FLGi"])
        self.tt("dve", DG, EQ1, G1.unsqueeze(2).to_broadcast([128, NT, 8]), ALU.mult, ["EQ", "G"], ["DG"])
        self.tt("dve", TM2, EQ2, G2.unsqueeze(2).to_broadcast([128, NT, 8]), ALU.mult, ["EQ", "G"], ["TM2"])
        self.tt("dve", DG, DG, TM2, ALU.add, ["DG", "TM2"], ["DG"])
        self.p.barrier()
        self.fork_begin(FLGi[0:1, 1:2])
        K = len(Cs)
        self.fork_vals = [K - i for i in range(K)]
        L_ = locals()
        for Cv in Cs:
            self.stage4_sparse_body(l, A, B, L_, Cv)
            self.fork_next()
        self.stage4(l, A, B, dg_ext=DG)
        self.fork_end()

    def stage4_sparse_body(self, l, A, B, L_, C):
        cfg = self.cfg
        S, T, NT = cfg.S, cfg.T, cfg.NT
        P, w = self.P, self.w
        E = self.RE
        sm = self.small
        v3, vw = self.v3, self.vw
        (wgb, wub, wdb, sil, actb, n8, EQ1, EQ2, POS, GI, TM1, TM2, G1, G2, IDX, IDXg, VAL, IDXi, IDXgi, EOFF,
         UT, xtm) = (L_[k] for k in ("wgb", "wub", "wdb", "sil", "actb", "n8", "EQ1", "EQ2", "POS", "GI",
                                     "TM1", "TM2", "G1", "G2", "IDX", "IDXg", "VAL", "IDXi", "IDXgi", "EOFF", "UT", "xtm"))
        NS = C // 128
        NSL = NE * C
        cgroups = [(c0, min(512, C - c0)) for c0 in range(0, C, 512)]
        for e_ in range(NE):
            self.memset("pool", EOFF[:, e_:e_ + 1], float(e_ * C), ["EOFF"])
        for t in range(NT):
            for t2 in range(t + 1):
                lhsT = UT if t2 == t else self.ones_f[:]
                self.mm(self.ps[1][:, t * 8:(t + 1) * 8], lhsT, TM1[:, t2, :], t2 == 0, t2 == t, ["UT", "ones", "MASK"], ["ps1"])
        self.cp("dve", POS, self.ps[1][:, 0:n8].rearrange("p (a b) -> p a b", a=NT), ["ps1"], ["POS"])
        self.tt("dve", GI, POS, EOFF.unsqueeze(1).to_broadcast([128, NT, 8]), ALU.add, ["POS", "EOFF"], ["GI"])
        self.ts("dve", TM2, POS, float(C), 1.0e6, ALU.is_ge, ALU.mult, ["POS"], ["TM2"])
        self.tt("dve", GI, GI, TM2, ALU.add, ["GI", "TM2"], ["GI"])
        for k_, EQ in enumerate((EQ1, EQ2)):
            self.tt("dve", TM2, EQ, GI, ALU.mult, ["EQ", "GI"], ["TM2"])
            self.red(IDX[k_], TM2, ALU.add, ["TM2"], ["IDX"])
            G = (G1, G2)[k_]
            self.ts("dve", VAL, IDX[k_], float(NSL), None, ALU.is_lt, None, ["IDX"], ["VAL"])
            self.tt("dve", G, G, VAL, ALU.mult, ["G", "VAL"], ["G"])
            self.ts("dve", IDXg[k_], IDX[k_], float(NSL - 1), None, ALU.min, None, ["IDX"], ["IDXg"])
            self.cp("dve", IDXi[k_], IDX[k_], ["IDX"], ["IDXi"])
            self.cp("dve", IDXgi[k_], IDXg[k_], ["IDXg"], ["IDXgi"])
        self.p.barrier()
        hb = [vw(A, i * 2048, 2048) for i in range(2)]
        hbb = [vw(A, 4096 + i * 1024, 1024, BF16) for i in range(2)]
        xbuf, ybuf = self.xbuf, self.ybuf
        for t in range(NT):
            i2 = t % 2
            self.dma("sp", hb[i2], self.h1tm[t * 128:(t + 1) * 128, :], ["h1tm"], ["hb%d" % i2])
            self.cp("act", hbb[i2], hb[i2], ["hb%d" % i2], ["hbb%d" % i2])
            for k_ in range(2):
                self.p.op("pool", (lambda src, ix: (lambda e: e.indirect_dma_start(
                    out=xbuf[:, :], out_offset=bass.IndirectOffsetOnAxis(ap=ix, axis=0), in_=src, in_offset=None)))(
                        hbb[i2], IDXgi[k_][:, t:t + 1]),
                    reads=self.Rs("hbb%d" % i2, "IDXgi"), writes=self.Rs("xbuf"), dma=True)
        self.p.barrier()
        XT = [v3(B, i * 8 * C, 16, C, BF16) for i in range(2)]
        assert 16 * C <= 18432 and NS * 2048 <= 18432
        acc = v3(A, 0, NS, 2048)
        nchunk = cfg.EFF // 128
        ngrp = NS * 2

        def load_xt(e_, s_):
            self.dma("sp", xtm[s_ % 2], xbuf[e_ * C + s_ * 128:e_ * C + (s_ + 1) * 128, :], ["xbuf"], ["xtm%d" % (s_ % 2)])

        def load_x(e_):
            for s_ in range(min(2, NS)):
                load_xt(e_, s_)

        def xpose_group(e_, g_):
            s_, hf = divmod(g_, 2)
            p16 = self.psb16(7)
            for j in range(8):
                k = hf * 8 + j
                self.tr(p16[:, j * 128:(j + 1) * 128], xtm[s_ % 2][:, k * 128:(k + 1) * 128], self.ident_b[:],
                        ["xtm%d" % (s_ % 2), "identb"], ["ps7"])
            self.cp("act", XT[e_ % 2][:, hf * 8:hf * 8 + 8, s_ * 128:(s_ + 1) * 128],
                    p16[:, 0:1024].rearrange("p (a b) -> p a b", a=8), ["ps7"], ["XT%d" % (e_ % 2)])
            if hf == 1 and s_ + 2 < NS:
                load_xt(e_, s_ + 2)

        load_x(0)
        for g_ in range(ngrp):
            xpose_group(0, g_)
        steps = [(e_, fc) for e_ in range(NE) for fc in range(nchunk)]
        per = -(-ngrp // nchunk)
        gnext = {}
        state = {"it2": 0, "dn": 0}

        def gate_up(i):
            e_, fc = steps[i]
            i2 = i % 2
            c0 = fc * 128
            wgn, wun, wdn = "wg%d" % i2, "wu%d" % i2, "wd%d" % i2
            if fc == 0 and e_ + 1 < NE:
                load_x(e_ + 1)
                gnext[e_ + 1] = 0
            self.wload(wgb[i2], w["moe_w_gate"][0][e_][:, c0:c0 + 128], wgn)
            self.wload(wub[i2], w["moe_w_up"][0][e_][:, c0:c0 + 128], wun)
            self.wload(wdb[i2], w["moe_w_down"][0][e_][c0:c0 + 128, :], wdn)
            xt, xn = XT[e_ % 2], "XT%d" % (e_ % 2)
            for ci, (cc0, n) in enumerate(cgroups):
                bg, bu = ((0, 1), (2, 3))[state["it2"] % 2]
                state["it2"] += 1
                for k in range(16):
                    self.mm(self.psb(bg, n), wgb[i2][:, k, :], xt[:, k, cc0:cc0 + n], k == 0, k == 15, [wgn, xn], ["ps%d" % bg])
                for k in range(16):
                    self.mm(self.psb(bu, n), wub[i2][:, k, :], xt[:, k, cc0:cc0 + n], k == 0, k == 15, [wun, xn], ["ps%d" % bu])
                sl = sil[state["it2"] % 2][:, 0:n]
                sn = "sil%d" % (state["it2"] % 2)
                self.act(sl, self.psb(bg, n), AF.Silu, ["ps%d" % bg], [sn])
                self.tt("dve", actb[i2][:, cc0:cc0 + n], sl, self.psb(bu, n), ALU.mult, [sn, "ps%d" % bu], ["ab%d_%d" % (i2, ci)])
            if e_ + 1 < NE:
                for _ in range(per):
                    if gnext[e_ + 1] < ngrp:
                        xpose_group(e_ + 1, gnext[e_ + 1])
                        gnext[e_ + 1] += 1

        def down(i):
            e_, fc = steps[i]
            i2 = i % 2
            wdn = "wd%d" % i2
            for s_ in range(NS):
                ci = (s_ * 128) // 512
                for q in range(4):
                    bank = 4 + state["dn"] % 3
                    state["dn"] += 1
                    self.mm(self.psb(bank), actb[i2][:, s_ * 128:(s_ + 1) * 128], wdb[i2][:, 0, q * 512:(q + 1) * 512], True, True,
                            ["ab%d_%d" % (i2, ci), wdn], ["ps%d" % bank])
                    av = acc[:, s_, q * 512:(q + 1) * 512]
                    if fc == 0:
                        self.cp("dve", av, self.psb(bank), ["ps%d" % bank], ["acc%d" % s_])
                    else:
                        self.tt("dve", av, av, self.psb(bank), ALU.add, ["acc%d" % s_, "ps%d" % bank], ["acc%d" % s_])
                if fc == nchunk - 1:
                    self.dma("sp", ybuf[e_ * C + s_ * 128:e_ * C + (s_ + 1) * 128, :], acc[:, s_, :], ["acc%d" % s_], ["ybuf"])

        for i in range(len(steps) + 1):
            if i < len(steps):
                gate_up(i)
            if i >= 1:
                down(i - 1)
        self.p.barrier()
        lng = vw(B, 0, 2048)
        lnb = vw(B, 2048, 2048)
        self.dma("sp", lng, w["ln2_g"][l].rearrange("(o n) -> o n", o=1).partition_broadcast(128), [], ["lng"])
        self.dma("sp", lnb, w["ln2_b"][l].rearrange("(o n) -> o n", o=1).partition_broadcast(128), [], ["lng"])
        sets = [dict(y1=vw(A, i * 8192, 2048), y2=vw(A, i * 8192 + 2048, 2048), hh=vw(A, i * 8192 + 4096, 2048),
                     tt=vw(A, i * 8192 + 6144, 2048)) for i in range(2)]
        for t in range(NT):
            i2 = t % 2
            st_ = sets[i2]
            y1, y2, hh, tt_ = st_["y1"], st_["y2"], st_["hh"], st_["tt"]
            n_ = "cb%d" % i2
            for k_, yb in enumerate((y1, y2)):
                self.p.op("pool", (lambda dst, ix: (lambda e: e.indirect_dma_start(
                    out=dst, out_offset=None, in_=ybuf[:, :], in_offset=bass.IndirectOffsetOnAxis(ap=ix, axis=0))))(
                        yb, IDXgi[k_][:, t:t + 1]),
                    reads=self.Rs("ybuf", "IDXgi"), writes=self.Rs(n_ + "y%d" % k_), dma=True)
            self.dma("sp", hh, self.h1tm[t * 128:(t + 1) * 128, :], ["h1tm"], [n_ + "h"])
            self.ts("dve", y1, y1, G1[:, t:t + 1], None, ALU.mult, None, [n_ + "y0", "G"], [n_ + "y0"])
            self.stt("dve", hh, hh, ALPHA, y1, ALU.mult, ALU.add, [n_ + "h", n_ + "y0"], [n_ + "h"])
            self.stt("dve", hh, y2, G2[:, t:t + 1], hh, ALU.mult, ALU.add, [n_ + "h", n_ + "y1", "G"], [n_ + "h"])
            so = 220 + i2 * 8
            ssum, ssq, mean, var, rstd = (sm[:, so + j:so + j + 1] for j in range(5))
            smn = n_ + "s"
            self.act(y1, hh, AF.Identity, [n_ + "h"], [n_ + "y0", smn + "a"], accum=ssum)
            self.act(y2, hh, AF.Square, [n_ + "h"], [n_ + "y1", smn + "b"], accum=ssq)
            self.ts("dve", mean, ssum, 1.0 / D, None, ALU.mult, None, [smn + "a"], [smn + "m"])
            self.tt("dve", var, mean, mean, ALU.mult, [smn + "m"], [smn + "v"])
            self.stt("dve", var, ssq, 1.0 / D, var, ALU.mult, ALU.subtract, [smn + "b", smn + "v"], [smn + "v"])
            self._eps = self.eps5[:, 0:1]
            self.rstd_from(rstd, var, 1.0, [smn + "v", "eps"], [smn + "r"], smn + "r")
            self.ts("dve", tt_, hh, mean, rstd, ALU.subtract, ALU.mult, [n_ + "h", smn + "m", smn + "r"], [n_ + "t"])
            self.tt("pool", tt_, tt_, lng, ALU.mult, [n_ + "t", "lng"], [n_ + "t"])
            self.tt("dve", tt_, tt_, lnb, ALU.add, [n_ + "t", "lng"], [n_ + "t"])
            on = "o_y_%d" % (t * 128)
            self.dma("sp", self.o_y[t * 128:(t + 1) * 128, :], tt_, [n_ + "t"], [on])
            self.outs.append(on)

def rope_tables(S, PAST):
    half = 32
    inv = (np.float32(10000.0) ** (-(np.arange(half, dtype=np.float32)) / np.float32(half))).astype(np.float32)
    pos = np.concatenate([np.arange(S), np.tile(PAST + np.arange(32), 4)]).astype(np.float32)
    ang = pos[:, None] * inv[None, :]
    cos = np.concatenate([np.cos(ang), np.cos(ang)], axis=-1).astype(np.float32)
    sin = np.concatenate([np.sin(ang), np.sin(ang)], axis=-1).astype(np.float32)
    tabM = np.ascontiguousarray(np.concatenate([cos, sin], axis=-1))
    tabT = np.ascontiguousarray(tabM.T)
    return tabT, tabM

_NC_CACHE = {}

def run(cfg, inputs, n_cores):
    key = (cfg.S, cfg.PAST, cfg.FF, cfg.EFF, cfg.DEPTH, cfg.stop, cfg.Cs)
    if key not in _NC_CACHE:
        _NC_CACHE[key] = KB(cfg).build()
    nc = _NC_CACHE[key]
    L = cfg.DEPTH
    tabT, tabM = rope_tables(cfg.S, cfg.PAST)
    f = lambda a: np.ascontiguousarray(np.asarray(a, dtype=np.float32))
    wmap = {}
    for k in W_INPUTS:
        a = f(inputs[k])
        if k == "w_uq":
            a = a.reshape(L, 768, 8 * 192)
        elif k in ("w_uk", "w_uv"):
            a = a.reshape(L, 512, 1024)
        wmap[k] = a
    in_maps = []
    for c in range(n_cores):
        m = dict(wmap)
        m["x"] = f(np.concatenate([inputs["x_prompt"][c], np.asarray(inputs["x_sample"][4 * c:4 * c + 4]).reshape(128, D)], axis=0))
        m["state_conv"] = f(inputs["state_conv"][:, 4 * c:4 * c + 4])
        m["cache_ckv"] = f(inputs["cache_ckv"][:, 4 * c:4 * c + 4])
        m["cache_krope"] = f(inputs["cache_krope"][:, 4 * c:4 * c + 4])
        m["tabT"] = tabT
        m["tabM"] = tabM
        in_maps.append(m)
    res = run_bass_kernel_spmd(nc, in_maps, core_ids=list(range(n_cores))).results
    if getattr(cfg, "debug", False):
        _NC_CACHE["last_res"] = res
    S = cfg.S
    y_p = np.stack([r["y"][:S] for r in res])
    y_s = np.concatenate([r["y"][S:].reshape(4, 32, D) for r in res])
    conv_p = np.stack([r["conv_p"] for r in res], axis=1)
    ckv_p = np.stack([r["ckv_p"] for r in res], axis=1)
    kr_p = np.stack([r["krope_p"] for r in res], axis=1)
    conv_s = np.concatenate([r["conv_s"] for r in res], axis=1)
    ckv_s = np.concatenate([r["ckv_s"].reshape(L, 4, 32, 512) for r in res], axis=1)
    kr_s = np.concatenate([r["krope_s"].reshape(L, 4, 32, 64) for r in res], axis=1)
    v_s = np.concatenate([r["sguv_s"].reshape(L, 4, 32, 512) for r in res], axis=1)
    return tuple(np.ascontiguousarray(a, dtype=np.float32) for a in
                 (y_p, y_s, conv_p, ckv_p, kr_p, conv_s, ckv_s, kr_s, v_s))

def kernel(**inputs):
    return run(Cfg(), inputs, 8)
```

```python
import math
import copy
import contextlib
import numpy as np
import concourse.bass as bass
import concourse.mybir as mybir
from concourse.bass_utils import run_bass_kernel_spmd

F32 = mybir.dt.float32
BF16 = mybir.dt.bfloat16
I32 = mybir.dt.int32
AF = mybir.ActivationFunctionType
ALU = mybir.AluOpType
AX = mybir.AxisListType

D = 2048
KT = 16
DIN = 3904
NE = 8
SM_SCALE = 192 ** -0.5
ALPHA = 4 ** 0.25
NEG = -30000.0

class Res:
    __slots__ = ("w", "rs", "excl")

    def __init__(self, excl=False):
        self.w = None
        self.rs = []
        self.excl = excl

class _Op:
    __slots__ = ("fn", "deps", "inc", "dma_sem", "dma_val", "val", "noinc")

    def __init__(self, fn):
        self.fn = fn
        self.noinc = False
        self.deps = []
        self.inc = False
        self.dma_sem = None
        self.dma_val = 0
        self.val = 0

class Prog:
    ENGS = ("pe", "act", "dve", "pool", "sp")
    NDMA = 8

    def __init__(self, nc, same=True):
        self.nc = nc
        self.ops = {e: [] for e in self.ENGS}
        self.same = same
        self.dma_ctr = {e: 0 for e in self.ENGS}
        self.dma_cnt = {}
        self.covered = {}

    def op(self, eng, fn, reads=(), writes=(), dma=False, extra=None, noinc=False):
        ops = self.ops[eng]
        seq = len(ops)
        o = _Op(fn)
        o.noinc = noinc
        deps = {}

        def add(d, force=False):
            if d is None:
                return
            e2, s2 = d
            od = self.ops[e2][s2]
            if od.dma_sem is not None:
                k = ("dma", od.dma_sem)
                deps[k] = max(deps.get(k, 0), od.dma_val)
                return
            if e2 == eng and not force and (eng == "pe" or not self.same):
                return
            k = ("eng", e2)
            if s2 > deps.get(k, -1):
                deps[k] = s2

        if any(r.excl for r in reads):
            writes = list(writes) + [r for r in reads if r.excl]
            reads = [r for r in reads if not r.excl]
        for r in reads:
            add(r.w)
        for r in writes:
            add(r.w)
            for d in r.rs:
                add(d)
        if extra:
            for kk, v in extra:
                if kk[0] == "dma":
                    deps[kk] = max(deps.get(kk, 0), v)
                else:
                    add((kk[1], v), force=True)
        if dma:
            k = self.dma_ctr[eng] % self.NDMA
            self.dma_ctr[eng] += 1
            key = (eng, k)
            prev = self.dma_cnt.get(key, 0)
            if prev > 0:
                kk = ("dma", key)
                deps[kk] = max(deps.get(kk, 0), prev * 16)
            self.dma_cnt[key] = prev + 1
            o.dma_sem = key
            o.dma_val = (prev + 1) * 16
        for kk, v in deps.items():
            ck = (eng, kk)
            if self.covered.get(ck, -1) >= v:
                continue
            self.covered[ck] = v
            o.deps.append((kk, v))
            if kk[0] == "eng":
                self.ops[kk[1]][v].inc = True
        for r in reads:
            r.rs.append((eng, seq))
        for r in writes:
            r.w = (eng, seq)
            r.rs = []
        ops.append(o)
        return o

    def clone(self):
        p = Prog(self.nc, self.same)
        for e in self.ENGS:
            lst = []
            for o in self.ops[e]:
                c = _Op(o.fn)
                c.deps, c.inc, c.dma_sem, c.dma_val, c.val, c.noinc = list(o.deps), o.inc, o.dma_sem, o.dma_val, o.val, o.noinc
                lst.append(c)
            p.ops[e] = lst
        p.dma_ctr = dict(self.dma_ctr)
        p.dma_cnt = dict(self.dma_cnt)
        p.covered = dict(self.covered)
        return p

    def barrier(self):
        extra = []
        for e in self.ENGS:
            n = len(self.ops[e])
            for s in range(n - 1, -1, -1):
                od = self.ops[e][s]
                if od.dma_sem is None and od.fn is not None and not od.noinc:
                    extra.append((("eng", e), s))
                    break
        for key, cnt in self.dma_cnt.items():
            extra.append((("dma", key), cnt * 16))
        for e in self.ENGS:
            self.op(e, None, extra=extra)

    def emit(self, final_waits=()):
        self.emit_fork([(self, final_waits)], None, None)

    def emit_fork(self, branches, npre, vals):
        nc = self.nc
        progs = [b[0] for b in branches]
        for pr, fw in branches:
            pr.op("sp", None, reads=list(fw))
        if len(progs) > 1:
            for e in self.ENGS:
                for i in range(npre[e]):
                    inc = any(pr.ops[e][i].inc for pr in progs)
                    for pr in progs:
                        pr.ops[e][i].inc = inc
        for pr in progs:
            for e in self.ENGS:
                c = 0
                for o in pr.ops[e]:
                    if o.inc:
                        c += 1
                        o.val = c
        with contextlib.ExitStack() as st:
            esem = {e: st.enter_context(nc.semaphore("s_" + e)) for e in self.ENGS}
            dsem = {}
            for pr in progs:
                for key in pr.dma_cnt:
                    if key not in dsem:
                        dsem[key] = st.enter_context(nc.semaphore("d_%s%d" % key))
            engobj = {"pe": nc.tensor, "act": nc.scalar, "dve": nc.vector, "pool": nc.gpsimd, "sp": nc.sync}
            regs = {e: st.enter_context(engobj[e].register("r_" + e)) for e in self.ENGS} if len(progs) > 1 else {}
            for pr in progs:
                pr.regs = regs
            block = st.enter_context(nc.Block())
            engmap = {"pe": block.tensor, "act": block.scalar, "dve": block.vector,
                      "pool": block.gpsimd, "sp": block.sync}

            def run(engine, e, pr, ops):
                for o in ops:
                    for kk, v in o.deps:
                        if kk[0] == "eng":
                            engine.wait_ge(esem[kk[1]], pr.ops[kk[1]][v].val)
                        else:
                            engine.wait_ge(dsem[kk[1]], v)
                    if o.fn is None:
                        continue
                    ins = o.fn(engine)
                    if o.dma_sem is not None:
                        ins.then_inc(dsem[o.dma_sem], 16)
                    elif o.inc:
                        ins.then_inc(esem[e], 1)

            def chain(engine, e, i):
                pr = progs[i]
                if i == len(progs) - 1:
                    run(engine, e, pr, pr.ops[e][npre[e]:])
                    return
                with engine.If_eq(regs[e], vals[i]):
                    run(engine, e, pr, pr.ops[e][npre[e]:])
                with engine.Else():
                    chain(engine, e, i + 1)

            def make(e):
                def body(engine):
                    if len(progs) == 1:
                        run(engine, e, self, self.ops[e])
                        return
                    run(engine, e, progs[0], progs[0].ops[e][:npre[e]])
                    chain(engine, e, 0)
                return body

            for e in self.ENGS:
                engmap[e](make(e))

class _Stop(Exception):
    pass

class Cfg:
    def __init__(self, S=2048, PAST=2048, FF=5632, EFF=5632, DEPTH=2, stop=None, C=(896, 1024, 1152)):
        self.S, self.PAST, self.FF, self.EFF, self.DEPTH = S, PAST, FF, EFF, DEPTH
        self.stop = stop
        self.Cs = tuple(sorted(C)) if isinstance(C, (tuple, list)) else (C,)
        self.C = self.Cs[-1]
        self.T = S + 128
        self.NT = self.T // 128
        self.NTP = S // 128
        self.groups = [(s, min(512, S - s)) for s in range(0, S, 512)] + [(S, 128)]
        self.sgs = []
        cur, tot = [], 0
        for gi, (s, n) in enumerate(self.groups):
            if tot + n > 1152:
                self.sgs.append(cur)
                cur, tot = [], 0
            cur.append(gi)
            tot += n
        self.sgs.append(cur)

W_INPUTS = ["w_in", "conv_w", "sgu_ln_g", "sgu_ln_b", "sgu_w", "sgu_b", "q_norm_g", "w_uq", "kv_norm_g",
            "w_uk", "w_uv", "mix_norm_g", "w_o", "ln1_g", "ln1_b", "ln2_g", "ln2_b", "ffn_w_gate",
            "ffn_w_up", "ffn_w_down", "router_w", "moe_w_gate", "moe_w_up", "moe_w_down"]

class KB:
    def __init__(self, cfg):
        self.cfg = cfg
        self.nc = bass.Bass("TRN2", target_bir_lowering=False)
        self.res = {}

    def R(self, name):
        r = self.res.get(name)
        if r is None:
            r = self.res[name] = Res(excl=(name[:2] == "ps" and name[2:].isdigit()))
        return r

    def Rs(self, *names):
        return [self.R(n) for n in names]

    def mm(self, out, lhsT, rhs, start, stop, rd, wr):
        self.p.op("pe", lambda e: e.matmul(out, lhsT=lhsT, rhs=rhs, start=start, stop=stop),
                  reads=self.Rs(*rd), writes=self.Rs(*wr))

    def tr(self, out, in_, ident, rd, wr):
        self.p.op("pe", lambda e: e.transpose(out=out, in_=in_, identity=ident),
                  reads=self.Rs(*rd), writes=self.Rs(*wr))

    def act(self, out, in_, func, rd, wr, bias=None, scale=None, accum=None):
        kw = {}
        if bias is not None:
            kw["bias"] = bias
        if scale is not None:
            kw["scale"] = scale
        if accum is not None:
            kw["accum_out"] = accum
        self.p.op("act", lambda e: e.activation(out=out, in_=in_, func=func, **kw),
                  reads=self.Rs(*rd), writes=self.Rs(*wr))

    def tt(self, eng, out, in0, in1, op, rd, wr):
        self.p.op(eng, lambda e: e.tensor_tensor(out=out, in0=in0, in1=in1, op=op),
                  reads=self.Rs(*rd), writes=self.Rs(*wr))

    def ts(self, eng, out, in0, s1, s2, op0, op1, rd, wr):
        if op1 is None:
            self.p.op(eng, lambda e: e.tensor_scalar(out=out, in0=in0, scalar1=s1, scalar2=None, op0=op0),
                      reads=self.Rs(*rd), writes=self.Rs(*wr))
        else:
            self.p.op(eng, lambda e: e.tensor_scalar(out=out, in0=in0, scalar1=s1, scalar2=s2, op0=op0, op1=op1),
                      reads=self.Rs(*rd), writes=self.Rs(*wr))

    def stt(self, eng, out, in0, scalar, in1, op0, op1, rd, wr):
        self.p.op(eng, lambda e: e.scalar_tensor_tensor(out=out, in0=in0, scalar=scalar, in1=in1, op0=op0, op1=op1),
                  reads=self.Rs(*rd), writes=self.Rs(*wr))

    def cp(self, eng, out, in_, rd, wr):
        if eng == "act":
            self.p.op("act", lambda e: e.activation(out=out, in_=in_, func=AF.Copy), reads=self.Rs(*rd), writes=self.Rs(*wr))
        else:
            self.p.op(eng, lambda e: e.tensor_copy(out=out, in_=in_), reads=self.Rs(*rd), writes=self.Rs(*wr))

    def red(self, out, in_, op, rd, wr):
        self.p.op("dve", lambda e: e.tensor_reduce(out=out, in_=in_, axis=AX.X, op=op),
                  reads=self.Rs(*rd), writes=self.Rs(*wr))

    def recip(self, out, in_, rd, wr):
        self.p.op("dve", lambda e: e.reciprocal(out=out, in_=in_), reads=self.Rs(*rd), writes=self.Rs(*wr))

    def memset(self, eng, ap, v, wr):
        self.p.op(eng, lambda e: e.memset(ap, v), writes=self.Rs(*wr))

    def dma(self, q, out, in_, rd, wr):
        self.p.op(q, lambda e: e.dma_start(out=out, in_=in_), reads=self.Rs(*rd), writes=self.Rs(*wr), dma=True)

    def rstd_from(self, out, in_, scale, rd, wr, tmpname):
        self.act(out, in_, AF.Sqrt, rd, [tmpname], bias=self._eps, scale=scale)
        self.recip(out, out, [tmpname], wr)

    @staticmethod
    def vw(reg, off, n, dt=F32):
        ap = reg[:, off:off + n]
        if dt != F32:
            ap = ap.bitcast(dt)
        return ap

    def v3(self, reg, off, a, b, dt=F32):
        n = a * b if dt == F32 else (a * b) // 2
        return self.vw(reg, off, n, dt).rearrange("p (a b) -> p a b", a=a)

    def build(self):
        cfg, nc = self.cfg, self.nc
        S, T, NT, NTP, PAST = cfg.S, cfg.T, cfg.NT, cfg.NTP, cfg.PAST
        self.p = Prog(nc)
        dt_in = lambda name, shape: nc.dram_tensor(name, list(shape), F32, kind="ExternalInput").ap()
        dt_out = lambda name, shape: nc.dram_tensor(name, list(shape), F32, kind="ExternalOutput").ap()
        L = cfg.DEPTH
        self.x = dt_in("x", [T, D])
        self.state_conv = dt_in("state_conv", [L, 4, 2, 512])
        self.cache_ckv = dt_in("cache_ckv", [L, 4, PAST, 512])
        self.cache_krope = dt_in("cache_krope", [L, 4, PAST, 64])
        self.tabT = dt_in("tabT", [128, T])
        self.tabM = dt_in("tabM", [T, 128])
        self.w = {}
        shapes = {"w_in": [L, D, DIN], "conv_w": [L, 3, 512], "sgu_ln_g": [L, 512], "sgu_ln_b": [L, 512],
                  "sgu_w": [L, 4, 128, 128], "sgu_b": [L, 4, 128], "q_norm_g": [L, 768],
                  "w_uq": [L, 768, 8 * 192], "kv_norm_g": [L, 512], "w_uk": [L, 512, 1024],
                  "w_uv": [L, 512, 1024], "mix_norm_g": [L, D], "w_o": [L, D, D], "ln1_g": [L, D],
                  "ln1_b": [L, D], "ln2_g": [L, D], "ln2_b": [L, D],
                  "ffn_w_gate": [1, D, cfg.FF], "ffn_w_up": [1, D, cfg.FF], "ffn_w_down": [1, cfg.FF, D],
                  "router_w": [1, D, NE], "moe_w_gate": [1, NE, D, cfg.EFF], "moe_w_up": [1, NE, D, cfg.EFF],
                  "moe_w_down": [1, NE, cfg.EFF, D]}
        for k in W_INPUTS:
            self.w[k] = dt_in(k, shapes[k])
        self.o_y = dt_out("y", [T, D])
        self.o_conv_p = dt_out("conv_p", [L, 2, 512])
        self.o_ckv_p = dt_out("ckv_p", [L, S, 512])
        self.o_krope_p = dt_out("krope_p", [L, S, 64])
        self.o_conv_s = dt_out("conv_s", [L, 4, 2, 512])
        self.o_ckv_s = dt_out("ckv_s", [L, 128, 512])
        self.o_krope_s = dt_out("krope_s", [L, 128, 64])
        self.o_sguv_s = dt_out("sguv_s", [L, 128, 512])
        self.hres = nc.dram_tensor("hresT", [D, T], F32, kind="Internal").ap()
        C = cfg.C
        dk = "ExternalOutput" if getattr(cfg, "debug", False) else "Internal"
        self.h1tm = nc.dram_tensor("h1tm", [T, D], F32, kind=dk).ap()
        self.xbuf = nc.dram_tensor("xbuf", [NE * C, D], BF16, kind=dk).ap()
        self.ybuf = nc.dram_tensor("ybuf", [NE * C, D], F32, kind=dk).ap()
        self.outs = []

        with contextlib.ExitStack() as st:
            st.enter_context(nc.allow_non_contiguous_dma(reason="small strided parameter loads"))
            sb = lambda n, s, d=F32: st.enter_context(nc.sbuf_tensor(n, s, d))
            RW = 18432
            self.EW = 13000
            self.RX = sb("RX", [128, RW])
            self.RY = sb("RY", [128, RW])
            self.RE = sb("RE", [128, self.EW])
            self.ident_f = sb("ident_f", [128, 128])
            self.ident_b = sb("ident_b", [128, 128], BF16)
            self.ones_f = sb("ones_f", [128, 128])
            self.eps5 = sb("eps5", [128, 1])
            self.eps6 = sb("eps6", [128, 1])
            self.mrow = sb("mrow", [1, 128], BF16)
            self.mcol = sb("mcol", [1, 128], BF16)
            self.small = sb("small", [128, 256])
            self.par = sb("par", [128, 1664])
            self.ps = [st.enter_context(nc.psum_tensor("ps%d" % i, [128, 512], F32)) for i in range(8)]
            self.setup_consts()
            if L > 1:
                zt = self.vw(self.RE, 0, 1024, BF16)
                self.memset("pool", zt, 0.0, ["zt"])
                for r in range(NE * cfg.C // 128):
                    self.dma("act", self.xbuf[r * 128:(r + 1) * 128, :], zt, ["zt"], ["xbuf"])
            try:
                for l in range(L):
                    A, B = (self.RX, self.RY) if l % 2 == 0 else (self.RY, self.RX)
                    self.layer(l, A, B)
            except _Stop:
                pass
            if getattr(self, "_fork", None) is not None:
                brs = []
                for (pr, res, outs) in self._fork["branches"]:
                    self.res = res
                    brs.append((pr, self.Rs(*outs)))
                brs[0][0].emit_fork(brs, self._fork["npre"], self.fork_vals)
            else:
                self.p.emit(final_waits=self.Rs(*self.outs))
        return nc

    def fork_begin(self, flag_ap):
        p = self.p
        for e in p.ENGS:
            p.op(e, (lambda en: (lambda eng: eng.reg_load(p.regs[en], flag_ap)))(e), reads=self.Rs("FLGi"), noinc=True)
        self._fork = dict(npre={e: len(p.ops[e]) for e in p.ENGS}, prog=p.clone(), res=copy.deepcopy(self.res),
                          outs=list(self.outs), branches=[])

    def fork_next(self, last=False):
        f = self._fork
        f["branches"].append((self.p, self.res, self.outs))
        if not last:
            self.p, self.res, self.outs = f["prog"].clone(), copy.deepcopy(f["res"]), list(f["outs"])

    def fork_end(self):
        self.fork_next(last=True)

    def chk(self, name):
        if self.cfg.stop == "l%d%s" % (self.l, name):
            raise _Stop()

    def psb(self, i, n=512):
        return self.ps[i][:, 0:n]

    def psb16(self, i):
        return self.ps[i][:, :].bitcast(BF16)

    def setup_consts(self):
        self.memset("pool", self.ones_f[:], 1.0, ["ones"])
        self.memset("pool", self.ident_f[:], 1.0, ["identf"])
        idf = self.ident_f
        self.p.op("pool", lambda e: e.affine_select(out=idf[:], in_=idf[:], pattern=[[-1, 128]],
                                                    compare_op=ALU.is_equal, fill=0.0, base=0,
                                                    channel_multiplier=1),
                  reads=self.Rs("identf"), writes=self.Rs("identf"))
        self.cp("dve", self.ident_b[:], self.ident_f[:], ["identf"], ["identb"])
        self.memset("pool", self.eps5[:], 1e-5, ["eps"])
        self.memset("pool", self.eps6[:], 1e-6, ["eps"])
        self.memset("dve", self.mrow[:], 0.0, ["mask"])
        self.memset("dve", self.mrow[:, 0:64], 1.0, ["mask"])
        self.memset("dve", self.mcol[:], 0.0, ["mask"])
        self.memset("dve", self.mcol[:, 64:128], NEG, ["mask"])

    def load_params(self, l):
        par, w = self.par, self.w
        P = {}
        off = [0]

        def alloc(n):
            o = off[0]
            off[0] += n
            return par[:, o:o + n]

        def fm(name, src, k):
            ap = alloc(k)
            self.dma("sp", ap, src.rearrange("(k p) -> p k", p=128), [], ["par"])
            P[name] = ap

        def bc(name, src, n):
            ap = alloc(n)
            self.dma("sp", ap, src.rearrange("(o n) -> o n", o=1).partition_broadcast(128), [], ["par"])
            P[name] = ap

        fm("ln1g", w["ln1_g"][l], 16); fm("ln1b", w["ln1_b"][l], 16)
        fm("ln2g", w["ln2_g"][l], 16); fm("ln2b", w["ln2_b"][l], 16)
        fm("mixg", w["mix_norm_g"][l], 16)
        fm("qg", w["q_norm_g"][l], 6); fm("kvg", w["kv_norm_g"][l], 4)
        ap = alloc(12)
        for r in range(3):
            self.dma("sp", ap.rearrange("p (j r) -> p j r", j=4)[:, :, r], w["conv_w"][l][r].rearrange("(j p) -> p j", p=128), [], ["par"])
        P["convw"] = ap.rearrange("p (j r) -> p j r", j=4)
        bc("sgug", w["sgu_ln_g"][l], 512); bc("sgub", w["sgu_ln_b"][l], 512)
        bc("kvg_bc", w["kv_norm_g"][l], 512)
        ap = alloc(4)
        self.dma("sp", ap, w["sgu_b"][l].rearrange("h p -> p h"), [], ["par"])
        P["sgubias"] = ap
        ap = alloc(4)
        for b in range(4):
            self.dma("sp", ap[32 * b:32 * b + 32, :], w["sgu_b"][l][:, 0:32].rearrange("h p -> p h"), [], ["par"])
        P["sgubias_s"] = ap
        self.P = P

    def layer(self, l, A, B):
        cfg = self.cfg
        S, T, NT, NTP = cfg.S, cfg.T, cfg.NT, cfg.NTP
        self.l = l
        self.HT = self.v3(A, 0, 16, T, BF16)
        self.MT = self.v3(B, 0, 16, T, BF16)
        self._eps = self.eps5[:, 0:1]
        self.p.barrier()
        self.load_params(l)
        self.chk("par")
        if l == 0:
            self.stage0(A, B)
            self.p.barrier()
        self.chk("s0")
        self.stage1(l, A, B)
        self.p.barrier()
        self.chk("s1")
        self.stage2(l, A, B)
        self.p.barrier()
        self.chk("s2")
        self.stage3(l, A, B)
        self.p.barrier()
        self.chk("s3")
        if l % 2 == 1:
            self.stage4_sparse(l, A, B)
        else:
            self.stage4(l, A, B)
        self.chk("s4")

    def stage0(self, A, B):
        cfg = self.cfg
        T, NT = cfg.T, cfg.NT
        xs = [self.vw(B, 0, 2048), self.vw(B, 2048, 2048)]
        stg = [self.v3(B, 4096 + i * 512, 4, 128) for i in range(4)]
        nst = 0
        for t in range(NT):
            xb = xs[t % 2]
            self.dma("sp", xb, self.x[t * 128:(t + 1) * 128, :], [], ["xs%d" % (t % 2)])
            for q in range(4):
                bank = (t * 4 + q) % 6
                for j in range(4):
                    k = q * 4 + j
                    self.tr(self.ps[bank][:, j * 128:(j + 1) * 128], xb[:, k * 128:(k + 1) * 128], self.ident_f[:],
                            ["xs%d" % (t % 2), "identf"], ["ps%d" % bank])
                pv = self.ps[bank][:, :].rearrange("p (a b) -> p a b", a=4)
                self.cp("act", self.HT[:, q * 4:q * 4 + 4, t * 128:(t + 1) * 128], pv, ["ps%d" % bank], ["HT"])
                sg = stg[nst % 4]
                sn = "stg%d" % (nst % 4)
                nst += 1
                self.cp("dve", sg, pv, ["ps%d" % bank], [sn])
                self.dma("sp", self.hres[q * 512:(q + 1) * 512, t * 128:(t + 1) * 128].rearrange("(a p) n -> p a n", p=128),
                         sg, [sn], ["hres"])

    def wload(self, dst, src, name, q="pool"):
        self.dma(q, dst, src.rearrange("(k p) n -> p k n", p=128), [], [name])

    def stage1(self, l, A, B):
        cfg = self.cfg
        S, T, NT, NTP, groups = cfg.S, cfg.T, cfg.NT, cfg.NTP, cfg.groups
        P, w = self.P, self.w
        win = w["w_in"][l]
        HT, MT = self.HT, self.MT
        BH = 8704
        E = self.RE
        self.cqnT = self.v3(E, 0, 6, T, BF16)
        self.ckvnT = self.v3(E, 6528, 4, T, BF16)
        self.kropeT = self.vw(E, 10880, 1088, BF16)
        self.ckvn_new = self.v3(E, 11968, 4, 512, BF16)

        wb = [self.v3(B, BH + i * 3072, 3 * 16, 128, BF16) for i in range(2)]
        zbuf = self.vw(E, 0, S + 2)
        zs = self.v3(E, 2052, 4, 34)
        wk = [[self.vw(E, 2200 + (i * 4 + j) * 512, 512) for j in range(4)] for i in range(2)]
        convw = P["convw"]
        it = 0
        for j in range(4):
            wbj = wb[j % 2]
            wn = "cw%d" % (j % 2)
            for r, c0 in enumerate((512, 1024, 0)):
                self.wload(wbj[:, r * 16:(r + 1) * 16, :], win[:, c0 + j * 128:c0 + (j + 1) * 128], wn)
            self.memset("pool", zbuf[:, 0:2], 0.0, ["zbuf"])
            for b in range(4):
                self.dma("sp", zs[:, b, 0:2], self.state_conv[l][b][:, j * 128:(j + 1) * 128].rearrange("r p -> p r"),
                         [], ["zs"])
            for gi, (g0, n) in enumerate(groups):
                samp = gi == len(groups) - 1
                tmpc, a32, sq, rstd = wk[it % 2]
                wkn = "cwk%d" % (it % 2)
                it += 1
                pc, ph, pb = 0 + 3 * (it % 2), 1 + 3 * (it % 2), 2 + 3 * (it % 2)
                for r, bank in enumerate((pc, ph, pb)):
                    for k in range(16):
                        self.mm(self.psb(bank, n), wbj[:, r * 16 + k, :], HT[:, k, g0:g0 + n], k == 0, k == 15,
                                [wn, "HT"], ["ps%d" % bank])
                self.cp("act", tmpc[:, 0:n], self.psb(pc, n), ["ps%d" % pc], [wkn])
                if not samp:
                    zc = [zbuf[:, g0 + r:g0 + r + n] for r in range(3)]
                    zn = "zbuf"
                    self.tt("dve", zc[2], tmpc[:, 0:n], self.psb(ph, n), ALU.mult, [wkn, "ps%d" % ph], [zn])
                    yv = a32[:, 0:n]
                    pbv = self.psb(pb, n)
                    sqv, rsv = sq[:, 0:n], rstd[:, 0:n]
                    mo = MT[:, j, g0:g0 + n]
                else:
                    zc = [zs[:, :, r:r + 32] for r in range(3)]
                    zn = "zs"
                    self.tt("dve", zc[2], tmpc[:, 0:n].rearrange("p (b q) -> p b q", b=4),
                            self.psb(ph, n).rearrange("p (b q) -> p b q", b=4), ALU.mult, [wkn, "ps%d" % ph], [zn])
                    yv = a32[:, 0:n].rearrange("p (b q) -> p b q", b=4)
                    pbv = self.psb(pb, n).rearrange("p (b q) -> p b q", b=4)
                    sqv, rsv = sq[:, 0:n], rstd[:, 0:n]
                    mo = MT[:, j, g0:g0 + n]
                self.ts("dve", yv, zc[0], convw[:, j, 0:1], None, ALU.mult, None, [zn, "par"], [wkn])
                self.stt("dve", yv, zc[1], convw[:, j, 1:2], yv, ALU.mult, ALU.add, [zn, "par", wkn], [wkn])
                self.stt("dve", yv, zc[2], convw[:, j, 2:3], yv, ALU.mult, ALU.add, [zn, "par", wkn], [wkn])
                self.tt("dve", yv, yv, pbv, ALU.mult, [wkn, "ps%d" % pb], [wkn])
                self.act(sqv, a32[:, 0:n], AF.Square, [wkn], [wkn + "s"])
                self.mm(self.psb(6 + it % 2, n), self.ones_f[:], sqv, True, True, ["ones", wkn + "s"], ["ps%d" % (6 + it % 2)])
                self._eps = self.eps6[:, 0:1]
                self.rstd_from(rsv, self.psb(6 + it % 2, n), 1.0 / 128, ["ps%d" % (6 + it % 2), "eps"], [wkn + "r"], wkn + "r")
                self.stt("dve", mo, a32[:, 0:n], P["mixg"][:, j:j + 1], rsv, ALU.mult, ALU.mult,
                         [wkn, wkn + "r", "par"], ["MT"])
            self.dma("sp", self.o_conv_p[l][:, j * 128:(j + 1) * 128].rearrange("r p -> p r"), zbuf[:, S:S + 2],
                     ["zbuf"], ["o_conv_p%d_%d" % (l, j)])
            self.outs.append("o_conv_p%d_%d" % (l, j))
            for b in range(4):
                on = "o_conv_s%d_%d_%d" % (l, j, b)
                self.dma("sp", self.o_conv_s[l][b][:, j * 128:(j + 1) * 128].rearrange("r p -> p r"), zs[:, b, 32:34],
                         ["zs"], [on])
                self.outs.append(on)
        self.p.barrier()
        self.chk("p1")

        wu = self.v3(B, BH, 16, 512, BF16)
        wv = self.v3(B, BH + 4096, 16, 512, BF16)
        self.wload(wu, win[:, 1536:2048], "wu")
        self.wload(wv, win[:, 2048:2560], "wv")
        wraw = self.v3(E, 0, 4, 128)
        WT = self.v3(E, 512, 4, 128, BF16)
        WTs = self.v3(E, 768, 4, 128, BF16)
        wtf = self.v3(E, 1024, 4, 128)
        self.dma("sp", wraw, w["sgu_w"][l].rearrange("h p q -> p h q"), [], ["wraw"])
        for h in range(4):
            self.tr(self.ps[0][:, h * 128:(h + 1) * 128], wraw[:, h, :], self.ident_f[:], ["wraw", "identf"], ["ps0"])
        self.cp("dve", wtf, self.ps[0][:, :].rearrange("p (a b) -> p a b", a=4), ["ps0"], ["wtf"])
        for h in range(4):
            wslice = wtf[:, h, :]
            self.p.op("pool", (lambda ws: (lambda e: e.affine_select(out=ws, in_=ws, pattern=[[1, 128]],
                                                                       compare_op=ALU.is_ge, fill=0.0, base=0,
                                                                       channel_multiplier=-1)))(wslice),
                      reads=self.Rs("wtf"), writes=self.Rs("wtf"))
        self.cp("dve", WT, wtf, ["wtf"], ["WT"])
        self.memset("pool", WTs, 0.0, ["WTs"])
        for b in range(4):
            self.dma("sp", WTs[32 * b:32 * b + 32, :, 32 * b:32 * b + 32], WT[0:32, :, 0:32], ["WT", "WTs"], ["WTs"])
        sw = 1600
        bufs = []
        for i in range(2):
            o = sw + i * 2700
            bufs.append(dict(gu=self.vw(E, o, 512), gv=self.vw(E, o + 512, 512), sq=self.vw(E, o + 1024, 512),
                             bo=self.vw(E, o + 1536, 512), vnb=self.vw(E, o + 2048, 256, BF16),
                             bon=self.vw(E, o + 2304, 256, BF16)))
        sm = self.small
        for t in range(NT):
            samp = t == NT - 1
            bf = bufs[t % 2]
            bn = "sg%d" % (t % 2)
            gu, gv, sq, bo, vnb, bon = bf["gu"], bf["gv"], bf["sq"], bf["bo"], bf["vnb"], bf["bon"]
            pu, pv, pss, ptt = (0, 1, 2, 3) if t % 2 == 0 else (4, 5, 6, 7)
            tok = slice(t * 128, (t + 1) * 128)
            for k in range(16):
                self.mm(self.psb(pu), HT[:, k, tok], wu[:, k, :], k == 0, k == 15, ["HT", "wu"], ["ps%d" % pu])
            for k in range(16):
                self.mm(self.psb(pv), HT[:, k, tok], wv[:, k, :], k == 0, k == 15, ["HT", "wv"], ["ps%d" % pv])
            self.act(gu, self.psb(pu), AF.Gelu_apprx_tanh, ["ps%d" % pu], [bn + "gu"])
            self.act(gv, self.psb(pv), AF.Gelu_apprx_tanh, ["ps%d" % pv], [bn + "gv"])
            so = (t % 2) * 40
            s1, s2, mean, var, rs = (sm[:, so + i * 4:so + i * 4 + 4] for i in range(5))
            smn = "sm%d" % (t % 2)
            g3 = lambda a: a.rearrange("p (h c) -> p h c", h=4)
            bc3 = lambda a: a.unsqueeze(2).to_broadcast([128, 4, 128])
            self.red(s1, g3(gv), ALU.add, [bn + "gv"], [smn])
            self.tt("pool", sq, gv, gv, ALU.mult, [bn + "gv"], [bn + "sq"])
            self.red(s2, g3(sq), ALU.add, [bn + "sq"], [smn])
            self.ts("dve", mean, s1, 1.0 / 128, None, ALU.mult, None, [smn], [smn])
            self.tt("dve", var, mean, mean, ALU.mult, [smn], [smn])
            self.stt("dve", var, s2, 1.0 / 128, var, ALU.mult, ALU.subtract, [smn], [smn])
            self._eps = self.eps5[:, 0:1]
            self.rstd_from(rs, var, 1.0, [smn, "eps"], [smn], smn)
            self.tt("dve", g3(gv), g3(gv), bc3(mean), ALU.subtract, [bn + "gv", smn], [bn + "gv"])
            self.tt("dve", g3(gv), g3(gv), bc3(rs), ALU.mult, [bn + "gv", smn], [bn + "gv"])
            self.tt("dve", gv, gv, P["sgug"], ALU.mult, [bn + "gv", "par"], [bn + "gv"])
            self.tt("dve", gv, gv, P["sgub"], ALU.add, [bn + "gv", "par"], [bn + "gv"])
            if samp:
                on = "o_sguv%d" % l
                self.dma("sp", self.o_sguv_s[l], gv, [bn + "gv"], [on])
                self.outs.append(on)
            self.cp("act", vnb, gv, [bn + "gv"], [bn + "vnb"])
            wt = WTs if samp else WT
            wtn = "WTs" if samp else "WT"
            for h in range(4):
                self.mm(self.ps[pss][:, h * 128:(h + 1) * 128], wt[:, h, :], vnb[:, h * 128:(h + 1) * 128], True, True,
                        [wtn, bn + "vnb"], ["ps%d" % pss])
            bias = P["sgubias_s"] if samp else P["sgubias"]
            for h in range(4):
                hs = slice(h * 128, (h + 1) * 128)
                self.stt("dve", bo[:, hs], self.ps[pss][:, hs], bias[:, h:h + 1], gu[:, hs], ALU.add, ALU.mult,
                         ["ps%d" % pss, "par", bn + "gu"], [bn + "bo"])
            self.tt("pool", sq, bo, bo, ALU.mult, [bn + "bo"], [bn + "sq"])
            ss, rs2 = sm[:, so + 20:so + 24], sm[:, so + 24:so + 28]
            self.red(ss, g3(sq), ALU.add, [bn + "sq"], [smn])
            self._eps = self.eps6[:, 0:1]
            self.rstd_from(rs2, ss, 1.0 / 128, [smn, "eps"], [smn], smn)
            self.tt("dve", g3(bon), g3(bo), bc3(rs2), ALU.mult, [bn + "bo", smn], [bn + "bon"])
            p16 = self.psb16(ptt)
            for h in range(4):
                self.tr(p16[:, h * 128:(h + 1) * 128], bon[:, h * 128:(h + 1) * 128], self.ident_b[:],
                        [bn + "bon", "identb"], ["ps%d" % ptt])
            self.tt("dve", MT[:, 4:8, tok], p16[:, 0:512].rearrange("p (a b) -> p a b", a=4),
                    P["mixg"][:, 4:8].unsqueeze(2).to_broadcast([128, 4, 128]), ALU.mult,
                    ["ps%d" % ptt, "par"], ["MT"])
        self.p.barrier()
        self.chk("p2")

        wq = self.v3(B, BH, 16, 768, BF16)
        self.wload(wq, win[:, 2560:3328], "wq")
        sqb = [self.vw(B, BH + 6144 + i * 512, 512) for i in range(2)]
        rsb = self.vw(B, BH + 6144 + 1024, 512)
        self._eps = self.eps6[:, 0:1]
        for gi, (g0, n) in enumerate(groups):
            for m in range(6):
                for k in range(16):
                    self.mm(self.psb(m, n), wq[:, k, m * 128:(m + 1) * 128], HT[:, k, g0:g0 + n], k == 0, k == 15,
                            ["wq", "HT"], ["ps%d" % m])
            for m in range(6):
                self.act(sqb[m % 2][:, 0:n], self.psb(m, n), AF.Square, ["ps%d" % m], ["sqb%d" % (m % 2)])
                self.mm(self.psb(7, n), self.ones_f[:], sqb[m % 2][:, 0:n], m == 0, m == 5, ["ones", "sqb%d" % (m % 2)], ["ps7"])
            self.rstd_from(rsb[:, 0:n], self.psb(7, n), 1.0 / 768, ["ps7", "eps"], ["rsb"], "rsb")
            for m in range(6):
                self.stt("dve", self.cqnT[:, m, g0:g0 + n], self.psb(m, n), P["qg"][:, m:m + 1], rsb[:, 0:n],
                         ALU.mult, ALU.mult, ["ps%d" % m, "par", "rsb"], ["cqnT"])
        self.p.barrier()
        self.chk("p3")

        wkv = self.v3(B, BH, 16, 512, BF16)
        self.wload(wkv, win[:, 3328:3840], "wkv")
        wkr = self.v3(B, BH + 4096, 16, 128, BF16)
        self.wload(wkr[:, :, 0:64], win[:, 3840:3904], "wkr")
        self.ts("dve", wkr[:, :, 64:96], wkr[:, :, 32:64], -1.0, None, ALU.mult, None, ["wkr"], ["wkr"])
        self.cp("dve", wkr[:, :, 96:128], wkr[:, :, 0:32], ["wkr"], ["wkr"])
        o5 = BH + 4096 + 1024
        sqb = [self.vw(B, o5 + i * 512, 512) for i in range(2)]
        rsb = self.vw(B, o5 + 1024, 512)
        ctm = [self.vw(B, o5 + 1536 + i * 512, 512) for i in range(2)]
        ctb = self.vw(B, o5 + 2560, 256, BF16)
        junk = self.vw(B, o5 + 2816, 512)
        for gi, (g0, n) in enumerate(groups):
            for m in range(4):
                for k in range(16):
                    self.mm(self.psb(m, n), wkv[:, k, m * 128:(m + 1) * 128], HT[:, k, g0:g0 + n], k == 0, k == 15,
                            ["wkv", "HT"], ["ps%d" % m])
            for m in range(4):
                self.act(sqb[m % 2][:, 0:n], self.psb(m, n), AF.Square, ["ps%d" % m], ["sqb%d" % (m % 2)])
                self.mm(self.psb(7, n), self.ones_f[:], sqb[m % 2][:, 0:n], m == 0, m == 3, ["ones", "sqb%d" % (m % 2)], ["ps7"])
            self.rstd_from(rsb[:, 0:n], self.psb(7, n), 1.0 / 512, ["ps7", "eps"], ["rsb"], "rsb")
            for m in range(4):
                self.stt("dve", self.ckvnT[:, m, g0:g0 + n], self.psb(m, n), P["kvg"][:, m:m + 1], rsb[:, 0:n],
                         ALU.mult, ALU.mult, ["ps%d" % m, "par", "rsb"], ["ckvnT"])
        sm = self.small
        for t in range(NT):
            samp = t == NT - 1
            tok = slice(t * 128, (t + 1) * 128)
            bank = 4 + t % 2
            for k in range(16):
                self.mm(self.psb(bank), HT[:, k, tok], wkv[:, k, :], k == 0, k == 15, ["HT", "wkv"], ["ps%d" % bank])
            ss, rs = sm[:, 100 + (t % 2) * 2:101 + (t % 2) * 2], sm[:, 101 + (t % 2) * 2:102 + (t % 2) * 2]
            smn = "smk%d" % (t % 2)
            self.act(junk, self.psb(bank), AF.Square, ["ps%d" % bank], ["junk", smn], accum=ss)
            self.rstd_from(rs, ss, 1.0 / 512, [smn, "eps"], [smn], smn)
            cb = ctm[t % 2]
            cbn = "ctm%d" % (t % 2)
            self.stt("dve", cb, self.psb(bank), rs, P["kvg_bc"], ALU.mult, ALU.mult, ["ps%d" % bank, smn, "par"], [cbn])
            if not samp:
                on = "o_ckv_p%d_%d" % (l, t)
                self.dma("sp", self.o_ckv_p[l][tok, :], cb, [cbn], [on])
            else:
                on = "o_ckv_s%d" % l
                self.dma("sp", self.o_ckv_s[l], cb, [cbn], [on])
                self.cp("act", ctb, cb, [cbn], ["ctb"])
                for b in range(4):
                    self.dma("sp", self.ckvn_new[0:32, b, :], ctb[32 * b:32 * b + 32, :], ["ctb"], ["ckvn_new"])
            self.outs.append(on)
        self.chk("p4")
        tabm = [self.vw(B, o5 + 3328 + i * 128, 128) for i in range(2)]
        prod = [self.vw(B, o5 + 3584 + i * 128, 128) for i in range(2)]
        dup = [self.vw(B, o5 + 3840 + i * 128, 128) for i in range(2)]
        for t in range(NT):
            samp = t == NT - 1
            tok = slice(t * 128, (t + 1) * 128)
            i2 = t % 2
            bank = 0 + i2
            self.dma("sp", tabm[i2], self.tabM[tok, :], [], ["tabm%d" % i2])
            for k in range(16):
                self.mm(self.ps[bank][:, 0:128], HT[:, k, tok], wkr[:, k, :], k == 0, k == 15, ["HT", "wkr"], ["ps%d" % bank])
            self.tt("dve", prod[i2], self.ps[bank][:, 0:128], tabm[i2], ALU.mult, ["ps%d" % bank, "tabm%d" % i2], ["prod%d" % i2])
            self.tt("dve", dup[i2][:, 0:64], prod[i2][:, 0:64], prod[i2][:, 64:128], ALU.add, ["prod%d" % i2], ["dup%d" % i2])
            self.cp("dve", dup[i2][:, 64:128], dup[i2][:, 0:64], ["dup%d" % i2], ["dup%d" % i2])
            if not samp:
                on = "o_kr_p%d_%d" % (l, t)
                self.dma("sp", self.o_krope_p[l][tok, :], dup[i2][:, 0:64], ["dup%d" % i2], [on])
            else:
                on = "o_kr_s%d" % l
                self.dma("sp", self.o_krope_s[l], dup[i2][:, 0:64], ["dup%d" % i2], [on])
            self.outs.append(on)
            self.tr(self.ps[2 + i2][:, 0:128], dup[i2], self.ident_f[:], ["dup%d" % i2, "identf"], ["ps%d" % (2 + i2)])
            self.cp("act", self.kropeT[:, tok], self.ps[2 + i2][:, 0:128], ["ps%d" % (2 + i2)], ["kropeT"])

    def attn_tail(self, l, h, c_ps, rinv, tok, i2, A):
        self.attn_tail_a(l, h, c_ps, rinv, i2, A)
        self.attn_tail_b(l, h, tok, i2, A)

    def attn_tail_a(self, l, h, c_ps, rinv, i2, A, rn="sm_rinv"):
        sm = self.small
        o = 16960 + i2 * 192
        c32 = self.vw(A, o, 128)
        cb = self.vw(A, o + 128, 64, BF16)
        n = "at%d" % i2
        ss, rs = sm[:, 120 + i2 * 2:121 + i2 * 2], sm[:, 121 + i2 * 2:122 + i2 * 2]
        if rinv is not None:
            self.ts("dve", c32, c_ps, rinv, None, ALU.mult, None, ["ps7", rn], [n])
        else:
            self.cp("dve", c32, c_ps, ["ps7"], [n])
        junk = self.vw(A, 16960 + 384, 64, BF16)
        self.act(junk, c32, AF.Square, [n], ["junkA", n + "s"], accum=ss)
        self._eps = self.eps6[:, 0:1]
        self.rstd_from(rs, ss, 1.0 / 128, [n + "s", "eps"], [n + "s"], n + "s")
        self.ts("dve", cb, c32, rs, None, ALU.mult, None, [n, n + "s"], [n + "b"])

    def attn_tail_b(self, l, h, tok, i2, A):
        P = self.P
        o = 16960 + i2 * 192
        cb = self.vw(A, o + 128, 64, BF16)
        n = "at%d" % i2
        p16 = self.psb16(6)
        self.tr(p16[:, i2 * 128:(i2 + 1) * 128], cb, self.ident_b[:], [n + "b", "identb"], ["ps6"])
        self.act(self.MT[:, 8 + h, tok], p16[:, i2 * 128:(i2 + 1) * 128], AF.Copy, ["ps6", "par"], ["MT"],
                 scale=P["mixg"][:, 8 + h:9 + h])

    def stage2(self, l, A, B):
        cfg = self.cfg
        S, T, NT, NTP, PAST, groups = cfg.S, cfg.T, cfg.NT, cfg.NTP, cfg.PAST, cfg.groups
        P, w = self.P, self.w
        MT = self.MT
        sm = self.small
        tabT = self.vw(A, 0, T)
        o = 2176
        wh = []
        for i in range(2):
            wh.append(dict(qn=self.v3(A, o, 6, 128, BF16), qr=self.v3(A, o + 384, 6, 128, BF16),
                           uk=self.v3(A, o + 768, 4, 128, BF16), uv=self.v3(A, o + 1024, 4, 128, BF16),
                           ukT=self.v3(A, o + 1280, 4, 128, BF16)))
            o += 1536
        qnT = self.vw(A, o, T // 2, BF16); o += T // 2
        qrT = self.vw(A, o, T // 2, BF16); o += T // 2
        knT = self.vw(A, o, S // 2, BF16); o += S // 2
        Vh = self.v3(A, o, NTP, 128, BF16); o += NTP * 64
        Pb = [self.vw(A, o + i * ((PAST + 128) // 2), (PAST + 128) // 2, BF16) for i in range(2)]
        o += (PAST + 128)
        PTb = [self.vw(A, o + i * 512, 512, BF16) for i in range(2)]
        o += 1024
        assert o <= 12672, o
        o = 12672
        QlatT = self.vw(A, o, 2048, BF16).rearrange("p (c b h t) -> p c b h t", c=4, b=4, h=8); o += 2048
        QlatF = self.vw(A, o - 2048, 2048, BF16).rearrange("p (c b m t) -> p c b m t", c=4, b=4, m=2)
        OlatT = self.vw(A, o, 2048, BF16).rearrange("p (c h b q) -> p c h b q", c=4, h=8, b=4)
        OlatF = self.vw(A, o, 2048, BF16).rearrange("p (c h t) -> p c h t", c=4, h=8); o += 2048
        wr_tmp = self.v3(A, o, 6, 64, BF16); o += 192
        assert o <= 16960, o
        self.qs_rope = self.vw(A, 17920, 512, BF16).rearrange("p (b h t) -> p b h t", b=4, h=8)
        qs_ropeF = self.vw(A, 17920, 512, BF16).rearrange("p (b m t) -> p b m t", b=4, m=2)
        self.dma("sp", tabT, self.tabT, [], ["tabT"])
        wuq = w["w_uq"][l]
        wuk = w["w_uk"][l]
        wuv = w["w_uv"][l]
        for h in range(8):
            wb = wh[h % 2]
            wn = "wh%d" % (h % 2)
            self.wload(wb["qn"], wuq[:, h * 192:h * 192 + 128], wn)
            self.wload(wr_tmp, wuq[:, h * 192 + 128:h * 192 + 192], "wrtmp")
            self.cp("dve", wb["qr"][:, :, 0:64], wr_tmp, ["wrtmp"], [wn])
            self.ts("dve", wb["qr"][:, :, 64:96], wr_tmp[:, :, 32:64], -1.0, None, ALU.mult, None, ["wrtmp"], [wn])
            self.cp("dve", wb["qr"][:, :, 96:128], wr_tmp[:, :, 0:32], ["wrtmp"], [wn])
            self.wload(wb["uk"], wuk[:, h * 128:(h + 1) * 128], wn)
            self.wload(wb["uv"], wuv[:, h * 128:(h + 1) * 128], wn)
            for gi, (g0, n) in enumerate(groups):
                b0 = (gi % 2) * 2
                for k in range(6):
                    self.mm(self.psb(b0, n), wb["qn"][:, k, :], self.cqnT[:, k, g0:g0 + n], k == 0, k == 5,
                            [wn, "cqnT"], ["ps%d" % b0])
                self.cp("act", qnT[:, g0:g0 + n], self.psb(b0, n), ["ps%d" % b0], ["qnT"])
                for k in range(6):
                    self.mm(self.psb(b0 + 1, n), wb["qr"][:, k, :], self.cqnT[:, k, g0:g0 + n], k == 0, k == 5,
                            [wn, "cqnT"], ["ps%d" % (b0 + 1)])
                self.tt("dve", qrT[:, g0:g0 + n], self.psb(b0 + 1, n), tabT[:, g0:g0 + n], ALU.mult,
                        ["ps%d" % (b0 + 1), "tabT"], ["qrT"])
            self.cp("dve", self.qs_rope[:, :, h, :], qrT[:, S:S + 128].rearrange("p (b t) -> p b t", b=4), ["qrT"], ["qs"])
            for gi, (g0, n) in enumerate(groups[:-1]):
                b0 = 4 + gi % 2
                for k in range(4):
                    self.mm(self.psb(b0, n), wb["uk"][:, k, :], self.ckvnT[:, k, g0:g0 + n], k == 0, k == 3,
                            [wn, "ckvnT"], ["ps%d" % b0])
                self.cp("act", knT[:, g0:g0 + n], self.psb(b0, n), ["ps%d" % b0], ["knT"])
            for t0 in range(0, NTP, 4):
                bank = 6 + (t0 // 4) % 2
                nt = min(4, NTP - t0)
                for j in range(nt):
                    t = t0 + j
                    for k in range(4):
                        self.mm(self.ps[bank][:, j * 128:(j + 1) * 128], self.ckvnT[:, k, t * 128:(t + 1) * 128],
                                wb["uv"][:, k, :], k == 0, k == 3, ["ckvnT", wn], ["ps%d" % bank])
                self.cp("dve", Vh[:, t0:t0 + nt, :], self.ps[bank][:, 0:nt * 128].rearrange("p (a b) -> p a b", a=nt),
                        ["ps%d" % bank], ["Vh"])
            p16 = self.psb16(5)
            for c in range(4):
                self.tr(p16[:, c * 128:(c + 1) * 128], wb["uk"][:, c, :], self.ident_b[:], [wn, "identb"], ["ps5"])
            self.cp("dve", wb["ukT"], p16[:, 0:512].rearrange("p (a b) -> p a b", a=4), ["ps5"], [wn + "T"])
            for c in range(4):
                self.mm(self.ps[4][:, c * 128:(c + 1) * 128], wb["ukT"][:, c, :], qnT[:, S:S + 128], True, True,
                        [wn + "T", "qnT"], ["ps4"])
            self.cp("act", QlatT[:, :, :, h, :], self.ps[4][:, :].rearrange("p (c b t) -> p c b t", c=4, b=4), ["ps4"], ["QlatT"])
            def geom(i):
                nk = (i + 1) * 128
                nb = (nk + 511) // 512
                so = 40 * (i % 2) + 140
                return dict(nk=nk, nb=nb, qs=slice(i * 128, (i + 1) * 128), Pq=Pb[i % 2], Pn="P%d" % (i % 2),
                            mx=sm[:, so:so + 4], rs4=sm[:, so + 4:so + 8], m1=sm[:, so + 8:so + 9], negm=sm[:, so + 9:so + 10],
                            rsum=sm[:, so + 10:so + 11], rinv=sm[:, so + 11:so + 12], smn="sma%d" % (i % 2),
                            rn="sm_rinv%d" % (i % 2))

            def s_pe(i):
                g = geom(i)
                for kb in range(g["nb"]):
                    ksz = min(512, g["nk"] - kb * 512)
                    ks = slice(kb * 512, kb * 512 + ksz)
                    last = kb == g["nb"] - 1
                    self.mm(self.psb(kb, ksz), qnT[:, g["qs"]], knT[:, ks], True, False, ["qnT", "knT"], ["ps%d" % kb])
                    self.mm(self.psb(kb, ksz), qrT[:, g["qs"]], self.kropeT[:, ks], False, not last, ["qrT", "kropeT"], ["ps%d" % kb])
                    if last:
                        self.mm(self.ps[kb][:, ksz - 128:ksz], self.mrow[:], self.mcol[:], False, True, ["mask"], ["ps%d" % kb])

            def s_sm(i):
                g = geom(i)
                nb, nk, smn = g["nb"], g["nk"], g["smn"]
                for kb in range(nb):
                    ksz = min(512, nk - kb * 512)
                    self.red(g["mx"][:, kb:kb + 1], self.psb(kb, ksz), ALU.max, ["ps%d" % kb], [smn + "m"])
                self.red(g["m1"], g["mx"][:, 0:nb], ALU.max, [smn + "m"], [smn + "m"])
                self.ts("dve", g["negm"], g["m1"], -SM_SCALE, None, ALU.mult, None, [smn + "m"], [smn + "n"])
                for kb in range(nb):
                    ksz = min(512, nk - kb * 512)
                    ks = slice(kb * 512, kb * 512 + ksz)
                    self.act(g["Pq"][:, ks], self.psb(kb, ksz), AF.Exp, ["ps%d" % kb, smn + "n"], [g["Pn"], smn + "r"],
                             bias=g["negm"], scale=SM_SCALE, accum=g["rs4"][:, kb:kb + 1])
                self.red(g["rsum"], g["rs4"][:, 0:nb], ALU.add, [smn + "r"], [smn + "r"])
                self.recip(g["rinv"], g["rsum"], [smn + "r"], [g["rn"]])

            def v_tr(i):
                g = geom(i)
                nblk = i + 1
                for c0 in range(0, nblk, 8):
                    cn = min(8, nblk - c0)
                    pi = (c0 // 8) % 2
                    p16 = self.psb16(4 + pi)
                    for j in range(cn):
                        kk = c0 + j
                        self.tr(p16[:, j * 128:(j + 1) * 128], g["Pq"][:, kk * 128:(kk + 1) * 128], self.ident_b[:],
                                [g["Pn"], "identb"], ["ps%d" % (4 + pi)])
                    eng = "act" if pi == 0 else "dve"
                    self.cp(eng, PTb[pi][:, 0:cn * 128], p16[:, 0:cn * 128], ["ps%d" % (4 + pi)], ["PT%d" % pi])

            def v_pv(i):
                g = geom(i)
                nblk = i + 1
                for c0 in range(0, nblk, 8):
                    cn = min(8, nblk - c0)
                    pi = (c0 // 8) % 2
                    for j in range(cn):
                        kk = c0 + j
                        self.mm(self.ps[7][:, 0:128], PTb[pi][:, j * 128:(j + 1) * 128], Vh[:, kk, :], kk == 0, kk == nblk - 1,
                                ["PT%d" % pi, "Vh"], ["ps7"])
                self.attn_tail_a(l, h, self.ps[7][:, 0:128], g["rinv"], i % 2, A, rn=g["rn"])

            assert NTP <= 16
            for j in range(NTP + 2):
                if j < NTP:
                    s_pe(j)
                if 1 <= j <= NTP:
                    v_tr(j - 1)
                if j < NTP:
                    s_sm(j)
                if 1 <= j <= NTP:
                    v_pv(j - 1)
                if j >= 2:
                    self.attn_tail_b(l, h, geom(j - 2)["qs"], (j - 2) % 2, A)
        self.p.barrier()
        self.chk("s2h")
        KTc = PAST // 128
        o = 0
        ckv = self.v3(A, o, KTc, 512, BF16); o += KTc * 256
        kr2 = self.v3(A, o, KTc, 128, BF16); o += KTc * 64
        ckvT = self.v3(A, o, 4, PAST, BF16); o += 2 * PAST
        krT = self.vw(A, o, PAST // 2, BF16); o += PAST // 2
        Ps = self.vw(A, o, (PAST + 128) // 2, BF16); o += (PAST + 128) // 2
        PTs = self.v3(A, o, KTc + 1, 128, BF16); o += (KTc + 1) * 64
        olb = self.vw(A, o, 256, BF16); o += 256
        assert o <= 12672, o
        uvb = [self.v3(A, i * 256, 4, 128, BF16) for i in range(2)]
        for b in range(4):
            self.wload(ckv, self.cache_ckv[l][b], "ckv")
            self.dma("pool", kr2[:, :, 0:64], self.cache_krope[l][b].rearrange("(k p) n -> p k n", p=128), [], ["kr2"])
            self.dma("pool", kr2[:, :, 64:128], self.cache_krope[l][b].rearrange("(k p) n -> p k n", p=128), [], ["kr2"])
            nbk = 0
            for c in range(4):
                for t0 in range(0, KTc, 8):
                    cn = min(8, KTc - t0)
                    bank = nbk % 4
                    nbk += 1
                    p16 = self.psb16(bank)
                    for j in range(cn):
                        self.tr(p16[:, j * 128:(j + 1) * 128], ckv[:, t0 + j, c * 128:(c + 1) * 128], self.ident_b[:],
                                ["ckv", "identb"], ["ps%d" % bank])
                    self.cp("act" if nbk % 2 else "dve", ckvT[:, c, t0 * 128:(t0 + cn) * 128], p16[:, 0:cn * 128],
                            ["ps%d" % bank], ["ckvT"])
            for t0 in range(0, KTc, 8):
                cn = min(8, KTc - t0)
                bank = nbk % 4
                nbk += 1
                p16 = self.psb16(bank)
                for j in range(cn):
                    self.tr(p16[:, j * 128:(j + 1) * 128], kr2[:, t0 + j, :], self.ident_b[:], ["kr2", "identb"], ["ps%d" % bank])
                self.cp("act" if nbk % 2 else "dve", krT[:, t0 * 128:(t0 + cn) * 128], p16[:, 0:cn * 128],
                        ["ps%d" % bank], ["krT"])
            newk = slice(S + 32 * b, S + 32 * b + 32)
            nblk = (PAST + 511) // 512
            for mt in range(2):
                hs = slice(4 * mt, 4 * mt + 4)
                qlat = lambda c: QlatF[:, c, b, mt, :]
                qrp = qs_ropeF[:, b, mt, :]
                so = 180
                mx = sm[:, so:so + 8]
                rs8 = sm[:, so + 8:so + 16]
                m1, negm, rsum, rinv = (sm[:, so + 16 + j:so + 17 + j] for j in range(4))
                for kb in range(nblk + 1):
                    bank = kb
                    if kb < nblk:
                        ksz = min(512, PAST - kb * 512)
                        ks = slice(kb * 512, kb * 512 + ksz)
                        for c in range(4):
                            self.mm(self.psb(bank, ksz), qlat(c), ckvT[:, c, ks], c == 0, False, ["QlatT", "ckvT"], ["ps%d" % bank])
                        self.mm(self.psb(bank, ksz), qrp, krT[:, ks], False, True, ["qs", "krT"], ["ps%d" % bank])
                    else:
                        ksz = 32
                        for c in range(4):
                            self.mm(self.psb(bank, ksz), qlat(c), self.ckvnT[:, c, newk], c == 0, False, ["QlatT", "ckvnT"], ["ps%d" % bank])
                        self.mm(self.psb(bank, ksz), qrp, self.kropeT[:, newk], False, True, ["qs", "kropeT"], ["ps%d" % bank])
                    self.red(mx[:, kb:kb + 1], self.psb(bank, ksz), ALU.max, ["ps%d" % bank], ["smsm"])
                self.red(m1, mx[:, 0:nblk + 1], ALU.max, ["smsm"], ["smsm"])
                self.ts("dve", negm, m1, -SM_SCALE, None, ALU.mult, None, ["smsm"], ["smsn"])
                for kb in range(nblk + 1):
                    if kb < nblk:
                        ksz = min(512, PAST - kb * 512)
                        ks = slice(kb * 512, kb * 512 + ksz)
                    else:
                        ksz = 32
                        ks = slice(PAST, PAST + 32)
                    self.act(Ps[:, ks], self.psb(kb, ksz), AF.Exp, ["ps%d" % kb, "smsn"], ["Ps", "smsr"],
                             bias=negm, scale=SM_SCALE, accum=rs8[:, kb:kb + 1])
                self.red(rsum, rs8[:, 0:nblk + 1], ALU.add, ["smsr"], ["smsr"])
                self.recip(rinv, rsum, ["smsr"], ["smsr"])
                for t0 in range(0, KTc, 8):
                    cn = min(8, KTc - t0)
                    bank = 5 + (t0 // 8) % 2
                    p16 = self.psb16(bank)
                    for j in range(cn):
                        self.tr(p16[:, j * 128:(j + 1) * 128], Ps[:, (t0 + j) * 128:(t0 + j + 1) * 128], self.ident_b[:],
                                ["Ps", "identb"], ["ps%d" % bank])
                    self.cp("act" if (t0 // 8) % 2 else "dve", PTs[:, t0:t0 + cn, :],
                            p16[:, 0:cn * 128].rearrange("p (a b) -> p a b", a=cn), ["ps%d" % bank], ["PTs"])
                p16 = self.psb16(5)
                self.tr(p16[0:32, 0:128], Ps[:, PAST:PAST + 32], self.ident_b[:], ["Ps", "identb"], ["ps5"])
                self.cp("dve", PTs[0:32, KTc, :], p16[0:32, 0:128], ["ps5"], ["PTs"])
                for kt in range(KTc):
                    self.mm(self.psb(7), PTs[:, kt, :], ckv[:, kt, :], kt == 0, False, ["PTs", "ckv"], ["ps7"])
                self.mm(self.psb(7), PTs[0:32, KTc, :], self.ckvn_new[0:32, b, :], False, True, ["PTs", "ckvn_new"], ["ps7"])
                self.ts("dve", olb, self.psb(7), rinv, None, ALU.mult, None, ["ps7", "smsr"], ["olb"])
                p16 = self.psb16(6)
                for c in range(4):
                    self.tr(p16[:, c * 128:(c + 1) * 128], olb[:, c * 128:(c + 1) * 128], self.ident_b[:], ["olb", "identb"], ["ps6"])
                for c in range(4):
                    self.cp("act" if c % 2 else "dve", OlatT[:, c, hs, b, :],
                            p16[:, c * 128:(c + 1) * 128].rearrange("p (h q) -> p h q", h=4), ["ps6"], ["OlatT"])
        self.p.barrier()
        self.chk("s2s")
        for h in range(8):
            ub = uvb[h % 2]
            un = "uvb%d" % (h % 2)
            self.wload(ub, wuv[:, h * 128:(h + 1) * 128], un)
            for c in range(4):
                self.mm(self.ps[7][:, 0:128], OlatF[:, c, h, :], ub[:, c, :], c == 0, c == 3, ["OlatT", un], ["ps7"])
            self.attn_tail(l, h, self.ps[7][:, 0:128], None, slice(S, S + 128), h % 2, A)

    def ln_apply(self, l, which, src_of, g0, n, gi, mean_bc, rstd_bc, work, last, A, B, dst_bf, ytm=None, tm_dst=None):
        P = self.P
        g, bta = P[which + "g"], P[which + "b"]
        if last:
            tm_dst = self.o_y
        tm = tm_dst is not None
        step = 256 if tm else n
        for c0 in range(0, n, step):
            nn = min(step, n - c0)
            cs = slice(c0, c0 + nn)
            for m in range(16):
                t1 = work[0][m % 2][:, 0:nn]
                o32 = work[1][m % 2][:, 0:nn]
                n1, n2 = "lnw%d" % (m % 2), "lno%d" % (m % 2)
                self.tt("pool", t1, src_of(m)[:, cs], mean_bc[:, cs], ALU.subtract, ["lnsrc", "lnmean"], [n1])
                self.tt("dve", t1, t1, rstd_bc[:, cs], ALU.mult, [n1, "lnrstd"], [n1])
                if not last:
                    self.act(dst_bf[:, m, g0 + c0:g0 + c0 + nn], t1, AF.Identity, [n1, "par"], ["MTout"],
                             scale=g[:, m:m + 1], bias=bta[:, m:m + 1])
                self.ts("dve", o32, t1, g[:, m:m + 1], bta[:, m:m + 1], ALU.mult, ALU.add, [n1, "par"], [n2])
                if not tm or not last:
                    self.dma("sp", self.hres[m * 128:(m + 1) * 128, g0 + c0:g0 + c0 + nn], o32, [n2], ["hres%d" % gi])
                if tm:
                    for j in range(nn // 128):
                        bank = 4 + j % 2
                        pcol = self.ps[bank][:, (m % 4) * 128:(m % 4 + 1) * 128]
                        self.tr(pcol, o32[:, j * 128:(j + 1) * 128], self.ident_f[:], [n2, "identf"], ["ps%d" % bank])
                        self.cp("act" if j % 2 else "dve", ytm[j][:, m * 128:(m + 1) * 128], pcol, ["ps%d" % bank], ["ytm%d" % j])
            if tm:
                for j in range(nn // 128):
                    t0 = g0 + c0 + j * 128
                    if last:
                        on = "o_y_%d" % t0
                        self.outs.append(on)
                    else:
                        on = "h1tm"
                    self.dma("sp", tm_dst[t0:t0 + 128, :], ytm[j], ["ytm%d" % j], [on])

    def ln_stats(self, n, work):
        mean_bc, rstd_bc, tmp = work
        self.act(mean_bc, self.psb(6, n), AF.Copy, ["ps6"], ["lnmean"], scale=1.0 / D)
        self.tt("dve", tmp, mean_bc, mean_bc, ALU.mult, ["lnmean"], ["lntmp"])
        self.stt("dve", tmp, self.psb(7, n), 1.0 / D, tmp, ALU.mult, ALU.subtract, ["ps7", "lntmp"], ["lntmp"])
        self._eps = self.eps5[:, 0:1]
        self.rstd_from(rstd_bc, tmp, 1.0, ["lntmp", "eps"], ["lnrstd"], "lnrstd")

    def stage3(self, l, A, B):
        cfg = self.cfg
        S, T, NT, groups = cfg.S, cfg.T, cfg.NT, cfg.groups
        P, w = self.P, self.w
        MT = self.MT
        wo = w["w_o"][l]
        o = 0
        rT = self.v3(A, o, 16, 512); o += 8192
        wob = [self.v3(A, o + i * 1024, 16, 128, BF16) for i in range(3)]; o += 3072
        res32 = [self.vw(A, o + i * 512, 512) for i in range(2)]; o += 1024
        sqb = [self.vw(A, o + i * 512, 512) for i in range(2)]; o += 1024
        mean_bc = self.vw(A, o, 512); o += 512
        rstd_bc = self.vw(A, o, 512); o += 512
        tmp = self.vw(A, o, 512); o += 512
        w0 = [self.vw(A, o + i * 512, 512) for i in range(2)]; o += 1024
        w1 = [self.vw(A, o + i * 512, 512) for i in range(2)]; o += 1024
        it = 0
        for gi, (g0, n) in enumerate(groups):
            for m in range(16):
                wb = wob[it % 3]
                wn = "wo%d" % (it % 3)
                it += 1
                self.wload(wb, wo[:, m * 128:(m + 1) * 128], wn)
                bank = m % 4
                for k in range(16):
                    self.mm(self.psb(bank, n), wb[:, k, :], MT[:, k, g0:g0 + n], k == 0, k == 15, [wn, "MT"], ["ps%d" % bank])
                rb = res32[m % 2][:, 0:n]
                self.dma("sp", rb, self.hres[m * 128:(m + 1) * 128, g0:g0 + n], ["hres%d" % gi], ["res%d" % (m % 2)])
                self.stt("dve", rT[:, m, 0:n], rb, ALPHA, self.psb(bank, n), ALU.mult, ALU.add,
                         ["res%d" % (m % 2), "ps%d" % bank], ["lnsrc"])
                sq = sqb[m % 2][:, 0:n]
                self.act(sq, rT[:, m, 0:n], AF.Square, ["lnsrc"], ["lsq%d" % (m % 2)])
                self.mm(self.psb(6, n), self.ones_f[:], rT[:, m, 0:n], m == 0, m == 15, ["ones", "lnsrc"], ["ps6"])
                self.mm(self.psb(7, n), self.ones_f[:], sq, m == 0, m == 15, ["ones", "lsq%d" % (m % 2)], ["ps7"])
            self.ln_stats(n, (mean_bc[:, 0:n], rstd_bc[:, 0:n], tmp[:, 0:n]))
            if l % 2 == 1:
                ytm = [self.vw(self.RE, j * 2048, 2048) for j in range(2)]
                self.ln_apply(l, "ln1", lambda m: rT[:, m, 0:n], g0, n, gi, mean_bc[:, 0:n], rstd_bc[:, 0:n], (w0, w1),
                              False, A, B, MT, ytm=ytm, tm_dst=self.h1tm)
            else:
                self.ln_apply(l, "ln1", lambda m: rT[:, m, 0:n], g0, n, gi, mean_bc[:, 0:n], rstd_bc[:, 0:n], (w0, w1),
                              False, A, B, MT)

    def stage4(self, l, A, B, dg_ext=None):
        cfg = self.cfg
        S, T, NT, groups = cfg.S, cfg.T, cfg.NT, cfg.groups
        P, w = self.P, self.w
        H1 = self.MT
        E = self.RE
        moe = (l % 2 == 1)
        last = (l == cfg.DEPTH - 1)
        sm = self.small
        acc = self.v3(A, 0, 16, 1152)
        FW = 1
        o = 0
        wgb = [self.v3(E, o + i * 1024, 16, 128, BF16) for i in range(2)]; o += 2048
        wub = [self.v3(E, o + i * 1024, 16, 128, BF16) for i in range(2)]; o += 2048
        wdb = [self.v3(E, o + i * 1024, 1, 2048, BF16) for i in range(2)]; o += 2048
        sil = [self.vw(E, o + i * 512, 512) for i in range(2)]; o += 1024
        actb = [self.v3(E, o + i * 256, 1, 512, BF16) for i in range(2)]; o += 512
        gbc = self.vw(E, o, 1152); o += 1152
        assert o <= 8832
        o = 8832
        if moe:
            dg = dg_ext
            dgb = self.vw(E, o, 128); o += 128
        lw = o
        mean_bc = self.vw(E, lw, 512); rstd_bc = self.vw(E, lw + 512, 512); tmp = self.vw(E, lw + 1024, 512)
        assert lw + 1536 <= self.EW, lw
        experts = list(range(NE)) if moe else [None]
        FFd = cfg.EFF if moe else cfg.FF
        nchunk = FFd // (128 * FW)
        it = 0
        it2 = 0
        for sgi, sg in enumerate(cfg.sgs):
            sg0 = groups[sg[0]][0]
            first = True
            for e in experts:
                if moe:
                    wg_d, wu_d, wd_d = w["moe_w_gate"][0][e], w["moe_w_up"][0][e], w["moe_w_down"][0][e]
                    for gi in sg:
                        g0, n = groups[gi]
                        for j in range(n // 128):
                            t = g0 // 128 + j
                            self.cp("dve", dgb, dg[:, t, e:e + 1].to_broadcast([128, 128]), ["dg"], ["dgb"])
                            self.mm(self.ps[5][:, j * 128:(j + 1) * 128], dgb, self.ident_f[:], True, True, ["dgb", "identf"], ["ps5"])
                        self.cp("act", gbc[:, g0 - sg0:g0 - sg0 + n], self.psb(5, n), ["ps5"], ["gbc"])
                else:
                    wg_d, wu_d, wd_d = w["ffn_w_gate"][0], w["ffn_w_up"][0], w["ffn_w_down"][0]
                for fc in range(nchunk):
                    i2 = it % 2
                    it += 1
                    wgn, wun, wdn = "wg%d" % i2, "wu%d" % i2, "wd%d" % i2
                    c0 = fc * 128 * FW
                    self.wload(wgb[i2], wg_d[:, c0:c0 + 128 * FW], wgn)
                    self.wload(wub[i2], wu_d[:, c0:c0 + 128 * FW], wun)
                    self.wload(wdb[i2], wd_d[c0:c0 + 128 * FW, :], wdn)
                    for gi in sg:
                        g0, n = groups[gi]
                        ab = actb[gi % 2]
                        abn = "actb%d" % (gi % 2)
                        for f in range(FW):
                            bg, bu = ((0, 1), (2, 3))[(it2 := it2 + 1) % 2]
                            for k in range(16):
                                self.mm(self.psb(bg, n), wgb[i2][:, k, f * 128:(f + 1) * 128], H1[:, k, g0:g0 + n], k == 0, k == 15,
                                        [wgn, "MT"], ["ps%d" % bg])
                            for k in range(16):
                                self.mm(self.psb(bu, n), wub[i2][:, k, f * 128:(f + 1) * 128], H1[:, k, g0:g0 + n], k == 0, k == 15,
                                        [wun, "MT"], ["ps%d" % bu])
                            sl = sil[it2 % 2][:, 0:n]
                            sn = "sil%d" % (it2 % 2)
                            self.act(sl, self.psb(bg, n), AF.Silu, ["ps%d" % bg], [sn])
                            if moe:
                                self.tt("pool", sl, sl, gbc[:, g0 - sg0:g0 - sg0 + n], ALU.mult, [sn, "gbc"], [sn])
                            self.tt("dve", ab[:, f, 0:n], sl, self.psb(bu, n), ALU.mult, [sn, "ps%d" % bu], [abn])
                        for m in range(16):
                            bank = 4 + m % 2 if not moe else 6 + m % 2
                            for f in range(FW):
                                self.mm(self.psb(bank, n), wdb[i2][:, f, m * 128:(m + 1) * 128], ab[:, f, 0:n], f == 0, f == FW - 1,
                                        [wdn, abn], ["ps%d" % bank])
                            av = acc[:, m, g0 - sg0:g0 - sg0 + n]
                            if first:
                                self.cp("dve", av, self.psb(bank, n), ["ps%d" % bank], ["acc"])
                            else:
                                self.tt("dve", av, av, self.psb(bank, n), ALU.add, ["acc", "ps%d" % bank], ["acc"])
                    first = False
            self.p.barrier()
            res32 = [self.vw(E, i * 512, 512) for i in range(2)]
            sqb = [self.vw(E, 1024 + i * 512, 512) for i in range(2)]
            w0 = [self.vw(E, 2048 + i * 512, 512) for i in range(2)]
            w1 = [self.vw(E, 3072 + i * 512, 512) for i in range(2)]
            ytm = [self.vw(E, 4096 + j * 2048, 2048) for j in range(2)]
            for gi in sg:
                g0, n = groups[gi]
                for m in range(16):
                    rb = res32[m % 2][:, 0:n]
                    self.dma("sp", rb, self.hres[m * 128:(m + 1) * 128, g0:g0 + n], ["hres%d" % gi], ["res%d" % (m % 2)])
                    av = acc[:, m, g0 - sg0:g0 - sg0 + n]
                    self.stt("dve", av, rb, ALPHA, av, ALU.mult, ALU.add, ["res%d" % (m % 2), "acc"], ["lnsrc"])
                    sq = sqb[m % 2][:, 0:n]
                    self.act(sq, av, AF.Square, ["lnsrc"], ["lsq%d" % (m % 2)])
                    self.mm(self.psb(6, n), self.ones_f[:], av, m == 0, m == 15, ["ones", "lnsrc"], ["ps6"])
                    self.mm(self.psb(7, n), self.ones_f[:], sq, m == 0, m == 15, ["ones", "lsq%d" % (m % 2)], ["ps7"])
                self.ln_stats(n, (mean_bc[:, 0:n], rstd_bc[:, 0:n], tmp[:, 0:n]))
                self.ln_apply(l, "ln2", lambda m: acc[:, m, g0 - sg0:g0 - sg0 + n], g0, n, gi, mean_bc[:, 0:n],
                              rstd_bc[:, 0:n], (w0, w1), last, A, B, H1, ytm)
            self.p.barrier()

    def stage4_sparse(self, l, A, B):
        cfg = self.cfg
        S, T, NT, C = cfg.S, cfg.T, cfg.NT, cfg.C
        P, w = self.P, self.w
        H1 = self.MT
        E = self.RE
        sm = self.small
        NS = C // 128
        NSL = NE * C
        cgroups = [(c0, min(512, C - c0)) for c0 in range(0, C, 512)]
        v3, vw = self.v3, self.vw
        o = 0
        wgb = [v3(E, o + i * 1024, 16, 128, BF16) for i in range(2)]; o += 2048
        wub = [v3(E, o + i * 1024, 16, 128, BF16) for i in range(2)]; o += 2048
        wdb = [v3(E, o + i * 1024, 1, 2048, BF16) for i in range(2)]; o += 2048
        sil = [vw(E, o + i * 512, 512) for i in range(2)]; o += 1024
        actb = [vw(E, o + i * (C // 2), C // 2, BF16) for i in range(2)]; o += C
        wr = v3(E, o, 16, 8, BF16); o += 64
        n8 = NT * 8
        so_ = self.EW - 1300
        EQ1 = v3(E, so_, NT, 8); so_ += n8
        EQ2 = v3(E, so_, NT, 8); so_ += n8
        DG = v3(E, so_, NT, 8); so_ += n8
        G1 = vw(E, so_, NT); so_ += NT
        G2 = vw(E, so_, NT); so_ += NT
        CNT = vw(E, so_, 8); so_ += 8
        FLG = vw(E, so_, 2); so_ += 2
        FLGi = vw(E, so_, 2).bitcast(I32); so_ += 2
        assert so_ <= self.EW
        POS = v3(E, o, NT, 8); o += n8
        GI = v3(E, o, NT, 8); o += n8
        TM1 = v3(E, o, NT, 8); o += n8
        TM2 = v3(E, o, NT, 8); o += n8
        IDX = [vw(E, o + i * NT, NT) for i in range(2)]; o += 2 * NT
        IDXg = [vw(E, o + i * NT, NT) for i in range(2)]; o += 2 * NT
        VAL = vw(E, o, NT); o += NT
        IDXi = [vw(E, o + i * NT, NT).bitcast(I32) for i in range(2)]; o += 2 * NT
        IDXgi = [vw(E, o + i * NT, NT).bitcast(I32) for i in range(2)]; o += 2 * NT
        EOFF = vw(E, o, 8); o += 8
        UT = vw(E, o, 128); o += 128
        xtm = [vw(E, o + i * 1024, 1024, BF16) for i in range(2)]; o += 2048
        assert o <= self.EW - 1300, o
        self.wload(wr, w["router_w"][0], "wr")
        self.memset("pool", UT, 1.0, ["UT"])
        self.p.op("pool", lambda e: e.affine_select(out=UT, in_=UT, pattern=[[1, 128]], compare_op=ALU.is_ge, fill=0.0,
                                                    base=-1, channel_multiplier=-1),
                  reads=self.Rs("UT"), writes=self.Rs("UT"))
        for t in range(NT):
            tok = slice(t * 128, (t + 1) * 128)
            for k in range(16):
                self.mm(self.ps[0][:, 0:8], H1[:, k, tok], wr[:, k, :], k == 0, k == 15, ["MT", "wr"], ["ps0"])
            lg = sm[:, 200:208]
            m8 = sm[:, 208:216]
            d12, e12 = sm[:, 216:217], sm[:, 217:218]
            self.cp("dve", lg, self.ps[0][:, 0:8], ["ps0"], ["rt"])
            self.p.op("dve", (lambda a, b: (lambda e: e.max(out=a, in_=b)))(m8, lg), reads=self.Rs("rt"), writes=self.Rs("rt8"))
            self.tt("dve", d12, m8[:, 1:2], m8[:, 0:1], ALU.subtract, ["rt8"], ["rtd"])
            self.act(e12, d12, AF.Exp, ["rtd"], ["rte"])
            self.ts("dve", G1[:, t:t + 1], e12, 1.0, None, ALU.add, None, ["rte"], ["G"])
            self.recip(G1[:, t:t + 1], G1[:, t:t + 1], ["G"], ["G"])
            self.ts("dve", G2[:, t:t + 1], G1[:, t:t + 1], -1.0, 1.0, ALU.mult, ALU.add, ["G"], ["G"])
            self.ts("dve", EQ1[:, t, :], lg, m8[:, 0:1], None, ALU.is_equal, None, ["rt", "rt8"], ["EQ"])
            self.ts("dve", EQ2[:, t, :], lg, m8[:, 1:2], None, ALU.is_equal, None, ["rt", "rt8"], ["EQ"])
        self.tt("dve", TM1, EQ1, EQ2, ALU.add, ["EQ"], ["MASK"])
        for t in range(NT):
            self.mm(self.ps[2][:, 0:8], self.ones_f[:], TM1[:, t, :], t == 0, t == NT - 1, ["ones", "MASK"], ["ps2"])
        self.cp("dve", CNT, self.ps[2][:, 0:8], ["ps2"], ["CNT"])
        self.red(FLG[:, 0:1], CNT, ALU.max, ["CNT"], ["FLG"])
        Cs = cfg.Cs
        self.ts("dve", FLG[:, 1:2], FLG[:, 0:1], float(Cs[0]), None, ALU.is_le, None, ["FLG"], ["FLG"])
        for Cv in Cs[1:]:
            self.stt("dve", FLG[:, 1:2], FLG[:, 0:1], float(Cv), FLG[:, 1:2], ALU.is_le, ALU.add, ["FLG"], ["FLG"])
        self.cp("dve", FLGi, FLG, ["FLG"], ["FLGi"])
        self.tt("dve", DG, EQ1, G1.unsqueeze(2).to_broadcast([128, NT, 8]), ALU.mult, ["EQ", "G"], ["DG"])
        self.tt("dve", TM2, EQ2, G2.unsqueeze(2).to_broadcast([128, NT, 8]), ALU.mult, ["EQ", "G"], ["TM2"])
        self.tt("dve", DG, DG, TM2, ALU.add, ["DG", "TM2"], ["DG"])
        self.p.barrier()
        self.fork_begin(FLGi[0:1, 1:2])
        K = len(Cs)
        self.fork_vals = [K - i for i in range(K)]
        L_ = locals()
        for Cv in Cs:
            self.stage4_sparse_body(l, A, B, L_, Cv)
            self.fork_next()
        self.stage4(l, A, B, dg_ext=DG)
        self.fork_end()

    def stage4_sparse_body(self, l, A, B, L_, C):
        cfg = self.cfg
        S, T, NT = cfg.S, cfg.T, cfg.NT
        P, w = self.P, self.w
        E = self.RE
        sm = self.small
        v3, vw = self.v3, self.vw
        (wgb, wub, wdb, sil, actb, n8, EQ1, EQ2, POS, GI, TM1, TM2, G1, G2, IDX, IDXg, VAL, IDXi, IDXgi, EOFF,
         UT, xtm) = (L_[k] for k in ("wgb", "wub", "wdb", "sil", "actb", "n8", "EQ1", "EQ2", "POS", "GI",
                                     "TM1", "TM2", "G1", "G2", "IDX", "IDXg", "VAL", "IDXi", "IDXgi", "EOFF", "UT", "xtm"))
        NS = C // 128
        NSL = NE * C
        cgroups = [(c0, min(512, C - c0)) for c0 in range(0, C, 512)]
        for e_ in range(NE):
            self.memset("pool", EOFF[:, e_:e_ + 1], float(e_ * C), ["EOFF"])
        for t in range(NT):
            for t2 in range(t + 1):
                lhsT = UT if t2 == t else self.ones_f[:]
                self.mm(self.ps[1][:, t * 8:(t + 1) * 8], lhsT, TM1[:, t2, :], t2 == 0, t2 == t, ["UT", "ones", "MASK"], ["ps1"])
        self.cp("dve", POS, self.ps[1][:, 0:n8].rearrange("p (a b) -> p a b", a=NT), ["ps1"], ["POS"])
        self.tt("dve", GI, POS, EOFF.unsqueeze(1).to_broadcast([128, NT, 8]), ALU.add, ["POS", "EOFF"], ["GI"])
        self.ts("dve", TM2, POS, float(C), 1.0e6, ALU.is_ge, ALU.mult, ["POS"], ["TM2"])
        self.tt("dve", GI, GI, TM2, ALU.add, ["GI", "TM2"], ["GI"])
        for k_, EQ in enumerate((EQ1, EQ2)):
            self.tt("dve", TM2, EQ, GI, ALU.mult, ["EQ", "GI"], ["TM2"])
            self.red(IDX[k_], TM2, ALU.add, ["TM2"], ["IDX"])
            G = (G1, G2)[k_]
            self.ts("dve", VAL, IDX[k_], float(NSL), None, ALU.is_lt, None, ["IDX"], ["VAL"])
            self.tt("dve", G, G, VAL, ALU.mult, ["G", "VAL"], ["G"])
            self.ts("dve", IDXg[k_], IDX[k_], float(NSL - 1), None, ALU.min, None, ["IDX"], ["IDXg"])
            self.cp("dve", IDXi[k_], IDX[k_], ["IDX"], ["IDXi"])
            self.cp("dve", IDXgi[k_], IDXg[k_], ["IDXg"], ["IDXgi"])
        self.p.barrier()
        hb = [vw(A, i * 2048, 2048) for i in range(2)]
        hbb = [vw(A, 4096 + i * 1024, 1024, BF16) for i in range(2)]
        xbuf, ybuf = self.xbuf, self.ybuf
        for t in range(NT):
            i2 = t % 2
            self.dma("sp", hb[i2], self.h1tm[t * 128:(t + 1) * 128, :], ["h1tm"], ["hb%d" % i2])
            self.cp("act", hbb[i2], hb[i2], ["hb%d" % i2], ["hbb%d" % i2])
            for k_ in range(2):
                self.p.op("pool", (lambda src, ix: (lambda e: e.indirect_dma_start(
                    out=xbuf[:, :], out_offset=bass.IndirectOffsetOnAxis(ap=ix, axis=0), in_=src, in_offset=None)))(
                        hbb[i2], IDXgi[k_][:, t:t + 1]),
                    reads=self.Rs("hbb%d" % i2, "IDXgi"), writes=self.Rs("xbuf"), dma=True)
        self.p.barrier()
        XT = [v3(B, i * 8 * C, 16, C, BF16) for i in range(2)]
        assert 16 * C <= 18432 and NS * 2048 <= 18432
        acc = v3(A, 0, NS, 2048)
        nchunk = cfg.EFF // 128
        ngrp = NS * 2

        def load_xt(e_, s_):
            self.dma("sp", xtm[s_ % 2], xbuf[e_ * C + s_ * 128:e_ * C + (s_ + 1) * 128, :], ["xbuf"], ["xtm%d" % (s_ % 2)])

        def load_x(e_):
            for s_ in range(min(2, NS)):
                load_xt(e_, s_)

        def xpose_group(e_, g_):
            s_, hf = divmod(g_, 2)
            p16 = self.psb16(7)
            for j in range(8):
                k = hf * 8 + j
                self.tr(p16[:, j * 128:(j + 1) * 128], xtm[s_ % 2][:, k * 128:(k + 1) * 128], self.ident_b[:],
                        ["xtm%d" % (s_ % 2), "identb"], ["ps7"])
            self.cp("act", XT[e_ % 2][:, hf * 8:hf * 8 + 8, s_ * 128:(s_ + 1) * 128],
                    p16[:, 0:1024].rearrange("p (a b) -> p a b", a=8), ["ps7"], ["XT%d" % (e_ % 2)])
            if hf == 1 and s_ + 2 < NS:
                load_xt(e_, s_ + 2)

        load_x(0)
        for g_ in range(ngrp):
            xpose_group(0, g_)
        steps = [(e_, fc) for e_ in range(NE) for fc in range(nchunk)]
        per = -(-ngrp // nchunk)
        gnext = {}
        state = {"it2": 0, "dn": 0}

        def gate_up(i):
            e_, fc = steps[i]
            i2 = i % 2
            c0 = fc * 128
            wgn, wun, wdn = "wg%d" % i2, "wu%d" % i2, "wd%d" % i2
            if fc == 0 and e_ + 1 < NE:
                load_x(e_ + 1)
                gnext[e_ + 1] = 0
            self.wload(wgb[i2], w["moe_w_gate"][0][e_][:, c0:c0 + 128], wgn)
            self.wload(wub[i2], w["moe_w_up"][0][e_][:, c0:c0 + 128], wun)
            self.wload(wdb[i2], w["moe_w_down"][0][e_][c0:c0 + 128, :], wdn)
            xt, xn = XT[e_ % 2], "XT%d" % (e_ % 2)
            for ci, (cc0, n) in enumerate(cgroups):
                bg, bu = ((0, 1), (2, 3))[state["it2"] % 2]
                state["it2"] += 1
                for k in range(16):
                    self.mm(self.psb(bg, n), wgb[i2][:, k, :], xt[:, k, cc0:cc0 + n], k == 0, k == 15, [wgn, xn], ["ps%d" % bg])
                for k in range(16):
                    self.mm(self.psb(bu, n), wub[i2][:, k, :], xt[:, k, cc0:cc0 + n], k == 0, k == 15, [wun, xn], ["ps%d" % bu])
                sl = sil[state["it2"] % 2][:, 0:n]
                sn = "sil%d" % (state["it2"] % 2)
                self.act(sl, self.psb(bg, n), AF.Silu, ["ps%d" % bg], [sn])
                self.tt("dve", actb[i2][:, cc0:cc0 + n], sl, self.psb(bu, n), ALU.mult, [sn, "ps%d" % bu], ["ab%d_%d" % (i2, ci)])
            if e_ + 1 < NE:
                for _ in range(per):
                    if gnext[e_ + 1] < ngrp:
                        xpose_group(e_ + 1, gnext[e_ + 1])
                        gnext[e_ + 1] += 1

        def down(i):
            e_, fc = steps[i]
            i2 = i % 2
            wdn = "wd%d" % i2
            for s_ in range(NS):
                ci = (s_ * 128) // 512
                for q in range(4):
                    bank = 4 + state["dn"] % 3
                    state["dn"] += 1
                    self.mm(self.psb(bank), actb[i2][:, s_ * 128:(s_ + 1) * 128], wdb[i2][:, 0, q * 512:(q + 1) * 512], True, True,
                            ["ab%d_%d" % (i2, ci), wdn], ["ps%d" % bank])
                    av = acc[:, s_, q * 512:(q + 1) * 512]
                    if fc == 0:
                        self.cp("dve", av, self.psb(bank), ["ps%d" % bank], ["acc%d" % s_])
                    else:
                        self.tt("dve", av, av, self.psb(bank), ALU.add, ["acc%d" % s_, "ps%d" % bank], ["acc%d" % s_])
                if fc == nchunk - 1:
                    self.dma("sp", ybuf[e_ * C + s_ * 128:e_ * C + (s_ + 1) * 128, :], acc[:, s_, :], ["acc%d" % s_], ["ybuf"])

        for i in range(len(steps) + 1):
            if i < len(steps):
                gate_up(i)
            if i >= 1:
                down(i - 1)
        self.p.barrier()
        lng = vw(B, 0, 2048)
        lnb = vw(B, 2048, 2048)
        self.dma("sp", lng, w["ln2_g"][l].rearrange("(o n) -> o n", o=1).partition_broadcast(128), [], ["lng"])
        self.dma("sp", lnb, w["ln2_b"][l].rearrange("(o n) -> o n", o=1).partition_broadcast(128), [], ["lng"])
        sets = [dict(y1=vw(A, i * 8192, 2048), y2=vw(A, i * 8192 + 2048, 2048), hh=vw(A, i * 8192 + 4096, 2048),
                     tt=vw(A, i * 8192 + 6144, 2048)) for i in range(2)]
        for t in range(NT):
            i2 = t % 2
            st_ = sets[i2]
            y1, y2, hh, tt_ = st_["y1"], st_["y2"], st_["hh"], st_["tt"]
            n_ = "cb%d" % i2
            for k_, yb in enumerate((y1, y2)):
                self.p.op("pool", (lambda dst, ix: (lambda e: e.indirect_dma_start(
                    out=dst, out_offset=None, in_=ybuf[:, :], in_offset=bass.IndirectOffsetOnAxis(ap=ix, axis=0))))(
                        yb, IDXgi[k_][:, t:t + 1]),
                    reads=self.Rs("ybuf", "IDXgi"), writes=self.Rs(n_ + "y%d" % k_), dma=True)
            self.dma("sp", hh, self.h1tm[t * 128:(t + 1) * 128, :], ["h1tm"], [n_ + "h"])
            self.ts("dve", y1, y1, G1[:, t:t + 1], None, ALU.mult, None, [n_ + "y0", "G"], [n_ + "y0"])
            self.stt("dve", hh, hh, ALPHA, y1, ALU.mult, ALU.add, [n_ + "h", n_ + "y0"], [n_ + "h"])
            self.stt("dve", hh, y2, G2[:, t:t + 1], hh, ALU.mult, ALU.add, [n_ + "h", n_ + "y1", "G"], [n_ + "h"])
            so = 220 + i2 * 8
            ssum, ssq, mean, var, rstd = (sm[:, so + j:so + j + 1] for j in range(5))
            smn = n_ + "s"
            self.act(y1, hh, AF.Identity, [n_ + "h"], [n_ + "y0", smn + "a"], accum=ssum)
            self.act(y2, hh, AF.Square, [n_ + "h"], [n_ + "y1", smn + "b"], accum=ssq)
            self.ts("dve", mean, ssum, 1.0 / D, None, ALU.mult, None, [smn + "a"], [smn + "m"])
            self.tt("dve", var, mean, mean, ALU.mult, [smn + "m"], [smn + "v"])
            self.stt("dve", var, ssq, 1.0 / D, var, ALU.mult, ALU.subtract, [smn + "b", smn + "v"], [smn + "v"])
            self._eps = self.eps5[:, 0:1]
            self.rstd_from(rstd, var, 1.0, [smn + "v", "eps"], [smn + "r"], smn + "r")
            self.ts("dve", tt_, hh, mean, rstd, ALU.subtract, ALU.mult, [n_ + "h", smn + "m", smn + "r"], [n_ + "t"])
            self.tt("pool", tt_, tt_, lng, ALU.mult, [n_ + "t", "lng"], [n_ + "t"])
            self.tt("dve", tt_, tt_, lnb, ALU.add, [n_ + "t", "lng"], [n_ + "t"])
            on = "o_y_%d" % (t * 128)
            self.dma("sp", self.o_y[t * 128:(t + 1) * 128, :], tt_, [n_ + "t"], [on])
            self.outs.append(on)

def rope_tables(S, PAST):
    half = 32
    inv = (np.float32(10000.0) ** (-(np.arange(half, dtype=np.float32)) / np.float32(half))).astype(np.float32)
    pos = np.concatenate([np.arange(S), np.tile(PAST + np.arange(32), 4)]).astype(np.float32)
    ang = pos[:, None] * inv[None, :]
    cos = np.concatenate([np.cos(ang), np.cos(ang)], axis=-1).astype(np.float32)
    sin = np.concatenate([np.sin(ang), np.sin(ang)], axis=-1).astype(np.float32)
    tabM = np.ascontiguousarray(np.concatenate([cos, sin], axis=-1))
    tabT = np.ascontiguousarray(tabM.T)
    return tabT, tabM

_NC_CACHE = {}

def run(cfg, inputs, n_cores):
    key = (cfg.S, cfg.PAST, cfg.FF, cfg.EFF, cfg.DEPTH, cfg.stop, cfg.Cs)
    if key not in _NC_CACHE:
        _NC_CACHE[key] = KB(cfg).build()
    nc = _NC_CACHE[key]
    L = cfg.DEPTH
    tabT, tabM = rope_tables(cfg.S, cfg.PAST)
    f = lambda a: np.ascontiguousarray(np.asarray(a, dtype=np.float32))
    wmap = {}
    for k in W_INPUTS:
        a = f(inputs[k])
        if k == "w_uq":
            a = a.reshape(L, 768, 8 * 192)
        elif k in ("w_uk", "w_uv"):
            a = a.reshape(L, 512, 1024)
        wmap[k] = a
    in_maps = []
    for c in range(n_cores):
        m = dict(wmap)
        m["x"] = f(np.concatenate([inputs["x_prompt"][c], np.asarray(inputs["x_sample"][4 * c:4 * c + 4]).reshape(128, D)], axis=0))
        m["state_conv"] = f(inputs["state_conv"][:, 4 * c:4 * c + 4])
        m["cache_ckv"] = f(inputs["cache_ckv"][:, 4 * c:4 * c + 4])
        m["cache_krope"] = f(inputs["cache_krope"][:, 4 * c:4 * c + 4])
        m["tabT"] = tabT
        m["tabM"] = tabM
        in_maps.append(m)
    res = run_bass_kernel_spmd(nc, in_maps, core_ids=list(range(n_cores))).results
    if getattr(cfg, "debug", False):
        _NC_CACHE["last_res"] = res
    S = cfg.S
    y_p = np.stack([r["y"][:S] for r in res])
    y_s = np.concatenate([r["y"][S:].reshape(4, 32, D) for r in res])
    conv_p = np.stack([r["conv_p"] for r in res], axis=1)
    ckv_p = np.stack([r["ckv_p"] for r in res], axis=1)
    kr_p = np.stack([r["krope_p"] for r in res], axis=1)
    conv_s = np.concatenate([r["conv_s"] for r in res], axis=1)
    ckv_s = np.concatenate([r["ckv_s"].reshape(L, 4, 32, 512) for r in res], axis=1)
    kr_s = np.concatenate([r["krope_s"].reshape(L, 4, 32, 64) for r in res], axis=1)
    v_s = np.concatenate([r["sguv_s"].reshape(L, 4, 32, 512) for r in res], axis=1)
    return tuple(np.ascontiguousarray(a, dtype=np.float32) for a in
                 (y_p, y_s, conv_p, ckv_p, kr_p, conv_s, ckv_s, kr_s, v_s))

def kernel(**inputs):
    return run(Cfg(), inputs, 8)
```
